# Optimizing a Trainium2 kernel written in Bass

```python
import math
import jax, jax.numpy as jnp
from jax import lax
import numpy as np

D_MODEL = 1024
BATCH = 8
SEQ = 8192
DEPTH = 2

GRID_W = 64
CTX_LEN = 256
NORM_EPS = 1e-6

RW_HEADS = 4
RW_HD = 64
RW_W = RW_HEADS * RW_HD
RW_DECAY_RANK = 64
RW_ICLR_RANK = 64
RW_GATE_RANK = 128
RW_GN_EPS = 64e-5
RW_SPLITS = [RW_W, 2 * RW_W, 3 * RW_W, 3 * RW_W + RW_DECAY_RANK, 3 * RW_W + RW_DECAY_RANK + RW_ICLR_RANK]
RW_COLS = 3 * RW_W + RW_DECAY_RANK + RW_ICLR_RANK + RW_GATE_RANK

SSM_HEADS = 8
SSM_HD = 64
SSM_W = SSM_HEADS * SSM_HD
SSM_GROUPS = 2
SSM_STATE = 64
SSM_CHUNK = 128
SSM_CONV = 3
SSM_CONV_CH = SSM_W + 2 * SSM_GROUPS * SSM_STATE
SSM_COLS = SSM_W + SSM_CONV_CH + SSM_HEADS

GLA_HEADS = 4
GLA_DK = 32
GLA_DV = 64
GLA_KW = GLA_HEADS * GLA_DK
GLA_VW = GLA_HEADS * GLA_DV
GLA_GATE_RANK = 16
GLA_TAU = 16.0
GLA_CHUNK = 64
GLA_COLS = 2 * GLA_KW + GLA_VW + GLA_GATE_RANK + GLA_VW

IN_COLS = RW_COLS + SSM_COLS + GLA_COLS
MIX_W = RW_W + SSM_W + GLA_VW

N_GROUPS = 4
EXPERTS_PER_GROUP = 4
N_EXPERTS = N_GROUPS * EXPERTS_PER_GROUP
TOP_K = 2
D_EXPERT = 512

kernel_name = 'hybrid_rwkv7_ssd_gla_hmoe_diffusion_block'


def rmsnorm(h, g):
    hf = h.astype(jnp.float32)
    hf = hf * lax.rsqrt(jnp.mean(hf * hf, -1, keepdims=True) + NORM_EPS)
    return (hf * g.astype(jnp.float32)).astype(h.dtype)


def modulate(h, shift, scale):
    return h * (1 + scale[:, None]) + shift[:, None]


def head_layernorm(y, g, b):
    yf = y.astype(jnp.float32)
    mu = jnp.mean(yf, -1, keepdims=True)
    var = jnp.mean(jnp.square(yf - mu), -1, keepdims=True)
    yn = (yf - mu) * lax.rsqrt(var + RW_GN_EPS)
    return yn.reshape(y.shape[:2] + (-1,)) * g + b


def head_rmsnorm(y, g):
    yf = y.astype(jnp.float32)
    yn = yf * lax.rsqrt(jnp.mean(yf * yf, -1, keepdims=True) + NORM_EPS)
    return yn.reshape(y.shape[:2] + (-1,)) * g


def token_shift(u, mu_prev, mu_next):
    pad = jnp.zeros_like(u[:, :1])
    prev = jnp.concatenate([pad, u[:, :-1]], 1)
    nxt = jnp.concatenate([u[:, 1:], pad], 1)
    return u + mu_prev * (prev - u) + mu_next * (nxt - u)


def dwconv_grid(u, w, b, rows):
    bsz, n, ch = u.shape
    img = u.reshape(bsz, rows, n // rows, ch)
    out = lax.conv_general_dilated(img, w[:, :, None, :].astype(u.dtype), (1, 1), 'SAME',
                                   dimension_numbers=('NHWC', 'HWIO', 'NHWC'), feature_group_count=ch)
    return out.reshape(bsz, n, ch) + b


def rwkv7_scan(r, w, k, v, a, b, s0):
    def step(s, inp):
        r_t, w_t, k_t, v_t, a_t, b_t = inp
        sa = jnp.einsum('bhvk,bhk->bhv', s, a_t)
        s = s * w_t[:, :, None, :] + sa[..., None] * b_t[:, :, None, :] + v_t[..., None] * k_t[:, :, None, :]
        return s, jnp.einsum('bhvk,bhk->bhv', s, r_t)
    xs = tuple(jnp.moveaxis(z.astype(jnp.float32), 1, 0) for z in (r, w, k, v, a, b))
    s_fin, ys = lax.scan(step, s0, xs)
    return jnp.moveaxis(ys, 0, 1), s_fin


def ssd_chunked(x, dA, bm, cm, s0):
    bsz, t, h, p = x.shape
    n = bm.shape[-1]
    L = SSM_CHUNK
    nc = t // L
    x = x.astype(jnp.float32).reshape(bsz, nc, L, h, p)
    bm = bm.astype(jnp.float32).reshape(bsz, nc, L, h, n)
    cm = cm.astype(jnp.float32).reshape(bsz, nc, L, h, n)
    a_cs = jnp.cumsum(dA.astype(jnp.float32).reshape(bsz, nc, L, h).transpose(0, 3, 1, 2), -1)
    causal = jnp.tril(jnp.ones((L, L), bool))
    decay_in = jnp.exp(jnp.where(causal, a_cs[..., :, None] - a_cs[..., None, :], -jnp.inf))
    scores = jnp.einsum('bclhn,bcshn->bhcls', cm, bm) * decay_in
    y_diag = jnp.einsum('bhcls,bcshp->bclhp', scores, x)
    decay_to_end = jnp.exp(a_cs[..., -1:] - a_cs)
    states = jnp.einsum('bclhn,bhcl,bclhp->bchpn', bm, decay_to_end, x)
    states = jnp.concatenate([s0.astype(jnp.float32)[:, None], states], 1)
    chunk_cs = jnp.cumsum(jnp.pad(a_cs[..., -1], ((0, 0), (0, 0), (1, 0))), -1)
    causal_c = jnp.tril(jnp.ones((nc + 1, nc + 1), bool))
    decay_chunk = jnp.exp(jnp.where(causal_c, chunk_cs[..., :, None] - chunk_cs[..., None, :], -jnp.inf))
    states = jnp.einsum('bhzc,bchpn->bzhpn', decay_chunk, states)
    y_off = jnp.einsum('bclhn,bchpn,bhcl->bclhp', cm, states[:, :-1], jnp.exp(a_cs))
    return (y_diag + y_off).reshape(bsz, t, h, p), states[:, -1]


def gla_chunked(q, k, v, logg, s0):
    bsz, t, h, dk = q.shape
    dv = v.shape[-1]
    L = GLA_CHUNK
    nc = t // L
    rs = lambda z: z.astype(jnp.float32).reshape(bsz, nc, L, h, z.shape[-1])
    q, k, v, logg = rs(q), rs(k), rs(v), rs(logg)
    b_cs = jnp.cumsum(logg, 2)
    ref = b_cs[:, :, L // 2:L // 2 + 1]
    att = jnp.einsum('bclhd,bcshd->bchls', q * jnp.exp(b_cs - ref), k * jnp.exp(ref - b_cs))
    att = jnp.where(jnp.tril(jnp.ones((L, L), bool)), att, 0.0)
    o_intra = jnp.einsum('bchls,bcshe->bclhe', att, v)
    b_end = b_cs[:, :, -1]
    kv = jnp.einsum('bcshd,bcshe->bchde', k * jnp.exp(b_end[:, :, None] - b_cs), v)
    def step(s, inp):
        kv_c, be_c = inp
        return s * jnp.exp(be_c)[..., None] + kv_c, s
    s_fin, s_start = lax.scan(step, s0.astype(jnp.float32), (jnp.moveaxis(kv, 1, 0), jnp.moveaxis(b_end, 1, 0)))
    o_inter = jnp.einsum('bclhd,bchde->bclhe', q * jnp.exp(b_cs), jnp.moveaxis(s_start, 0, 1))
    return (o_intra + o_inter).reshape(bsz, t, h, dv), s_fin


def bidir(scan_fn, ctx_fwd, lat_fwd, ctx_bwd, lat_bwd, s0, ctx_out):
    flip = lambda zs: tuple(jnp.flip(z, 1) for z in zs)
    yc_f, sc_f = scan_fn(*ctx_fwd, s0)
    yl_f, _ = scan_fn(*lat_fwd, sc_f)
    yc_b, sc_b = scan_fn(*flip(ctx_bwd), s0)
    yl_b, _ = scan_fn(*flip(lat_bwd), sc_b)
    yl = yl_f + jnp.flip(yl_b, 1)
    yc = yc_f + jnp.flip(yc_b, 1) if ctx_out else None
    return yc, yl


def rwkv_group(u_c, u_l, mu_prev, mu_next, w0, w2, a0, a2, g2, k_k, k_a, r_k, gn_g, gn_b, ctx_out):
    def heads(z):
        return z.reshape(z.shape[:2] + (RW_HEADS, RW_HD))

    def prep(u):
        r, k, v, wl, al, gl = jnp.split(token_shift(u, mu_prev, mu_next), RW_SPLITS, -1)
        return r, k, v, jnp.tanh(wl), al, gl

    def scan_inputs(p, d):
        r, k, v, wl, al, _ = p
        w_log = -jax.nn.softplus(-(w0[d] + wl @ w2[d])) - 0.5
        decay = jnp.exp(-jnp.exp(w_log.astype(jnp.float32)))
        a = jax.nn.sigmoid(a0[d] + al @ a2[d])
        kk = heads(k * k_k).astype(jnp.float32)
        kk = kk * lax.rsqrt(jnp.sum(kk * kk, -1, keepdims=True) + 1e-12)
        k_mod = k * (1 + (a - 1) * k_a)
        return (heads(r), heads(decay), heads(k_mod), heads(v), -kk, kk * heads(a))

    def finish(p, y):
        r, k, v, _, _, gl = p
        bonus = jnp.sum(heads(r * k * r_k), -1, keepdims=True) * heads(v)
        out = head_layernorm(y, gn_g, gn_b) + bonus.reshape(r.shape)
        return (out * (jax.nn.sigmoid(gl) @ g2)).astype(r.dtype)

    pc, pl = prep(u_c), prep(u_l)
    s0 = jnp.zeros((u_l.shape[0], RW_HEADS, RW_HD, RW_HD), jnp.float32)
    yc, yl = bidir(rwkv7_scan, scan_inputs(pc, 0), scan_inputs(pl, 0), scan_inputs(pc, 1), scan_inputs(pl, 1), s0, ctx_out)
    return (finish(pc, yc) if ctx_out else None), finish(pl, yl)


def ssm_group(u_c, u_l, rows, conv_w, conv_b, dt_bias, a_log, d_skip, norm_g, ctx_out):
    def prep(u, grid_rows):
        bsz, t = u.shape[:2]
        z, xbc, dt = jnp.split(u, [SSM_W, SSM_W + SSM_CONV_CH], -1)
        xbc = jax.nn.silu(dwconv_grid(xbc, conv_w, conv_b, grid_rows))
        xs, bm, cm = jnp.split(xbc, [SSM_W, SSM_W + SSM_GROUPS * SSM_STATE], -1)
        rep = SSM_HEADS // SSM_GROUPS
        bm = jnp.repeat(bm.reshape(bsz, t, SSM_GROUPS, SSM_STATE), rep, axis=2)
        cm = jnp.repeat(cm.reshape(bsz, t, SSM_GROUPS, SSM_STATE), rep, axis=2)
        return z, xs.reshape(bsz, t, SSM_HEADS, SSM_HD), bm, cm, dt

    def scan_inputs(p, d):
        _, xs, bm, cm, dt = p
        dtp = jax.nn.softplus(dt.astype(jnp.float32) + dt_bias[d])
        dA = -jnp.exp(a_log[d]) * dtp
        return (xs * dtp[..., None], dA, bm, cm)

    def finish(p, y):
        z, xs = p[0], p[1]
        y = (y + d_skip[:, None] * xs).reshape(z.shape[:2] + (SSM_W,))
        return rmsnorm(y * jax.nn.silu(z), norm_g).astype(z.dtype)

    pc, pl = prep(u_c, 1), prep(u_l, rows)
    s0 = jnp.zeros((u_l.shape[0], SSM_HEADS, SSM_HD, SSM_STATE), jnp.float32)
    yc, yl = bidir(ssd_chunked, scan_inputs(pc, 0), scan_inputs(pl, 0), scan_inputs(pc, 1), scan_inputs(pl, 1), s0, ctx_out)
    return (finish(pc, yc) if ctx_out else None), finish(pl, yl)


def gla_group(u_c, u_l, ga2, gb, norm_g, ctx_out):
    def heads(z):
        return z.reshape(z.shape[:2] + (GLA_HEADS, -1))

    def prep(u):
        q, k, v, gl, r = jnp.split(u, [GLA_KW, 2 * GLA_KW, 2 * GLA_KW + GLA_VW, 2 * GLA_KW + GLA_VW + GLA_GATE_RANK], -1)
        return heads(q) * GLA_DK ** -0.5, heads(k), heads(v), gl, r

    def scan_inputs(p, d):
        q, k, v, gl, _ = p
        logg = jax.nn.log_sigmoid((gl @ ga2[d] + gb[d]).astype(jnp.float32)) / GLA_TAU
        return (q, k, v, heads(logg))

    def finish(p, y):
        r = p[4]
        return (head_rmsnorm(y, norm_g) * jax.nn.silu(r)).astype(r.dtype)

    pc, pl = prep(u_c), prep(u_l)
    s0 = jnp.zeros((u_l.shape[0], GLA_HEADS, GLA_DK, GLA_DV), jnp.float32)
    yc, yl = bidir(gla_chunked, scan_inputs(pc, 0), scan_inputs(pl, 0), scan_inputs(pc, 1), scan_inputs(pl, 1), s0, ctx_out)
    return (finish(pc, yc) if ctx_out else None), finish(pl, yl)


def mixer(hc, hl, rows, w_in, w_out, rw_p, ssm_p, gla_p, ctx_out):
    cut = [RW_COLS, RW_COLS + SSM_COLS]
    rc, sc, gc = jnp.split(hc @ w_in, cut, -1)
    rl, sl, gl = jnp.split(hl @ w_in, cut, -1)
    a_c, a_l = rwkv_group(rc, rl, *rw_p, ctx_out)
    b_c, b_l = ssm_group(sc, sl, rows, *ssm_p, ctx_out)
    g_c, g_l = gla_group(gc, gl, *gla_p, ctx_out)
    out_l = jnp.concatenate([a_l, b_l, g_l], -1) @ w_out
    out_c = jnp.concatenate([a_c, b_c, g_c], -1) @ w_out if ctx_out else None
    return out_c, out_l


def hier_moe(h, rg_w, rg_b, re_w, re_b, w1, w3, w2):
    bsz, t, d = h.shape
    tok = h.reshape(bsz * t, d)
    g_logits = (tok @ rg_w + rg_b).astype(jnp.float32)
    g_prob = jax.nn.softmax(g_logits, -1)
    _, g_sel = lax.top_k(g_logits, 1)
    p_group = jnp.take_along_axis(g_prob, g_sel, 1)
    e_logits = (tok @ re_w + re_b).astype(jnp.float32).reshape(-1, N_GROUPS, EXPERTS_PER_GROUP)
    e_logits = jnp.take_along_axis(e_logits, g_sel[:, :, None], 1)[:, 0]
    top_p, top_i = lax.top_k(jax.nn.softmax(e_logits, -1), TOP_K)
    weight = p_group * top_p / jnp.sum(top_p, -1, keepdims=True)
    expert_id = g_sel * EXPERTS_PER_GROUP + top_i
    combine = jnp.sum(jax.nn.one_hot(expert_id, N_EXPERTS, dtype=jnp.float32) * weight[..., None], 1).astype(tok.dtype)
    out = jnp.zeros_like(tok)
    for e in range(N_EXPERTS):
        hid = jax.nn.silu(tok @ w1[e]) * (tok @ w3[e])
        out = out + combine[:, e:e + 1] * (hid @ w2[e])
    return out.reshape(bsz, t, d)


def setup_inputs(seed: int = 0) -> dict:
    key = jax.random.key(seed)
    ks = iter(jax.random.split(key, 64))
    nrm = lambda shape, s: jax.random.normal(next(ks), shape, jnp.float32) * s
    uni = lambda shape, lo, hi: jax.random.uniform(next(ks), shape, jnp.float32, lo, hi)
    L, D = DEPTH, D_MODEL
    dt = jnp.exp(uni((L, 2, SSM_HEADS), math.log(1e-3), math.log(1e-1)))
    return {
        'x': nrm((BATCH, SEQ, D), 1.0),
        'c': nrm((BATCH, D), 1.0),
        'ctx': nrm((BATCH, CTX_LEN, D), 1.0),
        'c_ctx': nrm((D,), 1.0),
        'ada_w': nrm((L, D, 6 * D), 0.5 * D ** -0.5),
        'ada_b': nrm((L, 6 * D), 0.02),
        'norm1_g': 1.0 + nrm((L, D), 0.1),
        'norm2_g': 1.0 + nrm((L, D), 0.1),
        'w_in': nrm((L, D, IN_COLS), D ** -0.5),
        'w_out': nrm((L, MIX_W, D), MIX_W ** -0.5),
        'rw_mu_prev': uni((L, RW_COLS), 0.0, 0.5),
        'rw_mu_next': uni((L, RW_COLS), 0.0, 0.5),
        'rw_w0': uni((L, 2, RW_W), -6.0, 1.0),
        'rw_w2': nrm((L, 2, RW_DECAY_RANK, RW_W), 0.1 * RW_DECAY_RANK ** -0.5),
        'rw_a0': nrm((L, 2, RW_W), 0.1),
        'rw_a2': nrm((L, 2, RW_ICLR_RANK, RW_W), 0.1 * RW_ICLR_RANK ** -0.5),
        'rw_g2': nrm((L, RW_GATE_RANK, RW_W), RW_GATE_RANK ** -0.5),
        'rw_k_k': 0.85 + nrm((L, RW_W), 0.05),
        'rw_k_a': 1.0 + nrm((L, RW_W), 0.05),
        'rw_r_k': nrm((L, RW_W), 0.1),
        'rw_gn_g': 1.0 + nrm((L, RW_W), 0.1),
        'rw_gn_b': nrm((L, RW_W), 0.02),
        'ssm_conv_w': nrm((L, SSM_CONV, SSM_CONV, SSM_CONV_CH), (SSM_CONV * SSM_CONV) ** -0.5),
        'ssm_conv_b': nrm((L, SSM_CONV_CH), 0.02),
        'ssm_dt_bias': dt + jnp.log(-jnp.expm1(-dt)),
        'ssm_a_log': jnp.log(uni((L, 2, SSM_HEADS), 1.0, 16.0)),
        'ssm_d': 1.0 + nrm((L, SSM_HEADS), 0.1),
        'ssm_norm_g': 1.0 + nrm((L, SSM_W), 0.1),
        'gla_ga2': nrm((L, 2, GLA_GATE_RANK, GLA_KW), GLA_GATE_RANK ** -0.5),
        'gla_gb': nrm((L, 2, GLA_KW), 0.5),
        'gla_norm_g': 1.0 + nrm((L, GLA_VW), 0.1),
        'moe_rg_w': nrm((L, D, N_GROUPS), D ** -0.5),
        'moe_rg_b': nrm((L, N_GROUPS), 0.01),
        'moe_re_w': nrm((L, D, N_EXPERTS), D ** -0.5),
        'moe_re_b': nrm((L, N_EXPERTS), 0.01),
        'moe_w1': nrm((L, N_EXPERTS, D, D_EXPERT), D ** -0.5),
        'moe_w3': nrm((L, N_EXPERTS, D, D_EXPERT), D ** -0.5),
        'moe_w2': nrm((L, N_EXPERTS, D_EXPERT, D), D_EXPERT ** -0.5),
        'final_g': 1.0 + nrm((D,), 0.1),
    }


def reference(x, c, ctx, c_ctx, ada_w, ada_b, norm1_g, norm2_g, w_in, w_out,
              rw_mu_prev, rw_mu_next, rw_w0, rw_w2, rw_a0, rw_a2, rw_g2, rw_k_k, rw_k_a, rw_r_k, rw_gn_g, rw_gn_b,
              ssm_conv_w, ssm_conv_b, ssm_dt_bias, ssm_a_log, ssm_d, ssm_norm_g,
              gla_ga2, gla_gb, gla_norm_g,
              moe_rg_w, moe_rg_b, moe_re_w, moe_re_b, moe_w1, moe_w3, moe_w2, final_g):
    rows = x.shape[1] // GRID_W
    cond_l = jax.nn.silu(c)
    cond_c = jax.nn.silu(c_ctx)[None]
    for l in range(DEPTH):
        ctx_out = l < DEPTH - 1
        mod_l = jnp.split(cond_l @ ada_w[l] + ada_b[l], 6, -1)
        mod_c = jnp.split(cond_c @ ada_w[l] + ada_b[l], 6, -1)
        hl = modulate(rmsnorm(x, norm1_g[l]), mod_l[0], mod_l[1])
        hc = modulate(rmsnorm(ctx, norm1_g[l]), mod_c[0], mod_c[1])
        rw_p = (rw_mu_prev[l], rw_mu_next[l], rw_w0[l], rw_w2[l], rw_a0[l], rw_a2[l], rw_g2[l],
                rw_k_k[l], rw_k_a[l], rw_r_k[l], rw_gn_g[l], rw_gn_b[l])
        ssm_p = (ssm_conv_w[l], ssm_conv_b[l], ssm_dt_bias[l], ssm_a_log[l], ssm_d[l], ssm_norm_g[l])
        gla_p = (gla_ga2[l], gla_gb[l], gla_norm_g[l])
        mc, ml = mixer(hc, hl, rows, w_in[l], w_out[l], rw_p, ssm_p, gla_p, ctx_out)
        moe_p = (moe_rg_w[l], moe_rg_b[l], moe_re_w[l], moe_re_b[l], moe_w1[l], moe_w3[l], moe_w2[l])
        x = x + mod_l[2][:, None] * ml
        x = x + mod_l[5][:, None] * hier_moe(modulate(rmsnorm(x, norm2_g[l]), mod_l[3], mod_l[4]), *moe_p)
        if ctx_out:
            ctx = ctx + mod_c[2][:, None] * mc
            ctx = ctx + mod_c[5][:, None] * hier_moe(modulate(rmsnorm(ctx, norm2_g[l]), mod_c[3], mod_c[4]), *moe_p)
    return rmsnorm(x, final_g)
```

```python
import numpy as np
import concourse.bass as bass
import concourse.mybir as mybir

F32 = mybir.dt.float32
BF16 = mybir.dt.bfloat16
ALU = mybir.AluOpType
AF = mybir.ActivationFunctionType
AX = mybir.AxisListType

SEM_CHUNK = 30000
N_DMA_SEMS = 12


class T:
    __slots__ = ("ap", "w", "r", "name")

    def __init__(self, ap, name=""):
        self.ap = ap
        self.w = None
        self.r = []
        self.name = name

    def __getitem__(self, idx):
        return self.ap[idx]


class KB:
    ENGS = ("pe", "act", "dve", "pool", "sp")

    def __init__(self, nc, same_engine_sync=True):
        self.nc = nc
        self.ops = {e: [] for e in self.ENGS}
        self.cnt = {e: 0 for e in self.ENGS}
        self.sem_names = []
        self.waited = {e: {} for e in self.ENGS}
        self.dma_rr = {e: 0 for e in self.ENGS}
        self.dma_val = {}
        self.same_engine_sync = same_engine_sync
        self.ctx = []
        self.final_waits = []
        self.sems = {}
        self.sem_guards = []
        self.last_tok = {}

    def sb(self, name, shape, dt):
        self.uid = getattr(self, "uid", 0) + 1
        name = "%s_%d" % (name, self.uid)
        g = self.nc.sbuf_tensor(name, list(shape), dt)
        t = g.__enter__()
        self.ctx.append(g)
        return T(t, name)

    def ps(self, name, shape, dt):
        self.uid = getattr(self, "uid", 0) + 1
        name = "%s_%d" % (name, self.uid)
        g = self.nc.psum_tensor(name, list(shape), dt)
        t = g.__enter__()
        self.ctx.append(g)
        return T(t, name)

    def dram(self, name, shape, dt, kind="Internal"):
        t = self.nc.dram_tensor(name, list(shape), dt, kind=kind)
        return T(t.ap(), name)

    def _sem_key(self, key):
        if key not in self.sem_names:
            self.sem_names.append(key)
        return key

    def op(self, eng, fn, reads=(), writes=(), dma=False, pe_acc=False):
        waits = {}

        def need(dep):
            if dep is None:
                return
            k, v, e = dep
            if e == eng and not dma and not self.same_engine_sync and not k[0] == "dma":
                return
            if e == eng and eng == "pe" and k[0] != "dma":
                return
            if waits.get(k, 0) < v:
                waits[k] = v

        for t in reads:
            need(t.w)
        for t in writes:
            if not (pe_acc and t.w is not None and t.w[2] == "pe" and eng == "pe"):
                need(t.w)
            for d in t.r:
                need(d)
        if dma:
            i = self.dma_rr[eng]
            self.dma_rr[eng] = (i + 1) % N_DMA_SEMS
            key = self._sem_key(("dma", eng, i))
            prev = self.dma_val.get(key, 0)
            if prev:
                if waits.get(key, 0) < prev:
                    waits[key] = prev
            val = prev + 16
            self.dma_val[key] = val
            inc = 16
        else:
            c = self.cnt[eng]
            self.cnt[eng] = c + 1
            key = self._sem_key(("eng", eng, c // SEM_CHUNK))
            val = (c % SEM_CHUNK) + 1
            inc = 1
        wl = []
        wd = self.waited[eng]
        for k, v in waits.items():
            if wd.get(k, 0) >= v:
                continue
            wd[k] = v
            wl.append((k, v))
        self.ops[eng].append((fn, wl, key, inc))
        tok = (key, val, eng)
        self.last_tok[key] = val
        for t in reads:
            t.r.append(tok)
        for t in writes:
            t.w = tok
            t.r = []
        return tok

    def mark(self):
        return len(self.ctx)

    def free_to(self, mark):
        while len(self.ctx) > mark:
            g = self.ctx.pop()
            g.__exit__(None, None, None)

    def barrier(self):
        for eng in self.ENGS:
            wl = []
            wd = self.waited[eng]
            for k, v in self.last_tok.items():
                if k[0] == "eng" and k[1] == eng:
                    continue
                if wd.get(k, 0) >= v:
                    continue
                wd[k] = v
                wl.append((k, v))
            if wl:
                self.ops[eng].append((None, wl, None, 0))

    def finish_wait(self, eng, toks):
        self.final_waits.append((eng, toks))

    def emit(self):
        nc = self.nc
        for key in self.sem_names:
            if key not in self.sems:
                g = nc.semaphore("s%d" % len(self.sems))
                self.sems[key] = g.__enter__()
                self.sem_guards.append(g)
        sems = self.sems
        ops = self.ops
        final_waits = self.final_waits

        def run(engname, h):
            for fn, wl, key, inc in ops[engname]:
                for k, v in wl:
                    h.wait_ge(sems[k], v)
                if fn is not None:
                    ins = fn(h)
                    ins.then_inc(sems[key], inc)
            for e, toks in final_waits:
                if e == engname:
                    for (k, v, _) in toks:
                        h.wait_ge(sems[k], v)

        with nc.Block() as block:
            @block.tensor
            def _(h):
                run("pe", h)

            @block.scalar
            def _(h):
                run("act", h)

            @block.vector
            def _(h):
                run("dve", h)

            @block.gpsimd
            def _(h):
                run("pool", h)

            @block.sync
            def _(h):
                run("sp", h)
        self.ops = {e: [] for e in self.ENGS}
        self.final_waits = []

    def close(self):
        self.free_to(0)
        for g in reversed(self.sem_guards):
            g.__exit__(None, None, None)


D = 1024
CTX = 256
INC = 3096
EPS = 1e-6
NEG_E05 = -0.6065306597126334


class MK:
    def __init__(self, T, L=2, dbg=None):
        self.T = T
        self.L = L
        self.NT = CTX + T
        self.dbg = dbg or set()
        nc = bass.Bass("TRN2", target_bir_lowering=False)
        self.nc = nc
        self.k = KB(nc)
        self.decl()

    def decl(self):
        k = self.k
        T, L, NT = self.T, self.L, self.NT
        I = lambda n, s: k.dram(n, s, F32, kind="ExternalInput")
        self.x = I("x", [T, D]); self.c = I("c", [D]); self.ctx = I("ctx", [CTX, D]); self.c_ctx = I("c_ctx", [D])
        self.ada_w = I("ada_w", [L, D, 6 * D]); self.ada_b = I("ada_b", [L, 6 * D])
        self.norm1_g = I("norm1_g", [L, D]); self.norm2_g = I("norm2_g", [L, D])
        self.w_in = I("w_in", [L, D, INC]); self.w_out = I("w_out", [L, D, D])
        self.rw_mu_prev = I("rw_mu_prev", [L, 1024]); self.rw_mu_next = I("rw_mu_next", [L, 1024])
        self.rw_w0 = I("rw_w0", [L, 2, 256]); self.rw_w2 = I("rw_w2", [L, 2, 64, 256])
        self.rw_a0 = I("rw_a0", [L, 2, 256]); self.rw_a2 = I("rw_a2", [L, 2, 64, 256])
        self.rw_g2 = I("rw_g2", [L, 128, 256])
        self.rw_k_k = I("rw_k_k", [L, 256]); self.rw_k_a = I("rw_k_a", [L, 256]); self.rw_r_k = I("rw_r_k", [L, 256])
        self.rw_gn_g = I("rw_gn_g", [L, 256]); self.rw_gn_b = I("rw_gn_b", [L, 256])
        self.ssm_conv_w = I("ssm_conv_w", [L, 3, 3, 768]); self.ssm_conv_b = I("ssm_conv_b", [L, 768])
        self.ssm_dt_bias = I("ssm_dt_bias", [L, 2, 8]); self.ssm_a_log = I("ssm_a_log", [L, 2, 8])
        self.ssm_d = I("ssm_d", [L, 8]); self.ssm_norm_g = I("ssm_norm_g", [L, 512])
        self.gla_ga2 = I("gla_ga2", [L, 2, 16, 128]); self.gla_gb = I("gla_gb", [L, 2, 128]); self.gla_norm_g = I("gla_norm_g", [L, 256])
        self.moe_rg_w = I("moe_rg_w", [L, D, 4]); self.moe_rg_b = I("moe_rg_b", [L, 4])
        self.moe_re_w = I("moe_re_w", [L, D, 16]); self.moe_re_b = I("moe_re_b", [L, 16])
        self.moe_w1 = I("moe_w1", [L, 16, D, 512]); self.moe_w3 = I("moe_w3", [L, 16, D, 512]); self.moe_w2 = I("moe_w2", [L, 16, 512, D])
        self.final_g = I("final_g", [D])
        self.c_ident = I("c_ident", [128, 128])
        self.out = k.dram("out", [T, D], F32, kind="ExternalOutput")
        def S(n, s, dt=F32):
            kind = "ExternalOutput" if n in self.dbg else "Internal"
            return k.dram(n, s, dt, kind=kind)
        self.xT = S("xT", [D, NT])
        self.uT = S("uT", [3200, NT])
        self.mixT = S("mixT", [D, NT])
        self.rw_consts()
        self.ssm_decl()
        self.moe_decl()

    def consts(self):
        k = self.k
        self.identf = k.sb("identf", [128, 128], F32)
        self.ident = k.sb("ident", [128, 128], BF16)
        self.ones = k.sb("ones", [128, 128], BF16)
        k.op("sp", lambda e: e.dma_start(out=self.identf[:, :], in_=self.c_ident.ap[:, :]), writes=[self.identf], dma=True)
        k.op("dve", lambda e: e.tensor_copy(self.ident[:, :], self.identf[:, :]), reads=[self.identf], writes=[self.ident])
        k.op("dve", lambda e: e.memset(self.ones[:, :], 1.0), writes=[self.ones])
        self.modT = k.sb("modT", [128, self.L, 48, 2], F32)
        self.gm = k.sb("gm", [128, self.L, 2, 8, 2], F32)
        self.load_consts2()

    def phase_mod(self):
        k = self.k
        L = self.L
        m = k.mark()
        cf = k.sb("cf", [128, 8, 2], F32)
        cb = k.sb("cb", [128, 8, 2], BF16)
        adab = k.sb("adab", [128, 48], F32)
        ng = k.sb("ng", [128, 2, 8], F32)
        aw = k.sb("aw", [128, 8, 3072], BF16)
        pm = k.ps("pm", [128, 48, 2], F32)
        k.op("sp", lambda e: e.dma_start(out=cf[:, :, 0], in_=self.c.ap.rearrange("(c p) -> p c", p=128), allow_slow_non_contiguous=True), writes=[cf], dma=True)
        k.op("sp", lambda e: e.dma_start(out=cf[:, :, 1], in_=self.c_ctx.ap.rearrange("(c p) -> p c", p=128), allow_slow_non_contiguous=True), writes=[cf], dma=True)
        k.op("act", lambda e: e.activation(out=cb[:, :, :], in_=cf[:, :, :], func=AF.Silu), reads=[cf], writes=[cb])
        for l in range(L):
            k.op("sp", lambda e, l=l: e.dma_start(out=adab[:, :], in_=self.ada_b.ap[l].rearrange("(c p) -> p c", p=128), allow_slow_non_contiguous=True), writes=[adab], dma=True)
            k.op("sp", lambda e, l=l: e.dma_start(out=ng[:, 0, :], in_=self.norm1_g.ap[l].rearrange("(c p) -> p c", p=128), allow_slow_non_contiguous=True), writes=[ng], dma=True)
            k.op("sp", lambda e, l=l: e.dma_start(out=ng[:, 1, :], in_=self.norm2_g.ap[l].rearrange("(c p) -> p c", p=128), allow_slow_non_contiguous=True), writes=[ng], dma=True)
            for half in range(2):
                for c in range(8):
                    k.op("pool", lambda e, l=l, c=c, half=half: e.dma_start(out=aw[:, c, :], in_=self.ada_w.ap[l, c * 128:(c + 1) * 128, half * 3072:(half + 1) * 3072]), writes=[aw], dma=True)
                for j in range(24):
                    for c in range(8):
                        k.op("pe", lambda e, c=c, j=j, half=half: e.matmul(pm[:, half * 24 + j, :], lhsT=aw[:, c, j * 128:(j + 1) * 128], rhs=cb[:, c, :], start=(c == 0), stop=(c == 7)), reads=[aw, cb], writes=[pm], pe_acc=True)
            k.op("dve", lambda e, l=l: e.tensor_tensor(self.modT[:, l, :, :], pm[:, :, :], adab[:, :].unsqueeze(2).to_broadcast([128, 48, 2]), ALU.add), reads=[pm, adab], writes=[self.modT])
            for w, j in ((0, 1), (1, 4)):
                k.op("dve", lambda e, l=l, w=w, j=j: e.scalar_tensor_tensor(out=self.gm[:, l, w, :, :], in0=self.modT[:, l, j * 8:(j + 1) * 8, :], scalar=1.0, in1=ng[:, w, :].unsqueeze(2).to_broadcast([128, 8, 2]), op0=ALU.add, op1=ALU.mult), reads=[self.modT, ng], writes=[self.gm])
        k.barrier()
        k.emit()
        k.free_to(m)

    def tok_tiles(self, lat_only=False, n=512):
        tiles = []
        if not lat_only:
            tiles.append((0, CTX, 1))
        for i in range(self.T // n):
            tiles.append((CTX + i * n, n, 0))
        return tiles

    def phase_x_in(self):
        k = self.k
        m = k.mark()
        xin = [k.sb("xin%d" % i, [128, D], F32) for i in range(2)]
        pt = [k.ps("ptx%d" % i, [128, 4, 128], F32) for i in range(2)]
        xo = [k.sb("xo%d" % i, [128, 8, 128], F32) for i in range(2)]
        nt = self.NT // 128
        for i in range(nt):
            src = self.ctx.ap[i * 128:(i + 1) * 128, :] if i < 2 else self.x.ap[(i - 2) * 128:(i - 1) * 128, :]
            b = i % 2
            k.op("sp", lambda e, b=b, src=src: e.dma_start(out=xin[b][:, :], in_=src), writes=[xin[b]], dma=True)
            for hh in range(2):
                for c in range(4):
                    cc = hh * 4 + c
                    k.op("pe", lambda e, b=b, hh=hh, c=c, cc=cc: e.transpose(pt[hh][:, c, :], xin[b][:, cc * 128:(cc + 1) * 128], self.identf[:, :]), reads=[xin[b], self.identf], writes=[pt[hh]], pe_acc=True)
                eng = "dve" if hh == 0 else "act"
                if eng == "dve":
                    k.op("dve", lambda e, b=b, hh=hh: e.tensor_copy(xo[b][:, hh * 4:(hh + 1) * 4, :], pt[hh][:, :, :]), reads=[pt[hh]], writes=[xo[b]])
                else:
                    k.op("act", lambda e, b=b, hh=hh: e.copy(xo[b][:, hh * 4:(hh + 1) * 4, :], pt[hh][:, :, :]), reads=[pt[hh]], writes=[xo[b]])
            k.op("pool", lambda e, b=b, i=i: e.dma_start(out=self.xT.ap.rearrange("(c p) t -> p c t", p=128)[:, :, i * 128:(i + 1) * 128], in_=xo[b][:, :, :]), reads=[xo[b]], dma=True)
        k.barrier()
        k.emit()
        k.free_to(m)

    def norm_tile(self, l, which, tok0, n, stream, xt, sq, pss, rstd, hT, keep_f32=False):
        k = self.k
        k.op("act", lambda e: e.activation(out=sq[:, :, :n], in_=xt[:, :, :n], func=AF.Square), reads=[xt], writes=[sq])
        for c in range(8):
            k.op("pe", lambda e, c=c: e.matmul(pss[:, :n], lhsT=self.ones[:, :], rhs=sq[:, c, :n], start=(c == 0), stop=(c == 7)), reads=[sq, self.ones], writes=[pss], pe_acc=True)
        k.op("act", lambda e: e.activation(out=rstd[:, :n], in_=pss[:, :n], func=AF.Sqrt, scale=1.0 / D, bias=EPS), reads=[pss], writes=[rstd])
        k.op("dve", lambda e: e.reciprocal(rstd[:, :n], rstd[:, :n]), reads=[rstd], writes=[rstd])
        k.op("dve", lambda e: e.tensor_tensor(xt[:, :, :n], xt[:, :, :n], rstd[:, :n].unsqueeze(1).to_broadcast([128, 8, n]), ALU.mult), reads=[xt, rstd], writes=[xt])
        sh_j = 0 if which == 0 else 3
        k.op("pool", lambda e: e.tensor_tensor(xt[:, :, :n], xt[:, :, :n], self.gm[:, l, which, :, stream].unsqueeze(2).to_broadcast([128, 8, n]), ALU.mult), reads=[xt, self.gm], writes=[xt])
        if keep_f32:
            k.op("dve", lambda e: e.tensor_tensor(xt[:, :, :n], xt[:, :, :n], self.modT[:, l, sh_j * 8:(sh_j + 1) * 8, stream].unsqueeze(2).to_broadcast([128, 8, n]), ALU.add), reads=[xt, self.modT], writes=[xt])
            k.op("act", lambda e: e.copy(hT[:, :, :n], xt[:, :, :n]), reads=[xt], writes=[hT])
        else:
            k.op("dve", lambda e: e.tensor_tensor(hT[:, :, :n], xt[:, :, :n], self.modT[:, l, sh_j * 8:(sh_j + 1) * 8, stream].unsqueeze(2).to_broadcast([128, 8, n]), ALU.add), reads=[xt, self.modT], writes=[hT])

    def phase_inproj(self, l):
        k = self.k
        m = k.mark()
        win = k.sb("win", [128, 8, INC], BF16)
        for c in range(8):
            k.op("pool", lambda e, c=c: e.dma_start(out=win[:, c, :], in_=self.w_in.ap[l, c * 128:(c + 1) * 128, :]), writes=[win], dma=True)
        xt = [k.sb("xt%d" % i, [128, 8, 512], F32) for i in range(2)]
        sq = k.sb("sq", [128, 8, 512], BF16)
        rstd = k.sb("rstd", [128, 512], F32)
        hT = [k.sb("hT%d" % i, [128, 8, 512], BF16) for i in range(2)]
        pss = k.ps("pss", [128, 512], F32)
        pu = [k.ps("pu%d" % i, [128, 512], F32) for i in range(4)]
        us = [k.sb("us%d" % i, [128, 512], F32) for i in range(4)]
        xTv = self.xT.ap.rearrange("(c p) t -> p c t", p=128)
        nchunk = (INC + 127) // 128
        cnt = 0
        for ti, (tok0, n, stream) in enumerate(self.tok_tiles()):
            b = ti % 2
            k.op("sp", lambda e, b=b, tok0=tok0, n=n: e.dma_start(out=xt[b][:, :, :n], in_=xTv[:, :, tok0:tok0 + n]), writes=[xt[b]], dma=True)
            self.norm_tile(l, 0, tok0, n, stream, xt[b], sq, pss, rstd, hT[b])
            for j in range(nchunk):
                c0 = j * 128
                nc_ = min(128, INC - c0)
                pb = cnt % 4
                cnt += 1
                for c in range(8):
                    k.op("pe", lambda e, c=c, c0=c0, nc_=nc_, pb=pb, b=b, n=n: e.matmul(pu[pb][:nc_, :n], lhsT=win[:, c, c0:c0 + nc_], rhs=hT[b][:, c, :n], start=(c == 0), stop=(c == 7)), reads=[win, hT[b]], writes=[pu[pb]], pe_acc=True)
                if j % 2 == 0:
                    k.op("dve", lambda e, pb=pb, nc_=nc_, n=n: e.tensor_copy(us[pb][:nc_, :n], pu[pb][:nc_, :n]), reads=[pu[pb]], writes=[us[pb]])
                else:
                    k.op("act", lambda e, pb=pb, nc_=nc_, n=n: e.copy(us[pb][:nc_, :n], pu[pb][:nc_, :n]), reads=[pu[pb]], writes=[us[pb]])
                k.op("pool", lambda e, pb=pb, nc_=nc_, n=n, c0=c0, tok0=tok0: e.dma_start(out=self.uT.ap[c0:c0 + nc_, tok0:tok0 + n], in_=us[pb][:nc_, :n]), reads=[us[pb]], dma=True)
        k.barrier()
        k.emit()
        k.free_to(m)

    def finish(self, last_tensor):
        k = self.k
        k.barrier()
        k.emit()
        k.close()


def _rw_consts(self):
    k = self.k
    I = lambda n, s: k.dram(n, s, F32, kind="ExternalInput")
    self.c_masks = I("c_masks", [4, 128, 512])
    self.c_id8 = I("c_id8", [128, 512])
    self.c_blk = I("c_blk", [128, 128])
    self.c_reset = I("c_reset", [128, 512])
    def S(n, s, dt=F32):
        kind = "ExternalOutput" if n in self.dbg else "Internal"
        return k.dram(n, s, dt, kind=kind)
    self.rw_yf = S("rw_yf", [256, self.NT])
    self.rw_bonus = S("rw_bonus", [256, self.NT])
    self.rw_gate = S("rw_gate", [256, self.NT])


def _load_consts2(self):
    k = self.k
    self.masks = k.sb("masks", [128, 4, 512], BF16)
    self.id8 = k.sb("id8", [128, 512], BF16)
    self.blk = k.sb("blk", [128, 128], BF16)
    self.reset = k.sb("reset", [128, 512], F32)
    for i in range(4):
        k.op("pool", lambda e, i=i: e.dma_start(out=self.masks[:, i, :], in_=self.c_masks.ap[i]), writes=[self.masks], dma=True)
    k.op("pool", lambda e: e.dma_start(out=self.id8[:, :], in_=self.c_id8.ap[:, :]), writes=[self.id8], dma=True)
    k.op("pool", lambda e: e.dma_start(out=self.blk[:, :], in_=self.c_blk.ap[:, :]), writes=[self.blk], dma=True)
    k.op("sp", lambda e: e.dma_start(out=self.reset[:, :], in_=self.c_reset.ap[:, :]), writes=[self.reset], dma=True)


def _pvec(self, name, src_ap, ncols, eng="sp", dt=F32):
    k = self.k
    t = k.sb(name, [128, ncols], dt)
    k.op(eng, lambda e: e.dma_start(out=t[:, :], in_=src_ap.rearrange("(c p) -> p c", p=128), allow_slow_non_contiguous=True), writes=[t], dma=True)
    return t


def phase_rwkv(self, l, d):
    k = self.k
    m = k.mark()
    NT = self.NT
    s = NEG_E05
    mp = self._pvec("mp", self.rw_mu_prev.ap[l], 8)
    mn = self._pvec("mn", self.rw_mu_next.ap[l], 8)
    cmix = k.sb("cmix", [128, 8], F32)
    k.op("dve", lambda e: e.tensor_tensor(cmix[:, :], mp[:, :], mn[:, :], ALU.add), reads=[mp, mn], writes=[cmix])
    k.op("dve", lambda e: e.tensor_scalar(cmix[:, :], cmix[:, :], -1.0, 1.0, ALU.mult, ALU.add), reads=[cmix], writes=[cmix])
    w0 = self._pvec("w0", self.rw_w0.ap[l, d], 2)
    a0 = self._pvec("a0", self.rw_a0.ap[l, d], 2)
    k_k = self._pvec("k_k", self.rw_k_k.ap[l], 2)
    k_a = self._pvec("k_a", self.rw_k_a.ap[l], 2)
    r_k = self._pvec("r_k", self.rw_r_k.ap[l], 2)
    gn_g = self._pvec("gn_g", self.rw_gn_g.ap[l], 2)
    gn_b = self._pvec("gn_b", self.rw_gn_b.ap[l], 2)
    w2s = k.sb("w2s", [128, 256], BF16)
    a2s = k.sb("a2s", [128, 256], BF16)
    g2s = k.sb("g2s", [128, 256], BF16)
    k.op("pool", lambda e: e.dma_start(out=w2s[0:64, :], in_=self.rw_w2.ap[l, d]), writes=[w2s], dma=True)
    k.op("pool", lambda e: e.dma_start(out=a2s[64:128, :], in_=self.rw_a2.ap[l, d]), writes=[a2s], dma=True)
    k.op("pool", lambda e: e.dma_start(out=g2s[:, :], in_=self.rw_g2.ap[l]), writes=[g2s], dma=True)

    U = [k.sb("rU%d" % i, [128, 8, 514], F32) for i in range(2)]
    S = k.sb("rS", [128, 8, 512], F32)
    wlt = k.sb("wlt", [128, 512], BF16)
    alb = k.sb("alb", [128, 512], BF16)
    sgl = k.sb("sgl", [128, 512], BF16)
    f32t = lambda n_: k.sb(n_, [128, 8, 64], F32)
    bft = lambda n_: k.sb(n_, [128, 8, 64], BF16)
    sgw, asig, kx, rn, cs, dcs, tmp, E, Etrue = [f32t("r_" + z) for z in ("sgw", "asig", "kx", "rn", "cs", "dcs", "tmp", "E", "Etrue")]
    kkn, bvec, kmod = f32t("kkn"), f32t("bvec"), f32t("kmod")
    sqk = bft("sqk")
    rt, kt, bt, at, Rtrue, Atrue, Bh, Kh, vb = [[bft("r_%s%d" % (z, hp)) for hp in range(2)] for z in ("rt", "kt", "bt", "at", "Rtrue", "Atrue", "Bh", "Kh", "vb")]
    WC = [k.sb("WC%d" % hp, [128, 8], F32) for hp in range(2)]
    Atm, Bhtm, Khtm, Vtm = [[bft("r_%s%d" % (z, hp)) for hp in range(2)] for z in ("Atm", "Bhtm", "Khtm", "Vtm")]
    Zs, Ns, Zs2, Ns2, Aak, Arb, Ark, X, AVs, PaT, Us = [[bft("r_%s%d" % (z, hp)) for hp in range(2)] for z in ("Zs", "Ns", "Zs2", "Ns2", "Aak", "Arb", "Ark", "X", "AVs", "PaT", "Us")]
    Qs = [f32t("Qs%d" % hp) for hp in range(2)]
    Mst = [k.sb("Mst%d" % hp, [128, 64], F32) for hp in range(2)]
    Mbf = [k.sb("Mbf%d" % hp, [128, 64], BF16) for hp in range(2)]
    ysb = [k.sb("ysb%d" % hp, [128, 512], F32) for hp in range(2)]
    bon = k.sb("bon", [128, 512], F32)
    gat = k.sb("gat", [128, 512], F32)
    yf_in = [k.sb("yfin%d" % hp, [128, 512], F32) for hp in range(2)]
    PS = [k.ps("rps%d" % i, [128, 512], F32) for i in range(2)]
    PY = [[k.ps("rpy%d%d" % (i, j), [128, 512], F32) for j in range(2)] for i in range(2)]
    PSB = [k.ps("rpsb%d" % i, [128, 8, 64], BF16) for i in range(2)]
    psi = [0]
    psbi = [0]

    def nps():
        p = PS[psi[0] % 2]
        psi[0] += 1
        return p

    def npsb():
        p = PSB[psbi[0] % 2]
        psbi[0] += 1
        return p

    for hp in range(2):
        k.op("dve", lambda e, hp=hp: e.memset(Mst[hp][:, :], 0.0), writes=[Mst[hp]])
        k.op("dve", lambda e, hp=hp: e.memset(Mbf[hp][:, :], 0.0), writes=[Mbf[hp]])

    M_SL, M_LE, M_SG, M_GE = 0, 1, 2, 3
    if d == 0:
        mZ, mN, mI = M_SL, M_SG, M_LE
    else:
        mZ, mN, mI = M_SG, M_SL, M_GE

    tiles = self.tok_tiles()
    lat = tiles[1:]
    order = [tiles[0]] + (lat if d == 0 else lat[::-1])
    uTv = self.uT.ap[0:1024, :].rearrange("(c p) t -> p c t", p=128)

    def v3(t, P, nch):
        return t[P, 0:nch, :]

    def v2(t, P, n):
        return t[P, :, :].rearrange("p c t -> p (c t)")[:, 0:n]

    def tile_body(ti, tok0, n, stream):
        nch = n // 64
        seq0, seq1 = (0, CTX) if stream == 1 else (CTX, NT)
        ub = U[ti % 2]
        lo = max(tok0 - 1, seq0)
        hi = min(tok0 + n + 1, seq1)
        if lo > tok0 - 1:
            k.op("pool", lambda e: e.memset(ub[:, :, 0:1], 0.0), writes=[ub])
        if hi < tok0 + n + 1:
            k.op("pool", lambda e: e.memset(ub[:, :, n + 1:n + 2], 0.0), writes=[ub])
        k.op("sp", lambda e: e.dma_start(out=ub[:, :, lo - (tok0 - 1):hi - (tok0 - 1)], in_=uTv[:, :, lo:hi]), writes=[ub], dma=True)
        fl = lambda t_: t_[:, :, :].rearrange("p c t -> p (c t)")[:, 0:n]

        def shift(c):
            k.op("dve", lambda e: e.tensor_scalar(S[:, c, :n], ub[:, c, 1:n + 1], cmix[:, c:c + 1], None, ALU.mult), reads=[ub, cmix], writes=[S])
            k.op("dve", lambda e: e.scalar_tensor_tensor(out=S[:, c, :n], in0=ub[:, c, 0:n], scalar=mp[:, c:c + 1], in1=S[:, c, :n], op0=ALU.mult, op1=ALU.add), reads=[ub, mp, S], writes=[S])
            k.op("dve", lambda e: e.scalar_tensor_tensor(out=S[:, c, :n], in0=ub[:, c, 2:n + 2], scalar=mn[:, c:c + 1], in1=S[:, c, :n], op0=ALU.mult, op1=ALU.add), reads=[ub, mn, S], writes=[S])
        for c in range(8):
            if d == 1 and c == 7:
                continue
            shift(c)
        k.op("act", lambda e: e.activation(out=wlt[0:64, :n], in_=S[0:64, 6, :n], func=AF.Tanh), reads=[S], writes=[wlt])
        k.op("act", lambda e: e.copy(alb[64:128, :n], S[64:128, 6, :n]), reads=[S], writes=[alb])
        if d == 0:
            k.op("act", lambda e: e.activation(out=sgl[:, :n], in_=S[:, 7, :n], func=AF.Sigmoid), reads=[S], writes=[sgl])

        def prep(hp):
            p1 = nps()
            k.op("pe", lambda e: e.matmul(p1[:, :n], lhsT=w2s[0:64, hp * 128:(hp + 1) * 128], rhs=wlt[0:64, :n], start=True, stop=True), reads=[w2s, wlt], writes=[p1])
            k.op("act", lambda e: e.activation(out=fl(sgw), in_=p1[:, :n], func=AF.Sigmoid, bias=w0[:, hp:hp + 1]), reads=[p1, w0], writes=[sgw])
            p2 = nps()
            k.op("pe", lambda e: e.matmul(p2[:, :n], lhsT=a2s[64:128, hp * 128:(hp + 1) * 128], rhs=alb[64:128, :n], start=True, stop=True), reads=[a2s, alb], writes=[p2])
            k.op("act", lambda e: e.activation(out=fl(asig), in_=p2[:, :n], func=AF.Sigmoid, bias=a0[:, hp:hp + 1]), reads=[p2, a0], writes=[asig])
            k.op("dve", lambda e: e.tensor_scalar(fl(kx), S[:, 2 + hp, :n], k_k[:, hp:hp + 1], None, ALU.mult), reads=[S, k_k], writes=[kx])
            k.op("act", lambda e: e.activation(out=fl(sqk), in_=fl(kx), func=AF.Square), reads=[kx], writes=[sqk])
            p3 = nps()
            k.op("pe", lambda e: e.matmul(p3[:, :n], lhsT=self.blk[:, :], rhs=fl(sqk), start=True, stop=True), reads=[self.blk, sqk], writes=[p3])
            k.op("act", lambda e: e.activation(out=fl(rn), in_=p3[:, :n], func=AF.Sqrt, bias=1e-12), reads=[p3], writes=[rn])
            k.op("dve", lambda e: e.reciprocal(fl(rn), fl(rn)), reads=[rn], writes=[rn])
            k.op("dve", lambda e: e.tensor_tensor(fl(kkn), fl(kx), fl(rn), ALU.mult), reads=[kx, rn], writes=[kkn])
            k.op("pool", lambda e: e.tensor_tensor(fl(bvec), fl(kkn), fl(asig), ALU.mult), reads=[kkn, asig], writes=[bvec])
            k.op("dve", lambda e: e.tensor_scalar(fl(tmp), fl(asig), -1.0, k_a[:, hp:hp + 1], ALU.add, ALU.mult), reads=[asig, k_a], writes=[tmp])
            k.op("dve", lambda e: e.scalar_tensor_tensor(out=fl(kmod), in0=fl(tmp), scalar=1.0, in1=S[:, 2 + hp, :n], op0=ALU.add, op1=ALU.mult), reads=[tmp, S], writes=[kmod])
            k.op("dve", lambda e: e.tensor_tensor_scan(fl(cs), self.reset[:, :n], fl(sgw), 0.0, ALU.mult, ALU.add), reads=[self.reset, sgw], writes=[cs])
            if d == 1:
                k.op("dve", lambda e: e.tensor_tensor(fl(tmp), fl(sgw), fl(cs), ALU.subtract), reads=[sgw, cs], writes=[tmp])
                k.op("dve", lambda e: e.tensor_tensor(cs[:, :nch, :], tmp[:, :nch, :], cs[:, :nch, 63:64].to_broadcast([128, nch, 64]), ALU.add), reads=[tmp, cs], writes=[cs])
            endi = 63 if d == 0 else 0
            k.op("pool", lambda e: e.tensor_tensor(dcs[:, :nch, :], cs[:, :nch, :], cs[:, :nch, 32:33].to_broadcast([128, nch, 64]), ALU.subtract), reads=[cs], writes=[dcs])
            k.op("act", lambda e: e.activation(out=fl(E), in_=fl(dcs), func=AF.Exp, scale=s), reads=[dcs], writes=[E])
            k.op("dve", lambda e: e.tensor_tensor(fl(rt[hp]), S[:, hp, :n], fl(E), ALU.mult), reads=[S, E], writes=[rt[hp]])
            k.op("pool", lambda e: e.tensor_tensor(fl(tmp), fl(dcs), fl(sgw), ALU.subtract), reads=[dcs, sgw], writes=[tmp])
            k.op("act", lambda e: e.activation(out=fl(E), in_=fl(tmp), func=AF.Exp, scale=s), reads=[tmp], writes=[E])
            k.op("dve", lambda e: e.scalar_tensor_tensor(out=fl(at[hp]), in0=fl(kkn), scalar=-1.0, in1=fl(E), op0=ALU.mult, op1=ALU.mult), reads=[kkn, E], writes=[at[hp]])
            k.op("act", lambda e: e.activation(out=fl(E), in_=fl(dcs), func=AF.Exp, scale=-s), reads=[dcs], writes=[E])
            k.op("dve", lambda e: e.tensor_tensor(fl(kt[hp]), fl(kmod), fl(E), ALU.mult), reads=[kmod, E], writes=[kt[hp]])
            k.op("pool", lambda e: e.tensor_tensor(fl(bt[hp]), fl(bvec), fl(E), ALU.mult), reads=[bvec, E], writes=[bt[hp]])
            k.op("act", lambda e: e.activation(out=fl(Etrue), in_=fl(cs), func=AF.Exp, scale=s), reads=[cs], writes=[Etrue])
            k.op("dve", lambda e: e.tensor_tensor(fl(Rtrue[hp]), S[:, hp, :n], fl(Etrue), ALU.mult), reads=[S, Etrue], writes=[Rtrue[hp]])
            k.op("dve", lambda e: e.tensor_copy(WC[hp][:, :nch], Etrue[:, :nch, endi]), reads=[Etrue], writes=[WC[hp]])
            k.op("pool", lambda e: e.tensor_tensor(fl(tmp), fl(cs), fl(sgw), ALU.subtract), reads=[cs, sgw], writes=[tmp])
            k.op("act", lambda e: e.activation(out=fl(E), in_=fl(tmp), func=AF.Exp, scale=s), reads=[tmp], writes=[E])
            k.op("dve", lambda e: e.scalar_tensor_tensor(out=fl(Atrue[hp]), in0=fl(kkn), scalar=-1.0, in1=fl(E), op0=ALU.mult, op1=ALU.mult), reads=[kkn, E], writes=[Atrue[hp]])
            k.op("pool", lambda e: e.tensor_tensor(tmp[:, :nch, :], cs[:, :nch, endi:endi + 1].to_broadcast([128, nch, 64]), cs[:, :nch, :], ALU.subtract), reads=[cs], writes=[tmp])
            k.op("act", lambda e: e.activation(out=fl(E), in_=fl(tmp), func=AF.Exp, scale=s), reads=[tmp], writes=[E])
            k.op("dve", lambda e: e.tensor_tensor(fl(Bh[hp]), fl(bvec), fl(E), ALU.mult), reads=[bvec, E], writes=[Bh[hp]])
            k.op("pool", lambda e: e.tensor_tensor(fl(Kh[hp]), fl(kmod), fl(E), ALU.mult), reads=[kmod, E], writes=[Kh[hp]])
            k.op("act", lambda e: e.copy(fl(vb[hp]), S[:, 4 + hp, :n]), reads=[S], writes=[vb[hp]])
            if d == 0:
                k.op("dve", lambda e: e.scalar_tensor_tensor(out=fl(sqk), in0=S[:, hp, :n], scalar=r_k[:, hp:hp + 1], in1=S[:, 2 + hp, :n], op0=ALU.mult, op1=ALU.mult), reads=[S, r_k], writes=[sqk])
                p4 = nps()
                k.op("pe", lambda e: e.matmul(p4[:, :n], lhsT=self.blk[:, :], rhs=fl(sqk), start=True, stop=True), reads=[self.blk, sqk], writes=[p4])
                k.op("dve", lambda e: e.tensor_tensor(bon[:, :n], p4[:, :n], S[:, 4 + hp, :n], ALU.mult), reads=[p4, S], writes=[bon])
                k.op("pool", lambda e: e.dma_start(out=self.rw_bonus.ap[hp * 128:(hp + 1) * 128, tok0:tok0 + n], in_=bon[:, :n]), reads=[bon], dma=True)
                p5 = nps()
                k.op("pe", lambda e: e.matmul(p5[:, :n], lhsT=g2s[:, hp * 128:(hp + 1) * 128], rhs=sgl[:, :n], start=True, stop=True), reads=[g2s, sgl], writes=[p5])
                k.op("act", lambda e: e.copy(gat[:, :n], p5[:, :n]), reads=[p5], writes=[gat])
                k.op("pool", lambda e: e.dma_start(out=self.rw_gate.ap[hp * 128:(hp + 1) * 128, tok0:tok0 + n], in_=gat[:, :n]), reads=[gat], dma=True)

        def units(dst_ps, lt, rt_, P, reads):
            pv = dst_ps[:, :].rearrange("p (c t) -> p c t", t=64)
            for ch in range(nch):
                k.op("pe", lambda e, ch=ch: e.matmul(pv[P, ch, :], lhsT=lt[P, ch, :], rhs=rt_[P, ch, :], start=True, stop=True), reads=reads, writes=[dst_ps], pe_acc=True)
            return pv

        def head_block(hp, hl):
            P = slice(hl * 64, hl * 64 + 64)

            def tm(src, dst):
                pb = npsb()
                for ch in range(nch):
                    k.op("pe", lambda e, ch=ch: e.transpose(pb[P, ch, :], src[hp][P, ch, :], self.ident[P, P]), reads=[src[hp], self.ident], writes=[pb], pe_acc=True)
                k.op("act", lambda e: e.copy(dst[hp][P, :nch, :], pb[P, :nch, :]), reads=[pb], writes=[dst[hp]])
            tm(Atrue, Atm); tm(Bh, Bhtm); tm(Kh, Khtm); tm(vb, Vtm)

            def pair(dst, lt, rt_, mask, eng="dve"):
                pp = nps()
                pv = units(pp, lt[hp], rt_[hp], P, [lt[hp], rt_[hp]])
                mv = self.masks[:, mask, :].rearrange("p (c t) -> p c t", t=64)
                k.op(eng, lambda e: e.tensor_tensor(dst[hp][P, :nch, :], pv[P, :nch, :], mv[P, :nch, :], ALU.mult), reads=[pp, self.masks], writes=[dst[hp]])
            pair(Zs, bt, at, mZ)
            pair(Ns, at, bt, mN)
            pair(Aak, kt, at, mZ)
            pair(Arb, bt, rt, mI)
            pair(Ark, kt, rt, mI)
            idv = self.id8[:, :].rearrange("p (c t) -> p c t", t=64)
            k.op("pool", lambda e: e.tensor_tensor(X[hp][P, :nch, :], Zs[hp][P, :nch, :], idv[P, :nch, :], ALU.add), reads=[Zs[hp], self.id8], writes=[X[hp]])
            Zc, Nc, Zn, Nn = Zs[hp], Ns[hp], Zs2[hp], Ns2[hp]
            for lev in range(1, 6):
                last = lev == 5

                def level(Zc, Nc, Zn, Nn, last):
                    if not last:
                        pz = nps()
                        pzv = units(pz, Nc, Zc, P, [Nc, Zc])
                    pn = nps()
                    pnv = units(pn, Zc, Nc, P, [Nc, Zc])
                    if not last:
                        k.op("act", lambda e: e.copy(Zn[P, :nch, :], pzv[P, :nch, :]), reads=[pz], writes=[Zn])
                    k.op("dve", lambda e: e.tensor_copy(Nn[P, :nch, :], pnv[P, :nch, :]), reads=[pn], writes=[Nn])
                    px = nps()
                    pxv = units(px, Nn, X[hp], P, [Nn, X[hp]])
                    k.op("dve", lambda e: e.tensor_tensor(X[hp][P, :nch, :], pxv[P, :nch, :], X[hp][P, :nch, :], ALU.add), reads=[px, X[hp]], writes=[X[hp]])
                level(Zc, Nc, Zn, Nn, last)
                Zc, Nc, Zn, Nn = Zn, Nn, Zc, Nc
            pa = nps()
            pav = units(pa, Aak[hp], Vtm[hp], P, [Aak[hp], Vtm[hp]])
            k.op("act", lambda e: e.copy(AVs[hp][P, :nch, :], pav[P, :nch, :]), reads=[pa], writes=[AVs[hp]])
            pq = nps()
            pqv = units(pq, X[hp], AVs[hp], P, [X[hp], AVs[hp]])
            k.op("act", lambda e: e.copy(Qs[hp][P, :nch, :], pqv[P, :nch, :]), reads=[pq], writes=[Qs[hp]])
            pp_ = nps()
            ppv_ = units(pp_, Atm[hp], X[hp], P, [Atm[hp], X[hp]])
            k.op("dve", lambda e: e.tensor_copy(PaT[hp][P, :nch, :], ppv_[P, :nch, :]), reads=[pp_], writes=[PaT[hp]])

        for hp in range(2):
            prep(hp)
            for hl in range(2):
                head_block(hp, hl)

        def seq_step(ch, hp, hl):
            P = slice(hl * 64, hl * 64 + 64)
            pyb = PY[hp][hl]
            pu_ = nps()
            k.op("pe", lambda e: e.matmul(pu_[P, 0:64], lhsT=PaT[hp][P, ch, :], rhs=Mbf[hp][P, :], start=True, stop=True), reads=[PaT[hp], Mbf[hp]], writes=[pu_])
            k.op("dve", lambda e: e.tensor_tensor(Us[hp][P, ch, :], pu_[P, 0:64], Qs[hp][P, ch, :], ALU.add), reads=[pu_, Qs[hp]], writes=[Us[hp]])
            pyv = pyb[:, :].rearrange("p (c t) -> p c t", t=64)
            k.op("pe", lambda e: e.matmul(pyv[P, ch, :], lhsT=Mbf[hp][P, :], rhs=Rtrue[hp][P, ch, :], start=True, stop=False), reads=[Mbf[hp], Rtrue[hp]], writes=[pyb], pe_acc=True)
            k.op("pe", lambda e: e.matmul(pyv[P, ch, :], lhsT=Vtm[hp][P, ch, :], rhs=Ark[hp][P, ch, :], start=False, stop=False), reads=[Vtm[hp], Ark[hp]], writes=[pyb], pe_acc=True)
            k.op("pe", lambda e: e.matmul(pyv[P, ch, :], lhsT=Us[hp][P, ch, :], rhs=Arb[hp][P, ch, :], start=False, stop=True), reads=[Us[hp], Arb[hp]], writes=[pyb], pe_acc=True)
            pm_ = nps()
            k.op("pe", lambda e: e.matmul(pm_[P, 0:64], lhsT=Bhtm[hp][P, ch, :], rhs=Us[hp][P, ch, :], start=True, stop=False), reads=[Bhtm[hp], Us[hp]], writes=[pm_], pe_acc=True)
            k.op("pe", lambda e: e.matmul(pm_[P, 0:64], lhsT=Khtm[hp][P, ch, :], rhs=Vtm[hp][P, ch, :], start=False, stop=True), reads=[Khtm[hp], Vtm[hp]], writes=[pm_], pe_acc=True)
            k.op("dve", lambda e: e.scalar_tensor_tensor(out=Mst[hp][P, :], in0=Mst[hp][P, :], scalar=WC[hp][P, ch:ch + 1], in1=pm_[P, 0:64], op0=ALU.mult, op1=ALU.add), reads=[Mst[hp], WC[hp], pm_], writes=[Mst[hp]])
            k.op("act", lambda e: e.copy(Mbf[hp][P, :], Mst[hp][P, :]), reads=[Mst[hp]], writes=[Mbf[hp]])

        chs = range(nch) if d == 0 else range(nch - 1, -1, -1)
        for ch in chs:
            for hp in range(2):
                for hl in range(2):
                    seq_step(ch, hp, hl)

        def outp(hp):
            for hl in range(2):
                P = slice(hl * 64, hl * 64 + 64)
                pp = PY[hp][hl]
                if hl == 0:
                    k.op("dve", lambda e, P=P, pp=pp: e.tensor_copy(ysb[hp][P, :n], pp[P, :n]), reads=[pp], writes=[ysb[hp]])
                else:
                    k.op("act", lambda e, P=P, pp=pp: e.copy(ysb[hp][P, :n], pp[P, :n]), reads=[pp], writes=[ysb[hp]])
            if d == 0:
                k.op("pool", lambda e: e.dma_start(out=self.rw_yf.ap[hp * 128:(hp + 1) * 128, tok0:tok0 + n], in_=ysb[hp][:, :n]), reads=[ysb[hp]], dma=True)
            else:
                self.rw_finish(l, hp, tok0, n, ysb[hp], yf_in[hp], bon, gat, gn_g, gn_b, nps, sqk, tmp, kx, rn)
        for hp in range(2):
            outp(hp)

    for ti, (tok0, n, stream) in enumerate(order):
        tile_body(ti, tok0, n, stream)
    k.barrier()
    k.emit()
    k.free_to(m)


def rw_finish(self, l, hp, tok0, n, yb, yf, bon, gat, gn_g, gn_b, nps, sqk, tmp, kx, rn):
    k = self.k
    fl = lambda t_: t_[:, :, :].rearrange("p c t -> p (c t)")[:, 0:n]
    k.op("sp", lambda e: e.dma_start(out=yf[:, :n], in_=self.rw_yf.ap[hp * 128:(hp + 1) * 128, tok0:tok0 + n]), writes=[yf], dma=True)
    k.op("sp", lambda e: e.dma_start(out=bon[:, :n], in_=self.rw_bonus.ap[hp * 128:(hp + 1) * 128, tok0:tok0 + n]), writes=[bon], dma=True)
    k.op("sp", lambda e: e.dma_start(out=gat[:, :n], in_=self.rw_gate.ap[hp * 128:(hp + 1) * 128, tok0:tok0 + n]), writes=[gat], dma=True)
    k.op("dve", lambda e: e.tensor_tensor(yb[:, :n], yb[:, :n], yf[:, :n], ALU.add), reads=[yb, yf], writes=[yb])
    k.op("act", lambda e: e.copy(fl(sqk), yb[:, :n]), reads=[yb], writes=[sqk])
    p1 = nps()
    k.op("pe", lambda e: e.matmul(p1[:, :n], lhsT=self.blk[:, :], rhs=fl(sqk), start=True, stop=True), reads=[self.blk, sqk], writes=[p1])
    k.op("dve", lambda e: e.scalar_tensor_tensor(out=fl(kx), in0=p1[:, :n], scalar=-1.0 / 64, in1=yb[:, :n], op0=ALU.mult, op1=ALU.add), reads=[p1, yb], writes=[kx])
    k.op("act", lambda e: e.copy(fl(sqk), fl(kx)), reads=[kx], writes=[sqk])
    p1b = nps()
    k.op("pe", lambda e: e.matmul(p1b[:, :n], lhsT=self.blk[:, :], rhs=fl(sqk), start=True, stop=True), reads=[self.blk, sqk], writes=[p1b])
    k.op("dve", lambda e: e.scalar_tensor_tensor(out=fl(kx), in0=p1b[:, :n], scalar=-1.0 / 64, in1=fl(kx), op0=ALU.mult, op1=ALU.add), reads=[p1b, kx], writes=[kx])
    k.op("act", lambda e: e.activation(out=fl(sqk), in_=fl(kx), func=AF.Square), reads=[kx], writes=[sqk])
    p2 = nps()
    k.op("pe", lambda e: e.matmul(p2[:, :n], lhsT=self.blk[:, :], rhs=fl(sqk), start=True, stop=True), reads=[self.blk, sqk], writes=[p2])
    k.op("act", lambda e: e.activation(out=fl(rn), in_=p2[:, :n], func=AF.Sqrt, scale=1.0 / 64, bias=64e-5), reads=[p2], writes=[rn])
    k.op("dve", lambda e: e.reciprocal(fl(rn), fl(rn)), reads=[rn], writes=[rn])
    k.op("dve", lambda e: e.tensor_tensor(fl(kx), fl(kx), fl(rn), ALU.mult), reads=[kx, rn], writes=[kx])
    k.op("dve", lambda e: e.tensor_scalar(fl(kx), fl(kx), gn_g[:, hp:hp + 1], gn_b[:, hp:hp + 1], ALU.mult, ALU.add), reads=[kx, gn_g, gn_b], writes=[kx])
    k.op("pool", lambda e: e.tensor_tensor(fl(kx), fl(kx), bon[:, :n], ALU.add), reads=[kx, bon], writes=[kx])
    k.op("dve", lambda e: e.tensor_tensor(fl(kx), fl(kx), gat[:, :n], ALU.mult), reads=[kx, gat], writes=[kx])
    k.op("pool", lambda e: e.dma_start(out=self.mixT.ap[hp * 128:(hp + 1) * 128, tok0:tok0 + n], in_=fl(kx)), reads=[kx], dma=True)


MK.rw_consts = _rw_consts
MK.load_consts2 = _load_consts2
MK._pvec = _pvec
MK.phase_rwkv = phase_rwkv
MK.rw_finish = rw_finish


GLA_OFF = 2312


def phase_gla(self, l, d):
    k = self.k
    m = k.mark()
    NT = self.NT
    s = 1.0 / 16.0
    qscale = 32 ** -0.5
    if not hasattr(self, "gla_yf"):
        kind = "ExternalOutput" if "gla_yf" in self.dbg else "Internal"
        self.gla_yf = k.dram("gla_yf", [256, NT], F32, kind=kind)
    ga2f = k.sb("ga2f", [16, 2, 128], F32)
    ga2p = k.sb("ga2p", [16, 2, 128], BF16)
    gbp = k.sb("gbp", [128, 2], F32)
    k.op("dve", lambda e: e.memset(ga2f[:, :, :], 0.0), writes=[ga2f])
    k.op("dve", lambda e: e.memset(gbp[:, :], 0.0), writes=[gbp])
    for h in range(4):
        hp, hl = h // 2, h % 2
        k.op("sp", lambda e, h=h, hp=hp, hl=hl: e.dma_start(out=ga2f[:, hp, hl * 64:hl * 64 + 32], in_=self.gla_ga2.ap[l, d, :, h * 32:(h + 1) * 32]), writes=[ga2f], dma=True)
        k.op("sp", lambda e, h=h, hp=hp, hl=hl: e.dma_start(out=gbp[hl * 64:hl * 64 + 32, hp:hp + 1], in_=self.gla_gb.ap[l, d, h * 32:(h + 1) * 32].rearrange("(p o) -> p o", o=1), allow_slow_non_contiguous=True), writes=[gbp], dma=True)
    k.op("dve", lambda e: e.tensor_copy(ga2p[:, :, :], ga2f[:, :, :]), reads=[ga2f], writes=[ga2p])
    ng = self._pvec("gng", self.gla_norm_g.ap[l], 2)

    f32t = lambda n_: k.sb(n_, [128, 8, 64], F32)
    bft = lambda n_: k.sb(n_, [128, 8, 64], BF16)
    q = [[f32t("gq%d%d" % (i, hp)) for hp in range(2)] for i in range(2)]
    kk_ = [[f32t("gk%d%d" % (i, hp)) for hp in range(2)] for i in range(2)]
    vv = [[f32t("gv%d%d" % (i, hp)) for hp in range(2)] for i in range(2)]
    glf = [k.sb("glf%d" % i, [16, 512], F32) for i in range(2)]
    glb = k.sb("glb", [16, 512], BF16)
    for i in range(2):
        for hp in range(2):
            k.op("pool", lambda e, i=i, hp=hp: e.memset(q[i][hp][:, :, :], 0.0), writes=[q[i][hp]])
            k.op("pool", lambda e, i=i, hp=hp: e.memset(kk_[i][hp][:, :, :], 0.0), writes=[kk_[i][hp]])
    lg, cs, dcs, tmp, E, Etrue = [f32t("g_" + z) for z in ("lg", "cs", "dcs", "tmp", "E", "Etrue")]
    qt, kt, Qtrue, Kh, vb, Khtm, Vtm, Ark = [[bft("g_%s%d" % (z, hp)) for hp in range(2)] for z in ("qt", "kt", "Qtrue", "Kh", "vb", "Khtm", "Vtm", "Ark")]
    WC = [k.sb("gWC%d" % hp, [128, 8], F32) for hp in range(2)]
    Mst = [k.sb("gMst%d" % hp, [128, 64], F32) for hp in range(2)]
    Mbf = [k.sb("gMbf%d" % hp, [128, 64], BF16) for hp in range(2)]
    ysb = [k.sb("gysb%d" % hp, [128, 512], F32) for hp in range(2)]
    yfin = [k.sb("gyfin%d" % hp, [128, 512], F32) for hp in range(2)]
    rin = [k.sb("grin%d" % hp, [128, 512], F32) for hp in range(2)]
    sq = k.sb("gsq", [128, 512], BF16)
    rn = k.sb("grn", [128, 512], F32)
    PS = [k.ps("gps%d" % i, [128, 512], F32) for i in range(2)]
    PY = [[k.ps("gpy%d%d" % (i, j), [128, 512], F32) for j in range(2)] for i in range(2)]
    PSB = [k.ps("gpsb%d" % i, [128, 8, 64], BF16) for i in range(2)]
    psi = [0]; psbi = [0]

    def nps():
        p = PS[psi[0] % 2]; psi[0] += 1; return p

    def npsb():
        p = PSB[psbi[0] % 2]; psbi[0] += 1; return p
    for hp in range(2):
        k.op("dve", lambda e, hp=hp: e.memset(Mst[hp][:, :], 0.0), writes=[Mst[hp]])
        k.op("dve", lambda e, hp=hp: e.memset(Mbf[hp][:, :], 0.0), writes=[Mbf[hp]])
    mI = 1 if d == 0 else 3
    tiles = self.tok_tiles()
    lat = tiles[1:]
    order = [tiles[0]] + (lat if d == 0 else lat[::-1])
    O = GLA_OFF

    def tile_body(ti, tok0, n, stream):
        nch = n // 64
        b = ti % 2
        fl = lambda t_: t_[:, :, :].rearrange("p c t -> p (c t)")[:, 0:n]
        for h in range(4):
            hp, hl = h // 2, h % 2
            P32 = slice(hl * 64, hl * 64 + 32)
            k.op("sp", lambda e, h=h, hp=hp, P32=P32: e.dma_start(out=fl(q[b][hp])[P32, :], in_=self.uT.ap[O + h * 32:O + (h + 1) * 32, tok0:tok0 + n]), writes=[q[b][hp]], dma=True)
            k.op("sp", lambda e, h=h, hp=hp, P32=P32: e.dma_start(out=fl(kk_[b][hp])[P32, :], in_=self.uT.ap[O + 128 + h * 32:O + 128 + (h + 1) * 32, tok0:tok0 + n]), writes=[kk_[b][hp]], dma=True)
        for hp in range(2):
            k.op("sp", lambda e, hp=hp: e.dma_start(out=fl(vv[b][hp]), in_=self.uT.ap[O + 256 + hp * 128:O + 256 + (hp + 1) * 128, tok0:tok0 + n]), writes=[vv[b][hp]], dma=True)
        k.op("sp", lambda e: e.dma_start(out=glf[b][:, :n], in_=self.uT.ap[O + 512:O + 528, tok0:tok0 + n]), writes=[glf[b]], dma=True)
        k.op("act", lambda e: e.copy(glb[:, :n], glf[b][:, :n]), reads=[glf[b]], writes=[glb])

        def prep(hp):
            p1 = nps()
            k.op("pe", lambda e: e.matmul(p1[:, :n], lhsT=ga2p[:, hp, :], rhs=glb[:, :n], start=True, stop=True), reads=[ga2p, glb], writes=[p1])
            k.op("act", lambda e: e.activation(out=fl(tmp), in_=p1[:, :n], func=AF.Sigmoid, bias=gbp[:, hp:hp + 1]), reads=[p1, gbp], writes=[tmp])
            k.op("act", lambda e: e.activation(out=fl(lg), in_=fl(tmp), func=AF.Ln), reads=[tmp], writes=[lg])
            k.op("dve", lambda e: e.tensor_tensor_scan(fl(cs), self.reset[:, :n], fl(lg), 0.0, ALU.mult, ALU.add), reads=[self.reset, lg], writes=[cs])
            if d == 1:
                k.op("dve", lambda e: e.tensor_tensor(fl(tmp), fl(lg), fl(cs), ALU.subtract), reads=[lg, cs], writes=[tmp])
                k.op("dve", lambda e: e.tensor_tensor(cs[:, :nch, :], tmp[:, :nch, :], cs[:, :nch, 63:64].to_broadcast([128, nch, 64]), ALU.add), reads=[tmp, cs], writes=[cs])
            endi = 63 if d == 0 else 0
            k.op("pool", lambda e: e.tensor_tensor(dcs[:, :nch, :], cs[:, :nch, :], cs[:, :nch, 32:33].to_broadcast([128, nch, 64]), ALU.subtract), reads=[cs], writes=[dcs])
            k.op("act", lambda e: e.activation(out=fl(E), in_=fl(dcs), func=AF.Exp, scale=s), reads=[dcs], writes=[E])
            k.op("dve", lambda e: e.scalar_tensor_tensor(out=fl(qt[hp]), in0=fl(q[b][hp]), scalar=qscale, in1=fl(E), op0=ALU.mult, op1=ALU.mult), reads=[q[b][hp], E], writes=[qt[hp]])
            k.op("act", lambda e: e.activation(out=fl(E), in_=fl(dcs), func=AF.Exp, scale=-s), reads=[dcs], writes=[E])
            k.op("dve", lambda e: e.tensor_tensor(fl(kt[hp]), fl(kk_[b][hp]), fl(E), ALU.mult), reads=[kk_[b][hp], E], writes=[kt[hp]])
            k.op("act", lambda e: e.activation(out=fl(Etrue), in_=fl(cs), func=AF.Exp, scale=s), reads=[cs], writes=[Etrue])
            k.op("dve", lambda e: e.scalar_tensor_tensor(out=fl(Qtrue[hp]), in0=fl(q[b][hp]), scalar=qscale, in1=fl(Etrue), op0=ALU.mult, op1=ALU.mult), reads=[q[b][hp], Etrue], writes=[Qtrue[hp]])
            k.op("dve", lambda e: e.tensor_copy(WC[hp][:, :nch], Etrue[:, :nch, endi]), reads=[Etrue], writes=[WC[hp]])
            k.op("pool", lambda e: e.tensor_tensor(tmp[:, :nch, :], cs[:, :nch, endi:endi + 1].to_broadcast([128, nch, 64]), cs[:, :nch, :], ALU.subtract), reads=[cs], writes=[tmp])
            k.op("act", lambda e: e.activation(out=fl(E), in_=fl(tmp), func=AF.Exp, scale=s), reads=[tmp], writes=[E])
            k.op("dve", lambda e: e.tensor_tensor(fl(Kh[hp]), fl(kk_[b][hp]), fl(E), ALU.mult), reads=[kk_[b][hp], E], writes=[Kh[hp]])
            k.op("act", lambda e: e.copy(fl(vb[hp]), fl(vv[b][hp])), reads=[vv[b][hp]], writes=[vb[hp]])

        def head_block(hp, hl):
            P = slice(hl * 64, hl * 64 + 64)

            def tm(src, dst):
                pb = npsb()
                for ch in range(nch):
                    k.op("pe", lambda e, ch=ch: e.transpose(pb[P, ch, :], src[hp][P, ch, :], self.ident[P, P]), reads=[src[hp], self.ident], writes=[pb], pe_acc=True)
                k.op("act", lambda e: e.copy(dst[hp][P, :nch, :], pb[P, :nch, :]), reads=[pb], writes=[dst[hp]])
            tm(Kh, Khtm); tm(vb, Vtm)
            pp = nps()
            pv = pp[:, :].rearrange("p (c t) -> p c t", t=64)
            for ch in range(nch):
                k.op("pe", lambda e, ch=ch: e.matmul(pv[P, ch, :], lhsT=kt[hp][P, ch, :], rhs=qt[hp][P, ch, :], start=True, stop=True), reads=[kt[hp], qt[hp]], writes=[pp], pe_acc=True)
            mv = self.masks[:, mI, :].rearrange("p (c t) -> p c t", t=64)
            k.op("dve", lambda e: e.tensor_tensor(Ark[hp][P, :nch, :], pv[P, :nch, :], mv[P, :nch, :], ALU.mult), reads=[pp, self.masks], writes=[Ark[hp]])

        for hp in range(2):
            prep(hp)
            for hl in range(2):
                head_block(hp, hl)

        def seq_step(ch, hp, hl):
            P = slice(hl * 64, hl * 64 + 64)
            pyb = PY[hp][hl]
            pyv = pyb[:, :].rearrange("p (c t) -> p c t", t=64)
            k.op("pe", lambda e: e.matmul(pyv[P, ch, :], lhsT=Mbf[hp][P, :], rhs=Qtrue[hp][P, ch, :], start=True, stop=False), reads=[Mbf[hp], Qtrue[hp]], writes=[pyb], pe_acc=True)
            k.op("pe", lambda e: e.matmul(pyv[P, ch, :], lhsT=Vtm[hp][P, ch, :], rhs=Ark[hp][P, ch, :], start=False, stop=True), reads=[Vtm[hp], Ark[hp]], writes=[pyb], pe_acc=True)
            pm_ = nps()
            k.op("pe", lambda e: e.matmul(pm_[P, 0:64], lhsT=Khtm[hp][P, ch, :], rhs=Vtm[hp][P, ch, :], start=True, stop=True), reads=[Khtm[hp], Vtm[hp]], writes=[pm_])
            k.op("dve", lambda e: e.scalar_tensor_tensor(out=Mst[hp][P, :], in0=Mst[hp][P, :], scalar=WC[hp][P, ch:ch + 1], in1=pm_[P, 0:64], op0=ALU.mult, op1=ALU.add), reads=[Mst[hp], WC[hp], pm_], writes=[Mst[hp]])
            k.op("act", lambda e: e.copy(Mbf[hp][P, :], Mst[hp][P, :]), reads=[Mst[hp]], writes=[Mbf[hp]])

        chs = range(nch) if d == 0 else range(nch - 1, -1, -1)
        for ch in chs:
            for hp in range(2):
                for hl in range(2):
                    seq_step(ch, hp, hl)

        def outp(hp):
            for hl in range(2):
                P = slice(hl * 64, hl * 64 + 64)
                pp = PY[hp][hl]
                if hl == 0:
                    k.op("dve", lambda e, P=P, pp=pp: e.tensor_copy(ysb[hp][P, :n], pp[P, :n]), reads=[pp], writes=[ysb[hp]])
                else:
                    k.op("act", lambda e, P=P, pp=pp: e.copy(ysb[hp][P, :n], pp[P, :n]), reads=[pp], writes=[ysb[hp]])
            if d == 0:
                k.op("pool", lambda e: e.dma_start(out=self.gla_yf.ap[hp * 128:(hp + 1) * 128, tok0:tok0 + n], in_=ysb[hp][:, :n]), reads=[ysb[hp]], dma=True)
            else:
                yb, yf, rr = ysb[hp], yfin[hp], rin[hp]
                k.op("sp", lambda e: e.dma_start(out=yf[:, :n], in_=self.gla_yf.ap[hp * 128:(hp + 1) * 128, tok0:tok0 + n]), writes=[yf], dma=True)
                k.op("sp", lambda e: e.dma_start(out=rr[:, :n], in_=self.uT.ap[O + 528 + hp * 128:O + 528 + (hp + 1) * 128, tok0:tok0 + n]), writes=[rr], dma=True)
                k.op("dve", lambda e: e.tensor_tensor(yb[:, :n], yb[:, :n], yf[:, :n], ALU.add), reads=[yb, yf], writes=[yb])
                k.op("act", lambda e: e.activation(out=sq[:, :n], in_=yb[:, :n], func=AF.Square), reads=[yb], writes=[sq])
                p2 = nps()
                k.op("pe", lambda e: e.matmul(p2[:, :n], lhsT=self.blk[:, :], rhs=sq[:, :n], start=True, stop=True), reads=[self.blk, sq], writes=[p2])
                k.op("act", lambda e: e.activation(out=rn[:, :n], in_=p2[:, :n], func=AF.Sqrt, scale=1.0 / 64, bias=EPS), reads=[p2], writes=[rn])
                k.op("dve", lambda e: e.reciprocal(rn[:, :n], rn[:, :n]), reads=[rn], writes=[rn])
                k.op("dve", lambda e: e.scalar_tensor_tensor(out=yb[:, :n], in0=yb[:, :n], scalar=ng[:, hp:hp + 1], in1=rn[:, :n], op0=ALU.mult, op1=ALU.mult), reads=[yb, ng, rn], writes=[yb])
                k.op("act", lambda e: e.activation(out=rr[:, :n], in_=rr[:, :n], func=AF.Silu), reads=[rr], writes=[rr])
                k.op("dve", lambda e: e.tensor_tensor(yb[:, :n], yb[:, :n], rr[:, :n], ALU.mult), reads=[yb, rr], writes=[yb])
                k.op("pool", lambda e: e.dma_start(out=self.mixT.ap[768 + hp * 128:768 + (hp + 1) * 128, tok0:tok0 + n], in_=yb[:, :n]), reads=[yb], dma=True)
        for hp in range(2):
            outp(hp)

    for ti, (tok0, n, stream) in enumerate(order):
        tile_body(ti, tok0, n, stream)
    k.barrier()
    k.emit()
    k.free_to(m)


MK.phase_gla = phase_gla


SSM_OFF = 1024


def _ssm_decl(self):
    k = self.k
    NT = self.NT
    self.c_m128 = k.dram("c_m128", [5, 128, 128], F32, kind="ExternalInput")
    def S(n, s, dt=F32):
        kind = "ExternalOutput" if n in self.dbg else "Internal"
        return k.dram(n, s, dt, kind=kind)
    self.s_xtm = S("s_xtm", [NT, 768])
    self.s_ztm = S("s_ztm", [NT, 512])
    self.s_dttm = S("s_dttm", [NT, 8])
    self.s_bcT = S("s_bcT", [256, NT])
    self.s_yf = S("s_yf", [NT, 512])


def phase_ssm_conv(self, l):
    k = self.k
    m = k.mark()
    NT, T = self.NT, self.T
    cw = k.sb("cw", [128, 6, 9], F32)
    cbias = self._pvec("cbias", self.ssm_conv_b.ap[l], 6)
    for tap in range(9):
        k.op("sp", lambda e, tap=tap: e.dma_start(out=cw[:, :, tap], in_=self.ssm_conv_w.ap[l, tap // 3, tap % 3].rearrange("(c p) -> p c", p=128), allow_slow_non_contiguous=True), writes=[cw], dma=True)
    xin = [k.sb("cxin%d" % i, [128, 642], F32) for i in range(3)]
    acc = [k.sb("cacc%d" % i, [128, 512], F32) for i in range(2)]
    acc2 = [k.sb("cacc2%d" % i, [128, 512], F32) for i in range(2)]
    res = [k.sb("cres%d" % i, [128, 512], F32) for i in range(2)]
    zin = [k.sb("czin%d" % i, [128, 512], F32) for i in range(2)]
    dtin = [k.sb("cdtin%d" % i, [8, 512], F32) for i in range(2)]
    otm = [k.sb("cotm%d" % i, [128, 4, 128], F32) for i in range(2)]
    odt = [k.sb("codt%d" % i, [128, 4, 8], F32) for i in range(2)]
    PT = [k.ps("cpt%d" % i, [128, 4, 128], F32) for i in range(3)]
    pti = [0]
    cnt = [0]
    O = SSM_OFF

    def transpose_store(src, npart, dst_ap_fn, n, ob):
        pt = PT[pti[0] % 3]; pti[0] += 1
        nb = n // 128
        for tb in range(nb):
            k.op("pe", lambda e, tb=tb: e.transpose(pt[:, tb, :npart], src[:npart, tb * 128:(tb + 1) * 128], self.identf[:npart, :npart]), reads=[src, self.identf], writes=[pt], pe_acc=True)
        eng = "act" if cnt[0] % 2 else "dve"
        cnt[0] += 1
        if eng == "act":
            k.op("act", lambda e: e.copy(ob[:, :nb, :npart], pt[:, :nb, :npart]), reads=[pt], writes=[ob])
        else:
            k.op("dve", lambda e: e.tensor_copy(ob[:, :nb, :npart], pt[:, :nb, :npart]), reads=[pt], writes=[ob])
        k.op("pool", lambda e: e.dma_start(out=dst_ap_fn(nb), in_=ob[:, :nb, :npart]), reads=[ob], dma=True)

    it = [0]
    for (tok0, n, stream) in self.tok_tiles():
        seq0, seq1 = (0, CTX) if stream == 1 else (CTX, NT)
        halo = 65 if stream == 0 else 1
        for c in range(6):
            i = it[0]; it[0] += 1
            xb = xin[i % 3]; ac = acc[i % 2]; ac2 = acc2[i % 2]; rs = res[i % 2]
            lo = max(tok0 - halo, seq0); hi = min(tok0 + n + halo, seq1)
            if lo > tok0 - halo:
                k.op("pool", lambda e, xb=xb, halo=halo: e.memset(xb[:, 0:halo], 0.0), writes=[xb])
            if hi < tok0 + n + halo:
                k.op("pool", lambda e, xb=xb, halo=halo, n=n: e.memset(xb[:, halo + n:halo + n + halo], 0.0), writes=[xb])
            k.op("sp", lambda e, xb=xb, lo=lo, hi=hi, tok0=tok0, halo=halo, c=c: e.dma_start(out=xb[:, lo - (tok0 - halo):hi - (tok0 - halo)], in_=self.uT.ap[O + 512 + c * 128:O + 512 + (c + 1) * 128, lo:hi]), writes=[xb], dma=True)
            first = True
            dys = (-1, 0, 1) if stream == 0 else (0,)
            for dy in dys:
                for dx in (0, -1, 1):
                    tap = (dy + 1) * 3 + (dx + 1)
                    off = halo + dy * 64 + dx
                    if first:
                        k.op("dve", lambda e, ac=ac, xb=xb, off=off, n=n, c=c, tap=tap: e.tensor_scalar(ac[:, :n], xb[:, off:off + n], cw[:, c, tap:tap + 1], None, ALU.mult), reads=[xb, cw], writes=[ac])
                        first = False
                        continue
                    c0, c1 = (0, 64) if (dx == 0 or stream == 1) else ((1, 64) if dx == -1 else (0, 63))
                    def tapop(ac=ac, xb=xb, off=off, n=n, c=c, tap=tap, c0=c0, c1=c1):
                        src = xb[:, off:off + n].rearrange("p (r w) -> p r w", w=64)[:, :, c0:c1]
                        dst = ac[:, :n].rearrange("p (r w) -> p r w", w=64)[:, :, c0:c1]
                        k.op("dve", lambda e: e.scalar_tensor_tensor(out=dst, in0=src, scalar=cw[:, c, tap:tap + 1], in1=dst, op0=ALU.mult, op1=ALU.add), reads=[xb, cw, ac], writes=[ac])
                    tapop()
            k.op("act", lambda e, ac=ac, rs=rs, n=n, c=c: e.activation(out=rs[:, :n], in_=ac[:, :n], func=AF.Silu, bias=cbias[:, c:c + 1]), reads=[ac, cbias], writes=[rs])
            if c >= 4:
                k.op("pool", lambda e, rs=rs, n=n, c=c, tok0=tok0: e.dma_start(out=self.s_bcT.ap[(c - 4) * 128:(c - 3) * 128, tok0:tok0 + n], in_=rs[:, :n]), reads=[rs], dma=True)
            ob = otm[i % 2]
            transpose_store(rs, 128, lambda nb, c=c, tok0=tok0: self.s_xtm.ap[tok0:tok0 + nb * 128, c * 128:(c + 1) * 128].rearrange("(b p) f -> p b f", p=128), n, ob)
        for c in range(4):
            i = it[0]; it[0] += 1
            zb = zin[i % 2]
            k.op("sp", lambda e, zb=zb, c=c, tok0=tok0, n=n: e.dma_start(out=zb[:, :n], in_=self.uT.ap[O + c * 128:O + (c + 1) * 128, tok0:tok0 + n]), writes=[zb], dma=True)
            ob = otm[i % 2]
            transpose_store(zb, 128, lambda nb, c=c, tok0=tok0: self.s_ztm.ap[tok0:tok0 + nb * 128, c * 128:(c + 1) * 128].rearrange("(b p) f -> p b f", p=128), n, ob)
        i = it[0]; it[0] += 1
        db = dtin[i % 2]
        k.op("sp", lambda e, db=db, tok0=tok0, n=n: e.dma_start(out=db[:, :n], in_=self.uT.ap[O + 1280:O + 1288, tok0:tok0 + n]), writes=[db], dma=True)
        ob = odt[i % 2]
        transpose_store(db, 8, lambda nb, tok0=tok0: self.s_dttm.ap[tok0:tok0 + nb * 128, :].rearrange("(b p) f -> p b f", p=128), n, ob)
    k.barrier()
    k.emit()
    k.free_to(m)


def phase_ssm_scan(self, l, d):
    k = self.k
    m = k.mark()
    NT = self.NT
    BIG = 30000.0
    m128 = k.sb("m128", [128, 5, 128], F32)
    for i in range(5):
        k.op("sp", lambda e, i=i: e.dma_start(out=m128[:, i, :], in_=self.c_m128.ap[i]), writes=[m128], dma=True)
    LE, GE, GT, LT, NEGI = 0, 1, 2, 3, 4
    if d == 0:
        mTri, mR, mNeg = LE, GT, GT
    else:
        mTri, mR, mNeg = GE, LT, LT
    onesf = k.sb("onesf", [128, 128], F32)
    k.op("dve", lambda e: e.memset(onesf[:, :], 1.0), writes=[onesf])
    dtb = k.sb("dtb", [128, 8], F32)
    aneg = k.sb("aneg", [128, 8], F32)
    dsk = k.sb("dsk", [128, 8], F32)
    ngb = k.sb("ngb", [128, 512], F32)
    k.op("sp", lambda e: e.dma_start(out=dtb[:, :], in_=self.ssm_dt_bias.ap[l, d].partition_broadcast(128)), writes=[dtb], dma=True)
    k.op("sp", lambda e: e.dma_start(out=aneg[:, :], in_=self.ssm_a_log.ap[l, d].partition_broadcast(128)), writes=[aneg], dma=True)
    k.op("sp", lambda e: e.dma_start(out=dsk[:, :], in_=self.ssm_d.ap[l].partition_broadcast(128)), writes=[dsk], dma=True)
    k.op("sp", lambda e: e.dma_start(out=ngb[:, :], in_=self.ssm_norm_g.ap[l].partition_broadcast(128)), writes=[ngb], dma=True)
    k.op("act", lambda e: e.activation(out=aneg[:, :], in_=aneg[:, :], func=AF.Exp), reads=[aneg], writes=[aneg])
    k.op("dve", lambda e: e.tensor_scalar(aneg[:, :], aneg[:, :], -1.0, None, ALU.mult), reads=[aneg], writes=[aneg])

    xs = [k.sb("sxs%d" % i, [128, 768], F32) for i in range(2)]
    dt = [k.sb("sdt%d" % i, [128, 8], F32) for i in range(2)]
    BT = [[k.sb("sBT%d%d" % (i, g), [64, 128], F32) for g in range(2)] for i in range(2)]
    CT = [[k.sb("sCT%d%d" % (i, g), [64, 128], F32) for g in range(2)] for i in range(2)]
    BTb = [k.sb("sBTb%d" % g, [64, 128], BF16) for g in range(2)]
    CTb = [k.sb("sCTb%d" % g, [64, 128], BF16) for g in range(2)]
    Btm = k.sb("sBtm", [128, 128], BF16)
    zt = [k.sb("szt%d" % i, [128, 512], F32) for i in range(2)]
    yfin = [k.sb("syf%d" % i, [128, 512], F32) for i in range(2)]
    xdt = k.sb("sx", [128, 8], F32)
    dA = k.sb("sdA", [128, 8], F32)
    cs = k.sb("scs", [128, 8], F32)
    dte = k.sb("sdte", [128, 8], F32)
    ecs = k.sb("secs", [128, 8], F32)
    eend = k.sb("seend", [128, 8], F32)
    dtp = k.sb("sdtp", [128, 8], F32)
    Xd = k.sb("sXd", [128, 8, 64], BF16)
    Xdd = k.sb("sXdd", [128, 8, 64], BF16)
    Rm = [k.sb("sRm%d" % i, [128, 128], F32) for i in range(4)]
    Lt = k.sb("sLt", [128, 8, 128], BF16)
    sc = k.sb("ssc", [128, 2, 128], BF16)
    Gt = k.sb("sGt", [128, 8, 128], BF16)
    Yt = k.sb("sYt", [128, 512], F32)
    Y = k.sb("sY", [128, 512], F32)
    junk = k.sb("sjunk", [128, 512], BF16)
    ss = k.sb("sss", [128, 1], F32)
    Ybf = k.sb("sYbf", [128, 512], BF16)
    ofm = k.sb("sofm", [128, 4, 128], F32)
    Sst = k.sb("sSst", [64, 8, 64], F32)
    Stmp = k.sb("sStmp", [64, 8, 64], F32)
    Sbf = k.sb("sSbf", [64, 8, 64], BF16)
    k.op("dve", lambda e: e.memset(Sst[:, :, :], 0.0), writes=[Sst])
    k.op("dve", lambda e: e.memset(Sbf[:, :, :], 0.0), writes=[Sbf])
    pcs = k.ps("spcs", [128, 2, 8], F32)
    pD = [k.ps("spD%d" % i, [128, 4, 128], F32) for i in range(2)]
    psc = k.ps("spsc", [128, 2, 128], F32)
    pYd = k.ps("spYd", [128, 512], F32)
    pYo = k.ps("spYo", [128, 512], F32)
    pSt = k.ps("spSt", [64, 512], F32)
    pT = k.ps("spT", [128, 4, 128], BF16)

    nchunks = NT // 128
    ctxc = [0, 1]
    latc = list(range(2, nchunks))
    order = (ctxc + latc) if d == 0 else (ctxc[::-1] + latc[::-1])
    ri = [0]

    def chunk(ci, c):
        b = ci % 2
        t0 = c * 128
        x_, dt_, z_, yf_ = xs[b], dt[b], zt[b], yfin[b]
        k.op("sp", lambda e: e.dma_start(out=x_[:, :], in_=self.s_xtm.ap[t0:t0 + 128, :]), writes=[x_], dma=True)
        k.op("sp", lambda e: e.dma_start(out=dt_[:, :], in_=self.s_dttm.ap[t0:t0 + 128, :]), writes=[dt_], dma=True)
        for g in range(2):
            k.op("sp", lambda e, g=g: e.dma_start(out=BT[b][g][:, :], in_=self.s_bcT.ap[g * 64:(g + 1) * 64, t0:t0 + 128]), writes=[BT[b][g]], dma=True)
            k.op("sp", lambda e, g=g: e.dma_start(out=CT[b][g][:, :], in_=self.s_bcT.ap[128 + g * 64:128 + (g + 1) * 64, t0:t0 + 128]), writes=[CT[b][g]], dma=True)
        if d == 1:
            k.op("sp", lambda e: e.dma_start(out=z_[:, :], in_=self.s_ztm.ap[t0:t0 + 128, :]), writes=[z_], dma=True)
            k.op("sp", lambda e: e.dma_start(out=yf_[:, :], in_=self.s_yf.ap[t0:t0 + 128, :]), writes=[yf_], dma=True)
        for g in range(2):
            k.op("act", lambda e, g=g: e.copy(BTb[g][:, :], BT[b][g][:, :]), reads=[BT[b][g]], writes=[BTb[g]])
            k.op("act", lambda e, g=g: e.copy(CTb[g][:, :], CT[b][g][:, :]), reads=[CT[b][g]], writes=[CTb[g]])
        k.op("act", lambda e: e.copy(Btm[:, :], x_[:, 512:640]), reads=[x_], writes=[Btm])
        k.op("dve", lambda e: e.tensor_tensor(xdt[:, :], dt_[:, :], dtb[:, :], ALU.add), reads=[dt_, dtb], writes=[xdt])
        k.op("act", lambda e: e.activation(out=xdt[:, :], in_=xdt[:, :], func=AF.Exp), reads=[xdt], writes=[xdt])
        k.op("act", lambda e: e.activation(out=dtp[:, :], in_=xdt[:, :], func=AF.Ln, bias=1.0), reads=[xdt], writes=[dtp])
        k.op("dve", lambda e: e.tensor_tensor(dA[:, :], dtp[:, :], aneg[:, :], ALU.mult), reads=[dtp, aneg], writes=[dA])
        k.op("pe", lambda e: e.matmul(pcs[:, 0, :], lhsT=m128[:, mTri, :], rhs=dA[:, :], start=True, stop=True), reads=[m128, dA], writes=[pcs])
        k.op("pe", lambda e: e.matmul(pcs[:, 1, :], lhsT=onesf[:, :], rhs=dA[:, :], start=True, stop=True), reads=[onesf, dA], writes=[pcs], pe_acc=True)
        k.op("dve", lambda e: e.tensor_copy(cs[:, :], pcs[:, 0, :]), reads=[pcs], writes=[cs])
        k.op("act", lambda e: e.activation(out=ecs[:, :], in_=pcs[:, 0, :], func=AF.Exp), reads=[pcs], writes=[ecs])
        k.op("act", lambda e: e.activation(out=eend[:, :], in_=pcs[:, 1, :], func=AF.Exp), reads=[pcs], writes=[eend])
        k.op("dve", lambda e: e.tensor_tensor(dte[:, :], pcs[:, 1, :], cs[:, :], ALU.subtract), reads=[pcs, cs], writes=[dte])
        k.op("act", lambda e: e.activation(out=dte[:, :], in_=dte[:, :], func=AF.Exp), reads=[dte], writes=[dte])
        xv = x_[:, 0:512].rearrange("p (h e) -> p h e", e=64)
        k.op("dve", lambda e: e.tensor_tensor(Xd[:, :, :], xv, dtp[:, :].unsqueeze(2).to_broadcast([128, 8, 64]), ALU.mult), reads=[x_, dtp], writes=[Xd])
        k.op("pool", lambda e: e.tensor_tensor(Xdd[:, :, :], Xd[:, :, :], dte[:, :].unsqueeze(2).to_broadcast([128, 8, 64]), ALU.mult), reads=[Xd, dte], writes=[Xdd])
        for h in range(8):
            r_ = Rm[ri[0] % 4]; ri[0] += 1
            pd = pD[h // 4]
            k.op("dve" if h % 2 == 0 else "pool", lambda e, h=h, r_=r_: e.tensor_tensor(r_[:, :], m128[:, mR, :], dA[:, h:h + 1].to_broadcast([128, 128]), ALU.mult), reads=[m128, dA], writes=[r_])
            k.op("pe", lambda e, h=h, r_=r_, pd=pd: e.matmul(pd[:, h % 4, :], lhsT=r_[:, :], rhs=m128[:, mTri, :], start=True, stop=False), reads=[r_, m128], writes=[pd], pe_acc=True)
            k.op("pe", lambda e, h=h, pd=pd: e.matmul(pd[:, h % 4, :], lhsT=m128[:, NEGI, :], rhs=m128[:, mNeg, :], start=False, stop=True), reads=[m128], writes=[pd], pe_acc=True)
        for hh in range(2):
            k.op("act", lambda e, hh=hh: e.activation(out=Lt[:, hh * 4:(hh + 1) * 4, :], in_=pD[hh][:, :, :], func=AF.Exp), reads=[pD[hh]], writes=[Lt])
        for g in range(2):
            k.op("pe", lambda e, g=g: e.matmul(psc[:, g, :], lhsT=BTb[g][:, :], rhs=CTb[g][:, :], start=True, stop=True), reads=[BTb[g], CTb[g]], writes=[psc], pe_acc=True)
        k.op("dve", lambda e: e.tensor_copy(sc[:, :, :], psc[:, :, :]), reads=[psc], writes=[sc])
        for g in range(2):
            k.op("dve" if g == 0 else "pool", lambda e, g=g: e.tensor_tensor(Gt[:, g * 4:(g + 1) * 4, :], Lt[:, g * 4:(g + 1) * 4, :], sc[:, g:g + 1, :].to_broadcast([128, 4, 128]), ALU.mult), reads=[Lt, sc], writes=[Gt])
        for h in range(8):
            k.op("pe", lambda e, h=h: e.matmul(pYd[:, h * 64:(h + 1) * 64], lhsT=Gt[:, h, :], rhs=Xd[:, h, :], start=True, stop=True), reads=[Gt, Xd], writes=[pYd], pe_acc=True)
        for g in range(2):
            k.op("pe", lambda e, g=g: e.matmul(pYo[:, g * 256:(g + 1) * 256], lhsT=CTb[g][:, :], rhs=Sbf[:, g * 4:(g + 1) * 4, :].rearrange("p h e -> p (h e)"), start=True, stop=True), reads=[CTb[g], Sbf], writes=[pYo], pe_acc=True)
        k.op("dve", lambda e: e.tensor_tensor(Yt[:, :].rearrange("p (h e) -> p h e", e=64), pYo[:, :].rearrange("p (h e) -> p h e", e=64), ecs[:, :].unsqueeze(2).to_broadcast([128, 8, 64]), ALU.mult), reads=[pYo, ecs], writes=[Yt])
        k.op("dve", lambda e: e.tensor_tensor(Y[:, :], Yt[:, :], pYd[:, :], ALU.add), reads=[Yt, pYd], writes=[Y])
        for g in range(2):
            k.op("pe", lambda e, g=g: e.matmul(pSt[:, g * 256:(g + 1) * 256], lhsT=Btm[:, g * 64:(g + 1) * 64], rhs=Xdd[:, g * 4:(g + 1) * 4, :].rearrange("p h e -> p (h e)"), start=True, stop=True), reads=[Btm, Xdd], writes=[pSt], pe_acc=True)
        k.op("pool", lambda e: e.tensor_tensor(Stmp[:, :, :], Sst[:, :, :], eend[0:64, :].unsqueeze(2).to_broadcast([64, 8, 64]), ALU.mult), reads=[Sst, eend], writes=[Stmp])
        k.op("dve", lambda e: e.tensor_tensor(Sst[:, :, :], Stmp[:, :, :], pSt[:, :].rearrange("p (h e) -> p h e", e=64), ALU.add), reads=[Stmp, pSt], writes=[Sst])
        k.op("act", lambda e: e.copy(Sbf[:, :, :], Sst[:, :, :]), reads=[Sst], writes=[Sbf])
        if d == 0:
            k.op("pool", lambda e: e.dma_start(out=self.s_yf.ap[t0:t0 + 128, :], in_=Y[:, :]), reads=[Y], dma=True)
        else:
            k.op("dve", lambda e: e.tensor_tensor(Y[:, :], Y[:, :], yf_[:, :], ALU.add), reads=[Y, yf_], writes=[Y])
            k.op("pool", lambda e: e.tensor_tensor(Yt[:, :].rearrange("p (h e) -> p h e", e=64), xv, dsk[:, :].unsqueeze(2).to_broadcast([128, 8, 64]), ALU.mult), reads=[x_, dsk], writes=[Yt])
            k.op("dve", lambda e: e.tensor_tensor(Y[:, :], Y[:, :], Yt[:, :], ALU.add), reads=[Y, Yt], writes=[Y])
            k.op("act", lambda e: e.activation(out=z_[:, :], in_=z_[:, :], func=AF.Silu), reads=[z_], writes=[z_])
            k.op("dve", lambda e: e.tensor_tensor(Y[:, :], Y[:, :], z_[:, :], ALU.mult), reads=[Y, z_], writes=[Y])
            k.op("act", lambda e: e.activation(out=junk[:, :], in_=Y[:, :], func=AF.Square, accum_out=ss[:, 0:1]), reads=[Y], writes=[junk, ss])
            k.op("act", lambda e: e.activation(out=ss[:, 0:1], in_=ss[:, 0:1], func=AF.Sqrt, scale=1.0 / 512, bias=EPS), reads=[ss], writes=[ss])
            k.op("dve", lambda e: e.reciprocal(ss[:, 0:1], ss[:, 0:1]), reads=[ss], writes=[ss])
            k.op("dve", lambda e: e.scalar_tensor_tensor(out=Ybf[:, :], in0=Y[:, :], scalar=ss[:, 0:1], in1=ngb[:, :], op0=ALU.mult, op1=ALU.mult), reads=[Y, ss, ngb], writes=[Ybf])
            for j in range(4):
                k.op("pe", lambda e, j=j: e.transpose(pT[:, j, :], Ybf[:, j * 128:(j + 1) * 128], self.ident[:, :]), reads=[Ybf, self.ident], writes=[pT], pe_acc=True)
            k.op("act", lambda e: e.copy(ofm[:, :, :], pT[:, :, :]), reads=[pT], writes=[ofm])
            k.op("pool", lambda e: e.dma_start(out=self.mixT.ap[256:768, t0:t0 + 128].rearrange("(j p) t -> p j t", p=128), in_=ofm[:, :, :]), reads=[ofm], dma=True)

    for ci, c in enumerate(order):
        chunk(ci, c)
    k.barrier()
    k.emit()
    k.free_to(m)


MK.ssm_decl = _ssm_decl
MK.phase_ssm_conv = phase_ssm_conv
MK.phase_ssm_scan = phase_ssm_scan


def _moe_decl(self):
    k = self.k
    NT = self.NT
    def S(n, s, dt=F32):
        kind = "ExternalOutput" if n in self.dbg else "Internal"
        return k.dram(n, s, dt, kind=kind)
    self.h2T = S("h2T", [D, NT], BF16)
    self.combT = S("combT", [16, NT])
    self.c_sel = k.dram("c_sel", [16, 16, 128], F32, kind="ExternalInput")


def phase_outproj(self, l, lat_only):
    k = self.k
    m = k.mark()
    NT = self.NT
    wout = k.sb("wout", [128, 8, D], BF16)
    for c in range(8):
        k.op("pool", lambda e, c=c: e.dma_start(out=wout[:, c, :], in_=self.w_out.ap[l, c * 128:(c + 1) * 128, :]), writes=[wout], dma=True)
    wr = k.sb("wr", [128, 8, 20], F32)
    k.op("sp", lambda e: e.dma_start(out=wr[:, :, 0:4], in_=self.moe_rg_w.ap[l].rearrange("(c p) g -> p c g", p=128)), writes=[wr], dma=True)
    k.op("sp", lambda e: e.dma_start(out=wr[:, :, 4:20], in_=self.moe_re_w.ap[l].rearrange("(c p) g -> p c g", p=128)), writes=[wr], dma=True)
    rb = k.sb("rb", [128, 20], F32)
    k.op("sp", lambda e: e.dma_start(out=rb[:, 0:4], in_=self.moe_rg_b.ap[l].partition_broadcast(128)), writes=[rb], dma=True)
    k.op("sp", lambda e: e.dma_start(out=rb[:, 4:20], in_=self.moe_re_b.ap[l].partition_broadcast(128)), writes=[rb], dma=True)
    mixf = [k.sb("mixf%d" % i, [128, 8, 512], F32) for i in range(2)]
    mixb = k.sb("mixb", [128, 8, 512], BF16)
    xt = [k.sb("oxt%d" % i, [128, 8, 512], F32) for i in range(2)]
    sq = k.sb("osq", [128, 8, 512], BF16)
    rstd = k.sb("orstd", [128, 512], F32)
    hT = k.sb("ohT", [128, 8, 512], BF16)
    pss = k.ps("opss", [128, 512], F32)
    po = [k.ps("opo%d" % i, [128, 512], F32) for i in range(3)]
    plg = k.ps("oplg", [128, 4, 20], F32)
    pct = k.ps("opct", [16, 4, 128], F32)
    R_ = lambda n_, s_: k.sb(n_, [128] + s_, F32)
    lg = R_("rlg", [4, 20]); gmax = R_("rgmax", [4, 1]); ohg = R_("rohg", [4, 4]); eg = R_("reg", [4, 4]); gsum = R_("rgsum", [4, 1])
    pgr = R_("rpgr", [4, 1]); esel3 = R_("resel3", [4, 4, 4]); esel = R_("resel", [4, 4]); m1 = R_("rm1", [4, 1]); oh1 = R_("roh1", [4, 4])
    es2 = R_("res2", [4, 4]); m2 = R_("rm2", [4, 1]); oh2 = R_("roh2", [4, 4]); ex2 = R_("rex2", [4, 1]); den = R_("rden", [4, 1])
    w1_ = R_("rw1", [4, 1]); w2_ = R_("rw2", [4, 1]); cig = R_("rcig", [4, 4]); comb = R_("rcomb", [4, 4, 4]); tmp4 = R_("rtmp4", [4, 4])
    combT_sb = k.sb("combT_sb", [16, 4, 128], F32)
    xTv = self.xT.ap.rearrange("(c p) t -> p c t", p=128)
    mTv = self.mixT.ap.rearrange("(c p) t -> p c t", p=128)
    hTv = self.h2T.ap.rearrange("(c p) t -> p c t", p=128)
    pi = [0]

    def tile_body(ti, tok0, n, stream):
        b = ti % 2
        mf, x_ = mixf[b], xt[b]
        k.op("sp", lambda e: e.dma_start(out=mf[:, :, :n], in_=mTv[:, :, tok0:tok0 + n]), writes=[mf], dma=True)
        k.op("sp", lambda e: e.dma_start(out=x_[:, :, :n], in_=xTv[:, :, tok0:tok0 + n]), writes=[x_], dma=True)
        k.op("act", lambda e: e.copy(mixb[:, 0:4, :n], mf[:, 0:4, :n]), reads=[mf], writes=[mixb])
        k.op("pool", lambda e: e.tensor_copy(mixb[:, 4:8, :n], mf[:, 4:8, :n]), reads=[mf], writes=[mixb])
        for j in range(8):
            p_ = po[pi[0] % 3]; pi[0] += 1
            for c in range(8):
                k.op("pe", lambda e, c=c, j=j, p_=p_: e.matmul(p_[:, :n], lhsT=wout[:, c, j * 128:(j + 1) * 128], rhs=mixb[:, c, :n], start=(c == 0), stop=(c == 7)), reads=[wout, mixb], writes=[p_], pe_acc=True)
            k.op("dve", lambda e, j=j, p_=p_: e.scalar_tensor_tensor(out=x_[:, j, :n], in0=p_[:, :n], scalar=self.modT[:, l, 16 + j, stream:stream + 1], in1=x_[:, j, :n], op0=ALU.mult, op1=ALU.add), reads=[p_, self.modT, x_], writes=[x_])
        k.op("pool", lambda e: e.dma_start(out=xTv[:, :, tok0:tok0 + n], in_=x_[:, :, :n]), reads=[x_], dma=True)
        self.norm_tile(l, 1, tok0, n, stream, x_, sq, pss, rstd, hT, keep_f32=True)
        k.op("pool", lambda e: e.dma_start(out=hTv[:, :, tok0:tok0 + n], in_=hT[:, :, :n]), reads=[hT], dma=True)
        nb = n // 128
        for tb in range(nb):
            for c in range(8):
                k.op("pe", lambda e, tb=tb, c=c: e.matmul(plg[:, tb, :], lhsT=x_[:, c, tb * 128:(tb + 1) * 128], rhs=wr[:, c, :], start=(c == 0), stop=(c == 7)), reads=[x_, wr], writes=[plg], pe_acc=True)
        V = lambda t_, *idx: t_[(slice(None), slice(0, nb)) + idx]
        op = lambda fn, r, w: k.op("dve", fn, reads=r, writes=w)
        op(lambda e: e.tensor_tensor(lg[:, :nb, :], plg[:, :nb, :], rb[:, :].unsqueeze(1).to_broadcast([128, nb, 20]), ALU.add), [plg, rb], [lg])
        op(lambda e: e.tensor_reduce(gmax[:, :nb, :], lg[:, :nb, 0:4], AX.X, ALU.max), [lg], [gmax])
        op(lambda e: e.tensor_tensor(ohg[:, :nb, :], lg[:, :nb, 0:4], gmax[:, :nb, :].to_broadcast([128, nb, 4]), ALU.is_ge), [lg, gmax], [ohg])
        op(lambda e: e.tensor_tensor(eg[:, :nb, :], lg[:, :nb, 0:4], gmax[:, :nb, :].to_broadcast([128, nb, 4]), ALU.subtract), [lg, gmax], [eg])
        k.op("act", lambda e: e.activation(out=eg[:, :nb, :], in_=eg[:, :nb, :], func=AF.Exp), reads=[eg], writes=[eg])
        op(lambda e: e.tensor_reduce(gsum[:, :nb, :], eg[:, :nb, :], AX.X, ALU.add), [eg], [gsum])
        op(lambda e: e.reciprocal(pgr[:, :nb, :], gsum[:, :nb, :]), [gsum], [pgr])
        elv = lg[:, :nb, 4:20].rearrange("p b (g e) -> p b g e", e=4)
        op(lambda e: e.tensor_tensor(esel3[:, :nb, :, :], elv, ohg[:, :nb, :].unsqueeze(3).to_broadcast([128, nb, 4, 4]), ALU.mult), [lg, ohg], [esel3])
        op(lambda e: e.tensor_reduce(esel[:, :nb, :], esel3[:, :nb, :, :].rearrange("p b g e -> p b e g"), AX.X, ALU.add), [esel3], [esel])
        op(lambda e: e.tensor_reduce(m1[:, :nb, :], esel[:, :nb, :], AX.X, ALU.max), [esel], [m1])
        op(lambda e: e.tensor_tensor(oh1[:, :nb, :], esel[:, :nb, :], m1[:, :nb, :].to_broadcast([128, nb, 4]), ALU.is_ge), [esel, m1], [oh1])
        op(lambda e: e.scalar_tensor_tensor(out=es2[:, :nb, :], in0=oh1[:, :nb, :], scalar=-1e30, in1=esel[:, :nb, :], op0=ALU.mult, op1=ALU.add), [oh1, esel], [es2])
        op(lambda e: e.tensor_reduce(m2[:, :nb, :], es2[:, :nb, :], AX.X, ALU.max), [es2], [m2])
        op(lambda e: e.tensor_tensor(oh2[:, :nb, :], es2[:, :nb, :], m2[:, :nb, :].to_broadcast([128, nb, 4]), ALU.is_ge), [es2, m2], [oh2])
        op(lambda e: e.tensor_tensor(ex2[:, :nb, :], m2[:, :nb, :], m1[:, :nb, :], ALU.subtract), [m2, m1], [ex2])
        k.op("act", lambda e: e.activation(out=ex2[:, :nb, :], in_=ex2[:, :nb, :], func=AF.Exp), reads=[ex2], writes=[ex2])
        op(lambda e: e.tensor_scalar(den[:, :nb, :], ex2[:, :nb, :], 1.0, None, ALU.add), [ex2], [den])
        op(lambda e: e.reciprocal(den[:, :nb, :], den[:, :nb, :]), [den], [den])
        op(lambda e: e.tensor_tensor(w1_[:, :nb, :], den[:, :nb, :], pgr[:, :nb, :], ALU.mult), [den, pgr], [w1_])
        op(lambda e: e.tensor_tensor(w2_[:, :nb, :], w1_[:, :nb, :], ex2[:, :nb, :], ALU.mult), [w1_, ex2], [w2_])
        op(lambda e: e.tensor_tensor(cig[:, :nb, :], oh1[:, :nb, :], w1_[:, :nb, :].to_broadcast([128, nb, 4]), ALU.mult), [oh1, w1_], [cig])
        op(lambda e: e.tensor_tensor(tmp4[:, :nb, :], oh2[:, :nb, :], w2_[:, :nb, :].to_broadcast([128, nb, 4]), ALU.mult), [oh2, w2_], [tmp4])
        op(lambda e: e.tensor_tensor(cig[:, :nb, :], cig[:, :nb, :], tmp4[:, :nb, :], ALU.add), [cig, tmp4], [cig])
        for g in range(4):
            op(lambda e, g=g: e.tensor_tensor(comb[:, :nb, g, :], cig[:, :nb, :], ohg[:, :nb, g:g + 1].to_broadcast([128, nb, 4]), ALU.mult), [cig, ohg], [comb])
        for tb in range(nb):
            k.op("pe", lambda e, tb=tb: e.transpose(pct[:, tb, :], comb[:, tb, :, :].rearrange("p g e -> p (g e)"), self.identf[:, :]), reads=[comb, self.identf], writes=[pct], pe_acc=True)
        k.op("act", lambda e: e.copy(combT_sb[:, :nb, :], pct[:, :nb, :]), reads=[pct], writes=[combT_sb])
        k.op("pool", lambda e: e.dma_start(out=self.combT.ap[:, tok0:tok0 + n].rearrange("k (b t) -> k b t", t=128), in_=combT_sb[:, :nb, :]), reads=[combT_sb], dma=True)

    for ti, (tok0, n, stream) in enumerate(self.tok_tiles(lat_only=lat_only)):
        tile_body(ti, tok0, n, stream)
    k.barrier()
    k.emit()
    k.free_to(m)


def phase_moe(self, l, lat_only):
    k = self.k
    m = k.mark()
    NT, T = self.NT, self.T
    TTL = min(2048, T)
    tiles = []
    start = CTX if lat_only else 0
    first = True
    t = start
    while t < NT:
        if first and not lat_only:
            n = CTX + TTL
        else:
            n = TTL
        n = min(n, NT - t)
        tiles.append((t, n))
        t += n
        first = False
    TTmax = max(n for _, n in tiles)
    acc = k.sb("macc", [128, 8, TTmax], F32)
    h2 = k.sb("mh2", [128, 8, TTmax], BF16)
    cTb = k.sb("mcTb", [16, TTmax], BF16)
    w1 = [k.sb("mw1%d" % i, [128, 8, 512], BF16) for i in range(2)]
    w3 = [k.sb("mw3%d" % i, [128, 8, 512], BF16) for i in range(2)]
    w2 = [k.sb("mw2%d" % i, [128, 4, D], BF16) for i in range(2)]
    self_sel = k.sb("msel", [16, 16, 128], BF16)
    k.op("pool", lambda e: e.dma_start(out=self_sel[:, :, :], in_=self.c_sel.ap.rearrange("e k m -> k e m")), writes=[self_sel], dma=True)
    sa = [k.sb("msa%d" % i, [128, 512], BF16) for i in range(2)]
    sa2 = [k.sb("msa2%d" % i, [128, 512], BF16) for i in range(2)]
    hid = [k.sb("mhid%d" % i, [128, 4, 512], BF16) for i in range(2)]
    cb = [k.sb("mcb%d" % i, [128, 512], BF16) for i in range(2)]
    xres = [k.sb("mxres%d" % i, [128, 512], F32) for i in range(2)]
    pa = [k.ps("mpa%d" % i, [128, 512], F32) for i in range(2)]
    pb = [k.ps("mpb%d" % i, [128, 512], F32) for i in range(2)]
    po = [k.ps("mpo%d" % i, [128, 512], F32) for i in range(3)]
    pcb = k.ps("mpcb", [128, 512], F32)
    hTv = self.h2T.ap.rearrange("(c p) t -> p c t", p=128)
    xTv = self.xT.ap.rearrange("(c p) t -> p c t", p=128)
    cnt = {"ab": 0, "o": 0, "h": 0, "w": 0, "s": 0}

    def blocks(t0, n):
        bl = []
        o = 0
        if t0 < CTX:
            bl.append((0, CTX, 1)); o = CTX
        while o < n:
            s_ = min(512, n - o)
            bl.append((o, s_, 0)); o += s_
        return bl

    for (t0, n) in tiles:
        for c in range(8):
            k.op("sp", lambda e, c=c, t0=t0, n=n: e.dma_start(out=h2[:, c, :n], in_=hTv[:, c, t0:t0 + n]), writes=[h2], dma=True)
        k.op("pool", lambda e, t0=t0, n=n: e.dma_start(out=cTb[:, :n], in_=self.combT.ap[:, t0:t0 + n]), writes=[cTb], dma=True)
        bl = blocks(t0, n)
        for ex in range(16):
            wb = cnt["w"] % 2; cnt["w"] += 1
            W1, W3, W2 = w1[wb], w3[wb], w2[wb]
            for c in range(8):
                k.op("pool", lambda e, c=c, ex=ex, W1=W1: e.dma_start(out=W1[:, c, :], in_=self.moe_w1.ap[l, ex, c * 128:(c + 1) * 128, :]), writes=[W1], dma=True)
                k.op("pool", lambda e, c=c, ex=ex, W3=W3: e.dma_start(out=W3[:, c, :], in_=self.moe_w3.ap[l, ex, c * 128:(c + 1) * 128, :]), writes=[W3], dma=True)
            for c in range(4):
                k.op("pool", lambda e, c=c, ex=ex, W2=W2: e.dma_start(out=W2[:, c, :], in_=self.moe_w2.ap[l, ex, c * 128:(c + 1) * 128, :]), writes=[W2], dma=True)
            for (o, s_, stream) in bl:
                cbb = cb[cnt["s"] % 2]; cnt["s"] += 1
                k.op("pe", lambda e, ex=ex, o=o, s_=s_: e.matmul(pcb[:, :s_], lhsT=self_sel[:, ex, :], rhs=cTb[:, o:o + s_], start=True, stop=True), reads=[self_sel, cTb], writes=[pcb])
                k.op("act", lambda e, cbb=cbb, s_=s_: e.copy(cbb[:, :s_], pcb[:, :s_]), reads=[pcb], writes=[cbb])
                hd = hid[cnt["h"] % 2]; cnt["h"] += 1
                for fc in range(4):
                    i = cnt["ab"] % 2; cnt["ab"] += 1
                    pa_, pb_, sa_, sa2_ = pa[i], pb[i], sa[i], sa2[i]
                    for c in range(8):
                        k.op("pe", lambda e, c=c, fc=fc, pa_=pa_, W1=W1, o=o, s_=s_: e.matmul(pa_[:, :s_], lhsT=W1[:, c, fc * 128:(fc + 1) * 128], rhs=h2[:, c, o:o + s_], start=(c == 0), stop=(c == 7)), reads=[W1, h2], writes=[pa_], pe_acc=True)
                    for c in range(8):
                        k.op("pe", lambda e, c=c, fc=fc, pb_=pb_, W3=W3, o=o, s_=s_: e.matmul(pb_[:, :s_], lhsT=W3[:, c, fc * 128:(fc + 1) * 128], rhs=h2[:, c, o:o + s_], start=(c == 0), stop=(c == 7)), reads=[W3, h2], writes=[pb_], pe_acc=True)
                    k.op("act", lambda e, pa_=pa_, sa_=sa_, s_=s_: e.activation(out=sa_[:, :s_], in_=pa_[:, :s_], func=AF.Silu), reads=[pa_], writes=[sa_])
                    k.op("pool", lambda e, sa_=sa_, sa2_=sa2_, cbb=cbb, s_=s_: e.tensor_tensor(sa2_[:, :s_], sa_[:, :s_], cbb[:, :s_], ALU.mult), reads=[sa_, cbb], writes=[sa2_])
                    k.op("dve", lambda e, sa2_=sa2_, pb_=pb_, hd=hd, fc=fc, s_=s_: e.tensor_tensor(hd[:, fc, :s_], sa2_[:, :s_], pb_[:, :s_], ALU.mult), reads=[sa2_, pb_], writes=[hd])
                for j in range(8):
                    po_ = po[cnt["o"] % 3]; cnt["o"] += 1
                    for fc in range(4):
                        k.op("pe", lambda e, fc=fc, j=j, po_=po_, W2=W2, hd=hd, s_=s_: e.matmul(po_[:, :s_], lhsT=W2[:, fc, j * 128:(j + 1) * 128], rhs=hd[:, fc, :s_], start=(fc == 0), stop=(fc == 3)), reads=[W2, hd], writes=[po_], pe_acc=True)
                    if ex == 0:
                        k.op("dve", lambda e, j=j, po_=po_, o=o, s_=s_: e.tensor_copy(acc[:, j, o:o + s_], po_[:, :s_]), reads=[po_], writes=[acc])
                    else:
                        k.op("dve", lambda e, j=j, po_=po_, o=o, s_=s_: e.tensor_tensor(acc[:, j, o:o + s_], acc[:, j, o:o + s_], po_[:, :s_], ALU.add), reads=[po_, acc], writes=[acc])
        ri = 0
        for (o, s_, stream) in bl:
            for j in range(8):
                xr = xres[ri % 2]; ri += 1
                k.op("sp", lambda e, xr=xr, o=o, s_=s_, t0=t0, j=j: e.dma_start(out=xr[:, :s_], in_=xTv[:, j, t0 + o:t0 + o + s_]), writes=[xr], dma=True)
                k.op("dve", lambda e, j=j, xr=xr, o=o, s_=s_, stream=stream: e.scalar_tensor_tensor(out=xr[:, :s_], in0=acc[:, j, o:o + s_], scalar=self.modT[:, l, 40 + j, stream:stream + 1], in1=xr[:, :s_], op0=ALU.mult, op1=ALU.add), reads=[acc, self.modT, xr], writes=[xr])
                k.op("pool", lambda e, xr=xr, o=o, s_=s_, t0=t0, j=j: e.dma_start(out=xTv[:, j, t0 + o:t0 + o + s_], in_=xr[:, :s_]), reads=[xr], dma=True)
    k.barrier()
    k.emit()
    k.free_to(m)


def phase_final(self):
    k = self.k
    m = k.mark()
    fg = self._pvec("fg", self.final_g.ap, 8)
    xt = [k.sb("fxt%d" % i, [128, 8, 512], F32) for i in range(2)]
    sq = k.sb("fsq", [128, 8, 512], BF16)
    rstd = k.sb("frstd", [128, 512], F32)
    pss = k.ps("fpss", [128, 512], F32)
    pt = [k.ps("fpt%d" % i, [128, 4, 128], F32) for i in range(2)]
    ot = [k.sb("fot%d" % i, [128, 8, 128], F32) for i in range(2)]
    xTv = self.xT.ap.rearrange("(c p) t -> p c t", p=128)
    cnt = [0]
    for ti, (tok0, n, stream) in enumerate(self.tok_tiles(lat_only=True)):
        x_ = xt[ti % 2]
        k.op("sp", lambda e, x_=x_, tok0=tok0, n=n: e.dma_start(out=x_[:, :, :n], in_=xTv[:, :, tok0:tok0 + n]), writes=[x_], dma=True)
        k.op("act", lambda e, x_=x_, n=n: e.activation(out=sq[:, :, :n], in_=x_[:, :, :n], func=AF.Square), reads=[x_], writes=[sq])
        for c in range(8):
            k.op("pe", lambda e, c=c, n=n: e.matmul(pss[:, :n], lhsT=self.ones[:, :], rhs=sq[:, c, :n], start=(c == 0), stop=(c == 7)), reads=[sq, self.ones], writes=[pss], pe_acc=True)
        k.op("act", lambda e, n=n: e.activation(out=rstd[:, :n], in_=pss[:, :n], func=AF.Sqrt, scale=1.0 / D, bias=EPS), reads=[pss], writes=[rstd])
        k.op("dve", lambda e, n=n: e.reciprocal(rstd[:, :n], rstd[:, :n]), reads=[rstd], writes=[rstd])
        k.op("dve", lambda e, x_=x_, n=n: e.tensor_tensor(x_[:, :, :n], x_[:, :, :n], rstd[:, :n].unsqueeze(1).to_broadcast([128, 8, n]), ALU.mult), reads=[x_, rstd], writes=[x_])
        k.op("pool", lambda e, x_=x_, n=n: e.tensor_tensor(x_[:, :, :n], x_[:, :, :n], fg[:, :].unsqueeze(2).to_broadcast([128, 8, n]), ALU.mult), reads=[x_, fg], writes=[x_])
        for tb in range(n // 128):
            o_ = ot[cnt[0] % 2]; cnt[0] += 1
            for hh in range(2):
                for c in range(4):
                    cc = hh * 4 + c
                    k.op("pe", lambda e, hh=hh, c=c, cc=cc, x_=x_, tb=tb: e.transpose(pt[hh][:, c, :], x_[:, cc, tb * 128:(tb + 1) * 128], self.identf[:, :]), reads=[x_, self.identf], writes=[pt[hh]], pe_acc=True)
                if hh == 0:
                    k.op("dve", lambda e, o_=o_, hh=hh: e.tensor_copy(o_[:, 0:4, :], pt[0][:, :, :]), reads=[pt[0]], writes=[o_])
                else:
                    k.op("act", lambda e, o_=o_, hh=hh: e.copy(o_[:, 4:8, :], pt[1][:, :, :]), reads=[pt[1]], writes=[o_])
            tt = tok0 - CTX + tb * 128
            k.op("pool", lambda e, o_=o_, tt=tt: e.dma_start(out=self.out.ap[tt:tt + 128, :], in_=o_[:, :, :].rearrange("p c f -> p (c f)")), reads=[o_], dma=True)
    k.barrier()
    k.emit()
    k.free_to(m)


MK.moe_decl = _moe_decl
MK.phase_outproj = phase_outproj
MK.phase_moe = phase_moe
MK.phase_final = phase_final


def build_all(T, dbg=None, L=2):
    mk_ = MK(T, L=L, dbg=dbg)
    mk_.consts(); mk_.phase_mod(); mk_.phase_x_in()
    for l in range(L):
        last = (l == L - 1)
        mk_.phase_inproj(l)
        mk_.phase_rwkv(l, 0); mk_.phase_rwkv(l, 1)
        mk_.phase_ssm_conv(l); mk_.phase_ssm_scan(l, 0); mk_.phase_ssm_scan(l, 1)
        mk_.phase_gla(l, 0); mk_.phase_gla(l, 1)
        mk_.phase_outproj(l, lat_only=last)
        mk_.phase_moe(l, lat_only=last)
    mk_.phase_final()
    mk_.finish(None)
    return mk_


def _host_consts():
    c = {}
    c["c_ident"] = np.eye(128, dtype=np.float32)
    p = np.arange(128)[:, None] % 64
    f = np.arange(512)[None, :] % 64
    c["c_masks"] = np.stack([(p < f), (p <= f), (p > f), (p >= f)]).astype(np.float32)
    c["c_id8"] = (p == f).astype(np.float32)
    pp = np.arange(128)
    c["c_blk"] = ((pp[:, None] // 64) == (pp[None, :] // 64)).astype(np.float32)
    j = np.arange(128)[:, None]; f128 = np.arange(128)[None, :]
    c["c_m128"] = np.stack([(j <= f128), (j >= f128), (j > f128), (j < f128), -30000.0 * (j == f128)]).astype(np.float32)
    sel = np.zeros((16, 16, 128), np.float32)
    for e_ in range(16):
        sel[e_, e_, :] = 1.0
    c["c_sel"] = sel
    c["c_reset"] = np.broadcast_to((np.arange(512) % 64 != 0).astype(np.float32)[None, :], (128, 512)).copy()
    return c


_CACHE = {}


def kernel(**inputs):
    from concourse.bass_utils import run_bass_kernel_spmd
    x = np.asarray(inputs["x"])
    B, T, _ = x.shape
    n_cores = 8
    assert B == n_cores
    mk_ = build_all(T)
    consts = _host_consts()
    in_maps = []
    for b in range(n_cores):
        m = {}
        for k_, v in inputs.items():
            v = np.asarray(v, dtype=np.float32)
            if k_ in ("x", "ctx", "c"):
                m[k_] = np.ascontiguousarray(v[b])
            else:
                m[k_] = np.ascontiguousarray(v)
        m.update(consts)
        in_maps.append(m)
    res = run_bass_kernel_spmd(mk_.nc, in_maps, core_ids=list(range(n_cores)))
    out = np.stack([np.asarray(r["out"], dtype=np.float32) for r in res.results], 0)
    return out
```

```python
import os
import numpy as np
import concourse.bass as bass
import concourse.mybir as mybir

F32 = mybir.dt.float32
BF16 = mybir.dt.bfloat16
ALU = mybir.AluOpType
AF = mybir.ActivationFunctionType
AX = mybir.AxisListType

SEM_CHUNK = int(os.environ.get("SEM_CHUNK", 8000))
N_DMA_SEMS = 12


class T:
    __slots__ = ("ap", "w", "r", "name", "psum")

    def __init__(self, ap, name="", psum=False):
        self.psum = psum
        self.ap = ap
        self.w = None
        self.r = []
        self.name = name

    def __getitem__(self, idx):
        return self.ap[idx]


class KB:
    ENGS = ("pe", "act", "dve", "pool", "sp")

    def __init__(self, nc, same_engine_sync=(os.environ.get("SES", "1") == "1")):
        self.nc = nc
        self.ops = {e: [] for e in self.ENGS}
        self.cnt = {e: 0 for e in self.ENGS}
        self.sem_names = []
        self.waited = {e: {} for e in self.ENGS}
        self.dma_rr = {e: 0 for e in self.ENGS}
        self.dma_val = {}
        self.same_engine_sync = same_engine_sync
        self.ctx = []
        self.final_waits = []
        self.sems = {}
        self.sem_guards = []
        self.last_tok = {}

    def sb(self, name, shape, dt):
        self.uid = getattr(self, "uid", 0) + 1
        name = "%s_%d" % (name, self.uid)
        g = self.nc.sbuf_tensor(name, list(shape), dt)
        t = g.__enter__()
        self.ctx.append(g)
        return T(t, name)

    def ps(self, name, shape, dt):
        self.uid = getattr(self, "uid", 0) + 1
        name = "%s_%d" % (name, self.uid)
        esz = 4 if dt == F32 else 2
        full = 2048 // esz
        g = self.nc.psum_tensor(name, [128, full], dt)
        t = g.__enter__()
        self.ctx.append(g)
        shape = list(shape)
        p = shape[0]
        if len(shape) == 2:
            assert shape[1] <= full
            ap = t[0:p, 0:shape[1]]
        else:
            assert len(shape) == 3 and shape[1] * shape[2] <= full
            ap = t[0:p, 0:shape[1] * shape[2]].rearrange("p (a b) -> p a b", b=shape[2])
        return T(ap, name, psum=True)

    def dram(self, name, shape, dt, kind="Internal"):
        t = self.nc.dram_tensor(name, list(shape), dt, kind=kind)
        return T(t.ap(), name)

    def _sem_key(self, key):
        if key not in self.sem_names:
            self.sem_names.append(key)
        return key

    def op(self, eng, fn, reads=(), writes=(), dma=False, pe_acc=False):
        waits = {}
        if any(t.psum for t in reads):
            writes = list(writes) + [t for t in reads if t.psum and t not in writes]
            reads = [t for t in reads if not t.psum]

        def need(dep):
            if dep is None:
                return
            k, v, e = dep
            if e == eng and not dma and not self.same_engine_sync and not k[0] == "dma":
                return
            if e == eng and eng == "pe" and k[0] != "dma":
                return
            if waits.get(k, 0) < v:
                waits[k] = v

        for t in reads:
            need(t.w)
        for t in writes:
            if not (pe_acc and t.w is not None and t.w[2] == "pe" and eng == "pe"):
                need(t.w)
            for d in t.r:
                need(d)
        if dma:
            i = self.dma_rr[eng]
            self.dma_rr[eng] = (i + 1) % N_DMA_SEMS
            key = self._sem_key(("dma", eng, i))
            prev = self.dma_val.get(key, 0)
            if prev:
                if waits.get(key, 0) < prev:
                    waits[key] = prev
            val = prev + 16
            self.dma_val[key] = val
            inc = 16
        else:
            c = self.cnt[eng]
            self.cnt[eng] = c + 1
            key = self._sem_key(("eng", eng, c // SEM_CHUNK))
            val = (c % SEM_CHUNK) + 1
            inc = 1
        wl = []
        wd = self.waited[eng]
        for k, v in waits.items():
            if wd.get(k, 0) >= v:
                continue
            wd[k] = v
            wl.append((k, v))
        self.ops[eng].append((fn, wl, key, inc))
        tok = (key, val, eng)
        self.last_tok[key] = val
        for t in reads:
            t.r.append(tok)
        for t in writes:
            t.w = tok
            t.r = []
        return tok

    def mark(self):
        return len(self.ctx)

    def free_to(self, mark):
        while len(self.ctx) > mark:
            g = self.ctx.pop()
            g.__exit__(None, None, None)

    def barrier(self):
        for eng in self.ENGS:
            wl = []
            wd = self.waited[eng]
            for k, v in self.last_tok.items():
                if k[0] == "eng" and k[1] == eng:
                    continue
                if wd.get(k, 0) >= v:
                    continue
                wd[k] = v
                wl.append((k, v))
            if wl:
                self.ops[eng].append((None, wl, None, 0))

    def finish_wait(self, eng, toks):
        self.final_waits.append((eng, toks))

    def emit(self):
        nc = self.nc
        for key in self.sem_names:
            if key not in self.sems:
                g = nc.semaphore("s%d" % len(self.sems))
                self.sems[key] = g.__enter__()
                self.sem_guards.append(g)
        sems = self.sems
        ops = self.ops
        final_waits = self.final_waits

        def run(engname, h):
            for fn, wl, key, inc in ops[engname]:
                for k, v in wl:
                    h.wait_ge(sems[k], v)
                if fn is not None:
                    ins = fn(h)
                    ins.then_inc(sems[key], inc)
            for e, toks in final_waits:
                if e == engname:
                    for (k, v, _) in toks:
                        h.wait_ge(sems[k], v)

        with nc.Block() as block:
            @block.tensor
            def _(h):
                run("pe", h)

            @block.scalar
            def _(h):
                run("act", h)

            @block.vector
            def _(h):
                run("dve", h)

            @block.gpsimd
            def _(h):
                run("pool", h)

            @block.sync
            def _(h):
                run("sp", h)
        self.ops = {e: [] for e in self.ENGS}
        self.final_waits = []

    def close(self):
        self.free_to(0)
        for g in reversed(self.sem_guards):
            g.__exit__(None, None, None)


D = 1024
CTX = 256
INC = 3096
EPS = 1e-6
NEG_E05 = -0.6065306597126334


class MK:
    def __init__(self, T, L=2, dbg=None):
        self.T = T
        self.L = L
        self.NT = CTX + T
        self.dbg = dbg or set()
        nc = bass.Bass("TRN2", target_bir_lowering=False)
        self.nc = nc
        self.k = KB(nc)
        self.decl()

    def decl(self):
        k = self.k
        T, L, NT = self.T, self.L, self.NT
        I = lambda n, s: k.dram(n, s, F32, kind="ExternalInput")
        self.x = I("x", [T, D]); self.c = I("c", [D]); self.ctx = I("ctx", [CTX, D]); self.c_ctx = I("c_ctx", [D])
        self.ada_w = I("ada_w", [L, D, 6 * D]); self.ada_b = I("ada_b", [L, 6 * D])
        self.norm1_g = I("norm1_g", [L, D]); self.norm2_g = I("norm2_g", [L, D])
        self.w_in = I("w_in", [L, D, INC]); self.w_out = I("w_out", [L, D, D])
        self.rw_mu_prev = I("rw_mu_prev", [L, 1024]); self.rw_mu_next = I("rw_mu_next", [L, 1024])
        self.rw_w0 = I("rw_w0", [L, 2, 256]); self.rw_w2 = I("rw_w2", [L, 2, 64, 256])
        self.rw_a0 = I("rw_a0", [L, 2, 256]); self.rw_a2 = I("rw_a2", [L, 2, 64, 256])
        self.rw_g2 = I("rw_g2", [L, 128, 256])
        self.rw_k_k = I("rw_k_k", [L, 256]); self.rw_k_a = I("rw_k_a", [L, 256]); self.rw_r_k = I("rw_r_k", [L, 256])
        self.rw_gn_g = I("rw_gn_g", [L, 256]); self.rw_gn_b = I("rw_gn_b", [L, 256])
        self.ssm_conv_w = I("ssm_conv_w", [L, 3, 3, 768]); self.ssm_conv_b = I("ssm_conv_b", [L, 768])
        self.ssm_dt_bias = I("ssm_dt_bias", [L, 2, 8]); self.ssm_a_log = I("ssm_a_log", [L, 2, 8])
        self.ssm_d = I("ssm_d", [L, 8]); self.ssm_norm_g = I("ssm_norm_g", [L, 512])
        self.gla_ga2 = I("gla_ga2", [L, 2, 16, 128]); self.gla_gb = I("gla_gb", [L, 2, 128]); self.gla_norm_g = I("gla_norm_g", [L, 256])
        self.moe_rg_w = I("moe_rg_w", [L, D, 4]); self.moe_rg_b = I("moe_rg_b", [L, 4])
        self.moe_re_w = I("moe_re_w", [L, D, 16]); self.moe_re_b = I("moe_re_b", [L, 16])
        self.moe_w1 = I("moe_w1", [L, 16, D, 512]); self.moe_w3 = I("moe_w3", [L, 16, D, 512]); self.moe_w2 = I("moe_w2", [L, 16, 512, D])
        self.final_g = I("final_g", [D])
        self.c_ident = I("c_ident", [128, 128])
        self.out = k.dram("out", [T, D], F32, kind="ExternalOutput")
        def S(n, s, dt=F32):
            kind = "ExternalOutput" if n in self.dbg else "Internal"
            return k.dram(n, s, dt, kind=kind)
        self.xT = S("xT", [D, NT])
        self.uT = S("uT", [3200, NT])
        self.mixT = S("mixT", [D, NT])
        self.rw_consts()
        self.ssm_decl()
        self.moe_decl()

    def consts(self):
        k = self.k
        self.identf = k.sb("identf", [128, 128], F32)
        self.ident = k.sb("ident", [128, 128], BF16)
        self.ones = k.sb("ones", [128, 128], BF16)
        k.op("sp", lambda e: e.dma_start(out=self.identf[:, :], in_=self.c_ident.ap[:, :]), writes=[self.identf], dma=True)
        k.op("dve", lambda e: e.tensor_copy(self.ident[:, :], self.identf[:, :]), reads=[self.identf], writes=[self.ident])
        k.op("dve", lambda e: e.memset(self.ones[:, :], 1.0), writes=[self.ones])
        self.modT = k.sb("modT", [128, self.L, 48, 2], F32)
        self.gm = k.sb("gm", [128, self.L, 2, 8, 2], F32)
        self.load_consts2()

    def phase_mod(self):
        k = self.k
        L = self.L
        m = k.mark()
        cf = k.sb("cf", [128, 8, 2], F32)
        cb = k.sb("cb", [128, 8, 2], BF16)
        adab = k.sb("adab", [128, 48], F32)
        ng = k.sb("ng", [128, 2, 8], F32)
        aw = k.sb("aw", [128, 8, 3072], BF16)
        pm = k.ps("pm", [128, 48, 2], F32)
        k.op("sp", lambda e: e.dma_start(out=cf[:, :, 0], in_=self.c.ap.rearrange("(c p) -> p c", p=128), allow_slow_non_contiguous=True), writes=[cf], dma=True)
        k.op("sp", lambda e: e.dma_start(out=cf[:, :, 1], in_=self.c_ctx.ap.rearrange("(c p) -> p c", p=128), allow_slow_non_contiguous=True), writes=[cf], dma=True)
        k.op("act", lambda e: e.activation(out=cb[:, :, :], in_=cf[:, :, :], func=AF.Silu), reads=[cf], writes=[cb])
        for l in range(L):
            k.op("sp", lambda e, l=l: e.dma_start(out=adab[:, :], in_=self.ada_b.ap[l].rearrange("(c p) -> p c", p=128), allow_slow_non_contiguous=True), writes=[adab], dma=True)
            k.op("sp", lambda e, l=l: e.dma_start(out=ng[:, 0, :], in_=self.norm1_g.ap[l].rearrange("(c p) -> p c", p=128), allow_slow_non_contiguous=True), writes=[ng], dma=True)
            k.op("sp", lambda e, l=l: e.dma_start(out=ng[:, 1, :], in_=self.norm2_g.ap[l].rearrange("(c p) -> p c", p=128), allow_slow_non_contiguous=True), writes=[ng], dma=True)
            for half in range(2):
                for c in range(8):
                    k.op("pool", lambda e, l=l, c=c, half=half: e.dma_start(out=aw[:, c, :], in_=self.ada_w.ap[l, c * 128:(c + 1) * 128, half * 3072:(half + 1) * 3072]), writes=[aw], dma=True)
                for j in range(24):
                    for c in range(8):
                        k.op("pe", lambda e, c=c, j=j, half=half: e.matmul(pm[:, half * 24 + j, :], lhsT=aw[:, c, j * 128:(j + 1) * 128], rhs=cb[:, c, :], start=(c == 0), stop=(c == 7)), reads=[aw, cb], writes=[pm], pe_acc=True)
            k.op("dve", lambda e, l=l: e.tensor_tensor(self.modT[:, l, :, :], pm[:, :, :], adab[:, :].unsqueeze(2).to_broadcast([128, 48, 2]), ALU.add), reads=[pm, adab], writes=[self.modT])
            for w, j in ((0, 1), (1, 4)):
                k.op("dve", lambda e, l=l, w=w, j=j: e.scalar_tensor_tensor(out=self.gm[:, l, w, :, :], in0=self.modT[:, l, j * 8:(j + 1) * 8, :], scalar=1.0, in1=ng[:, w, :].unsqueeze(2).to_broadcast([128, 8, 2]), op0=ALU.add, op1=ALU.mult), reads=[self.modT, ng], writes=[self.gm])
        k.barrier()
        k.emit()
        k.free_to(m)

    def tok_tiles(self, lat_only=False, n=512):
        tiles = []
        if not lat_only:
            tiles.append((0, CTX, 1))
        for i in range(self.T // n):
            tiles.append((CTX + i * n, n, 0))
        return tiles

    def phase_x_in(self):
        k = self.k
        m = k.mark()
        xin = [k.sb("xin%d" % i, [128, D], F32) for i in range(2)]
        pt = [k.ps("ptx%d" % i, [128, 4, 128], F32) for i in range(2)]
        xo = [k.sb("xo%d" % i, [128, 8, 128], F32) for i in range(2)]
        nt = self.NT // 128
        for i in range(nt):
            src = self.ctx.ap[i * 128:(i + 1) * 128, :] if i < 2 else self.x.ap[(i - 2) * 128:(i - 1) * 128, :]
            b = i % 2
            k.op("sp", lambda e, b=b, src=src: e.dma_start(out=xin[b][:, :], in_=src), writes=[xin[b]], dma=True)
            for hh in range(2):
                for c in range(4):
                    cc = hh * 4 + c
                    k.op("pe", lambda e, b=b, hh=hh, c=c, cc=cc: e.transpose(pt[hh][:, c, :], xin[b][:, cc * 128:(cc + 1) * 128], self.identf[:, :]), reads=[xin[b], self.identf], writes=[pt[hh]], pe_acc=True)
                eng = "dve" if hh == 0 else "act"
                if eng == "dve":
                    k.op("dve", lambda e, b=b, hh=hh: e.tensor_copy(xo[b][:, hh * 4:(hh + 1) * 4, :], pt[hh][:, :, :]), reads=[pt[hh]], writes=[xo[b]])
                else:
                    k.op("act", lambda e, b=b, hh=hh: e.copy(xo[b][:, hh * 4:(hh + 1) * 4, :], pt[hh][:, :, :]), reads=[pt[hh]], writes=[xo[b]])
            k.op("pool", lambda e, b=b, i=i: e.dma_start(out=self.xT.ap.rearrange("(c p) t -> p c t", p=128)[:, :, i * 128:(i + 1) * 128], in_=xo[b][:, :, :]), reads=[xo[b]], dma=True)
        k.barrier()
        k.emit()
        k.free_to(m)

    def norm_tile(self, l, which, tok0, n, stream, xt, sq, pss, rstd, hT, keep_f32=False):
        k = self.k
        k.op("act", lambda e: e.activation(out=sq[:, :, :n], in_=xt[:, :, :n], func=AF.Square), reads=[xt], writes=[sq])
        for c in range(8):
            k.op("pe", lambda e, c=c: e.matmul(pss[:, :n], lhsT=self.ones[:, :], rhs=sq[:, c, :n], start=(c == 0), stop=(c == 7)), reads=[sq, self.ones], writes=[pss], pe_acc=True)
        k.op("act", lambda e: e.activation(out=rstd[:, :n], in_=pss[:, :n], func=AF.Sqrt, scale=1.0 / D, bias=EPS), reads=[pss], writes=[rstd])
        k.op("dve", lambda e: e.reciprocal(rstd[:, :n], rstd[:, :n]), reads=[rstd], writes=[rstd])
        k.op("dve", lambda e: e.tensor_tensor(xt[:, :, :n], xt[:, :, :n], rstd[:, :n].unsqueeze(1).to_broadcast([128, 8, n]), ALU.mult), reads=[xt, rstd], writes=[xt])
        sh_j = 0 if which == 0 else 3
        k.op("pool", lambda e: e.tensor_tensor(xt[:, :, :n], xt[:, :, :n], self.gm[:, l, which, :, stream].unsqueeze(2).to_broadcast([128, 8, n]), ALU.mult), reads=[xt, self.gm], writes=[xt])
        if keep_f32:
            k.op("dve", lambda e: e.tensor_tensor(xt[:, :, :n], xt[:, :, :n], self.modT[:, l, sh_j * 8:(sh_j + 1) * 8, stream].unsqueeze(2).to_broadcast([128, 8, n]), ALU.add), reads=[xt, self.modT], writes=[xt])
            k.op("act", lambda e: e.copy(hT[:, :, :n], xt[:, :, :n]), reads=[xt], writes=[hT])
        else:
            k.op("dve", lambda e: e.tensor_tensor(hT[:, :, :n], xt[:, :, :n], self.modT[:, l, sh_j * 8:(sh_j + 1) * 8, stream].unsqueeze(2).to_broadcast([128, 8, n]), ALU.add), reads=[xt, self.modT], writes=[hT])

    def phase_inproj(self, l):
        k = self.k
        m = k.mark()
        win = k.sb("win", [128, 8, INC], BF16)
        for c in range(8):
            k.op("pool", lambda e, c=c: e.dma_start(out=win[:, c, :], in_=self.w_in.ap[l, c * 128:(c + 1) * 128, :]), writes=[win], dma=True)
        xt = [k.sb("xt%d" % i, [128, 8, 512], F32) for i in range(2)]
        sq = k.sb("sq", [128, 8, 512], BF16)
        rstd = k.sb("rstd", [128, 512], F32)
        hT = [k.sb("hT%d" % i, [128, 8, 512], BF16) for i in range(2)]
        pss = k.ps("pss", [128, 512], F32)
        pu = [k.ps("pu%d" % i, [128, 512], F32) for i in range(4)]
        us = [k.sb("us%d" % i, [128, 512], F32) for i in range(4)]
        xTv = self.xT.ap.rearrange("(c p) t -> p c t", p=128)
        nchunk = (INC + 127) // 128
        cnt = 0
        for ti, (tok0, n, stream) in enumerate(self.tok_tiles()):
            b = ti % 2
            k.op("sp", lambda e, b=b, tok0=tok0, n=n: e.dma_start(out=xt[b][:, :, :n], in_=xTv[:, :, tok0:tok0 + n]), writes=[xt[b]], dma=True)
            self.norm_tile(l, 0, tok0, n, stream, xt[b], sq, pss, rstd, hT[b])
            for j in range(nchunk):
                c0 = j * 128
                nc_ = min(128, INC - c0)
                pb = cnt % 4
                cnt += 1
                for c in range(8):
                    k.op("pe", lambda e, c=c, c0=c0, nc_=nc_, pb=pb, b=b, n=n: e.matmul(pu[pb][:nc_, :n], lhsT=win[:, c, c0:c0 + nc_], rhs=hT[b][:, c, :n], start=(c == 0), stop=(c == 7)), reads=[win, hT[b]], writes=[pu[pb]], pe_acc=True)
                if j % 2 == 0:
                    k.op("dve", lambda e, pb=pb, nc_=nc_, n=n: e.tensor_copy(us[pb][:nc_, :n], pu[pb][:nc_, :n]), reads=[pu[pb]], writes=[us[pb]])
                else:
                    k.op("act", lambda e, pb=pb, nc_=nc_, n=n: e.copy(us[pb][:nc_, :n], pu[pb][:nc_, :n]), reads=[pu[pb]], writes=[us[pb]])
                k.op("pool", lambda e, pb=pb, nc_=nc_, n=n, c0=c0, tok0=tok0: e.dma_start(out=self.uT.ap[c0:c0 + nc_, tok0:tok0 + n], in_=us[pb][:nc_, :n]), reads=[us[pb]], dma=True)
        k.barrier()
        k.emit()
        k.free_to(m)

    def finish(self, last_tensor):
        k = self.k
        k.barrier()
        k.emit()
        k.close()


def _rw_consts(self):
    k = self.k
    I = lambda n, s: k.dram(n, s, F32, kind="ExternalInput")
    self.c_masks = I("c_masks", [4, 128, 512])
    self.c_id8 = I("c_id8", [128, 512])
    self.c_blk = I("c_blk", [128, 128])
    self.c_reset = I("c_reset", [128, 512])
    def S(n, s, dt=F32):
        kind = "ExternalOutput" if n in self.dbg else "Internal"
        return k.dram(n, s, dt, kind=kind)
    self.rw_yf = S("rw_yf", [256, self.NT])
    self.rw_bonus = S("rw_bonus", [256, self.NT])
    self.rw_gate = S("rw_gate", [256, self.NT])


def _load_consts2(self):
    k = self.k
    self.masks = k.sb("masks", [128, 4, 512], BF16)
    self.id8 = k.sb("id8", [128, 512], BF16)
    self.blk = k.sb("blk", [128, 128], BF16)
    self.reset = k.sb("reset", [128, 512], F32)
    for i in range(4):
        k.op("pool", lambda e, i=i: e.dma_start(out=self.masks[:, i, :], in_=self.c_masks.ap[i]), writes=[self.masks], dma=True)
    k.op("pool", lambda e: e.dma_start(out=self.id8[:, :], in_=self.c_id8.ap[:, :]), writes=[self.id8], dma=True)
    k.op("pool", lambda e: e.dma_start(out=self.blk[:, :], in_=self.c_blk.ap[:, :]), writes=[self.blk], dma=True)
    k.op("sp", lambda e: e.dma_start(out=self.reset[:, :], in_=self.c_reset.ap[:, :]), writes=[self.reset], dma=True)


def _pvec(self, name, src_ap, ncols, eng="sp", dt=F32):
    k = self.k
    t = k.sb(name, [128, ncols], dt)
    k.op(eng, lambda e: e.dma_start(out=t[:, :], in_=src_ap.rearrange("(c p) -> p c", p=128), allow_slow_non_contiguous=True), writes=[t], dma=True)
    return t


def phase_rwkv(self, l, d):
    k = self.k
    m = k.mark()
    NT = self.NT
    s = NEG_E05
    mp = self._pvec("mp", self.rw_mu_prev.ap[l], 8)
    mn = self._pvec("mn", self.rw_mu_next.ap[l], 8)
    cmix = k.sb("cmix", [128, 8], F32)
    k.op("dve", lambda e: e.tensor_tensor(cmix[:, :], mp[:, :], mn[:, :], ALU.add), reads=[mp, mn], writes=[cmix])
    k.op("dve", lambda e: e.tensor_scalar(cmix[:, :], cmix[:, :], -1.0, 1.0, ALU.mult, ALU.add), reads=[cmix], writes=[cmix])
    w0 = self._pvec("w0", self.rw_w0.ap[l, d], 2)
    a0 = self._pvec("a0", self.rw_a0.ap[l, d], 2)
    k_k = self._pvec("k_k", self.rw_k_k.ap[l], 2)
    k_a = self._pvec("k_a", self.rw_k_a.ap[l], 2)
    r_k = self._pvec("r_k", self.rw_r_k.ap[l], 2)
    gn_g = self._pvec("gn_g", self.rw_gn_g.ap[l], 2)
    gn_b = self._pvec("gn_b", self.rw_gn_b.ap[l], 2)
    w2s = k.sb("w2s", [128, 256], BF16)
    a2s = k.sb("a2s", [128, 256], BF16)
    g2s = k.sb("g2s", [128, 256], BF16)
    k.op("pool", lambda e: e.dma_start(out=w2s[0:64, :], in_=self.rw_w2.ap[l, d]), writes=[w2s], dma=True)
    k.op("pool", lambda e: e.dma_start(out=a2s[64:128, :], in_=self.rw_a2.ap[l, d]), writes=[a2s], dma=True)
    k.op("pool", lambda e: e.dma_start(out=g2s[:, :], in_=self.rw_g2.ap[l]), writes=[g2s], dma=True)

    U = [k.sb("rU%d" % i, [128, 8, 514], F32) for i in range(2)]
    S = k.sb("rS", [128, 8, 512], F32)
    wlt = k.sb("wlt", [128, 512], BF16)
    alb = k.sb("alb", [128, 512], BF16)
    sgl = k.sb("sgl", [128, 512], BF16)
    f32t = lambda n_: k.sb(n_, [128, 8, 64], F32)
    bft = lambda n_: k.sb(n_, [128, 8, 64], BF16)
    sgw, asig, kx, rn, cs, dcs, tmp, E, Etrue = [f32t("r_" + z) for z in ("sgw", "asig", "kx", "rn", "cs", "dcs", "tmp", "E", "Etrue")]
    kkn, bvec, kmod = f32t("kkn"), f32t("bvec"), f32t("kmod")
    sqk = bft("sqk")
    rt, kt, bt, at, Rtrue, Atrue, Bh, Kh, vb = [[bft("r_%s%d" % (z, hp)) for hp in range(2)] for z in ("rt", "kt", "bt", "at", "Rtrue", "Atrue", "Bh", "Kh", "vb")]
    WC = [k.sb("WC%d" % hp, [128, 8], F32) for hp in range(2)]
    Atm, Bhtm, Khtm, Vtm = [[bft("r_%s%d" % (z, hp)) for hp in range(2)] for z in ("Atm", "Bhtm", "Khtm", "Vtm")]
    Zs, Ns, Zs2, Ns2, Aak, Arb, Ark, X, AVs, PaT, Us = [[bft("r_%s%d" % (z, hp)) for hp in range(2)] for z in ("Zs", "Ns", "Zs2", "Ns2", "Aak", "Arb", "Ark", "X", "AVs", "PaT", "Us")]
    Qs = [f32t("Qs%d" % hp) for hp in range(2)]
    Mst = [k.sb("Mst%d" % hp, [128, 64], F32) for hp in range(2)]
    Mbf = [k.sb("Mbf%d" % hp, [128, 64], BF16) for hp in range(2)]
    ysb = [k.sb("ysb%d" % hp, [128, 512], F32) for hp in range(2)]
    bon = k.sb("bon", [128, 512], F32)
    gat = k.sb("gat", [128, 512], F32)
    yf_in = [k.sb("yfin%d" % hp, [128, 512], F32) for hp in range(2)]
    PS = [k.ps("rps%d" % i, [128, 512], F32) for i in range(2)]
    PY = [[k.ps("rpy%d%d" % (i, j), [128, 512], F32) for j in range(2)] for i in range(2)]
    PSB = [k.ps("rpsb%d" % i, [128, 8, 64], BF16) for i in range(2)]
    psi = [0]
    psbi = [0]

    def nps():
        p = PS[psi[0] % 2]
        psi[0] += 1
        return p

    def npsb():
        p = PSB[psbi[0] % 2]
        psbi[0] += 1
        return p

    for hp in range(2):
        k.op("dve", lambda e, hp=hp: e.memset(Mst[hp][:, :], 0.0), writes=[Mst[hp]])
        k.op("dve", lambda e, hp=hp: e.memset(Mbf[hp][:, :], 0.0), writes=[Mbf[hp]])

    M_SL, M_LE, M_SG, M_GE = 0, 1, 2, 3
    if d == 0:
        mZ, mN, mI = M_SL, M_SG, M_LE
    else:
        mZ, mN, mI = M_SG, M_SL, M_GE

    tiles = self.tok_tiles()
    lat = tiles[1:]
    order = [tiles[0]] + (lat if d == 0 else lat[::-1])
    uTv = self.uT.ap[0:1024, :].rearrange("(c p) t -> p c t", p=128)

    def v3(t, P, nch):
        return t[P, 0:nch, :]

    def v2(t, P, n):
        return t[P, :, :].rearrange("p c t -> p (c t)")[:, 0:n]

    def tile_body(ti, tok0, n, stream):
        nch = n // 64
        seq0, seq1 = (0, CTX) if stream == 1 else (CTX, NT)
        ub = U[ti % 2]
        lo = max(tok0 - 1, seq0)
        hi = min(tok0 + n + 1, seq1)
        if lo > tok0 - 1:
            k.op("pool", lambda e: e.memset(ub[:, :, 0:1], 0.0), writes=[ub])
        if hi < tok0 + n + 1:
            k.op("pool", lambda e: e.memset(ub[:, :, n + 1:n + 2], 0.0), writes=[ub])
        k.op("sp", lambda e: e.dma_start(out=ub[:, :, lo - (tok0 - 1):hi - (tok0 - 1)], in_=uTv[:, :, lo:hi]), writes=[ub], dma=True)
        fl = lambda t_: t_[:, :, :].rearrange("p c t -> p (c t)")[:, 0:n]

        def shift(c):
            k.op("dve", lambda e: e.tensor_scalar(S[:, c, :n], ub[:, c, 1:n + 1], cmix[:, c:c + 1], None, ALU.mult), reads=[ub, cmix], writes=[S])
            k.op("dve", lambda e: e.scalar_tensor_tensor(out=S[:, c, :n], in0=ub[:, c, 0:n], scalar=mp[:, c:c + 1], in1=S[:, c, :n], op0=ALU.mult, op1=ALU.add), reads=[ub, mp, S], writes=[S])
            k.op("dve", lambda e: e.scalar_tensor_tensor(out=S[:, c, :n], in0=ub[:, c, 2:n + 2], scalar=mn[:, c:c + 1], in1=S[:, c, :n], op0=ALU.mult, op1=ALU.add), reads=[ub, mn, S], writes=[S])
        for c in range(8):
            if d == 1 and c == 7:
                continue
            shift(c)
        k.op("act", lambda e: e.activation(out=wlt[0:64, :n], in_=S[0:64, 6, :n], func=AF.Tanh), reads=[S], writes=[wlt])
        k.op("act", lambda e: e.copy(alb[64:128, :n], S[64:128, 6, :n]), reads=[S], writes=[alb])
        if d == 0:
            k.op("act", lambda e: e.activation(out=sgl[:, :n], in_=S[:, 7, :n], func=AF.Sigmoid), reads=[S], writes=[sgl])

        def prep(hp):
            p1 = nps()
            k.op("pe", lambda e: e.matmul(p1[:, :n], lhsT=w2s[0:64, hp * 128:(hp + 1) * 128], rhs=wlt[0:64, :n], start=True, stop=True), reads=[w2s, wlt], writes=[p1])
            k.op("act", lambda e: e.activation(out=fl(sgw), in_=p1[:, :n], func=AF.Sigmoid, bias=w0[:, hp:hp + 1]), reads=[p1, w0], writes=[sgw])
            p2 = nps()
            k.op("pe", lambda e: e.matmul(p2[:, :n], lhsT=a2s[64:128, hp * 128:(hp + 1) * 128], rhs=alb[64:128, :n], start=True, stop=True), reads=[a2s, alb], writes=[p2])
            k.op("act", lambda e: e.activation(out=fl(asig), in_=p2[:, :n], func=AF.Sigmoid, bias=a0[:, hp:hp + 1]), reads=[p2, a0], writes=[asig])
            k.op("dve", lambda e: e.tensor_scalar(fl(kx), S[:, 2 + hp, :n], k_k[:, hp:hp + 1], None, ALU.mult), reads=[S, k_k], writes=[kx])
            k.op("act", lambda e: e.activation(out=fl(sqk), in_=fl(kx), func=AF.Square), reads=[kx], writes=[sqk])
            p3 = nps()
            k.op("pe", lambda e: e.matmul(p3[:, :n], lhsT=self.blk[:, :], rhs=fl(sqk), start=True, stop=True), reads=[self.blk, sqk], writes=[p3])
            k.op("act", lambda e: e.activation(out=fl(rn), in_=p3[:, :n], func=AF.Sqrt, bias=1e-12), reads=[p3], writes=[rn])
            k.op("dve", lambda e: e.reciprocal(fl(rn), fl(rn)), reads=[rn], writes=[rn])
            k.op("dve", lambda e: e.tensor_tensor(fl(kkn), fl(kx), fl(rn), ALU.mult), reads=[kx, rn], writes=[kkn])
            k.op("pool", lambda e: e.tensor_tensor(fl(bvec), fl(kkn), fl(asig), ALU.mult), reads=[kkn, asig], writes=[bvec])
            k.op("dve", lambda e: e.tensor_scalar(fl(tmp), fl(asig), -1.0, k_a[:, hp:hp + 1], ALU.add, ALU.mult), reads=[asig, k_a], writes=[tmp])
            k.op("dve", lambda e: e.scalar_tensor_tensor(out=fl(kmod), in0=fl(tmp), scalar=1.0, in1=S[:, 2 + hp, :n], op0=ALU.add, op1=ALU.mult), reads=[tmp, S], writes=[kmod])
            k.op("dve", lambda e: e.tensor_tensor_scan(fl(cs), self.reset[:, :n], fl(sgw), 0.0, ALU.mult, ALU.add), reads=[self.reset, sgw], writes=[cs])
            if d == 1:
                k.op("dve", lambda e: e.tensor_tensor(fl(tmp), fl(sgw), fl(cs), ALU.subtract), reads=[sgw, cs], writes=[tmp])
                k.op("dve", lambda e: e.tensor_tensor(cs[:, :nch, :], tmp[:, :nch, :], cs[:, :nch, 63:64].to_broadcast([128, nch, 64]), ALU.add), reads=[tmp, cs], writes=[cs])
            endi = 63 if d == 0 else 0
            k.op("pool", lambda e: e.tensor_tensor(dcs[:, :nch, :], cs[:, :nch, :], cs[:, :nch, 32:33].to_broadcast([128, nch, 64]), ALU.subtract), reads=[cs], writes=[dcs])
            k.op("act", lambda e: e.activation(out=fl(E), in_=fl(dcs), func=AF.Exp, scale=s), reads=[dcs], writes=[E])
            k.op("dve", lambda e: e.tensor_tensor(fl(rt[hp]), S[:, hp, :n], fl(E), ALU.mult), reads=[S, E], writes=[rt[hp]])
            k.op("pool", lambda e: e.tensor_tensor(fl(tmp), fl(dcs), fl(sgw), ALU.subtract), reads=[dcs, sgw], writes=[tmp])
            k.op("act", lambda e: e.activation(out=fl(E), in_=fl(tmp), func=AF.Exp, scale=s), reads=[tmp], writes=[E])
            k.op("dve", lambda e: e.scalar_tensor_tensor(out=fl(at[hp]), in0=fl(kkn), scalar=-1.0, in1=fl(E), op0=ALU.mult, op1=ALU.mult), reads=[kkn, E], writes=[at[hp]])
            k.op("act", lambda e: e.activation(out=fl(E), in_=fl(dcs), func=AF.Exp, scale=-s), reads=[dcs], writes=[E])
            k.op("dve", lambda e: e.tensor_tensor(fl(kt[hp]), fl(kmod), fl(E), ALU.mult), reads=[kmod, E], writes=[kt[hp]])
            k.op("pool", lambda e: e.tensor_tensor(fl(bt[hp]), fl(bvec), fl(E), ALU.mult), reads=[bvec, E], writes=[bt[hp]])
            k.op("act", lambda e: e.activation(out=fl(Etrue), in_=fl(cs), func=AF.Exp, scale=s), reads=[cs], writes=[Etrue])
            k.op("dve", lambda e: e.tensor_tensor(fl(Rtrue[hp]), S[:, hp, :n], fl(Etrue), ALU.mult), reads=[S, Etrue], writes=[Rtrue[hp]])
            k.op("dve", lambda e: e.tensor_copy(WC[hp][:, :nch], Etrue[:, :nch, endi]), reads=[Etrue], writes=[WC[hp]])
            k.op("pool", lambda e: e.tensor_tensor(fl(tmp), fl(cs), fl(sgw), ALU.subtract), reads=[cs, sgw], writes=[tmp])
            k.op("act", lambda e: e.activation(out=fl(E), in_=fl(tmp), func=AF.Exp, scale=s), reads=[tmp], writes=[E])
            k.op("dve", lambda e: e.scalar_tensor_tensor(out=fl(Atrue[hp]), in0=fl(kkn), scalar=-1.0, in1=fl(E), op0=ALU.mult, op1=ALU.mult), reads=[kkn, E], writes=[Atrue[hp]])
            k.op("pool", lambda e: e.tensor_tensor(tmp[:, :nch, :], cs[:, :nch, endi:endi + 1].to_broadcast([128, nch, 64]), cs[:, :nch, :], ALU.subtract), reads=[cs], writes=[tmp])
            k.op("act", lambda e: e.activation(out=fl(E), in_=fl(tmp), func=AF.Exp, scale=s), reads=[tmp], writes=[E])
            k.op("dve", lambda e: e.tensor_tensor(fl(Bh[hp]), fl(bvec), fl(E), ALU.mult), reads=[bvec, E], writes=[Bh[hp]])
            k.op("pool", lambda e: e.tensor_tensor(fl(Kh[hp]), fl(kmod), fl(E), ALU.mult), reads=[kmod, E], writes=[Kh[hp]])
            k.op("act", lambda e: e.copy(fl(vb[hp]), S[:, 4 + hp, :n]), reads=[S], writes=[vb[hp]])
            if d == 0:
                k.op("dve", lambda e: e.scalar_tensor_tensor(out=fl(sqk), in0=S[:, hp, :n], scalar=r_k[:, hp:hp + 1], in1=S[:, 2 + hp, :n], op0=ALU.mult, op1=ALU.mult), reads=[S, r_k], writes=[sqk])
                p4 = nps()
                k.op("pe", lambda e: e.matmul(p4[:, :n], lhsT=self.blk[:, :], rhs=fl(sqk), start=True, stop=True), reads=[self.blk, sqk], writes=[p4])
                k.op("dve", lambda e: e.tensor_tensor(bon[:, :n], p4[:, :n], S[:, 4 + hp, :n], ALU.mult), reads=[p4, S], writes=[bon])
                k.op("pool", lambda e: e.dma_start(out=self.rw_bonus.ap[hp * 128:(hp + 1) * 128, tok0:tok0 + n], in_=bon[:, :n]), reads=[bon], dma=True)
                p5 = nps()
                k.op("pe", lambda e: e.matmul(p5[:, :n], lhsT=g2s[:, hp * 128:(hp + 1) * 128], rhs=sgl[:, :n], start=True, stop=True), reads=[g2s, sgl], writes=[p5])
                k.op("act", lambda e: e.copy(gat[:, :n], p5[:, :n]), reads=[p5], writes=[gat])
                k.op("pool", lambda e: e.dma_start(out=self.rw_gate.ap[hp * 128:(hp + 1) * 128, tok0:tok0 + n], in_=gat[:, :n]), reads=[gat], dma=True)

        def units(dst_ps, lt, rt_, P, reads):
            pv = dst_ps[:, :].rearrange("p (c t) -> p c t", t=64)
            for ch in range(nch):
                k.op("pe", lambda e, ch=ch: e.matmul(pv[P, ch, :], lhsT=lt[P, ch, :], rhs=rt_[P, ch, :], start=True, stop=True), reads=reads, writes=[dst_ps], pe_acc=True)
            return pv

        def head_block(hp, hl):
            P = slice(hl * 64, hl * 64 + 64)

            def tm(src, dst):
                pb = npsb()
                for ch in range(nch):
                    k.op("pe", lambda e, ch=ch: e.transpose(pb[P, ch, :], src[hp][P, ch, :], self.ident[P, P]), reads=[src[hp], self.ident], writes=[pb], pe_acc=True)
                k.op("act", lambda e: e.copy(dst[hp][P, :nch, :], pb[P, :nch, :]), reads=[pb], writes=[dst[hp]])
            tm(Atrue, Atm); tm(Bh, Bhtm); tm(Kh, Khtm); tm(vb, Vtm)

            def pair(dst, lt, rt_, mask, eng="dve"):
                pp = nps()
                pv = units(pp, lt[hp], rt_[hp], P, [lt[hp], rt_[hp]])
                mv = self.masks[:, mask, :].rearrange("p (c t) -> p c t", t=64)
                k.op(eng, lambda e: e.tensor_tensor(dst[hp][P, :nch, :], pv[P, :nch, :], mv[P, :nch, :], ALU.mult), reads=[pp, self.masks], writes=[dst[hp]])
            pair(Zs, bt, at, mZ)
            pair(Ns, at, bt, mN)
            pair(Aak, kt, at, mZ)
            pair(Arb, bt, rt, mI)
            pair(Ark, kt, rt, mI)
            idv = self.id8[:, :].rearrange("p (c t) -> p c t", t=64)
            k.op("pool", lambda e: e.tensor_tensor(X[hp][P, :nch, :], Zs[hp][P, :nch, :], idv[P, :nch, :], ALU.add), reads=[Zs[hp], self.id8], writes=[X[hp]])
            Zc, Nc, Zn, Nn = Zs[hp], Ns[hp], Zs2[hp], Ns2[hp]
            for lev in range(1, 6):
                last = lev == 5

                def level(Zc, Nc, Zn, Nn, last):
                    if not last:
                        pz = nps()
                        pzv = units(pz, Nc, Zc, P, [Nc, Zc])
                    pn = nps()
                    pnv = units(pn, Zc, Nc, P, [Nc, Zc])
                    if not last:
                        k.op("act", lambda e: e.copy(Zn[P, :nch, :], pzv[P, :nch, :]), reads=[pz], writes=[Zn])
                    k.op("dve", lambda e: e.tensor_copy(Nn[P, :nch, :], pnv[P, :nch, :]), reads=[pn], writes=[Nn])
                    px = nps()
                    pxv = units(px, Nn, X[hp], P, [Nn, X[hp]])
                    k.op("dve", lambda e: e.tensor_tensor(X[hp][P, :nch, :], pxv[P, :nch, :], X[hp][P, :nch, :], ALU.add), reads=[px, X[hp]], writes=[X[hp]])
                level(Zc, Nc, Zn, Nn, last)
                Zc, Nc, Zn, Nn = Zn, Nn, Zc, Nc
            pa = nps()
            pav = units(pa, Aak[hp], Vtm[hp], P, [Aak[hp], Vtm[hp]])
            k.op("act", lambda e: e.copy(AVs[hp][P, :nch, :], pav[P, :nch, :]), reads=[pa], writes=[AVs[hp]])
            pq = nps()
            pqv = units(pq, X[hp], AVs[hp], P, [X[hp], AVs[hp]])
            k.op("act", lambda e: e.copy(Qs[hp][P, :nch, :], pqv[P, :nch, :]), reads=[pq], writes=[Qs[hp]])
            pp_ = nps()
            ppv_ = units(pp_, Atm[hp], X[hp], P, [Atm[hp], X[hp]])
            k.op("dve", lambda e: e.tensor_copy(PaT[hp][P, :nch, :], ppv_[P, :nch, :]), reads=[pp_], writes=[PaT[hp]])

        for hp in range(2):
            prep(hp)
            for hl in range(2):
                head_block(hp, hl)

        def seq_step(ch, hp, hl):
            P = slice(hl * 64, hl * 64 + 64)
            pyb = PY[hp][hl]
            pu_ = nps()
            k.op("pe", lambda e: e.matmul(pu_[P, 0:64], lhsT=PaT[hp][P, ch, :], rhs=Mbf[hp][P, :], start=True, stop=True), reads=[PaT[hp], Mbf[hp]], writes=[pu_])
            k.op("dve", lambda e: e.tensor_tensor(Us[hp][P, ch, :], pu_[P, 0:64], Qs[hp][P, ch, :], ALU.add), reads=[pu_, Qs[hp]], writes=[Us[hp]])
            pyv = pyb[:, :].rearrange("p (c t) -> p c t", t=64)
            k.op("pe", lambda e: e.matmul(pyv[P, ch, :], lhsT=Mbf[hp][P, :], rhs=Rtrue[hp][P, ch, :], start=True, stop=False), reads=[Mbf[hp], Rtrue[hp]], writes=[pyb], pe_acc=True)
            k.op("pe", lambda e: e.matmul(pyv[P, ch, :], lhsT=Vtm[hp][P, ch, :], rhs=Ark[hp][P, ch, :], start=False, stop=False), reads=[Vtm[hp], Ark[hp]], writes=[pyb], pe_acc=True)
            k.op("pe", lambda e: e.matmul(pyv[P, ch, :], lhsT=Us[hp][P, ch, :], rhs=Arb[hp][P, ch, :], start=False, stop=True), reads=[Us[hp], Arb[hp]], writes=[pyb], pe_acc=True)
            pm_ = nps()
            k.op("pe", lambda e: e.matmul(pm_[P, 0:64], lhsT=Bhtm[hp][P, ch, :], rhs=Us[hp][P, ch, :], start=True, stop=False), reads=[Bhtm[hp], Us[hp]], writes=[pm_], pe_acc=True)
            k.op("pe", lambda e: e.matmul(pm_[P, 0:64], lhsT=Khtm[hp][P, ch, :], rhs=Vtm[hp][P, ch, :], start=False, stop=True), reads=[Khtm[hp], Vtm[hp]], writes=[pm_], pe_acc=True)
            k.op("dve", lambda e: e.scalar_tensor_tensor(out=Mst[hp][P, :], in0=Mst[hp][P, :], scalar=WC[hp][P, ch:ch + 1], in1=pm_[P, 0:64], op0=ALU.mult, op1=ALU.add), reads=[Mst[hp], WC[hp], pm_], writes=[Mst[hp]])
            k.op("act", lambda e: e.copy(Mbf[hp][P, :], Mst[hp][P, :]), reads=[Mst[hp]], writes=[Mbf[hp]])

        chs = range(nch) if d == 0 else range(nch - 1, -1, -1)
        for ch in chs:
            for hp in range(2):
                for hl in range(2):
                    seq_step(ch, hp, hl)

        def outp(hp):
            for hl in range(2):
                P = slice(hl * 64, hl * 64 + 64)
                pp = PY[hp][hl]
                if hl == 0:
                    k.op("dve", lambda e, P=P, pp=pp: e.tensor_copy(ysb[hp][P, :n], pp[P, :n]), reads=[pp], writes=[ysb[hp]])
                else:
                    k.op("act", lambda e, P=P, pp=pp: e.copy(ysb[hp][P, :n], pp[P, :n]), reads=[pp], writes=[ysb[hp]])
            if d == 0:
                k.op("pool", lambda e: e.dma_start(out=self.rw_yf.ap[hp * 128:(hp + 1) * 128, tok0:tok0 + n], in_=ysb[hp][:, :n]), reads=[ysb[hp]], dma=True)
            else:
                self.rw_finish(l, hp, tok0, n, ysb[hp], yf_in[hp], bon, gat, gn_g, gn_b, nps, sqk, tmp, kx, rn)
        for hp in range(2):
            outp(hp)

    for ti, (tok0, n, stream) in enumerate(order):
        tile_body(ti, tok0, n, stream)
    k.barrier()
    k.emit()
    k.free_to(m)


def rw_finish(self, l, hp, tok0, n, yb, yf, bon, gat, gn_g, gn_b, nps, sqk, tmp, kx, rn):
    k = self.k
    fl = lambda t_: t_[:, :, :].rearrange("p c t -> p (c t)")[:, 0:n]
    k.op("sp", lambda e: e.dma_start(out=yf[:, :n], in_=self.rw_yf.ap[hp * 128:(hp + 1) * 128, tok0:tok0 + n]), writes=[yf], dma=True)
    k.op("sp", lambda e: e.dma_start(out=bon[:, :n], in_=self.rw_bonus.ap[hp * 128:(hp + 1) * 128, tok0:tok0 + n]), writes=[bon], dma=True)
    k.op("sp", lambda e: e.dma_start(out=gat[:, :n], in_=self.rw_gate.ap[hp * 128:(hp + 1) * 128, tok0:tok0 + n]), writes=[gat], dma=True)
    k.op("dve", lambda e: e.tensor_tensor(yb[:, :n], yb[:, :n], yf[:, :n], ALU.add), reads=[yb, yf], writes=[yb])
    k.op("act", lambda e: e.copy(fl(sqk), yb[:, :n]), reads=[yb], writes=[sqk])
    p1 = nps()
    k.op("pe", lambda e: e.matmul(p1[:, :n], lhsT=self.blk[:, :], rhs=fl(sqk), start=True, stop=True), reads=[self.blk, sqk], writes=[p1])
    k.op("dve", lambda e: e.scalar_tensor_tensor(out=fl(kx), in0=p1[:, :n], scalar=-1.0 / 64, in1=yb[:, :n], op0=ALU.mult, op1=ALU.add), reads=[p1, yb], writes=[kx])
    k.op("act", lambda e: e.copy(fl(sqk), fl(kx)), reads=[kx], writes=[sqk])
    p1b = nps()
    k.op("pe", lambda e: e.matmul(p1b[:, :n], lhsT=self.blk[:, :], rhs=fl(sqk), start=True, stop=True), reads=[self.blk, sqk], writes=[p1b])
    k.op("dve", lambda e: e.scalar_tensor_tensor(out=fl(kx), in0=p1b[:, :n], scalar=-1.0 / 64, in1=fl(kx), op0=ALU.mult, op1=ALU.add), reads=[p1b, kx], writes=[kx])
    k.op("act", lambda e: e.activation(out=fl(sqk), in_=fl(kx), func=AF.Square), reads=[kx], writes=[sqk])
    p2 = nps()
    k.op("pe", lambda e: e.matmul(p2[:, :n], lhsT=self.blk[:, :], rhs=fl(sqk), start=True, stop=True), reads=[self.blk, sqk], writes=[p2])
    k.op("act", lambda e: e.activation(out=fl(rn), in_=p2[:, :n], func=AF.Sqrt, scale=1.0 / 64, bias=64e-5), reads=[p2], writes=[rn])
    k.op("dve", lambda e: e.reciprocal(fl(rn), fl(rn)), reads=[rn], writes=[rn])
    k.op("dve", lambda e: e.tensor_tensor(fl(kx), fl(kx), fl(rn), ALU.mult), reads=[kx, rn], writes=[kx])
    k.op("dve", lambda e: e.tensor_scalar(fl(kx), fl(kx), gn_g[:, hp:hp + 1], gn_b[:, hp:hp + 1], ALU.mult, ALU.add), reads=[kx, gn_g, gn_b], writes=[kx])
    k.op("pool", lambda e: e.tensor_tensor(fl(kx), fl(kx), bon[:, :n], ALU.add), reads=[kx, bon], writes=[kx])
    k.op("dve", lambda e: e.tensor_tensor(fl(kx), fl(kx), gat[:, :n], ALU.mult), reads=[kx, gat], writes=[kx])
    k.op("pool", lambda e: e.dma_start(out=self.mixT.ap[hp * 128:(hp + 1) * 128, tok0:tok0 + n], in_=fl(kx)), reads=[kx], dma=True)


MK.rw_consts = _rw_consts
MK.load_consts2 = _load_consts2
MK._pvec = _pvec
MK.phase_rwkv = phase_rwkv
MK.rw_finish = rw_finish


GLA_OFF = 2312


def phase_gla(self, l, d):
    k = self.k
    m = k.mark()
    NT = self.NT
    s = 1.0 / 16.0
    qscale = 32 ** -0.5
    if not hasattr(self, "gla_yf"):
        kind = "ExternalOutput" if "gla_yf" in self.dbg else "Internal"
        self.gla_yf = k.dram("gla_yf", [256, NT], F32, kind=kind)
    ga2f = k.sb("ga2f", [16, 2, 128], F32)
    ga2p = k.sb("ga2p", [16, 2, 128], BF16)
    gbp = k.sb("gbp", [128, 2], F32)
    k.op("dve", lambda e: e.memset(ga2f[:, :, :], 0.0), writes=[ga2f])
    k.op("dve", lambda e: e.memset(gbp[:, :], 0.0), writes=[gbp])
    for h in range(4):
        hp, hl = h // 2, h % 2
        k.op("sp", lambda e, h=h, hp=hp, hl=hl: e.dma_start(out=ga2f[:, hp, hl * 64:hl * 64 + 32], in_=self.gla_ga2.ap[l, d, :, h * 32:(h + 1) * 32]), writes=[ga2f], dma=True)
        k.op("sp", lambda e, h=h, hp=hp, hl=hl: e.dma_start(out=gbp[hl * 64:hl * 64 + 32, hp:hp + 1], in_=self.gla_gb.ap[l, d, h * 32:(h + 1) * 32].rearrange("(p o) -> p o", o=1), allow_slow_non_contiguous=True), writes=[gbp], dma=True)
    k.op("dve", lambda e: e.tensor_copy(ga2p[:, :, :], ga2f[:, :, :]), reads=[ga2f], writes=[ga2p])
    ng = self._pvec("gng", self.gla_norm_g.ap[l], 2)

    f32t = lambda n_: k.sb(n_, [128, 8, 64], F32)
    bft = lambda n_: k.sb(n_, [128, 8, 64], BF16)
    q = [[f32t("gq%d%d" % (i, hp)) for hp in range(2)] for i in range(2)]
    kk_ = [[f32t("gk%d%d" % (i, hp)) for hp in range(2)] for i in range(2)]
    vv = [[f32t("gv%d%d" % (i, hp)) for hp in range(2)] for i in range(2)]
    glf = [k.sb("glf%d" % i, [16, 512], F32) for i in range(2)]
    glb = k.sb("glb", [16, 512], BF16)
    for i in range(2):
        for hp in range(2):
            k.op("pool", lambda e, i=i, hp=hp: e.memset(q[i][hp][:, :, :], 0.0), writes=[q[i][hp]])
            k.op("pool", lambda e, i=i, hp=hp: e.memset(kk_[i][hp][:, :, :], 0.0), writes=[kk_[i][hp]])
    lg, cs, dcs, tmp, E, Etrue = [f32t("g_" + z) for z in ("lg", "cs", "dcs", "tmp", "E", "Etrue")]
    qt, kt, Qtrue, Kh, vb, Khtm, Vtm, Ark = [[bft("g_%s%d" % (z, hp)) for hp in range(2)] for z in ("qt", "kt", "Qtrue", "Kh", "vb", "Khtm", "Vtm", "Ark")]
    WC = [k.sb("gWC%d" % hp, [128, 8], F32) for hp in range(2)]
    Mst = [k.sb("gMst%d" % hp, [128, 64], F32) for hp in range(2)]
    Mbf = [k.sb("gMbf%d" % hp, [128, 64], BF16) for hp in range(2)]
    ysb = [k.sb("gysb%d" % hp, [128, 512], F32) for hp in range(2)]
    yfin = [k.sb("gyfin%d" % hp, [128, 512], F32) for hp in range(2)]
    rin = [k.sb("grin%d" % hp, [128, 512], F32) for hp in range(2)]
    sq = k.sb("gsq", [128, 512], BF16)
    rn = k.sb("grn", [128, 512], F32)
    PS = [k.ps("gps%d" % i, [128, 512], F32) for i in range(2)]
    PY = [[k.ps("gpy%d%d" % (i, j), [128, 512], F32) for j in range(2)] for i in range(2)]
    PSB = [k.ps("gpsb%d" % i, [128, 8, 64], BF16) for i in range(2)]
    psi = [0]; psbi = [0]

    def nps():
        p = PS[psi[0] % 2]; psi[0] += 1; return p

    def npsb():
        p = PSB[psbi[0] % 2]; psbi[0] += 1; return p
    for hp in range(2):
        k.op("dve", lambda e, hp=hp: e.memset(Mst[hp][:, :], 0.0), writes=[Mst[hp]])
        k.op("dve", lambda e, hp=hp: e.memset(Mbf[hp][:, :], 0.0), writes=[Mbf[hp]])
    mI = 1 if d == 0 else 3
    tiles = self.tok_tiles()
    lat = tiles[1:]
    order = [tiles[0]] + (lat if d == 0 else lat[::-1])
    O = GLA_OFF

    def tile_body(ti, tok0, n, stream):
        nch = n // 64
        b = ti % 2
        fl = lambda t_: t_[:, :, :].rearrange("p c t -> p (c t)")[:, 0:n]
        for h in range(4):
            hp, hl = h // 2, h % 2
            P32 = slice(hl * 64, hl * 64 + 32)
            k.op("sp", lambda e, h=h, hp=hp, P32=P32: e.dma_start(out=fl(q[b][hp])[P32, :], in_=self.uT.ap[O + h * 32:O + (h + 1) * 32, tok0:tok0 + n]), writes=[q[b][hp]], dma=True)
            k.op("sp", lambda e, h=h, hp=hp, P32=P32: e.dma_start(out=fl(kk_[b][hp])[P32, :], in_=self.uT.ap[O + 128 + h * 32:O + 128 + (h + 1) * 32, tok0:tok0 + n]), writes=[kk_[b][hp]], dma=True)
        for hp in range(2):
            k.op("sp", lambda e, hp=hp: e.dma_start(out=fl(vv[b][hp]), in_=self.uT.ap[O + 256 + hp * 128:O + 256 + (hp + 1) * 128, tok0:tok0 + n]), writes=[vv[b][hp]], dma=True)
        k.op("sp", lambda e: e.dma_start(out=glf[b][:, :n], in_=self.uT.ap[O + 512:O + 528, tok0:tok0 + n]), writes=[glf[b]], dma=True)
        k.op("act", lambda e: e.copy(glb[:, :n], glf[b][:, :n]), reads=[glf[b]], writes=[glb])

        def prep(hp):
            p1 = nps()
            k.op("pe", lambda e: e.matmul(p1[:, :n], lhsT=ga2p[:, hp, :], rhs=glb[:, :n], start=True, stop=True), reads=[ga2p, glb], writes=[p1])
            k.op("act", lambda e: e.activation(out=fl(tmp), in_=p1[:, :n], func=AF.Sigmoid, bias=gbp[:, hp:hp + 1]), reads=[p1, gbp], writes=[tmp])
            k.op("act", lambda e: e.activation(out=fl(lg), in_=fl(tmp), func=AF.Ln), reads=[tmp], writes=[lg])
            k.op("dve", lambda e: e.tensor_tensor_scan(fl(cs), self.reset[:, :n], fl(lg), 0.0, ALU.mult, ALU.add), reads=[self.reset, lg], writes=[cs])
            if d == 1:
                k.op("dve", lambda e: e.tensor_tensor(fl(tmp), fl(lg), fl(cs), ALU.subtract), reads=[lg, cs], writes=[tmp])
                k.op("dve", lambda e: e.tensor_tensor(cs[:, :nch, :], tmp[:, :nch, :], cs[:, :nch, 63:64].to_broadcast([128, nch, 64]), ALU.add), reads=[tmp, cs], writes=[cs])
            endi = 63 if d == 0 else 0
            k.op("pool", lambda e: e.tensor_tensor(dcs[:, :nch, :], cs[:, :nch, :], cs[:, :nch, 32:33].to_broadcast([128, nch, 64]), ALU.subtract), reads=[cs], writes=[dcs])
            k.op("act", lambda e: e.activation(out=fl(E), in_=fl(dcs), func=AF.Exp, scale=s), reads=[dcs], writes=[E])
            k.op("dve", lambda e: e.scalar_tensor_tensor(out=fl(qt[hp]), in0=fl(q[b][hp]), scalar=qscale, in1=fl(E), op0=ALU.mult, op1=ALU.mult), reads=[q[b][hp], E], writes=[qt[hp]])
            k.op("act", lambda e: e.activation(out=fl(E), in_=fl(dcs), func=AF.Exp, scale=-s), reads=[dcs], writes=[E])
            k.op("dve", lambda e: e.tensor_tensor(fl(kt[hp]), fl(kk_[b][hp]), fl(E), ALU.mult), reads=[kk_[b][hp], E], writes=[kt[hp]])
            k.op("act", lambda e: e.activation(out=fl(Etrue), in_=fl(cs), func=AF.Exp, scale=s), reads=[cs], writes=[Etrue])
            k.op("dve", lambda e: e.scalar_tensor_tensor(out=fl(Qtrue[hp]), in0=fl(q[b][hp]), scalar=qscale, in1=fl(Etrue), op0=ALU.mult, op1=ALU.mult), reads=[q[b][hp], Etrue], writes=[Qtrue[hp]])
            k.op("dve", lambda e: e.tensor_copy(WC[hp][:, :nch], Etrue[:, :nch, endi]), reads=[Etrue], writes=[WC[hp]])
            k.op("pool", lambda e: e.tensor_tensor(tmp[:, :nch, :], cs[:, :nch, endi:endi + 1].to_broadcast([128, nch, 64]), cs[:, :nch, :], ALU.subtract), reads=[cs], writes=[tmp])
            k.op("act", lambda e: e.activation(out=fl(E), in_=fl(tmp), func=AF.Exp, scale=s), reads=[tmp], writes=[E])
            k.op("dve", lambda e: e.tensor_tensor(fl(Kh[hp]), fl(kk_[b][hp]), fl(E), ALU.mult), reads=[kk_[b][hp], E], writes=[Kh[hp]])
            k.op("act", lambda e: e.copy(fl(vb[hp]), fl(vv[b][hp])), reads=[vv[b][hp]], writes=[vb[hp]])

        def head_block(hp, hl):
            P = slice(hl * 64, hl * 64 + 64)

            def tm(src, dst):
                pb = npsb()
                for ch in range(nch):
                    k.op("pe", lambda e, ch=ch: e.transpose(pb[P, ch, :], src[hp][P, ch, :], self.ident[P, P]), reads=[src[hp], self.ident], writes=[pb], pe_acc=True)
                k.op("act", lambda e: e.copy(dst[hp][P, :nch, :], pb[P, :nch, :]), reads=[pb], writes=[dst[hp]])
            tm(Kh, Khtm); tm(vb, Vtm)
            pp = nps()
            pv = pp[:, :].rearrange("p (c t) -> p c t", t=64)
            for ch in range(nch):
                k.op("pe", lambda e, ch=ch: e.matmul(pv[P, ch, :], lhsT=kt[hp][P, ch, :], rhs=qt[hp][P, ch, :], start=True, stop=True), reads=[kt[hp], qt[hp]], writes=[pp], pe_acc=True)
            mv = self.masks[:, mI, :].rearrange("p (c t) -> p c t", t=64)
            k.op("dve", lambda e: e.tensor_tensor(Ark[hp][P, :nch, :], pv[P, :nch, :], mv[P, :nch, :], ALU.mult), reads=[pp, self.masks], writes=[Ark[hp]])

        for hp in range(2):
            prep(hp)
            for hl in range(2):
                head_block(hp, hl)

        def seq_step(ch, hp, hl):
            P = slice(hl * 64, hl * 64 + 64)
            pyb = PY[hp][hl]
            pyv = pyb[:, :].rearrange("p (c t) -> p c t", t=64)
            k.op("pe", lambda e: e.matmul(pyv[P, ch, :], lhsT=Mbf[hp][P, :], rhs=Qtrue[hp][P, ch, :], start=True, stop=False), reads=[Mbf[hp], Qtrue[hp]], writes=[pyb], pe_acc=True)
            k.op("pe", lambda e: e.matmul(pyv[P, ch, :], lhsT=Vtm[hp][P, ch, :], rhs=Ark[hp][P, ch, :], start=False, stop=True), reads=[Vtm[hp], Ark[hp]], writes=[pyb], pe_acc=True)
            pm_ = nps()
            k.op("pe", lambda e: e.matmul(pm_[P, 0:64], lhsT=Khtm[hp][P, ch, :], rhs=Vtm[hp][P, ch, :], start=True, stop=True), reads=[Khtm[hp], Vtm[hp]], writes=[pm_])
            k.op("dve", lambda e: e.scalar_tensor_tensor(out=Mst[hp][P, :], in0=Mst[hp][P, :], scalar=WC[hp][P, ch:ch + 1], in1=pm_[P, 0:64], op0=ALU.mult, op1=ALU.add), reads=[Mst[hp], WC[hp], pm_], writes=[Mst[hp]])
            k.op("act", lambda e: e.copy(Mbf[hp][P, :], Mst[hp][P, :]), reads=[Mst[hp]], writes=[Mbf[hp]])

        chs = range(nch) if d == 0 else range(nch - 1, -1, -1)
        for ch in chs:
            for hp in range(2):
                for hl in range(2):
                    seq_step(ch, hp, hl)

        def outp(hp):
            for hl in range(2):
                P = slice(hl * 64, hl * 64 + 64)
                pp = PY[hp][hl]
                if hl == 0:
                    k.op("dve", lambda e, P=P, pp=pp: e.tensor_copy(ysb[hp][P, :n], pp[P, :n]), reads=[pp], writes=[ysb[hp]])
                else:
                    k.op("act", lambda e, P=P, pp=pp: e.copy(ysb[hp][P, :n], pp[P, :n]), reads=[pp], writes=[ysb[hp]])
            if d == 0:
                k.op("pool", lambda e: e.dma_start(out=self.gla_yf.ap[hp * 128:(hp + 1) * 128, tok0:tok0 + n], in_=ysb[hp][:, :n]), reads=[ysb[hp]], dma=True)
            else:
                yb, yf, rr = ysb[hp], yfin[hp], rin[hp]
                k.op("sp", lambda e: e.dma_start(out=yf[:, :n], in_=self.gla_yf.ap[hp * 128:(hp + 1) * 128, tok0:tok0 + n]), writes=[yf], dma=True)
                k.op("sp", lambda e: e.dma_start(out=rr[:, :n], in_=self.uT.ap[O + 528 + hp * 128:O + 528 + (hp + 1) * 128, tok0:tok0 + n]), writes=[rr], dma=True)
                k.op("dve", lambda e: e.tensor_tensor(yb[:, :n], yb[:, :n], yf[:, :n], ALU.add), reads=[yb, yf], writes=[yb])
                k.op("act", lambda e: e.activation(out=sq[:, :n], in_=yb[:, :n], func=AF.Square), reads=[yb], writes=[sq])
                p2 = nps()
                k.op("pe", lambda e: e.matmul(p2[:, :n], lhsT=self.blk[:, :], rhs=sq[:, :n], start=True, stop=True), reads=[self.blk, sq], writes=[p2])
                k.op("act", lambda e: e.activation(out=rn[:, :n], in_=p2[:, :n], func=AF.Sqrt, scale=1.0 / 64, bias=EPS), reads=[p2], writes=[rn])
                k.op("dve", lambda e: e.reciprocal(rn[:, :n], rn[:, :n]), reads=[rn], writes=[rn])
                k.op("dve", lambda e: e.scalar_tensor_tensor(out=yb[:, :n], in0=yb[:, :n], scalar=ng[:, hp:hp + 1], in1=rn[:, :n], op0=ALU.mult, op1=ALU.mult), reads=[yb, ng, rn], writes=[yb])
                k.op("act", lambda e: e.activation(out=rr[:, :n], in_=rr[:, :n], func=AF.Silu), reads=[rr], writes=[rr])
                k.op("dve", lambda e: e.tensor_tensor(yb[:, :n], yb[:, :n], rr[:, :n], ALU.mult), reads=[yb, rr], writes=[yb])
                k.op("pool", lambda e: e.dma_start(out=self.mixT.ap[768 + hp * 128:768 + (hp + 1) * 128, tok0:tok0 + n], in_=yb[:, :n]), reads=[yb], dma=True)
        for hp in range(2):
            outp(hp)

    for ti, (tok0, n, stream) in enumerate(order):
        tile_body(ti, tok0, n, stream)
    k.barrier()
    k.emit()
    k.free_to(m)


MK.phase_gla = phase_gla


SSM_OFF = 1024


def _ssm_decl(self):
    k = self.k
    NT = self.NT
    self.c_m128 = k.dram("c_m128", [5, 128, 128], F32, kind="ExternalInput")
    def S(n, s, dt=F32):
        kind = "ExternalOutput" if n in self.dbg else "Internal"
        return k.dram(n, s, dt, kind=kind)
    self.s_xtm = S("s_xtm", [NT, 768])
    self.s_ztm = S("s_ztm", [NT, 512])
    self.s_dttm = S("s_dttm", [NT, 8])
    self.s_bcT = S("s_bcT", [256, NT])
    self.s_yf = S("s_yf", [NT, 512])


def phase_ssm_conv(self, l):
    k = self.k
    m = k.mark()
    NT, T = self.NT, self.T
    cw = k.sb("cw", [128, 6, 9], F32)
    cbias = self._pvec("cbias", self.ssm_conv_b.ap[l], 6)
    for tap in range(9):
        k.op("sp", lambda e, tap=tap: e.dma_start(out=cw[:, :, tap], in_=self.ssm_conv_w.ap[l, tap // 3, tap % 3].rearrange("(c p) -> p c", p=128), allow_slow_non_contiguous=True), writes=[cw], dma=True)
    xin = [k.sb("cxin%d" % i, [128, 642], F32) for i in range(3)]
    acc = [k.sb("cacc%d" % i, [128, 512], F32) for i in range(2)]
    acc2 = [k.sb("cacc2%d" % i, [128, 512], F32) for i in range(2)]
    res = [k.sb("cres%d" % i, [128, 512], F32) for i in range(2)]
    zin = [k.sb("czin%d" % i, [128, 512], F32) for i in range(2)]
    dtin = [k.sb("cdtin%d" % i, [8, 512], F32) for i in range(2)]
    otm = [k.sb("cotm%d" % i, [128, 4, 128], F32) for i in range(2)]
    odt = [k.sb("codt%d" % i, [128, 4, 8], F32) for i in range(2)]
    PT = [k.ps("cpt%d" % i, [128, 4, 128], F32) for i in range(3)]
    pti = [0]
    cnt = [0]
    O = SSM_OFF

    def transpose_store(src, npart, dst_ap_fn, n, ob):
        pt = PT[pti[0] % 3]; pti[0] += 1
        nb = n // 128
        for tb in range(nb):
            k.op("pe", lambda e, tb=tb: e.transpose(pt[:, tb, :npart], src[:npart, tb * 128:(tb + 1) * 128], self.identf[:npart, :npart]), reads=[src, self.identf], writes=[pt], pe_acc=True)
        eng = "act" if cnt[0] % 2 else "dve"
        cnt[0] += 1
        if eng == "act":
            k.op("act", lambda e: e.copy(ob[:, :nb, :npart], pt[:, :nb, :npart]), reads=[pt], writes=[ob])
        else:
            k.op("dve", lambda e: e.tensor_copy(ob[:, :nb, :npart], pt[:, :nb, :npart]), reads=[pt], writes=[ob])
        k.op("pool", lambda e: e.dma_start(out=dst_ap_fn(nb), in_=ob[:, :nb, :npart]), reads=[ob], dma=True)

    it = [0]
    for (tok0, n, stream) in self.tok_tiles():
        seq0, seq1 = (0, CTX) if stream == 1 else (CTX, NT)
        halo = 65 if stream == 0 else 1
        for c in range(6):
            i = it[0]; it[0] += 1
            xb = xin[i % 3]; ac = acc[i % 2]; ac2 = acc2[i % 2]; rs = res[i % 2]
            lo = max(tok0 - halo, seq0); hi = min(tok0 + n + halo, seq1)
            if lo > tok0 - halo:
                k.op("pool", lambda e, xb=xb, halo=halo: e.memset(xb[:, 0:halo], 0.0), writes=[xb])
            if hi < tok0 + n + halo:
                k.op("pool", lambda e, xb=xb, halo=halo, n=n: e.memset(xb[:, halo + n:halo + n + halo], 0.0), writes=[xb])
            k.op("sp", lambda e, xb=xb, lo=lo, hi=hi, tok0=tok0, halo=halo, c=c: e.dma_start(out=xb[:, lo - (tok0 - halo):hi - (tok0 - halo)], in_=self.uT.ap[O + 512 + c * 128:O + 512 + (c + 1) * 128, lo:hi]), writes=[xb], dma=True)
            first = True
            dys = (-1, 0, 1) if stream == 0 else (0,)
            for dy in dys:
                for dx in (0, -1, 1):
                    tap = (dy + 1) * 3 + (dx + 1)
                    off = halo + dy * 64 + dx
                    if first:
                        k.op("dve", lambda e, ac=ac, xb=xb, off=off, n=n, c=c, tap=tap: e.tensor_scalar(ac[:, :n], xb[:, off:off + n], cw[:, c, tap:tap + 1], None, ALU.mult), reads=[xb, cw], writes=[ac])
                        first = False
                        continue
                    c0, c1 = (0, 64) if (dx == 0 or stream == 1) else ((1, 64) if dx == -1 else (0, 63))
                    def tapop(ac=ac, xb=xb, off=off, n=n, c=c, tap=tap, c0=c0, c1=c1):
                        src = xb[:, off:off + n].rearrange("p (r w) -> p r w", w=64)[:, :, c0:c1]
                        dst = ac[:, :n].rearrange("p (r w) -> p r w", w=64)[:, :, c0:c1]
                        k.op("dve", lambda e: e.scalar_tensor_tensor(out=dst, in0=src, scalar=cw[:, c, tap:tap + 1], in1=dst, op0=ALU.mult, op1=ALU.add), reads=[xb, cw, ac], writes=[ac])
                    tapop()
            k.op("act", lambda e, ac=ac, rs=rs, n=n, c=c: e.activation(out=rs[:, :n], in_=ac[:, :n], func=AF.Silu, bias=cbias[:, c:c + 1]), reads=[ac, cbias], writes=[rs])
            if c >= 4:
                k.op("pool", lambda e, rs=rs, n=n, c=c, tok0=tok0: e.dma_start(out=self.s_bcT.ap[(c - 4) * 128:(c - 3) * 128, tok0:tok0 + n], in_=rs[:, :n]), reads=[rs], dma=True)
            ob = otm[i % 2]
            transpose_store(rs, 128, lambda nb, c=c, tok0=tok0: self.s_xtm.ap[tok0:tok0 + nb * 128, c * 128:(c + 1) * 128].rearrange("(b p) f -> p b f", p=128), n, ob)
        for c in range(4):
            i = it[0]; it[0] += 1
            zb = zin[i % 2]
            k.op("sp", lambda e, zb=zb, c=c, tok0=tok0, n=n: e.dma_start(out=zb[:, :n], in_=self.uT.ap[O + c * 128:O + (c + 1) * 128, tok0:tok0 + n]), writes=[zb], dma=True)
            ob = otm[i % 2]
            transpose_store(zb, 128, lambda nb, c=c, tok0=tok0: self.s_ztm.ap[tok0:tok0 + nb * 128, c * 128:(c + 1) * 128].rearrange("(b p) f -> p b f", p=128), n, ob)
        i = it[0]; it[0] += 1
        db = dtin[i % 2]
        k.op("sp", lambda e, db=db, tok0=tok0, n=n: e.dma_start(out=db[:, :n], in_=self.uT.ap[O + 1280:O + 1288, tok0:tok0 + n]), writes=[db], dma=True)
        ob = odt[i % 2]
        transpose_store(db, 8, lambda nb, tok0=tok0: self.s_dttm.ap[tok0:tok0 + nb * 128, :].rearrange("(b p) f -> p b f", p=128), n, ob)
    k.barrier()
    k.emit()
    k.free_to(m)


def phase_ssm_scan(self, l, d):
    k = self.k
    m = k.mark()
    NT = self.NT
    BIG = 30000.0
    m128 = k.sb("m128", [128, 5, 128], F32)
    for i in range(5):
        k.op("sp", lambda e, i=i: e.dma_start(out=m128[:, i, :], in_=self.c_m128.ap[i]), writes=[m128], dma=True)
    LE, GE, GT, LT, NEGI = 0, 1, 2, 3, 4
    if d == 0:
        mTri, mR, mNeg = LE, GT, GT
    else:
        mTri, mR, mNeg = GE, LT, LT
    onesf = k.sb("onesf", [128, 128], F32)
    k.op("dve", lambda e: e.memset(onesf[:, :], 1.0), writes=[onesf])
    dtb = k.sb("dtb", [128, 8], F32)
    aneg = k.sb("aneg", [128, 8], F32)
    dsk = k.sb("dsk", [128, 8], F32)
    ngb = k.sb("ngb", [128, 512], F32)
    k.op("sp", lambda e: e.dma_start(out=dtb[:, :], in_=self.ssm_dt_bias.ap[l, d].partition_broadcast(128)), writes=[dtb], dma=True)
    k.op("sp", lambda e: e.dma_start(out=aneg[:, :], in_=self.ssm_a_log.ap[l, d].partition_broadcast(128)), writes=[aneg], dma=True)
    k.op("sp", lambda e: e.dma_start(out=dsk[:, :], in_=self.ssm_d.ap[l].partition_broadcast(128)), writes=[dsk], dma=True)
    k.op("sp", lambda e: e.dma_start(out=ngb[:, :], in_=self.ssm_norm_g.ap[l].partition_broadcast(128)), writes=[ngb], dma=True)
    k.op("act", lambda e: e.activation(out=aneg[:, :], in_=aneg[:, :], func=AF.Exp), reads=[aneg], writes=[aneg])
    k.op("dve", lambda e: e.tensor_scalar(aneg[:, :], aneg[:, :], -1.0, None, ALU.mult), reads=[aneg], writes=[aneg])

    xs = [k.sb("sxs%d" % i, [128, 768], F32) for i in range(2)]
    dt = [k.sb("sdt%d" % i, [128, 8], F32) for i in range(2)]
    BT = [[k.sb("sBT%d%d" % (i, g), [64, 128], F32) for g in range(2)] for i in range(2)]
    CT = [[k.sb("sCT%d%d" % (i, g), [64, 128], F32) for g in range(2)] for i in range(2)]
    BTb = [k.sb("sBTb%d" % g, [64, 128], BF16) for g in range(2)]
    CTb = [k.sb("sCTb%d" % g, [64, 128], BF16) for g in range(2)]
    Btm = k.sb("sBtm", [128, 128], BF16)
    zt = [k.sb("szt%d" % i, [128, 512], F32) for i in range(2)]
    yfin = [k.sb("syf%d" % i, [128, 512], F32) for i in range(2)]
    xdt = k.sb("sx", [128, 8], F32)
    dA = k.sb("sdA", [128, 8], F32)
    cs = k.sb("scs", [128, 8], F32)
    dte = k.sb("sdte", [128, 8], F32)
    ecs = k.sb("secs", [128, 8], F32)
    eend = k.sb("seend", [128, 8], F32)
    dtp = k.sb("sdtp", [128, 8], F32)
    Xd = k.sb("sXd", [128, 8, 64], BF16)
    Xdd = k.sb("sXdd", [128, 8, 64], BF16)
    Rm = [k.sb("sRm%d" % i, [128, 128], F32) for i in range(4)]
    Lt = k.sb("sLt", [128, 8, 128], BF16)
    sc = k.sb("ssc", [128, 2, 128], BF16)
    Gt = k.sb("sGt", [128, 8, 128], BF16)
    Yt = k.sb("sYt", [128, 512], F32)
    Y = k.sb("sY", [128, 512], F32)
    junk = k.sb("sjunk", [128, 512], BF16)
    ss = k.sb("sss", [128, 1], F32)
    Ybf = k.sb("sYbf", [128, 512], BF16)
    ofm = k.sb("sofm", [128, 4, 128], F32)
    Sst = k.sb("sSst", [64, 8, 64], F32)
    Stmp = k.sb("sStmp", [64, 8, 64], F32)
    Sbf = k.sb("sSbf", [64, 8, 64], BF16)
    k.op("dve", lambda e: e.memset(Sst[:, :, :], 0.0), writes=[Sst])
    k.op("dve", lambda e: e.memset(Sbf[:, :, :], 0.0), writes=[Sbf])
    pcs = k.ps("spcs", [128, 2, 8], F32)
    pD = [k.ps("spD%d" % i, [128, 4, 128], F32) for i in range(2)]
    psc = k.ps("spsc", [128, 2, 128], F32)
    pYd = k.ps("spYd", [128, 512], F32)
    pYo = k.ps("spYo", [128, 512], F32)
    pSt = k.ps("spSt", [64, 512], F32)
    pT = k.ps("spT", [128, 4, 128], BF16)

    nchunks = NT // 128
    ctxc = [0, 1]
    latc = list(range(2, nchunks))
    order = (ctxc + latc) if d == 0 else (ctxc[::-1] + latc[::-1])
    ri = [0]

    def chunk(ci, c):
        b = ci % 2
        t0 = c * 128
        x_, dt_, z_, yf_ = xs[b], dt[b], zt[b], yfin[b]
        k.op("sp", lambda e: e.dma_start(out=x_[:, :], in_=self.s_xtm.ap[t0:t0 + 128, :]), writes=[x_], dma=True)
        k.op("sp", lambda e: e.dma_start(out=dt_[:, :], in_=self.s_dttm.ap[t0:t0 + 128, :]), writes=[dt_], dma=True)
        for g in range(2):
            k.op("sp", lambda e, g=g: e.dma_start(out=BT[b][g][:, :], in_=self.s_bcT.ap[g * 64:(g + 1) * 64, t0:t0 + 128]), writes=[BT[b][g]], dma=True)
            k.op("sp", lambda e, g=g: e.dma_start(out=CT[b][g][:, :], in_=self.s_bcT.ap[128 + g * 64:128 + (g + 1) * 64, t0:t0 + 128]), writes=[CT[b][g]], dma=True)
        if d == 1:
            k.op("sp", lambda e: e.dma_start(out=z_[:, :], in_=self.s_ztm.ap[t0:t0 + 128, :]), writes=[z_], dma=True)
            k.op("sp", lambda e: e.dma_start(out=yf_[:, :], in_=self.s_yf.ap[t0:t0 + 128, :]), writes=[yf_], dma=True)
        for g in range(2):
            k.op("act", lambda e, g=g: e.copy(BTb[g][:, :], BT[b][g][:, :]), reads=[BT[b][g]], writes=[BTb[g]])
            k.op("act", lambda e, g=g: e.copy(CTb[g][:, :], CT[b][g][:, :]), reads=[CT[b][g]], writes=[CTb[g]])
        k.op("act", lambda e: e.copy(Btm[:, :], x_[:, 512:640]), reads=[x_], writes=[Btm])
        k.op("dve", lambda e: e.tensor_tensor(xdt[:, :], dt_[:, :], dtb[:, :], ALU.add), reads=[dt_, dtb], writes=[xdt])
        k.op("act", lambda e: e.activation(out=xdt[:, :], in_=xdt[:, :], func=AF.Exp), reads=[xdt], writes=[xdt])
        k.op("act", lambda e: e.activation(out=dtp[:, :], in_=xdt[:, :], func=AF.Ln, bias=1.0), reads=[xdt], writes=[dtp])
        k.op("dve", lambda e: e.tensor_tensor(dA[:, :], dtp[:, :], aneg[:, :], ALU.mult), reads=[dtp, aneg], writes=[dA])
        k.op("pe", lambda e: e.matmul(pcs[:, 0, :], lhsT=m128[:, mTri, :], rhs=dA[:, :], start=True, stop=True), reads=[m128, dA], writes=[pcs])
        k.op("pe", lambda e: e.matmul(pcs[:, 1, :], lhsT=onesf[:, :], rhs=dA[:, :], start=True, stop=True), reads=[onesf, dA], writes=[pcs], pe_acc=True)
        k.op("dve", lambda e: e.tensor_copy(cs[:, :], pcs[:, 0, :]), reads=[pcs], writes=[cs])
        k.op("act", lambda e: e.activation(out=ecs[:, :], in_=pcs[:, 0, :], func=AF.Exp), reads=[pcs], writes=[ecs])
        k.op("act", lambda e: e.activation(out=eend[:, :], in_=pcs[:, 1, :], func=AF.Exp), reads=[pcs], writes=[eend])
        k.op("dve", lambda e: e.tensor_tensor(dte[:, :], pcs[:, 1, :], cs[:, :], ALU.subtract), reads=[pcs, cs], writes=[dte])
        k.op("act", lambda e: e.activation(out=dte[:, :], in_=dte[:, :], func=AF.Exp), reads=[dte], writes=[dte])
        xv = x_[:, 0:512].rearrange("p (h e) -> p h e", e=64)
        k.op("dve", lambda e: e.tensor_tensor(Xd[:, :, :], xv, dtp[:, :].unsqueeze(2).to_broadcast([128, 8, 64]), ALU.mult), reads=[x_, dtp], writes=[Xd])
        k.op("pool", lambda e: e.tensor_tensor(Xdd[:, :, :], Xd[:, :, :], dte[:, :].unsqueeze(2).to_broadcast([128, 8, 64]), ALU.mult), reads=[Xd, dte], writes=[Xdd])
        for h in range(8):
            r_ = Rm[ri[0] % 4]; ri[0] += 1
            pd = pD[h // 4]
            k.op("dve" if h % 2 == 0 else "pool", lambda e, h=h, r_=r_: e.tensor_tensor(r_[:, :], m128[:, mR, :], dA[:, h:h + 1].to_broadcast([128, 128]), ALU.mult), reads=[m128, dA], writes=[r_])
            k.op("pe", lambda e, h=h, r_=r_, pd=pd: e.matmul(pd[:, h % 4, :], lhsT=r_[:, :], rhs=m128[:, mTri, :], start=True, stop=False), reads=[r_, m128], writes=[pd], pe_acc=True)
            k.op("pe", lambda e, h=h, pd=pd: e.matmul(pd[:, h % 4, :], lhsT=m128[:, NEGI, :], rhs=m128[:, mNeg, :], start=False, stop=True), reads=[m128], writes=[pd], pe_acc=True)
        for hh in range(2):
            k.op("act", lambda e, hh=hh: e.activation(out=Lt[:, hh * 4:(hh + 1) * 4, :], in_=pD[hh][:, :, :], func=AF.Exp), reads=[pD[hh]], writes=[Lt])
        for g in range(2):
            k.op("pe", lambda e, g=g: e.matmul(psc[:, g, :], lhsT=BTb[g][:, :], rhs=CTb[g][:, :], start=True, stop=True), reads=[BTb[g], CTb[g]], writes=[psc], pe_acc=True)
        k.op("dve", lambda e: e.tensor_copy(sc[:, :, :], psc[:, :, :]), reads=[psc], writes=[sc])
        for g in range(2):
            k.op("dve" if g == 0 else "pool", lambda e, g=g: e.tensor_tensor(Gt[:, g * 4:(g + 1) * 4, :], Lt[:, g * 4:(g + 1) * 4, :], sc[:, g:g + 1, :].to_broadcast([128, 4, 128]), ALU.mult), reads=[Lt, sc], writes=[Gt])
        for h in range(8):
            k.op("pe", lambda e, h=h: e.matmul(pYd[:, h * 64:(h + 1) * 64], lhsT=Gt[:, h, :], rhs=Xd[:, h, :], start=True, stop=True), reads=[Gt, Xd], writes=[pYd], pe_acc=True)
        for g in range(2):
            k.op("pe", lambda e, g=g: e.matmul(pYo[:, g * 256:(g + 1) * 256], lhsT=CTb[g][:, :], rhs=Sbf[:, g * 4:(g + 1) * 4, :].rearrange("p h e -> p (h e)"), start=True, stop=True), reads=[CTb[g], Sbf], writes=[pYo], pe_acc=True)
        k.op("dve", lambda e: e.tensor_tensor(Yt[:, :].rearrange("p (h e) -> p h e", e=64), pYo[:, :].rearrange("p (h e) -> p h e", e=64), ecs[:, :].unsqueeze(2).to_broadcast([128, 8, 64]), ALU.mult), reads=[pYo, ecs], writes=[Yt])
        k.op("dve", lambda e: e.tensor_tensor(Y[:, :], Yt[:, :], pYd[:, :], ALU.add), reads=[Yt, pYd], writes=[Y])
        for g in range(2):
            k.op("pe", lambda e, g=g: e.matmul(pSt[:, g * 256:(g + 1) * 256], lhsT=Btm[:, g * 64:(g + 1) * 64], rhs=Xdd[:, g * 4:(g + 1) * 4, :].rearrange("p h e -> p (h e)"), start=True, stop=True), reads=[Btm, Xdd], writes=[pSt], pe_acc=True)
        k.op("pool", lambda e: e.tensor_tensor(Stmp[:, :, :], Sst[:, :, :], eend[0:64, :].unsqueeze(2).to_broadcast([64, 8, 64]), ALU.mult), reads=[Sst, eend], writes=[Stmp])
        k.op("dve", lambda e: e.tensor_tensor(Sst[:, :, :], Stmp[:, :, :], pSt[:, :].rearrange("p (h e) -> p h e", e=64), ALU.add), reads=[Stmp, pSt], writes=[Sst])
        k.op("act", lambda e: e.copy(Sbf[:, :, :], Sst[:, :, :]), reads=[Sst], writes=[Sbf])
        if d == 0:
            k.op("pool", lambda e: e.dma_start(out=self.s_yf.ap[t0:t0 + 128, :], in_=Y[:, :]), reads=[Y], dma=True)
        else:
            k.op("dve", lambda e: e.tensor_tensor(Y[:, :], Y[:, :], yf_[:, :], ALU.add), reads=[Y, yf_], writes=[Y])
            k.op("pool", lambda e: e.tensor_tensor(Yt[:, :].rearrange("p (h e) -> p h e", e=64), xv, dsk[:, :].unsqueeze(2).to_broadcast([128, 8, 64]), ALU.mult), reads=[x_, dsk], writes=[Yt])
            k.op("dve", lambda e: e.tensor_tensor(Y[:, :], Y[:, :], Yt[:, :], ALU.add), reads=[Y, Yt], writes=[Y])
            k.op("act", lambda e: e.activation(out=z_[:, :], in_=z_[:, :], func=AF.Silu), reads=[z_], writes=[z_])
            k.op("dve", lambda e: e.tensor_tensor(Y[:, :], Y[:, :], z_[:, :], ALU.mult), reads=[Y, z_], writes=[Y])
            k.op("act", lambda e: e.activation(out=junk[:, :], in_=Y[:, :], func=AF.Square, accum_out=ss[:, 0:1]), reads=[Y], writes=[junk, ss])
            k.op("act", lambda e: e.activation(out=ss[:, 0:1], in_=ss[:, 0:1], func=AF.Sqrt, scale=1.0 / 512, bias=EPS), reads=[ss], writes=[ss])
            k.op("dve", lambda e: e.reciprocal(ss[:, 0:1], ss[:, 0:1]), reads=[ss], writes=[ss])
            k.op("dve", lambda e: e.scalar_tensor_tensor(out=Ybf[:, :], in0=Y[:, :], scalar=ss[:, 0:1], in1=ngb[:, :], op0=ALU.mult, op1=ALU.mult), reads=[Y, ss, ngb], writes=[Ybf])
            for j in range(4):
                k.op("pe", lambda e, j=j: e.transpose(pT[:, j, :], Ybf[:, j * 128:(j + 1) * 128], self.ident[:, :]), reads=[Ybf, self.ident], writes=[pT], pe_acc=True)
            k.op("act", lambda e: e.copy(ofm[:, :, :], pT[:, :, :]), reads=[pT], writes=[ofm])
            k.op("pool", lambda e: e.dma_start(out=self.mixT.ap[256:768, t0:t0 + 128].rearrange("(j p) t -> p j t", p=128), in_=ofm[:, :, :]), reads=[ofm], dma=True)

    for ci, c in enumerate(order):
        chunk(ci, c)
    k.barrier()
    k.emit()
    k.free_to(m)


MK.ssm_decl = _ssm_decl
MK.phase_ssm_conv = phase_ssm_conv
MK.phase_ssm_scan = phase_ssm_scan


def _moe_decl(self):
    k = self.k
    NT = self.NT
    def S(n, s, dt=F32):
        kind = "ExternalOutput" if n in self.dbg else "Internal"
        return k.dram(n, s, dt, kind=kind)
    self.h2T = S("h2T", [D, NT], BF16)
    self.combT = S("combT", [16, NT])
    self.c_sel = k.dram("c_sel", [16, 16, 128], F32, kind="ExternalInput")


def phase_outproj(self, l, lat_only):
    k = self.k
    m = k.mark()
    NT = self.NT
    wout = k.sb("wout", [128, 8, D], BF16)
    for c in range(8):
        k.op("pool", lambda e, c=c: e.dma_start(out=wout[:, c, :], in_=self.w_out.ap[l, c * 128:(c + 1) * 128, :]), writes=[wout], dma=True)
    wr = k.sb("wr", [128, 8, 20], F32)
    k.op("sp", lambda e: e.dma_start(out=wr[:, :, 0:4], in_=self.moe_rg_w.ap[l].rearrange("(c p) g -> p c g", p=128)), writes=[wr], dma=True)
    k.op("sp", lambda e: e.dma_start(out=wr[:, :, 4:20], in_=self.moe_re_w.ap[l].rearrange("(c p) g -> p c g", p=128)), writes=[wr], dma=True)
    rb = k.sb("rb", [128, 20], F32)
    k.op("sp", lambda e: e.dma_start(out=rb[:, 0:4], in_=self.moe_rg_b.ap[l].partition_broadcast(128)), writes=[rb], dma=True)
    k.op("sp", lambda e: e.dma_start(out=rb[:, 4:20], in_=self.moe_re_b.ap[l].partition_broadcast(128)), writes=[rb], dma=True)
    mixf = [k.sb("mixf%d" % i, [128, 8, 512], F32) for i in range(2)]
    mixb = k.sb("mixb", [128, 8, 512], BF16)
    xt = [k.sb("oxt%d" % i, [128, 8, 512], F32) for i in range(2)]
    sq = k.sb("osq", [128, 8, 512], BF16)
    rstd = k.sb("orstd", [128, 512], F32)
    hT = k.sb("ohT", [128, 8, 512], BF16)
    pss = k.ps("opss", [128, 512], F32)
    po = [k.ps("opo%d" % i, [128, 512], F32) for i in range(3)]
    plg = k.ps("oplg", [128, 4, 20], F32)
    pct = k.ps("opct", [16, 4, 128], F32)
    R_ = lambda n_, s_: k.sb(n_, [128] + s_, F32)
    lg = R_("rlg", [4, 20]); gmax = R_("rgmax", [4, 1]); ohg = R_("rohg", [4, 4]); eg = R_("reg", [4, 4]); gsum = R_("rgsum", [4, 1])
    pgr = R_("rpgr", [4, 1]); esel3 = R_("resel3", [4, 4, 4]); esel = R_("resel", [4, 4]); m1 = R_("rm1", [4, 1]); oh1 = R_("roh1", [4, 4])
    es2 = R_("res2", [4, 4]); m2 = R_("rm2", [4, 1]); oh2 = R_("roh2", [4, 4]); ex2 = R_("rex2", [4, 1]); den = R_("rden", [4, 1])
    w1_ = R_("rw1", [4, 1]); w2_ = R_("rw2", [4, 1]); cig = R_("rcig", [4, 4]); comb = R_("rcomb", [4, 4, 4]); tmp4 = R_("rtmp4", [4, 4])
    combT_sb = k.sb("combT_sb", [16, 4, 128], F32)
    xTv = self.xT.ap.rearrange("(c p) t -> p c t", p=128)
    mTv = self.mixT.ap.rearrange("(c p) t -> p c t", p=128)
    hTv = self.h2T.ap.rearrange("(c p) t -> p c t", p=128)
    pi = [0]

    def tile_body(ti, tok0, n, stream):
        b = ti % 2
        mf, x_ = mixf[b], xt[b]
        k.op("sp", lambda e: e.dma_start(out=mf[:, :, :n], in_=mTv[:, :, tok0:tok0 + n]), writes=[mf], dma=True)
        k.op("sp", lambda e: e.dma_start(out=x_[:, :, :n], in_=xTv[:, :, tok0:tok0 + n]), writes=[x_], dma=True)
        k.op("act", lambda e: e.copy(mixb[:, 0:4, :n], mf[:, 0:4, :n]), reads=[mf], writes=[mixb])
        k.op("pool", lambda e: e.tensor_copy(mixb[:, 4:8, :n], mf[:, 4:8, :n]), reads=[mf], writes=[mixb])
        for j in range(8):
            p_ = po[pi[0] % 3]; pi[0] += 1
            for c in range(8):
                k.op("pe", lambda e, c=c, j=j, p_=p_: e.matmul(p_[:, :n], lhsT=wout[:, c, j * 128:(j + 1) * 128], rhs=mixb[:, c, :n], start=(c == 0), stop=(c == 7)), reads=[wout, mixb], writes=[p_], pe_acc=True)
            k.op("dve", lambda e, j=j, p_=p_: e.scalar_tensor_tensor(out=x_[:, j, :n], in0=p_[:, :n], scalar=self.modT[:, l, 16 + j, stream:stream + 1], in1=x_[:, j, :n], op0=ALU.mult, op1=ALU.add), reads=[p_, self.modT, x_], writes=[x_])
        k.op("pool", lambda e: e.dma_start(out=xTv[:, :, tok0:tok0 + n], in_=x_[:, :, :n]), reads=[x_], dma=True)
        self.norm_tile(l, 1, tok0, n, stream, x_, sq, pss, rstd, hT, keep_f32=True)
        k.op("pool", lambda e: e.dma_start(out=hTv[:, :, tok0:tok0 + n], in_=hT[:, :, :n]), reads=[hT], dma=True)
        nb = n // 128
        for tb in range(nb):
            for c in range(8):
                k.op("pe", lambda e, tb=tb, c=c: e.matmul(plg[:, tb, :], lhsT=x_[:, c, tb * 128:(tb + 1) * 128], rhs=wr[:, c, :], start=(c == 0), stop=(c == 7)), reads=[x_, wr], writes=[plg], pe_acc=True)
        V = lambda t_, *idx: t_[(slice(None), slice(0, nb)) + idx]
        op = lambda fn, r, w: k.op("dve", fn, reads=r, writes=w)
        op(lambda e: e.tensor_tensor(lg[:, :nb, :], plg[:, :nb, :], rb[:, :].unsqueeze(1).to_broadcast([128, nb, 20]), ALU.add), [plg, rb], [lg])
        op(lambda e: e.tensor_reduce(gmax[:, :nb, :], lg[:, :nb, 0:4], AX.X, ALU.max), [lg], [gmax])
        op(lambda e: e.tensor_tensor(ohg[:, :nb, :], lg[:, :nb, 0:4], gmax[:, :nb, :].to_broadcast([128, nb, 4]), ALU.is_ge), [lg, gmax], [ohg])
        op(lambda e: e.tensor_tensor(eg[:, :nb, :], lg[:, :nb, 0:4], gmax[:, :nb, :].to_broadcast([128, nb, 4]), ALU.subtract), [lg, gmax], [eg])
        k.op("act", lambda e: e.activation(out=eg[:, :nb, :], in_=eg[:, :nb, :], func=AF.Exp), reads=[eg], writes=[eg])
        op(lambda e: e.tensor_reduce(gsum[:, :nb, :], eg[:, :nb, :], AX.X, ALU.add), [eg], [gsum])
        op(lambda e: e.reciprocal(pgr[:, :nb, :], gsum[:, :nb, :]), [gsum], [pgr])
        elv = lg[:, :nb, 4:20].rearrange("p b (g e) -> p b g e", e=4)
        op(lambda e: e.tensor_tensor(esel3[:, :nb, :, :], elv, ohg[:, :nb, :].unsqueeze(3).to_broadcast([128, nb, 4, 4]), ALU.mult), [lg, ohg], [esel3])
        op(lambda e: e.tensor_reduce(esel[:, :nb, :], esel3[:, :nb, :, :].rearrange("p b g e -> p b e g"), AX.X, ALU.add), [esel3], [esel])
        op(lambda e: e.tensor_reduce(m1[:, :nb, :], esel[:, :nb, :], AX.X, ALU.max), [esel], [m1])
        op(lambda e: e.tensor_tensor(oh1[:, :nb, :], esel[:, :nb, :], m1[:, :nb, :].to_broadcast([128, nb, 4]), ALU.is_ge), [esel, m1], [oh1])
        op(lambda e: e.scalar_tensor_tensor(out=es2[:, :nb, :], in0=oh1[:, :nb, :], scalar=-1e30, in1=esel[:, :nb, :], op0=ALU.mult, op1=ALU.add), [oh1, esel], [es2])
        op(lambda e: e.tensor_reduce(m2[:, :nb, :], es2[:, :nb, :], AX.X, ALU.max), [es2], [m2])
        op(lambda e: e.tensor_tensor(oh2[:, :nb, :], es2[:, :nb, :], m2[:, :nb, :].to_broadcast([128, nb, 4]), ALU.is_ge), [es2, m2], [oh2])
        op(lambda e: e.tensor_tensor(ex2[:, :nb, :], m2[:, :nb, :], m1[:, :nb, :], ALU.subtract), [m2, m1], [ex2])
        k.op("act", lambda e: e.activation(out=ex2[:, :nb, :], in_=ex2[:, :nb, :], func=AF.Exp), reads=[ex2], writes=[ex2])
        op(lambda e: e.tensor_scalar(den[:, :nb, :], ex2[:, :nb, :], 1.0, None, ALU.add), [ex2], [den])
        op(lambda e: e.reciprocal(den[:, :nb, :], den[:, :nb, :]), [den], [den])
        op(lambda e: e.tensor_tensor(w1_[:, :nb, :], den[:, :nb, :], pgr[:, :nb, :], ALU.mult), [den, pgr], [w1_])
        op(lambda e: e.tensor_tensor(w2_[:, :nb, :], w1_[:, :nb, :], ex2[:, :nb, :], ALU.mult), [w1_, ex2], [w2_])
        op(lambda e: e.tensor_tensor(cig[:, :nb, :], oh1[:, :nb, :], w1_[:, :nb, :].to_broadcast([128, nb, 4]), ALU.mult), [oh1, w1_], [cig])
        op(lambda e: e.tensor_tensor(tmp4[:, :nb, :], oh2[:, :nb, :], w2_[:, :nb, :].to_broadcast([128, nb, 4]), ALU.mult), [oh2, w2_], [tmp4])
        op(lambda e: e.tensor_tensor(cig[:, :nb, :], cig[:, :nb, :], tmp4[:, :nb, :], ALU.add), [cig, tmp4], [cig])
        for g in range(4):
            op(lambda e, g=g: e.tensor_tensor(comb[:, :nb, g, :], cig[:, :nb, :], ohg[:, :nb, g:g + 1].to_broadcast([128, nb, 4]), ALU.mult), [cig, ohg], [comb])
        for tb in range(nb):
            k.op("pe", lambda e, tb=tb: e.transpose(pct[:, tb, :], comb[:, tb, :, :].rearrange("p g e -> p (g e)"), self.identf[:, :]), reads=[comb, self.identf], writes=[pct], pe_acc=True)
        k.op("act", lambda e: e.copy(combT_sb[:, :nb, :], pct[:, :nb, :]), reads=[pct], writes=[combT_sb])
        k.op("pool", lambda e: e.dma_start(out=self.combT.ap[:, tok0:tok0 + n].rearrange("k (b t) -> k b t", t=128), in_=combT_sb[:, :nb, :]), reads=[combT_sb], dma=True)

    for ti, (tok0, n, stream) in enumerate(self.tok_tiles(lat_only=lat_only)):
        tile_body(ti, tok0, n, stream)
    k.barrier()
    k.emit()
    k.free_to(m)


def phase_moe(self, l, lat_only):
    k = self.k
    m = k.mark()
    NT, T = self.NT, self.T
    TTL = min(2048, T)
    tiles = []
    start = CTX if lat_only else 0
    first = True
    t = start
    while t < NT:
        if first and not lat_only:
            n = CTX + TTL
        else:
            n = TTL
        n = min(n, NT - t)
        tiles.append((t, n))
        t += n
        first = False
    TTmax = max(n for _, n in tiles)
    acc = k.sb("macc", [128, 8, TTmax], F32)
    h2 = k.sb("mh2", [128, 8, TTmax], BF16)
    cTb = k.sb("mcTb", [16, TTmax], BF16)
    w1 = [k.sb("mw1%d" % i, [128, 8, 512], BF16) for i in range(2)]
    w3 = [k.sb("mw3%d" % i, [128, 8, 512], BF16) for i in range(2)]
    w2 = [k.sb("mw2%d" % i, [128, 4, D], BF16) for i in range(2)]
    self_sel = k.sb("msel", [16, 16, 128], BF16)
    k.op("pool", lambda e: e.dma_start(out=self_sel[:, :, :], in_=self.c_sel.ap.rearrange("e k m -> k e m")), writes=[self_sel], dma=True)
    sa = [k.sb("msa%d" % i, [128, 512], BF16) for i in range(2)]
    sa2 = [k.sb("msa2%d" % i, [128, 512], BF16) for i in range(2)]
    hid = [k.sb("mhid%d" % i, [128, 4, 512], BF16) for i in range(2)]
    cb = [k.sb("mcb%d" % i, [128, 512], BF16) for i in range(2)]
    xres = [k.sb("mxres%d" % i, [128, 512], F32) for i in range(2)]
    pa = [k.ps("mpa%d" % i, [128, 512], F32) for i in range(2)]
    pb = [k.ps("mpb%d" % i, [128, 512], F32) for i in range(2)]
    po = [k.ps("mpo%d" % i, [128, 512], F32) for i in range(3)]
    pcb = k.ps("mpcb", [128, 512], F32)
    hTv = self.h2T.ap.rearrange("(c p) t -> p c t", p=128)
    xTv = self.xT.ap.rearrange("(c p) t -> p c t", p=128)
    cnt = {"ab": 0, "o": 0, "h": 0, "w": 0, "s": 0}

    def blocks(t0, n):
        bl = []
        o = 0
        if t0 < CTX:
            bl.append((0, CTX, 1)); o = CTX
        while o < n:
            s_ = min(512, n - o)
            bl.append((o, s_, 0)); o += s_
        return bl

    for (t0, n) in tiles:
        for c in range(8):
            k.op("sp", lambda e, c=c, t0=t0, n=n: e.dma_start(out=h2[:, c, :n], in_=hTv[:, c, t0:t0 + n]), writes=[h2], dma=True)
        k.op("pool", lambda e, t0=t0, n=n: e.dma_start(out=cTb[:, :n], in_=self.combT.ap[:, t0:t0 + n]), writes=[cTb], dma=True)
        bl = blocks(t0, n)
        for ex in range(16):
            wb = cnt["w"] % 2; cnt["w"] += 1
            W1, W3, W2 = w1[wb], w3[wb], w2[wb]
            for c in range(8):
                k.op("pool", lambda e, c=c, ex=ex, W1=W1: e.dma_start(out=W1[:, c, :], in_=self.moe_w1.ap[l, ex, c * 128:(c + 1) * 128, :]), writes=[W1], dma=True)
                k.op("pool", lambda e, c=c, ex=ex, W3=W3: e.dma_start(out=W3[:, c, :], in_=self.moe_w3.ap[l, ex, c * 128:(c + 1) * 128, :]), writes=[W3], dma=True)
            for c in range(4):
                k.op("pool", lambda e, c=c, ex=ex, W2=W2: e.dma_start(out=W2[:, c, :], in_=self.moe_w2.ap[l, ex, c * 128:(c + 1) * 128, :]), writes=[W2], dma=True)
            for (o, s_, stream) in bl:
                cbb = cb[cnt["s"] % 2]; cnt["s"] += 1
                k.op("pe", lambda e, ex=ex, o=o, s_=s_: e.matmul(pcb[:, :s_], lhsT=self_sel[:, ex, :], rhs=cTb[:, o:o + s_], start=True, stop=True), reads=[self_sel, cTb], writes=[pcb])
                k.op("act", lambda e, cbb=cbb, s_=s_: e.copy(cbb[:, :s_], pcb[:, :s_]), reads=[pcb], writes=[cbb])
                hd = hid[cnt["h"] % 2]; cnt["h"] += 1
                for fc in range(4):
                    i = cnt["ab"] % 2; cnt["ab"] += 1
                    pa_, pb_, sa_, sa2_ = pa[i], pb[i], sa[i], sa2[i]
                    for c in range(8):
                        k.op("pe", lambda e, c=c, fc=fc, pa_=pa_, W1=W1, o=o, s_=s_: e.matmul(pa_[:, :s_], lhsT=W1[:, c, fc * 128:(fc + 1) * 128], rhs=h2[:, c, o:o + s_], start=(c == 0), stop=(c == 7)), reads=[W1, h2], writes=[pa_], pe_acc=True)
                    for c in range(8):
                        k.op("pe", lambda e, c=c, fc=fc, pb_=pb_, W3=W3, o=o, s_=s_: e.matmul(pb_[:, :s_], lhsT=W3[:, c, fc * 128:(fc + 1) * 128], rhs=h2[:, c, o:o + s_], start=(c == 0), stop=(c == 7)), reads=[W3, h2], writes=[pb_], pe_acc=True)
                    k.op("act", lambda e, pa_=pa_, sa_=sa_, s_=s_: e.activation(out=sa_[:, :s_], in_=pa_[:, :s_], func=AF.Silu), reads=[pa_], writes=[sa_])
                    k.op("pool", lambda e, sa_=sa_, sa2_=sa2_, cbb=cbb, s_=s_: e.tensor_tensor(sa2_[:, :s_], sa_[:, :s_], cbb[:, :s_], ALU.mult), reads=[sa_, cbb], writes=[sa2_])
                    k.op("dve", lambda e, sa2_=sa2_, pb_=pb_, hd=hd, fc=fc, s_=s_: e.tensor_tensor(hd[:, fc, :s_], sa2_[:, :s_], pb_[:, :s_], ALU.mult), reads=[sa2_, pb_], writes=[hd])
                for j in range(8):
                    po_ = po[cnt["o"] % 3]; cnt["o"] += 1
                    for fc in range(4):
                        k.op("pe", lambda e, fc=fc, j=j, po_=po_, W2=W2, hd=hd, s_=s_: e.matmul(po_[:, :s_], lhsT=W2[:, fc, j * 128:(j + 1) * 128], rhs=hd[:, fc, :s_], start=(fc == 0), stop=(fc == 3)), reads=[W2, hd], writes=[po_], pe_acc=True)
                    if ex == 0:
                        k.op("dve", lambda e, j=j, po_=po_, o=o, s_=s_: e.tensor_copy(acc[:, j, o:o + s_], po_[:, :s_]), reads=[po_], writes=[acc])
                    else:
                        k.op("dve", lambda e, j=j, po_=po_, o=o, s_=s_: e.tensor_tensor(acc[:, j, o:o + s_], acc[:, j, o:o + s_], po_[:, :s_], ALU.add), reads=[po_, acc], writes=[acc])
        ri = 0
        for (o, s_, stream) in bl:
            for j in range(8):
                xr = xres[ri % 2]; ri += 1
                k.op("sp", lambda e, xr=xr, o=o, s_=s_, t0=t0, j=j: e.dma_start(out=xr[:, :s_], in_=xTv[:, j, t0 + o:t0 + o + s_]), writes=[xr], dma=True)
                k.op("dve", lambda e, j=j, xr=xr, o=o, s_=s_, stream=stream: e.scalar_tensor_tensor(out=xr[:, :s_], in0=acc[:, j, o:o + s_], scalar=self.modT[:, l, 40 + j, stream:stream + 1], in1=xr[:, :s_], op0=ALU.mult, op1=ALU.add), reads=[acc, self.modT, xr], writes=[xr])
                k.op("pool", lambda e, xr=xr, o=o, s_=s_, t0=t0, j=j: e.dma_start(out=xTv[:, j, t0 + o:t0 + o + s_], in_=xr[:, :s_]), reads=[xr], dma=True)
    k.barrier()
    k.emit()
    k.free_to(m)


def phase_final(self):
    k = self.k
    m = k.mark()
    fg = self._pvec("fg", self.final_g.ap, 8)
    xt = [k.sb("fxt%d" % i, [128, 8, 512], F32) for i in range(2)]
    sq = k.sb("fsq", [128, 8, 512], BF16)
    rstd = k.sb("frstd", [128, 512], F32)
    pss = k.ps("fpss", [128, 512], F32)
    pt = [k.ps("fpt%d" % i, [128, 4, 128], F32) for i in range(2)]
    ot = [k.sb("fot%d" % i, [128, 8, 128], F32) for i in range(2)]
    xTv = self.xT.ap.rearrange("(c p) t -> p c t", p=128)
    cnt = [0]
    for ti, (tok0, n, stream) in enumerate(self.tok_tiles(lat_only=True)):
        x_ = xt[ti % 2]
        k.op("sp", lambda e, x_=x_, tok0=tok0, n=n: e.dma_start(out=x_[:, :, :n], in_=xTv[:, :, tok0:tok0 + n]), writes=[x_], dma=True)
        k.op("act", lambda e, x_=x_, n=n: e.activation(out=sq[:, :, :n], in_=x_[:, :, :n], func=AF.Square), reads=[x_], writes=[sq])
        for c in range(8):
            k.op("pe", lambda e, c=c, n=n: e.matmul(pss[:, :n], lhsT=self.ones[:, :], rhs=sq[:, c, :n], start=(c == 0), stop=(c == 7)), reads=[sq, self.ones], writes=[pss], pe_acc=True)
        k.op("act", lambda e, n=n: e.activation(out=rstd[:, :n], in_=pss[:, :n], func=AF.Sqrt, scale=1.0 / D, bias=EPS), reads=[pss], writes=[rstd])
        k.op("dve", lambda e, n=n: e.reciprocal(rstd[:, :n], rstd[:, :n]), reads=[rstd], writes=[rstd])
        k.op("dve", lambda e, x_=x_, n=n: e.tensor_tensor(x_[:, :, :n], x_[:, :, :n], rstd[:, :n].unsqueeze(1).to_broadcast([128, 8, n]), ALU.mult), reads=[x_, rstd], writes=[x_])
        k.op("pool", lambda e, x_=x_, n=n: e.tensor_tensor(x_[:, :, :n], x_[:, :, :n], fg[:, :].unsqueeze(2).to_broadcast([128, 8, n]), ALU.mult), reads=[x_, fg], writes=[x_])
        for tb in range(n // 128):
            o_ = ot[cnt[0] % 2]; cnt[0] += 1
            for hh in range(2):
                for c in range(4):
                    cc = hh * 4 + c
                    k.op("pe", lambda e, hh=hh, c=c, cc=cc, x_=x_, tb=tb: e.transpose(pt[hh][:, c, :], x_[:, cc, tb * 128:(tb + 1) * 128], self.identf[:, :]), reads=[x_, self.identf], writes=[pt[hh]], pe_acc=True)
                if hh == 0:
                    k.op("dve", lambda e, o_=o_, hh=hh: e.tensor_copy(o_[:, 0:4, :], pt[0][:, :, :]), reads=[pt[0]], writes=[o_])
                else:
                    k.op("act", lambda e, o_=o_, hh=hh: e.copy(o_[:, 4:8, :], pt[1][:, :, :]), reads=[pt[1]], writes=[o_])
            tt = tok0 - CTX + tb * 128
            k.op("pool", lambda e, o_=o_, tt=tt: e.dma_start(out=self.out.ap[tt:tt + 128, :], in_=o_[:, :, :].rearrange("p c f -> p (c f)")), reads=[o_], dma=True)
    k.barrier()
    k.emit()
    k.free_to(m)


MK.moe_decl = _moe_decl
MK.phase_outproj = phase_outproj
MK.phase_moe = phase_moe
MK.phase_final = phase_final


def build_all(T, dbg=None, L=2):
    mk_ = MK(T, L=L, dbg=dbg)
    mk_.consts(); mk_.phase_mod(); mk_.phase_x_in()
    for l in range(L):
        last = (l == L - 1)
        mk_.phase_inproj(l)
        mk_.phase_rwkv(l, 0); mk_.phase_rwkv(l, 1)
        mk_.phase_ssm_conv(l); mk_.phase_ssm_scan(l, 0); mk_.phase_ssm_scan(l, 1)
        mk_.phase_gla(l, 0); mk_.phase_gla(l, 1)
        mk_.phase_outproj(l, lat_only=last)
        mk_.phase_moe(l, lat_only=last)
    mk_.phase_final()
    mk_.finish(None)
    return mk_


def _host_consts():
    c = {}
    c["c_ident"] = np.eye(128, dtype=np.float32)
    p = np.arange(128)[:, None] % 64
    f = np.arange(512)[None, :] % 64
    c["c_masks"] = np.stack([(p < f), (p <= f), (p > f), (p >= f)]).astype(np.float32)
    c["c_id8"] = (p == f).astype(np.float32)
    pp = np.arange(128)
    c["c_blk"] = ((pp[:, None] // 64) == (pp[None, :] // 64)).astype(np.float32)
    j = np.arange(128)[:, None]; f128 = np.arange(128)[None, :]
    c["c_m128"] = np.stack([(j <= f128), (j >= f128), (j > f128), (j < f128), -30000.0 * (j == f128)]).astype(np.float32)
    sel = np.zeros((16, 16, 128), np.float32)
    for e_ in range(16):
        sel[e_, e_, :] = 1.0
    c["c_sel"] = sel
    c["c_reset"] = np.broadcast_to((np.arange(512) % 64 != 0).astype(np.float32)[None, :], (128, 512)).copy()
    return c


_CACHE = {}


def kernel(**inputs):
    from concourse.bass_utils import run_bass_kernel_spmd
    x = np.asarray(inputs["x"])
    B, T, _ = x.shape
    n_cores = 8
    assert B == n_cores
    mk_ = build_all(T)
    consts = _host_consts()
    in_maps = []
    for b in range(n_cores):
        m = {}
        for k_, v in inputs.items():
            v = np.asarray(v, dtype=np.float32)
            if k_ in ("x", "ctx", "c"):
                m[k_] = np.ascontiguousarray(v[b])
            else:
                m[k_] = np.ascontiguousarray(v)
        m.update(consts)
        in_maps.append(m)
    res = run_bass_kernel_spmd(mk_.nc, in_maps, core_ids=list(range(n_cores)))
    out = np.stack([np.asarray(r["out"], dtype=np.float32) for r in res.results], 0)
    return out
```

```python
import os
import numpy as np
import concourse.bass as bass
import concourse.mybir as mybir

F32 = mybir.dt.float32
BF16 = mybir.dt.bfloat16
ALU = mybir.AluOpType
AF = mybir.ActivationFunctionType
AX = mybir.AxisListType

SEM_CHUNK = int(os.environ.get("SEM_CHUNK", 8000))
N_DMA_SEMS = 12


class T:
    __slots__ = ("ap", "w", "r", "name", "psum")

    def __init__(self, ap, name="", psum=False):
        self.psum = psum
        self.ap = ap
        self.w = None
        self.r = []
        self.name = name

    def __getitem__(self, idx):
        return self.ap[idx]


class KB:
    ENGS = ("pe", "act", "dve", "pool", "sp")

    def __init__(self, nc, same_engine_sync=(os.environ.get("SES", "1") == "1")):
        self.nc = nc
        self.ops = {e: [] for e in self.ENGS}
        self.cnt = {e: 0 for e in self.ENGS}
        self.sem_names = []
        self.waited = {e: {} for e in self.ENGS}
        self.dma_rr = {e: 0 for e in self.ENGS}
        self.dma_val = {}
        self.same_engine_sync = same_engine_sync
        self.ctx = []
        self.final_waits = []
        self.sems = {}
        self.sem_guards = []
        self.last_tok = {}

    def sb(self, name, shape, dt):
        self.uid = getattr(self, "uid", 0) + 1
        name = "%s_%d" % (name, self.uid)
        g = self.nc.sbuf_tensor(name, list(shape), dt)
        t = g.__enter__()
        self.ctx.append(g)
        return T(t, name)

    def ps(self, name, shape, dt):
        self.uid = getattr(self, "uid", 0) + 1
        name = "%s_%d" % (name, self.uid)
        esz = 4 if dt == F32 else 2
        full = 2048 // esz
        g = self.nc.psum_tensor(name, [128, full], dt)
        t = g.__enter__()
        self.ctx.append(g)
        shape = list(shape)
        p = shape[0]
        if len(shape) == 2:
            assert shape[1] <= full
            ap = t[0:p, 0:shape[1]]
        else:
            assert len(shape) == 3 and shape[1] * shape[2] <= full
            ap = t[0:p, 0:shape[1] * shape[2]].rearrange("p (a b) -> p a b", b=shape[2])
        return T(ap, name, psum=True)

    def dram(self, name, shape, dt, kind="Internal"):
        t = self.nc.dram_tensor(name, list(shape), dt, kind=kind)
        return T(t.ap(), name)

    def _sem_key(self, key):
        if key not in self.sem_names:
            self.sem_names.append(key)
        return key

    def op(self, eng, fn, reads=(), writes=(), dma=False, pe_acc=False):
        waits = {}
        if any(t.psum for t in reads):
            writes = list(writes) + [t for t in reads if t.psum and t not in writes]
            reads = [t for t in reads if not t.psum]

        def need(dep):
            if dep is None:
                return
            k, v, e = dep
            if e == eng and not dma and not self.same_engine_sync and not k[0] == "dma":
                return
            if e == eng and eng == "pe" and k[0] != "dma":
                return
            if waits.get(k, 0) < v:
                waits[k] = v

        for t in reads:
            need(t.w)
        for t in writes:
            if not (pe_acc and t.w is not None and t.w[2] == "pe" and eng == "pe"):
                need(t.w)
            for d in t.r:
                need(d)
        if dma:
            i = self.dma_rr[eng]
            self.dma_rr[eng] = (i + 1) % N_DMA_SEMS
            key = self._sem_key(("dma", eng, i))
            prev = self.dma_val.get(key, 0)
            if prev:
                if waits.get(key, 0) < prev:
                    waits[key] = prev
            val = prev + 16
            self.dma_val[key] = val
            inc = 16
        else:
            c = self.cnt[eng]
            self.cnt[eng] = c + 1
            key = self._sem_key(("eng", eng, c // SEM_CHUNK))
            val = (c % SEM_CHUNK) + 1
            inc = 1
        wl = []
        wd = self.waited[eng]
        for k, v in waits.items():
            if wd.get(k, 0) >= v:
                continue
            wd[k] = v
            wl.append((k, v))
        self.ops[eng].append((fn, wl, key, inc))
        tok = (key, val, eng)
        self.last_tok[key] = val
        for t in reads:
            t.r.append(tok)
        for t in writes:
            t.w = tok
            t.r = []
        return tok

    def mark(self):
        return len(self.ctx)

    def free_to(self, mark):
        while len(self.ctx) > mark:
            g = self.ctx.pop()
            g.__exit__(None, None, None)

    def barrier(self):
        for eng in self.ENGS:
            wl = []
            wd = self.waited[eng]
            for k, v in self.last_tok.items():
                if k[0] == "eng" and k[1] == eng:
                    continue
                if wd.get(k, 0) >= v:
                    continue
                wd[k] = v
                wl.append((k, v))
            if wl:
                self.ops[eng].append((None, wl, None, 0))

    def finish_wait(self, eng, toks):
        self.final_waits.append((eng, toks))

    def emit(self):
        nc = self.nc
        for key in self.sem_names:
            if key not in self.sems:
                g = nc.semaphore("s%d" % len(self.sems))
                self.sems[key] = g.__enter__()
                self.sem_guards.append(g)
        sems = self.sems
        ops = self.ops
        final_waits = self.final_waits

        def run(engname, h):
            for fn, wl, key, inc in ops[engname]:
                for k, v in wl:
                    h.wait_ge(sems[k], v)
                if fn is not None:
                    ins = fn(h)
                    ins.then_inc(sems[key], inc)
            for e, toks in final_waits:
                if e == engname:
                    for (k, v, _) in toks:
                        h.wait_ge(sems[k], v)

        with nc.Block() as block:
            @block.tensor
            def _(h):
                run("pe", h)

            @block.scalar
            def _(h):
                run("act", h)

            @block.vector
            def _(h):
                run("dve", h)

            @block.gpsimd
            def _(h):
                run("pool", h)

            @block.sync
            def _(h):
                run("sp", h)
        self.ops = {e: [] for e in self.ENGS}
        self.final_waits = []

    def close(self):
        self.free_to(0)
        for g in reversed(self.sem_guards):
            g.__exit__(None, None, None)


D = 1024
CTX = 256
INC = 3096
EPS = 1e-6
NEG_E05 = -0.6065306597126334


class MK:
    def __init__(self, T, L=2, dbg=None):
        self.T = T
        self.L = L
        self.NT = CTX + T
        self.dbg = dbg or set()
        nc = bass.Bass("TRN2", target_bir_lowering=False)
        self.nc = nc
        self.k = KB(nc)
        self.decl()

    def decl(self):
        k = self.k
        T, L, NT = self.T, self.L, self.NT
        I = lambda n, s: k.dram(n, s, F32, kind="ExternalInput")
        self.x = I("x", [T, D]); self.c = I("c", [D]); self.ctx = I("ctx", [CTX, D]); self.c_ctx = I("c_ctx", [D])
        self.ada_w = I("ada_w", [L, D, 6 * D]); self.ada_b = I("ada_b", [L, 6 * D])
        self.norm1_g = I("norm1_g", [L, D]); self.norm2_g = I("norm2_g", [L, D])
        self.w_in = I("w_in", [L, D, INC]); self.w_out = I("w_out", [L, D, D])
        self.rw_mu_prev = I("rw_mu_prev", [L, 1024]); self.rw_mu_next = I("rw_mu_next", [L, 1024])
        self.rw_w0 = I("rw_w0", [L, 2, 256]); self.rw_w2 = I("rw_w2", [L, 2, 64, 256])
        self.rw_a0 = I("rw_a0", [L, 2, 256]); self.rw_a2 = I("rw_a2", [L, 2, 64, 256])
        self.rw_g2 = I("rw_g2", [L, 128, 256])
        self.rw_k_k = I("rw_k_k", [L, 256]); self.rw_k_a = I("rw_k_a", [L, 256]); self.rw_r_k = I("rw_r_k", [L, 256])
        self.rw_gn_g = I("rw_gn_g", [L, 256]); self.rw_gn_b = I("rw_gn_b", [L, 256])
        self.ssm_conv_w = I("ssm_conv_w", [L, 3, 3, 768]); self.ssm_conv_b = I("ssm_conv_b", [L, 768])
        self.ssm_dt_bias = I("ssm_dt_bias", [L, 2, 8]); self.ssm_a_log = I("ssm_a_log", [L, 2, 8])
        self.ssm_d = I("ssm_d", [L, 8]); self.ssm_norm_g = I("ssm_norm_g", [L, 512])
        self.gla_ga2 = I("gla_ga2", [L, 2, 16, 128]); self.gla_gb = I("gla_gb", [L, 2, 128]); self.gla_norm_g = I("gla_norm_g", [L, 256])
        self.moe_rg_w = I("moe_rg_w", [L, D, 4]); self.moe_rg_b = I("moe_rg_b", [L, 4])
        self.moe_re_w = I("moe_re_w", [L, D, 16]); self.moe_re_b = I("moe_re_b", [L, 16])
        self.moe_w1 = I("moe_w1", [L, 16, D, 512]); self.moe_w3 = I("moe_w3", [L, 16, D, 512]); self.moe_w2 = I("moe_w2", [L, 16, 512, D])
        self.final_g = I("final_g", [D])
        self.c_ident = I("c_ident", [128, 128])
        self.out = k.dram("out", [T, D], F32, kind="ExternalOutput")
        def S(n, s, dt=F32):
            kind = "ExternalOutput" if n in self.dbg else "Internal"
            return k.dram(n, s, dt, kind=kind)
        self.xT = S("xT", [D, NT])
        self.uT = S("uT", [3200, NT])
        self.mixT = S("mixT", [D, NT])
        self.rw_consts()
        self.ssm_decl()
        self.moe_decl()

    def consts(self):
        k = self.k
        self.identf = k.sb("identf", [128, 128], F32)
        self.ident = k.sb("ident", [128, 128], BF16)
        self.ones = k.sb("ones", [128, 128], BF16)
        k.op("sp", lambda e: e.dma_start(out=self.identf[:, :], in_=self.c_ident.ap[:, :]), writes=[self.identf], dma=True)
        k.op("dve", lambda e: e.tensor_copy(self.ident[:, :], self.identf[:, :]), reads=[self.identf], writes=[self.ident])
        k.op("dve", lambda e: e.memset(self.ones[:, :], 1.0), writes=[self.ones])
        self.modT = k.sb("modT", [128, self.L, 48, 2], F32)
        self.gm = k.sb("gm", [128, self.L, 2, 8, 2], F32)
        self.load_consts2()

    def phase_mod(self):
        k = self.k
        L = self.L
        m = k.mark()
        cf = k.sb("cf", [128, 8, 2], F32)
        cb = k.sb("cb", [128, 8, 2], BF16)
        adab = k.sb("adab", [128, 48], F32)
        ng = k.sb("ng", [128, 2, 8], F32)
        aw = k.sb("aw", [128, 8, 3072], BF16)
        pm = k.ps("pm", [128, 48, 2], F32)
        k.op("sp", lambda e: e.dma_start(out=cf[:, :, 0], in_=self.c.ap.rearrange("(c p) -> p c", p=128), allow_slow_non_contiguous=True), writes=[cf], dma=True)
        k.op("sp", lambda e: e.dma_start(out=cf[:, :, 1], in_=self.c_ctx.ap.rearrange("(c p) -> p c", p=128), allow_slow_non_contiguous=True), writes=[cf], dma=True)
        k.op("act", lambda e: e.activation(out=cb[:, :, :], in_=cf[:, :, :], func=AF.Silu), reads=[cf], writes=[cb])
        for l in range(L):
            k.op("sp", lambda e, l=l: e.dma_start(out=adab[:, :], in_=self.ada_b.ap[l].rearrange("(c p) -> p c", p=128), allow_slow_non_contiguous=True), writes=[adab], dma=True)
            k.op("sp", lambda e, l=l: e.dma_start(out=ng[:, 0, :], in_=self.norm1_g.ap[l].rearrange("(c p) -> p c", p=128), allow_slow_non_contiguous=True), writes=[ng], dma=True)
            k.op("sp", lambda e, l=l: e.dma_start(out=ng[:, 1, :], in_=self.norm2_g.ap[l].rearrange("(c p) -> p c", p=128), allow_slow_non_contiguous=True), writes=[ng], dma=True)
            for half in range(2):
                for c in range(8):
                    k.op("pool", lambda e, l=l, c=c, half=half: e.dma_start(out=aw[:, c, :], in_=self.ada_w.ap[l, c * 128:(c + 1) * 128, half * 3072:(half + 1) * 3072]), writes=[aw], dma=True)
                for j in range(24):
                    for c in range(8):
                        k.op("pe", lambda e, c=c, j=j, half=half: e.matmul(pm[:, half * 24 + j, :], lhsT=aw[:, c, j * 128:(j + 1) * 128], rhs=cb[:, c, :], start=(c == 0), stop=(c == 7)), reads=[aw, cb], writes=[pm], pe_acc=True)
            k.op("dve", lambda e, l=l: e.tensor_tensor(self.modT[:, l, :, :], pm[:, :, :], adab[:, :].unsqueeze(2).to_broadcast([128, 48, 2]), ALU.add), reads=[pm, adab], writes=[self.modT])
            for w, j in ((0, 1), (1, 4)):
                k.op("dve", lambda e, l=l, w=w, j=j: e.scalar_tensor_tensor(out=self.gm[:, l, w, :, :], in0=self.modT[:, l, j * 8:(j + 1) * 8, :], scalar=1.0, in1=ng[:, w, :].unsqueeze(2).to_broadcast([128, 8, 2]), op0=ALU.add, op1=ALU.mult), reads=[self.modT, ng], writes=[self.gm])
        k.barrier()
        k.emit()
        k.free_to(m)

    def tok_tiles(self, lat_only=False, n=512):
        tiles = []
        if not lat_only:
            tiles.append((0, CTX, 1))
        for i in range(self.T // n):
            tiles.append((CTX + i * n, n, 0))
        return tiles

    def phase_x_in(self):
        k = self.k
        m = k.mark()
        xin = [k.sb("xin%d" % i, [128, D], F32) for i in range(2)]
        pt = [k.ps("ptx%d" % i, [128, 4, 128], F32) for i in range(2)]
        xo = [k.sb("xo%d" % i, [128, 8, 128], F32) for i in range(2)]
        nt = self.NT // 128
        for i in range(nt):
            src = self.ctx.ap[i * 128:(i + 1) * 128, :] if i < 2 else self.x.ap[(i - 2) * 128:(i - 1) * 128, :]
            b = i % 2
            k.op("sp", lambda e, b=b, src=src: e.dma_start(out=xin[b][:, :], in_=src), writes=[xin[b]], dma=True)
            for hh in range(2):
                for c in range(4):
                    cc = hh * 4 + c
                    k.op("pe", lambda e, b=b, hh=hh, c=c, cc=cc: e.transpose(pt[hh][:, c, :], xin[b][:, cc * 128:(cc + 1) * 128], self.identf[:, :]), reads=[xin[b], self.identf], writes=[pt[hh]], pe_acc=True)
                eng = "dve" if hh == 0 else "act"
                if eng == "dve":
                    k.op("dve", lambda e, b=b, hh=hh: e.tensor_copy(xo[b][:, hh * 4:(hh + 1) * 4, :], pt[hh][:, :, :]), reads=[pt[hh]], writes=[xo[b]])
                else:
                    k.op("act", lambda e, b=b, hh=hh: e.copy(xo[b][:, hh * 4:(hh + 1) * 4, :], pt[hh][:, :, :]), reads=[pt[hh]], writes=[xo[b]])
            k.op("pool", lambda e, b=b, i=i: e.dma_start(out=self.xT.ap.rearrange("(c p) t -> p c t", p=128)[:, :, i * 128:(i + 1) * 128], in_=xo[b][:, :, :]), reads=[xo[b]], dma=True)
        k.barrier()
        k.emit()
        k.free_to(m)

    def norm_tile(self, l, which, tok0, n, stream, xt, sq, pss, rstd, hT, keep_f32=False):
        k = self.k
        k.op("act", lambda e: e.activation(out=sq[:, :, :n], in_=xt[:, :, :n], func=AF.Square), reads=[xt], writes=[sq])
        for c in range(8):
            k.op("pe", lambda e, c=c: e.matmul(pss[:, :n], lhsT=self.ones[:, :], rhs=sq[:, c, :n], start=(c == 0), stop=(c == 7)), reads=[sq, self.ones], writes=[pss], pe_acc=True)
        k.op("act", lambda e: e.activation(out=rstd[:, :n], in_=pss[:, :n], func=AF.Sqrt, scale=1.0 / D, bias=EPS), reads=[pss], writes=[rstd])
        k.op("dve", lambda e: e.reciprocal(rstd[:, :n], rstd[:, :n]), reads=[rstd], writes=[rstd])
        k.op("dve", lambda e: e.tensor_tensor(xt[:, :, :n], xt[:, :, :n], rstd[:, :n].unsqueeze(1).to_broadcast([128, 8, n]), ALU.mult), reads=[xt, rstd], writes=[xt])
        sh_j = 0 if which == 0 else 3
        k.op("pool", lambda e: e.tensor_tensor(xt[:, :, :n], xt[:, :, :n], self.gm[:, l, which, :, stream].unsqueeze(2).to_broadcast([128, 8, n]), ALU.mult), reads=[xt, self.gm], writes=[xt])
        if keep_f32:
            k.op("dve", lambda e: e.tensor_tensor(xt[:, :, :n], xt[:, :, :n], self.modT[:, l, sh_j * 8:(sh_j + 1) * 8, stream].unsqueeze(2).to_broadcast([128, 8, n]), ALU.add), reads=[xt, self.modT], writes=[xt])
            k.op("act", lambda e: e.copy(hT[:, :, :n], xt[:, :, :n]), reads=[xt], writes=[hT])
        else:
            k.op("dve", lambda e: e.tensor_tensor(hT[:, :, :n], xt[:, :, :n], self.modT[:, l, sh_j * 8:(sh_j + 1) * 8, stream].unsqueeze(2).to_broadcast([128, 8, n]), ALU.add), reads=[xt, self.modT], writes=[hT])

    def phase_inproj(self, l):
        k = self.k
        m = k.mark()
        win = k.sb("win", [128, 8, INC], BF16)
        for c in range(8):
            k.op("pool", lambda e, c=c: e.dma_start(out=win[:, c, :], in_=self.w_in.ap[l, c * 128:(c + 1) * 128, :]), writes=[win], dma=True)
        xt = [k.sb("xt%d" % i, [128, 8, 512], F32) for i in range(2)]
        sq = k.sb("sq", [128, 8, 512], BF16)
        rstd = k.sb("rstd", [128, 512], F32)
        hT = [k.sb("hT%d" % i, [128, 8, 512], BF16) for i in range(2)]
        pss = k.ps("pss", [128, 512], F32)
        pu = [k.ps("pu%d" % i, [128, 512], F32) for i in range(4)]
        us = [k.sb("us%d" % i, [128, 512], F32) for i in range(4)]
        xTv = self.xT.ap.rearrange("(c p) t -> p c t", p=128)
        nchunk = (INC + 127) // 128
        cnt = 0
        for ti, (tok0, n, stream) in enumerate(self.tok_tiles()):
            b = ti % 2
            k.op("sp", lambda e, b=b, tok0=tok0, n=n: e.dma_start(out=xt[b][:, :, :n], in_=xTv[:, :, tok0:tok0 + n]), writes=[xt[b]], dma=True)
            self.norm_tile(l, 0, tok0, n, stream, xt[b], sq, pss, rstd, hT[b])
            for j in range(nchunk):
                c0 = j * 128
                nc_ = min(128, INC - c0)
                pb = cnt % 4
                cnt += 1
                for c in range(8):
                    k.op("pe", lambda e, c=c, c0=c0, nc_=nc_, pb=pb, b=b, n=n: e.matmul(pu[pb][:nc_, :n], lhsT=win[:, c, c0:c0 + nc_], rhs=hT[b][:, c, :n], start=(c == 0), stop=(c == 7)), reads=[win, hT[b]], writes=[pu[pb]], pe_acc=True)
                if j % 2 == 0:
                    k.op("dve", lambda e, pb=pb, nc_=nc_, n=n: e.tensor_copy(us[pb][:nc_, :n], pu[pb][:nc_, :n]), reads=[pu[pb]], writes=[us[pb]])
                else:
                    k.op("act", lambda e, pb=pb, nc_=nc_, n=n: e.copy(us[pb][:nc_, :n], pu[pb][:nc_, :n]), reads=[pu[pb]], writes=[us[pb]])
                k.op("pool", lambda e, pb=pb, nc_=nc_, n=n, c0=c0, tok0=tok0: e.dma_start(out=self.uT.ap[c0:c0 + nc_, tok0:tok0 + n], in_=us[pb][:nc_, :n]), reads=[us[pb]], dma=True)
        k.barrier()
        k.emit()
        k.free_to(m)

    def finish(self, last_tensor):
        k = self.k
        k.barrier()
        k.emit()
        k.close()


def _rw_consts(self):
    k = self.k
    I = lambda n, s: k.dram(n, s, F32, kind="ExternalInput")
    self.c_masks = I("c_masks", [4, 128, 512])
    self.c_id8 = I("c_id8", [128, 512])
    self.c_blk = I("c_blk", [128, 128])
    self.c_reset = I("c_reset", [128, 512])
    def S(n, s, dt=F32):
        kind = "ExternalOutput" if n in self.dbg else "Internal"
        return k.dram(n, s, dt, kind=kind)
    self.rw_yf = S("rw_yf", [256, self.NT])
    self.rw_bonus = S("rw_bonus", [256, self.NT])
    self.rw_gate = S("rw_gate", [256, self.NT])


def _load_consts2(self):
    k = self.k
    self.masks = k.sb("masks", [128, 4, 512], BF16)
    self.id8 = k.sb("id8", [128, 512], BF16)
    self.blk = k.sb("blk", [128, 128], BF16)
    self.reset = k.sb("reset", [128, 512], F32)
    for i in range(4):
        k.op("pool", lambda e, i=i: e.dma_start(out=self.masks[:, i, :], in_=self.c_masks.ap[i]), writes=[self.masks], dma=True)
    k.op("pool", lambda e: e.dma_start(out=self.id8[:, :], in_=self.c_id8.ap[:, :]), writes=[self.id8], dma=True)
    k.op("pool", lambda e: e.dma_start(out=self.blk[:, :], in_=self.c_blk.ap[:, :]), writes=[self.blk], dma=True)
    k.op("sp", lambda e: e.dma_start(out=self.reset[:, :], in_=self.c_reset.ap[:, :]), writes=[self.reset], dma=True)


def _pvec(self, name, src_ap, ncols, eng="sp", dt=F32):
    k = self.k
    t = k.sb(name, [128, ncols], dt)
    k.op(eng, lambda e: e.dma_start(out=t[:, :], in_=src_ap.rearrange("(c p) -> p c", p=128), allow_slow_non_contiguous=True), writes=[t], dma=True)
    return t


def interleave(gens, weights=None):
    gens = [(g, (weights[i] if weights else 1)) for i, g in enumerate(gens)]
    while gens:
        for item in list(gens):
            g, w = item
            try:
                for _ in range(w):
                    next(g)
            except StopIteration:
                gens.remove(item)


def phase_rwkv(self, l, d):
    k = self.k
    m = k.mark()
    NT = self.NT
    s = NEG_E05
    mp = self._pvec("mp", self.rw_mu_prev.ap[l], 8)
    mn = self._pvec("mn", self.rw_mu_next.ap[l], 8)
    cmix = k.sb("cmix", [128, 8], F32)
    k.op("dve", lambda e: e.tensor_tensor(cmix[:, :], mp[:, :], mn[:, :], ALU.add), reads=[mp, mn], writes=[cmix])
    k.op("dve", lambda e: e.tensor_scalar(cmix[:, :], cmix[:, :], -1.0, 1.0, ALU.mult, ALU.add), reads=[cmix], writes=[cmix])
    w0 = self._pvec("w0", self.rw_w0.ap[l, d], 2)
    a0 = self._pvec("a0", self.rw_a0.ap[l, d], 2)
    k_k = self._pvec("k_k", self.rw_k_k.ap[l], 2)
    k_a = self._pvec("k_a", self.rw_k_a.ap[l], 2)
    r_k = self._pvec("r_k", self.rw_r_k.ap[l], 2)
    gn_g = self._pvec("gn_g", self.rw_gn_g.ap[l], 2)
    gn_b = self._pvec("gn_b", self.rw_gn_b.ap[l], 2)
    w2s = k.sb("w2s", [128, 256], BF16)
    a2s = k.sb("a2s", [128, 256], BF16)
    g2s = k.sb("g2s", [128, 256], BF16)
    k.op("pool", lambda e: e.dma_start(out=w2s[0:64, :], in_=self.rw_w2.ap[l, d]), writes=[w2s], dma=True)
    k.op("pool", lambda e: e.dma_start(out=a2s[64:128, :], in_=self.rw_a2.ap[l, d]), writes=[a2s], dma=True)
    k.op("pool", lambda e: e.dma_start(out=g2s[:, :], in_=self.rw_g2.ap[l]), writes=[g2s], dma=True)

    U = [k.sb("rU%d" % i, [128, 8, 514], F32) for i in range(2)]
    S = k.sb("rS", [128, 8, 512], F32)
    wlt = k.sb("wlt", [128, 512], BF16)
    alb = k.sb("alb", [128, 512], BF16)
    sgl = k.sb("sgl", [128, 512], BF16)
    f32t = lambda n_: k.sb(n_, [128, 8, 64], F32)
    bft = lambda n_: k.sb(n_, [128, 8, 64], BF16)
    sgw, asig, kx, rn, cs, dcs, tmp, E, Etrue = [f32t("r_" + z) for z in ("sgw", "asig", "kx", "rn", "cs", "dcs", "tmp", "E", "Etrue")]
    E2_, E3_ = f32t("r_E2"), f32t("r_E3")
    kkn, bvec, kmod = f32t("kkn"), f32t("bvec"), f32t("kmod")
    sqk = bft("sqk")
    AB = {z: [[bft("r_%s%d%d" % (z, b, hp)) for hp in range(2)] for b in range(2)] for z in ("rt", "kt", "bt", "at", "Rtrue", "Atrue", "Bh", "Kh", "vb")}
    WCs = [[k.sb("WC%d%d" % (b, hp), [128, 8], F32) for hp in range(2)] for b in range(2)]
    Atm, Bhtm, Khtm, Vtm = [[bft("r_%s%d" % (z, hp)) for hp in range(2)] for z in ("Atm", "Bhtm", "Khtm", "Vtm")]
    Zs, Ns, Zs2, Ns2, Aak, Arb, Ark, X, AVs, PaT, Us = [[bft("r_%s%d" % (z, hp)) for hp in range(2)] for z in ("Zs", "Ns", "Zs2", "Ns2", "Aak", "Arb", "Ark", "X", "AVs", "PaT", "Us")]
    Qs = [f32t("Qs%d" % hp) for hp in range(2)]
    Mst = [k.sb("Mst%d" % hp, [128, 64], F32) for hp in range(2)]
    Mbf = [k.sb("Mbf%d" % hp, [128, 64], BF16) for hp in range(2)]
    ysb = [k.sb("ysb%d" % hp, [128, 512], F32) for hp in range(2)]
    bon = k.sb("bon", [128, 512], F32)
    gat = k.sb("gat", [128, 512], F32)
    fbon = k.sb("fbon", [128, 512], F32)
    fgat = k.sb("fgat", [128, 512], F32)
    fkx, frn = f32t("r_fkx"), f32t("r_frn")
    fsq = bft("r_fsq")
    yf_in = [k.sb("yfin%d" % hp, [128, 512], F32) for hp in range(2)]
    PS = [k.ps("rps%d" % i, [128, 512], F32) for i in range(4)]
    PY = [k.ps("rpy%d" % j, [128, 512], F32) for j in range(2)]
    PSB = [k.ps("rpsb%d" % i, [128, 8, 64], BF16) for i in range(2)]
    psi = [0]
    psbi = [0]

    def nps():
        p = PS[psi[0] % 4]
        psi[0] += 1
        return p

    def npsb():
        p = PSB[psbi[0] % 2]
        psbi[0] += 1
        return p

    for hp in range(2):
        k.op("dve", lambda e, hp=hp: e.memset(Mst[hp][:, :], 0.0), writes=[Mst[hp]])
        k.op("dve", lambda e, hp=hp: e.memset(Mbf[hp][:, :], 0.0), writes=[Mbf[hp]])

    M_SL, M_LE, M_SG, M_GE = 0, 1, 2, 3
    if d == 0:
        mZ, mN, mI = M_SL, M_SG, M_LE
    else:
        mZ, mN, mI = M_SG, M_SL, M_GE

    tiles = self.tok_tiles()
    lat = tiles[1:]
    order = [tiles[0]] + (lat if d == 0 else lat[::-1])
    uTv = self.uT.ap[0:1024, :].rearrange("(c p) t -> p c t", p=128)

    def stageA(ti, tok0, n, stream):
        nch = n // 64
        b = ti % 2
        rt, kt, bt, at, Rtrue, Atrue, Bh, Kh, vb = [AB[z][b] for z in ("rt", "kt", "bt", "at", "Rtrue", "Atrue", "Bh", "Kh", "vb")]
        WC = WCs[b]
        seq0, seq1 = (0, CTX) if stream == 1 else (CTX, NT)
        ub = U[ti % 2]
        lo = max(tok0 - 1, seq0)
        hi = min(tok0 + n + 1, seq1)
        if lo > tok0 - 1:
            k.op("pool", lambda e: e.memset(ub[:, :, 0:1], 0.0), writes=[ub])
        if hi < tok0 + n + 1:
            k.op("pool", lambda e: e.memset(ub[:, :, n + 1:n + 2], 0.0), writes=[ub])
        k.op("sp", lambda e: e.dma_start(out=ub[:, :, lo - (tok0 - 1):hi - (tok0 - 1)], in_=uTv[:, :, lo:hi]), writes=[ub], dma=True)
        fl = lambda t_: t_[:, :, :].rearrange("p c t -> p (c t)")[:, 0:n]
        yield

        def shift(c):
            k.op("dve", lambda e: e.tensor_scalar(S[:, c, :n], ub[:, c, 1:n + 1], cmix[:, c:c + 1], None, ALU.mult), reads=[ub, cmix], writes=[S])
            k.op("dve", lambda e: e.scalar_tensor_tensor(out=S[:, c, :n], in0=ub[:, c, 0:n], scalar=mp[:, c:c + 1], in1=S[:, c, :n], op0=ALU.mult, op1=ALU.add), reads=[ub, mp, S], writes=[S])
            k.op("dve", lambda e: e.scalar_tensor_tensor(out=S[:, c, :n], in0=ub[:, c, 2:n + 2], scalar=mn[:, c:c + 1], in1=S[:, c, :n], op0=ALU.mult, op1=ALU.add), reads=[ub, mn, S], writes=[S])
        for c in range(8):
            if d == 1 and c == 7:
                continue
            shift(c)
            yield
        k.op("act", lambda e: e.activation(out=wlt[0:64, :n], in_=S[0:64, 6, :n], func=AF.Tanh), reads=[S], writes=[wlt])
        k.op("act", lambda e: e.copy(alb[64:128, :n], S[64:128, 6, :n]), reads=[S], writes=[alb])
        if d == 0:
            k.op("act", lambda e: e.activation(out=sgl[:, :n], in_=S[:, 7, :n], func=AF.Sigmoid), reads=[S], writes=[sgl])
        yield

        def prep(hp):
            p1 = nps()
            k.op("pe", lambda e: e.matmul(p1[:, :n], lhsT=w2s[0:64, hp * 128:(hp + 1) * 128], rhs=wlt[0:64, :n], start=True, stop=True), reads=[w2s, wlt], writes=[p1])
            k.op("act", lambda e: e.activation(out=fl(sgw), in_=p1[:, :n], func=AF.Sigmoid, bias=w0[:, hp:hp + 1]), reads=[p1, w0], writes=[sgw])
            p2 = nps()
            k.op("pe", lambda e: e.matmul(p2[:, :n], lhsT=a2s[64:128, hp * 128:(hp + 1) * 128], rhs=alb[64:128, :n], start=True, stop=True), reads=[a2s, alb], writes=[p2])
            k.op("act", lambda e: e.activation(out=fl(asig), in_=p2[:, :n], func=AF.Sigmoid, bias=a0[:, hp:hp + 1]), reads=[p2, a0], writes=[asig])
            yield
            k.op("dve", lambda e: e.tensor_scalar(fl(kx), S[:, 2 + hp, :n], k_k[:, hp:hp + 1], None, ALU.mult), reads=[S, k_k], writes=[kx])
            k.op("act", lambda e: e.activation(out=fl(sqk), in_=fl(kx), func=AF.Square), reads=[kx], writes=[sqk])
            p3 = nps()
            k.op("pe", lambda e: e.matmul(p3[:, :n], lhsT=self.blk[:, :], rhs=fl(sqk), start=True, stop=True), reads=[self.blk, sqk], writes=[p3])
            k.op("act", lambda e: e.activation(out=fl(rn), in_=p3[:, :n], func=AF.Ln, bias=1e-12), reads=[p3], writes=[rn])
            k.op("act", lambda e: e.activation(out=fl(rn), in_=fl(rn), func=AF.Exp, scale=-0.5), reads=[rn], writes=[rn])
            yield
            k.op("dve", lambda e: e.tensor_tensor(fl(kkn), fl(kx), fl(rn), ALU.mult), reads=[kx, rn], writes=[kkn])
            k.op("pool", lambda e: e.tensor_tensor(fl(bvec), fl(kkn), fl(asig), ALU.mult), reads=[kkn, asig], writes=[bvec])
            k.op("dve", lambda e: e.tensor_scalar(fl(tmp), fl(asig), -1.0, k_a[:, hp:hp + 1], ALU.add, ALU.mult), reads=[asig, k_a], writes=[tmp])
            k.op("dve", lambda e: e.scalar_tensor_tensor(out=fl(kmod), in0=fl(tmp), scalar=1.0, in1=S[:, 2 + hp, :n], op0=ALU.add, op1=ALU.mult), reads=[tmp, S], writes=[kmod])
            yield
            k.op("dve", lambda e: e.tensor_tensor_scan(fl(cs), self.reset[:, :n], fl(sgw), 0.0, ALU.mult, ALU.add), reads=[self.reset, sgw], writes=[cs])
            if d == 1:
                k.op("dve", lambda e: e.tensor_tensor(fl(tmp), fl(sgw), fl(cs), ALU.subtract), reads=[sgw, cs], writes=[tmp])
                k.op("dve", lambda e: e.tensor_tensor(cs[:, :nch, :], tmp[:, :nch, :], cs[:, :nch, 63:64].to_broadcast([128, nch, 64]), ALU.add), reads=[tmp, cs], writes=[cs])
            endi = 63 if d == 0 else 0
            k.op("pool", lambda e: e.tensor_tensor(dcs[:, :nch, :], cs[:, :nch, :], cs[:, :nch, 32:33].to_broadcast([128, nch, 64]), ALU.subtract), reads=[cs], writes=[dcs])
            yield
            k.op("act", lambda e: e.activation(out=fl(E), in_=fl(dcs), func=AF.Exp, scale=s), reads=[dcs], writes=[E])
            k.op("pool", lambda e: e.tensor_tensor(fl(tmp), fl(dcs), fl(sgw), ALU.subtract), reads=[dcs, sgw], writes=[tmp])
            k.op("act", lambda e: e.activation(out=fl(E2_), in_=fl(tmp), func=AF.Exp, scale=s), reads=[tmp], writes=[E2_])
            k.op("act", lambda e: e.activation(out=fl(E3_), in_=fl(dcs), func=AF.Exp, scale=-s), reads=[dcs], writes=[E3_])
            k.op("act", lambda e: e.activation(out=fl(Etrue), in_=fl(cs), func=AF.Exp, scale=s), reads=[cs], writes=[Etrue])
            yield
            k.op("dve", lambda e: e.tensor_tensor(fl(rt[hp]), S[:, hp, :n], fl(E), ALU.mult), reads=[S, E], writes=[rt[hp]])
            k.op("dve", lambda e: e.scalar_tensor_tensor(out=fl(at[hp]), in0=fl(kkn), scalar=-1.0, in1=fl(E2_), op0=ALU.mult, op1=ALU.mult), reads=[kkn, E2_], writes=[at[hp]])
            k.op("dve", lambda e: e.tensor_tensor(fl(kt[hp]), fl(kmod), fl(E3_), ALU.mult), reads=[kmod, E3_], writes=[kt[hp]])
            k.op("pool", lambda e: e.tensor_tensor(fl(bt[hp]), fl(bvec), fl(E3_), ALU.mult), reads=[bvec, E3_], writes=[bt[hp]])
            yield
            k.op("dve", lambda e: e.tensor_tensor(fl(Rtrue[hp]), S[:, hp, :n], fl(Etrue), ALU.mult), reads=[S, Etrue], writes=[Rtrue[hp]])
            k.op("dve", lambda e: e.tensor_copy(WC[hp][:, :nch], Etrue[:, :nch, endi]), reads=[Etrue], writes=[WC[hp]])
            k.op("pool", lambda e: e.tensor_tensor(fl(tmp), fl(cs), fl(sgw), ALU.subtract), reads=[cs, sgw], writes=[tmp])
            k.op("act", lambda e: e.activation(out=fl(E), in_=fl(tmp), func=AF.Exp, scale=s), reads=[tmp], writes=[E])
            k.op("dve", lambda e: e.scalar_tensor_tensor(out=fl(Atrue[hp]), in0=fl(kkn), scalar=-1.0, in1=fl(E), op0=ALU.mult, op1=ALU.mult), reads=[kkn, E], writes=[Atrue[hp]])
            yield
            k.op("pool", lambda e: e.tensor_tensor(tmp[:, :nch, :], cs[:, :nch, endi:endi + 1].to_broadcast([128, nch, 64]), cs[:, :nch, :], ALU.subtract), reads=[cs], writes=[tmp])
            k.op("act", lambda e: e.activation(out=fl(E2_), in_=fl(tmp), func=AF.Exp, scale=s), reads=[tmp], writes=[E2_])
            k.op("dve", lambda e: e.tensor_tensor(fl(Bh[hp]), fl(bvec), fl(E2_), ALU.mult), reads=[bvec, E2_], writes=[Bh[hp]])
            k.op("pool", lambda e: e.tensor_tensor(fl(Kh[hp]), fl(kmod), fl(E2_), ALU.mult), reads=[kmod, E2_], writes=[Kh[hp]])
            k.op("act", lambda e: e.copy(fl(vb[hp]), S[:, 4 + hp, :n]), reads=[S], writes=[vb[hp]])
            yield
            if d == 0:
                k.op("dve", lambda e: e.scalar_tensor_tensor(out=fl(sqk), in0=S[:, hp, :n], scalar=r_k[:, hp:hp + 1], in1=S[:, 2 + hp, :n], op0=ALU.mult, op1=ALU.mult), reads=[S, r_k], writes=[sqk])
                p4 = nps()
                k.op("pe", lambda e: e.matmul(p4[:, :n], lhsT=self.blk[:, :], rhs=fl(sqk), start=True, stop=True), reads=[self.blk, sqk], writes=[p4])
                k.op("dve", lambda e: e.tensor_tensor(bon[:, :n], p4[:, :n], S[:, 4 + hp, :n], ALU.mult), reads=[p4, S], writes=[bon])
                k.op("pool", lambda e: e.dma_start(out=self.rw_bonus.ap[hp * 128:(hp + 1) * 128, tok0:tok0 + n], in_=bon[:, :n]), reads=[bon], dma=True)
                p5 = nps()
                k.op("pe", lambda e: e.matmul(p5[:, :n], lhsT=g2s[:, hp * 128:(hp + 1) * 128], rhs=sgl[:, :n], start=True, stop=True), reads=[g2s, sgl], writes=[p5])
                k.op("act", lambda e: e.copy(gat[:, :n], p5[:, :n]), reads=[p5], writes=[gat])
                k.op("pool", lambda e: e.dma_start(out=self.rw_gate.ap[hp * 128:(hp + 1) * 128, tok0:tok0 + n], in_=gat[:, :n]), reads=[gat], dma=True)
                yield
        for hp in range(2):
            yield from prep(hp)

    def stageB(ti, tok0, n, stream):
        nch = n // 64
        b = ti % 2
        rt, kt, bt, at, Rtrue, Atrue, Bh, Kh, vb = [AB[z][b] for z in ("rt", "kt", "bt", "at", "Rtrue", "Atrue", "Bh", "Kh", "vb")]
        WC = WCs[b]

        def units(dst_ps, lt, rt_, P, reads):
            pv = dst_ps[:, :].rearrange("p (c t) -> p c t", t=64)
            for ch in range(nch):
                k.op("pe", lambda e, ch=ch: e.matmul(pv[P, ch, :], lhsT=lt[P, ch, :], rhs=rt_[P, ch, :], start=True, stop=True), reads=reads, writes=[dst_ps], pe_acc=True)
            return pv

        def head_block(hp, hl):
            P = slice(hl * 64, hl * 64 + 64)

            def tm(src, dst):
                pb = npsb()
                for ch in range(nch):
                    k.op("pe", lambda e, ch=ch: e.transpose(pb[P, ch, :], src[hp][P, ch, :], self.ident[P, P]), reads=[src[hp], self.ident], writes=[pb], pe_acc=True)
                k.op("act", lambda e: e.copy(dst[hp][P, :nch, :], pb[P, :nch, :]), reads=[pb], writes=[dst[hp]])
            for (a_, b_) in ((Atrue, Atm), (Bh, Bhtm), (Kh, Khtm), (vb, Vtm)):
                tm(a_, b_)
                yield

            def pair(dst, lt, rt_, mask, eng="dve"):
                pp = nps()
                pv = units(pp, lt[hp], rt_[hp], P, [lt[hp], rt_[hp]])
                mv = self.masks[:, mask, :].rearrange("p (c t) -> p c t", t=64)
                k.op(eng, lambda e: e.tensor_tensor(dst[hp][P, :nch, :], pv[P, :nch, :], mv[P, :nch, :], ALU.mult), reads=[pp, self.masks], writes=[dst[hp]])
            for args in ((Zs, bt, at, mZ), (Ns, at, bt, mN), (Aak, kt, at, mZ), (Arb, bt, rt, mI), (Ark, kt, rt, mI)):
                pair(*args)
                yield
            idv = self.id8[:, :].rearrange("p (c t) -> p c t", t=64)
            k.op("pool", lambda e: e.tensor_tensor(X[hp][P, :nch, :], Zs[hp][P, :nch, :], idv[P, :nch, :], ALU.add), reads=[Zs[hp], self.id8], writes=[X[hp]])
            Zc, Nc, Zn, Nn = Zs[hp], Ns[hp], Zs2[hp], Ns2[hp]
            for lev in range(1, 6):
                last = lev == 5

                def level(Zc, Nc, Zn, Nn, last):
                    if not last:
                        pz = nps()
                        pzv = units(pz, Nc, Zc, P, [Nc, Zc])
                    pn = nps()
                    pnv = units(pn, Zc, Nc, P, [Nc, Zc])
                    if not last:
                        k.op("act", lambda e: e.copy(Zn[P, :nch, :], pzv[P, :nch, :]), reads=[pz], writes=[Zn])
                    k.op("dve", lambda e: e.tensor_copy(Nn[P, :nch, :], pnv[P, :nch, :]), reads=[pn], writes=[Nn])
                    yield
                    px = nps()
                    pxv = units(px, Nn, X[hp], P, [Nn, X[hp]])
                    k.op("dve", lambda e: e.tensor_tensor(X[hp][P, :nch, :], pxv[P, :nch, :], X[hp][P, :nch, :], ALU.add), reads=[px, X[hp]], writes=[X[hp]])
                    yield
                yield from level(Zc, Nc, Zn, Nn, last)
                Zc, Nc, Zn, Nn = Zn, Nn, Zc, Nc
            pa = nps()
            pav = units(pa, Aak[hp], Vtm[hp], P, [Aak[hp], Vtm[hp]])
            k.op("act", lambda e: e.copy(AVs[hp][P, :nch, :], pav[P, :nch, :]), reads=[pa], writes=[AVs[hp]])
            yield
            pq = nps()
            pqv = units(pq, X[hp], AVs[hp], P, [X[hp], AVs[hp]])
            k.op("act", lambda e: e.copy(Qs[hp][P, :nch, :], pqv[P, :nch, :]), reads=[pq], writes=[Qs[hp]])
            yield
            pp_ = nps()
            ppv_ = units(pp_, Atm[hp], X[hp], P, [Atm[hp], X[hp]])
            k.op("dve", lambda e: e.tensor_copy(PaT[hp][P, :nch, :], ppv_[P, :nch, :]), reads=[pp_], writes=[PaT[hp]])
            yield

        interleave_here = [head_block(0, 0), head_block(0, 1), head_block(1, 0), head_block(1, 1)]
        alive = list(interleave_here)
        while alive:
            for g in list(alive):
                try:
                    next(g)
                except StopIteration:
                    alive.remove(g)
            yield

        def seq_step(ch, hp, hl):
            P = slice(hl * 64, hl * 64 + 64)
            pyb = PY[hl]
            col = hp * 256 + (ch % 4) * 64
            pu_ = nps()
            k.op("pe", lambda e: e.matmul(pu_[P, 0:64], lhsT=PaT[hp][P, ch, :], rhs=Mbf[hp][P, :], start=True, stop=True), reads=[PaT[hp], Mbf[hp]], writes=[pu_])
            k.op("dve", lambda e: e.tensor_tensor(Us[hp][P, ch, :], pu_[P, 0:64], Qs[hp][P, ch, :], ALU.add), reads=[pu_, Qs[hp]], writes=[Us[hp]])
            yield
            k.op("pe", lambda e: e.matmul(pyb[P, col:col + 64], lhsT=Mbf[hp][P, :], rhs=Rtrue[hp][P, ch, :], start=True, stop=False), reads=[Mbf[hp], Rtrue[hp]], writes=[pyb], pe_acc=True)
            k.op("pe", lambda e: e.matmul(pyb[P, col:col + 64], lhsT=Vtm[hp][P, ch, :], rhs=Ark[hp][P, ch, :], start=False, stop=False), reads=[Vtm[hp], Ark[hp]], writes=[pyb], pe_acc=True)
            k.op("pe", lambda e: e.matmul(pyb[P, col:col + 64], lhsT=Us[hp][P, ch, :], rhs=Arb[hp][P, ch, :], start=False, stop=True), reads=[Us[hp], Arb[hp]], writes=[pyb], pe_acc=True)
            pm_ = nps()
            k.op("pe", lambda e: e.matmul(pm_[P, 0:64], lhsT=Bhtm[hp][P, ch, :], rhs=Us[hp][P, ch, :], start=True, stop=False), reads=[Bhtm[hp], Us[hp]], writes=[pm_], pe_acc=True)
            k.op("pe", lambda e: e.matmul(pm_[P, 0:64], lhsT=Khtm[hp][P, ch, :], rhs=Vtm[hp][P, ch, :], start=False, stop=True), reads=[Khtm[hp], Vtm[hp]], writes=[pm_], pe_acc=True)
            k.op("dve", lambda e: e.scalar_tensor_tensor(out=Mst[hp][P, :], in0=Mst[hp][P, :], scalar=WC[hp][P, ch:ch + 1], in1=pm_[P, 0:64], op0=ALU.mult, op1=ALU.add), reads=[Mst[hp], WC[hp], pm_], writes=[Mst[hp]])
            k.op("act", lambda e: e.copy(Mbf[hp][P, :], Mst[hp][P, :]), reads=[Mst[hp]], writes=[Mbf[hp]])
            yield

        def evac_y(grp):
            for hp in range(2):
                for hl in range(2):
                    P = slice(hl * 64, hl * 64 + 64)
                    pp = PY[hl]
                    nc4 = min(4, nch - grp * 4)
                    src = pp[P, hp * 256:hp * 256 + nc4 * 64]
                    dst = ysb[hp][P, grp * 256:grp * 256 + nc4 * 64]
                    if hl == 0:
                        k.op("dve", lambda e, src=src, dst=dst: e.tensor_copy(dst, src), reads=[pp], writes=[ysb[hp]])
                    else:
                        k.op("act", lambda e, src=src, dst=dst: e.copy(dst, src), reads=[pp], writes=[ysb[hp]])

        chs = list(range(nch)) if d == 0 else list(range(nch - 1, -1, -1))
        done = 0
        for ch in chs:
            gens = [seq_step(ch, hp, hl) for hp in range(2) for hl in range(2)]
            alive = list(gens)
            while alive:
                for g in list(alive):
                    try:
                        next(g)
                    except StopIteration:
                        alive.remove(g)
                yield
            done += 1
            if done % 4 == 0 or done == nch:
                evac_y(ch // 4)
                yield

        def outp(hp):
            if d == 0:
                k.op("pool", lambda e: e.dma_start(out=self.rw_yf.ap[hp * 128:(hp + 1) * 128, tok0:tok0 + n], in_=ysb[hp][:, :n]), reads=[ysb[hp]], dma=True)
            else:
                self.rw_finish(l, hp, tok0, n, ysb[hp], yf_in[hp], fbon, fgat, gn_g, gn_b, nps, fsq, None, fkx, frn)
        for hp in range(2):
            outp(hp)
            yield

    nt = len(order)
    gA = stageA(0, *order[0])
    for _ in gA:
        pass
    for ti in range(nt):
        gens = [stageB(ti, *order[ti])]
        wts = [3]
        if ti + 1 < nt:
            gens.append(stageA(ti + 1, *order[ti + 1]))
            wts.append(1)
        if os.environ.get("RW_IL", "0") == "1":
            interleave(gens, wts)
        else:
            for g_ in gens:
                for _ in g_:
                    pass
    k.barrier()
    k.emit()
    k.free_to(m)


def phase_rwkv_old(self, l, d):
    k = self.k
    m = k.mark()
    NT = self.NT
    s = NEG_E05
    mp = self._pvec("mp", self.rw_mu_prev.ap[l], 8)
    mn = self._pvec("mn", self.rw_mu_next.ap[l], 8)
    cmix = k.sb("cmix", [128, 8], F32)
    k.op("dve", lambda e: e.tensor_tensor(cmix[:, :], mp[:, :], mn[:, :], ALU.add), reads=[mp, mn], writes=[cmix])
    k.op("dve", lambda e: e.tensor_scalar(cmix[:, :], cmix[:, :], -1.0, 1.0, ALU.mult, ALU.add), reads=[cmix], writes=[cmix])
    w0 = self._pvec("w0", self.rw_w0.ap[l, d], 2)
    a0 = self._pvec("a0", self.rw_a0.ap[l, d], 2)
    k_k = self._pvec("k_k", self.rw_k_k.ap[l], 2)
    k_a = self._pvec("k_a", self.rw_k_a.ap[l], 2)
    r_k = self._pvec("r_k", self.rw_r_k.ap[l], 2)
    gn_g = self._pvec("gn_g", self.rw_gn_g.ap[l], 2)
    gn_b = self._pvec("gn_b", self.rw_gn_b.ap[l], 2)
    w2s = k.sb("w2s", [128, 256], BF16)
    a2s = k.sb("a2s", [128, 256], BF16)
    g2s = k.sb("g2s", [128, 256], BF16)
    k.op("pool", lambda e: e.dma_start(out=w2s[0:64, :], in_=self.rw_w2.ap[l, d]), writes=[w2s], dma=True)
    k.op("pool", lambda e: e.dma_start(out=a2s[64:128, :], in_=self.rw_a2.ap[l, d]), writes=[a2s], dma=True)
    k.op("pool", lambda e: e.dma_start(out=g2s[:, :], in_=self.rw_g2.ap[l]), writes=[g2s], dma=True)

    U = [k.sb("rU%d" % i, [128, 8, 514], F32) for i in range(2)]
    S = k.sb("rS", [128, 8, 512], F32)
    wlt = k.sb("wlt", [128, 512], BF16)
    alb = k.sb("alb", [128, 512], BF16)
    sgl = k.sb("sgl", [128, 512], BF16)
    f32t = lambda n_: k.sb(n_, [128, 8, 64], F32)
    bft = lambda n_: k.sb(n_, [128, 8, 64], BF16)
    sgw, asig, kx, rn, cs, dcs, tmp, E, Etrue = [f32t("r_" + z) for z in ("sgw", "asig", "kx", "rn", "cs", "dcs", "tmp", "E", "Etrue")]
    kkn, bvec, kmod = f32t("kkn"), f32t("bvec"), f32t("kmod")
    sqk = bft("sqk")
    rt, kt, bt, at, Rtrue, Atrue, Bh, Kh, vb = [[bft("r_%s%d" % (z, hp)) for hp in range(2)] for z in ("rt", "kt", "bt", "at", "Rtrue", "Atrue", "Bh", "Kh", "vb")]
    WC = [k.sb("WC%d" % hp, [128, 8], F32) for hp in range(2)]
    Atm, Bhtm, Khtm, Vtm = [[bft("r_%s%d" % (z, hp)) for hp in range(2)] for z in ("Atm", "Bhtm", "Khtm", "Vtm")]
    Zs, Ns, Zs2, Ns2, Aak, Arb, Ark, X, AVs, PaT, Us = [[bft("r_%s%d" % (z, hp)) for hp in range(2)] for z in ("Zs", "Ns", "Zs2", "Ns2", "Aak", "Arb", "Ark", "X", "AVs", "PaT", "Us")]
    Qs = [f32t("Qs%d" % hp) for hp in range(2)]
    Mst = [k.sb("Mst%d" % hp, [128, 64], F32) for hp in range(2)]
    Mbf = [k.sb("Mbf%d" % hp, [128, 64], BF16) for hp in range(2)]
    ysb = [k.sb("ysb%d" % hp, [128, 512], F32) for hp in range(2)]
    bon = k.sb("bon", [128, 512], F32)
    gat = k.sb("gat", [128, 512], F32)
    yf_in = [k.sb("yfin%d" % hp, [128, 512], F32) for hp in range(2)]
    PS = [k.ps("rps%d" % i, [128, 512], F32) for i in range(2)]
    PY = [[k.ps("rpy%d%d" % (i, j), [128, 512], F32) for j in range(2)] for i in range(2)]
    PSB = [k.ps("rpsb%d" % i, [128, 8, 64], BF16) for i in range(2)]
    psi = [0]
    psbi = [0]

    def nps():
        p = PS[psi[0] % 2]
        psi[0] += 1
        return p

    def npsb():
        p = PSB[psbi[0] % 2]
        psbi[0] += 1
        return p

    for hp in range(2):
        k.op("dve", lambda e, hp=hp: e.memset(Mst[hp][:, :], 0.0), writes=[Mst[hp]])
        k.op("dve", lambda e, hp=hp: e.memset(Mbf[hp][:, :], 0.0), writes=[Mbf[hp]])

    M_SL, M_LE, M_SG, M_GE = 0, 1, 2, 3
    if d == 0:
        mZ, mN, mI = M_SL, M_SG, M_LE
    else:
        mZ, mN, mI = M_SG, M_SL, M_GE

    tiles = self.tok_tiles()
    lat = tiles[1:]
    order = [tiles[0]] + (lat if d == 0 else lat[::-1])
    uTv = self.uT.ap[0:1024, :].rearrange("(c p) t -> p c t", p=128)

    def v3(t, P, nch):
        return t[P, 0:nch, :]

    def v2(t, P, n):
        return t[P, :, :].rearrange("p c t -> p (c t)")[:, 0:n]

    def tile_body(ti, tok0, n, stream):
        nch = n // 64
        seq0, seq1 = (0, CTX) if stream == 1 else (CTX, NT)
        ub = U[ti % 2]
        lo = max(tok0 - 1, seq0)
        hi = min(tok0 + n + 1, seq1)
        if lo > tok0 - 1:
            k.op("pool", lambda e: e.memset(ub[:, :, 0:1], 0.0), writes=[ub])
        if hi < tok0 + n + 1:
            k.op("pool", lambda e: e.memset(ub[:, :, n + 1:n + 2], 0.0), writes=[ub])
        k.op("sp", lambda e: e.dma_start(out=ub[:, :, lo - (tok0 - 1):hi - (tok0 - 1)], in_=uTv[:, :, lo:hi]), writes=[ub], dma=True)
        fl = lambda t_: t_[:, :, :].rearrange("p c t -> p (c t)")[:, 0:n]

        def shift(c):
            k.op("dve", lambda e: e.tensor_scalar(S[:, c, :n], ub[:, c, 1:n + 1], cmix[:, c:c + 1], None, ALU.mult), reads=[ub, cmix], writes=[S])
            k.op("dve", lambda e: e.scalar_tensor_tensor(out=S[:, c, :n], in0=ub[:, c, 0:n], scalar=mp[:, c:c + 1], in1=S[:, c, :n], op0=ALU.mult, op1=ALU.add), reads=[ub, mp, S], writes=[S])
            k.op("dve", lambda e: e.scalar_tensor_tensor(out=S[:, c, :n], in0=ub[:, c, 2:n + 2], scalar=mn[:, c:c + 1], in1=S[:, c, :n], op0=ALU.mult, op1=ALU.add), reads=[ub, mn, S], writes=[S])
        for c in range(8):
            if d == 1 and c == 7:
                continue
            shift(c)
        k.op("act", lambda e: e.activation(out=wlt[0:64, :n], in_=S[0:64, 6, :n], func=AF.Tanh), reads=[S], writes=[wlt])
        k.op("act", lambda e: e.copy(alb[64:128, :n], S[64:128, 6, :n]), reads=[S], writes=[alb])
        if d == 0:
            k.op("act", lambda e: e.activation(out=sgl[:, :n], in_=S[:, 7, :n], func=AF.Sigmoid), reads=[S], writes=[sgl])

        def prep(hp):
            p1 = nps()
            k.op("pe", lambda e: e.matmul(p1[:, :n], lhsT=w2s[0:64, hp * 128:(hp + 1) * 128], rhs=wlt[0:64, :n], start=True, stop=True), reads=[w2s, wlt], writes=[p1])
            k.op("act", lambda e: e.activation(out=fl(sgw), in_=p1[:, :n], func=AF.Sigmoid, bias=w0[:, hp:hp + 1]), reads=[p1, w0], writes=[sgw])
            p2 = nps()
            k.op("pe", lambda e: e.matmul(p2[:, :n], lhsT=a2s[64:128, hp * 128:(hp + 1) * 128], rhs=alb[64:128, :n], start=True, stop=True), reads=[a2s, alb], writes=[p2])
            k.op("act", lambda e: e.activation(out=fl(asig), in_=p2[:, :n], func=AF.Sigmoid, bias=a0[:, hp:hp + 1]), reads=[p2, a0], writes=[asig])
            k.op("dve", lambda e: e.tensor_scalar(fl(kx), S[:, 2 + hp, :n], k_k[:, hp:hp + 1], None, ALU.mult), reads=[S, k_k], writes=[kx])
            k.op("act", lambda e: e.activation(out=fl(sqk), in_=fl(kx), func=AF.Square), reads=[kx], writes=[sqk])
            p3 = nps()
            k.op("pe", lambda e: e.matmul(p3[:, :n], lhsT=self.blk[:, :], rhs=fl(sqk), start=True, stop=True), reads=[self.blk, sqk], writes=[p3])
            k.op("act", lambda e: e.activation(out=fl(rn), in_=p3[:, :n], func=AF.Sqrt, bias=1e-12), reads=[p3], writes=[rn])
            k.op("dve", lambda e: e.reciprocal(fl(rn), fl(rn)), reads=[rn], writes=[rn])
            k.op("dve", lambda e: e.tensor_tensor(fl(kkn), fl(kx), fl(rn), ALU.mult), reads=[kx, rn], writes=[kkn])
            k.op("pool", lambda e: e.tensor_tensor(fl(bvec), fl(kkn), fl(asig), ALU.mult), reads=[kkn, asig], writes=[bvec])
            k.op("dve", lambda e: e.tensor_scalar(fl(tmp), fl(asig), -1.0, k_a[:, hp:hp + 1], ALU.add, ALU.mult), reads=[asig, k_a], writes=[tmp])
            k.op("dve", lambda e: e.scalar_tensor_tensor(out=fl(kmod), in0=fl(tmp), scalar=1.0, in1=S[:, 2 + hp, :n], op0=ALU.add, op1=ALU.mult), reads=[tmp, S], writes=[kmod])
            k.op("dve", lambda e: e.tensor_tensor_scan(fl(cs), self.reset[:, :n], fl(sgw), 0.0, ALU.mult, ALU.add), reads=[self.reset, sgw], writes=[cs])
            if d == 1:
                k.op("dve", lambda e: e.tensor_tensor(fl(tmp), fl(sgw), fl(cs), ALU.subtract), reads=[sgw, cs], writes=[tmp])
                k.op("dve", lambda e: e.tensor_tensor(cs[:, :nch, :], tmp[:, :nch, :], cs[:, :nch, 63:64].to_broadcast([128, nch, 64]), ALU.add), reads=[tmp, cs], writes=[cs])
            endi = 63 if d == 0 else 0
            k.op("pool", lambda e: e.tensor_tensor(dcs[:, :nch, :], cs[:, :nch, :], cs[:, :nch, 32:33].to_broadcast([128, nch, 64]), ALU.subtract), reads=[cs], writes=[dcs])
            k.op("act", lambda e: e.activation(out=fl(E), in_=fl(dcs), func=AF.Exp, scale=s), reads=[dcs], writes=[E])
            k.op("dve", lambda e: e.tensor_tensor(fl(rt[hp]), S[:, hp, :n], fl(E), ALU.mult), reads=[S, E], writes=[rt[hp]])
            k.op("pool", lambda e: e.tensor_tensor(fl(tmp), fl(dcs), fl(sgw), ALU.subtract), reads=[dcs, sgw], writes=[tmp])
            k.op("act", lambda e: e.activation(out=fl(E), in_=fl(tmp), func=AF.Exp, scale=s), reads=[tmp], writes=[E])
            k.op("dve", lambda e: e.scalar_tensor_tensor(out=fl(at[hp]), in0=fl(kkn), scalar=-1.0, in1=fl(E), op0=ALU.mult, op1=ALU.mult), reads=[kkn, E], writes=[at[hp]])
            k.op("act", lambda e: e.activation(out=fl(E), in_=fl(dcs), func=AF.Exp, scale=-s), reads=[dcs], writes=[E])
            k.op("dve", lambda e: e.tensor_tensor(fl(kt[hp]), fl(kmod), fl(E), ALU.mult), reads=[kmod, E], writes=[kt[hp]])
            k.op("pool", lambda e: e.tensor_tensor(fl(bt[hp]), fl(bvec), fl(E), ALU.mult), reads=[bvec, E], writes=[bt[hp]])
            k.op("act", lambda e: e.activation(out=fl(Etrue), in_=fl(cs), func=AF.Exp, scale=s), reads=[cs], writes=[Etrue])
            k.op("dve", lambda e: e.tensor_tensor(fl(Rtrue[hp]), S[:, hp, :n], fl(Etrue), ALU.mult), reads=[S, Etrue], writes=[Rtrue[hp]])
            k.op("dve", lambda e: e.tensor_copy(WC[hp][:, :nch], Etrue[:, :nch, endi]), reads=[Etrue], writes=[WC[hp]])
            k.op("pool", lambda e: e.tensor_tensor(fl(tmp), fl(cs), fl(sgw), ALU.subtract), reads=[cs, sgw], writes=[tmp])
            k.op("act", lambda e: e.activation(out=fl(E), in_=fl(tmp), func=AF.Exp, scale=s), reads=[tmp], writes=[E])
            k.op("dve", lambda e: e.scalar_tensor_tensor(out=fl(Atrue[hp]), in0=fl(kkn), scalar=-1.0, in1=fl(E), op0=ALU.mult, op1=ALU.mult), reads=[kkn, E], writes=[Atrue[hp]])
            k.op("pool", lambda e: e.tensor_tensor(tmp[:, :nch, :], cs[:, :nch, endi:endi + 1].to_broadcast([128, nch, 64]), cs[:, :nch, :], ALU.subtract), reads=[cs], writes=[tmp])
            k.op("act", lambda e: e.activation(out=fl(E), in_=fl(tmp), func=AF.Exp, scale=s), reads=[tmp], writes=[E])
            k.op("dve", lambda e: e.tensor_tensor(fl(Bh[hp]), fl(bvec), fl(E), ALU.mult), reads=[bvec, E], writes=[Bh[hp]])
            k.op("pool", lambda e: e.tensor_tensor(fl(Kh[hp]), fl(kmod), fl(E), ALU.mult), reads=[kmod, E], writes=[Kh[hp]])
            k.op("act", lambda e: e.copy(fl(vb[hp]), S[:, 4 + hp, :n]), reads=[S], writes=[vb[hp]])
            if d == 0:
                k.op("dve", lambda e: e.scalar_tensor_tensor(out=fl(sqk), in0=S[:, hp, :n], scalar=r_k[:, hp:hp + 1], in1=S[:, 2 + hp, :n], op0=ALU.mult, op1=ALU.mult), reads=[S, r_k], writes=[sqk])
                p4 = nps()
                k.op("pe", lambda e: e.matmul(p4[:, :n], lhsT=self.blk[:, :], rhs=fl(sqk), start=True, stop=True), reads=[self.blk, sqk], writes=[p4])
                k.op("dve", lambda e: e.tensor_tensor(bon[:, :n], p4[:, :n], S[:, 4 + hp, :n], ALU.mult), reads=[p4, S], writes=[bon])
                k.op("pool", lambda e: e.dma_start(out=self.rw_bonus.ap[hp * 128:(hp + 1) * 128, tok0:tok0 + n], in_=bon[:, :n]), reads=[bon], dma=True)
                p5 = nps()
                k.op("pe", lambda e: e.matmul(p5[:, :n], lhsT=g2s[:, hp * 128:(hp + 1) * 128], rhs=sgl[:, :n], start=True, stop=True), reads=[g2s, sgl], writes=[p5])
                k.op("act", lambda e: e.copy(gat[:, :n], p5[:, :n]), reads=[p5], writes=[gat])
                k.op("pool", lambda e: e.dma_start(out=self.rw_gate.ap[hp * 128:(hp + 1) * 128, tok0:tok0 + n], in_=gat[:, :n]), reads=[gat], dma=True)

        def units(dst_ps, lt, rt_, P, reads):
            pv = dst_ps[:, :].rearrange("p (c t) -> p c t", t=64)
            for ch in range(nch):
                k.op("pe", lambda e, ch=ch: e.matmul(pv[P, ch, :], lhsT=lt[P, ch, :], rhs=rt_[P, ch, :], start=True, stop=True), reads=reads, writes=[dst_ps], pe_acc=True)
            return pv

        def head_block(hp, hl):
            P = slice(hl * 64, hl * 64 + 64)

            def tm(src, dst):
                pb = npsb()
                for ch in range(nch):
                    k.op("pe", lambda e, ch=ch: e.transpose(pb[P, ch, :], src[hp][P, ch, :], self.ident[P, P]), reads=[src[hp], self.ident], writes=[pb], pe_acc=True)
                k.op("act", lambda e: e.copy(dst[hp][P, :nch, :], pb[P, :nch, :]), reads=[pb], writes=[dst[hp]])
            tm(Atrue, Atm); tm(Bh, Bhtm); tm(Kh, Khtm); tm(vb, Vtm)

            def pair(dst, lt, rt_, mask, eng="dve"):
                pp = nps()
                pv = units(pp, lt[hp], rt_[hp], P, [lt[hp], rt_[hp]])
                mv = self.masks[:, mask, :].rearrange("p (c t) -> p c t", t=64)
                k.op(eng, lambda e: e.tensor_tensor(dst[hp][P, :nch, :], pv[P, :nch, :], mv[P, :nch, :], ALU.mult), reads=[pp, self.masks], writes=[dst[hp]])
            pair(Zs, bt, at, mZ)
            pair(Ns, at, bt, mN)
            pair(Aak, kt, at, mZ)
            pair(Arb, bt, rt, mI)
            pair(Ark, kt, rt, mI)
            idv = self.id8[:, :].rearrange("p (c t) -> p c t", t=64)
            k.op("pool", lambda e: e.tensor_tensor(X[hp][P, :nch, :], Zs[hp][P, :nch, :], idv[P, :nch, :], ALU.add), reads=[Zs[hp], self.id8], writes=[X[hp]])
            Zc, Nc, Zn, Nn = Zs[hp], Ns[hp], Zs2[hp], Ns2[hp]
            for lev in range(1, 6):
                last = lev == 5

                def level(Zc, Nc, Zn, Nn, last):
                    if not last:
                        pz = nps()
                        pzv = units(pz, Nc, Zc, P, [Nc, Zc])
                    pn = nps()
                    pnv = units(pn, Zc, Nc, P, [Nc, Zc])
                    if not last:
                        k.op("act", lambda e: e.copy(Zn[P, :nch, :], pzv[P, :nch, :]), reads=[pz], writes=[Zn])
                    k.op("dve", lambda e: e.tensor_copy(Nn[P, :nch, :], pnv[P, :nch, :]), reads=[pn], writes=[Nn])
                    px = nps()
                    pxv = units(px, Nn, X[hp], P, [Nn, X[hp]])
                    k.op("dve", lambda e: e.tensor_tensor(X[hp][P, :nch, :], pxv[P, :nch, :], X[hp][P, :nch, :], ALU.add), reads=[px, X[hp]], writes=[X[hp]])
                level(Zc, Nc, Zn, Nn, last)
                Zc, Nc, Zn, Nn = Zn, Nn, Zc, Nc
            pa = nps()
            pav = units(pa, Aak[hp], Vtm[hp], P, [Aak[hp], Vtm[hp]])
            k.op("act", lambda e: e.copy(AVs[hp][P, :nch, :], pav[P, :nch, :]), reads=[pa], writes=[AVs[hp]])
            pq = nps()
            pqv = units(pq, X[hp], AVs[hp], P, [X[hp], AVs[hp]])
            k.op("act", lambda e: e.copy(Qs[hp][P, :nch, :], pqv[P, :nch, :]), reads=[pq], writes=[Qs[hp]])
            pp_ = nps()
            ppv_ = units(pp_, Atm[hp], X[hp], P, [Atm[hp], X[hp]])
            k.op("dve", lambda e: e.tensor_copy(PaT[hp][P, :nch, :], ppv_[P, :nch, :]), reads=[pp_], writes=[PaT[hp]])

        for hp in range(2):
            prep(hp)
            for hl in range(2):
                head_block(hp, hl)

        def seq_step(ch, hp, hl):
            P = slice(hl * 64, hl * 64 + 64)
            pyb = PY[hp][hl]
            pu_ = nps()
            k.op("pe", lambda e: e.matmul(pu_[P, 0:64], lhsT=PaT[hp][P, ch, :], rhs=Mbf[hp][P, :], start=True, stop=True), reads=[PaT[hp], Mbf[hp]], writes=[pu_])
            k.op("dve", lambda e: e.tensor_tensor(Us[hp][P, ch, :], pu_[P, 0:64], Qs[hp][P, ch, :], ALU.add), reads=[pu_, Qs[hp]], writes=[Us[hp]])
            pyv = pyb[:, :].rearrange("p (c t) -> p c t", t=64)
            k.op("pe", lambda e: e.matmul(pyv[P, ch, :], lhsT=Mbf[hp][P, :], rhs=Rtrue[hp][P, ch, :], start=True, stop=False), reads=[Mbf[hp], Rtrue[hp]], writes=[pyb], pe_acc=True)
            k.op("pe", lambda e: e.matmul(pyv[P, ch, :], lhsT=Vtm[hp][P, ch, :], rhs=Ark[hp][P, ch, :], start=False, stop=False), reads=[Vtm[hp], Ark[hp]], writes=[pyb], pe_acc=True)
            k.op("pe", lambda e: e.matmul(pyv[P, ch, :], lhsT=Us[hp][P, ch, :], rhs=Arb[hp][P, ch, :], start=False, stop=True), reads=[Us[hp], Arb[hp]], writes=[pyb], pe_acc=True)
            pm_ = nps()
            k.op("pe", lambda e: e.matmul(pm_[P, 0:64], lhsT=Bhtm[hp][P, ch, :], rhs=Us[hp][P, ch, :], start=True, stop=False), reads=[Bhtm[hp], Us[hp]], writes=[pm_], pe_acc=True)
            k.op("pe", lambda e: e.matmul(pm_[P, 0:64], lhsT=Khtm[hp][P, ch, :], rhs=Vtm[hp][P, ch, :], start=False, stop=True), reads=[Khtm[hp], Vtm[hp]], writes=[pm_], pe_acc=True)
            k.op("dve", lambda e: e.scalar_tensor_tensor(out=Mst[hp][P, :], in0=Mst[hp][P, :], scalar=WC[hp][P, ch:ch + 1], in1=pm_[P, 0:64], op0=ALU.mult, op1=ALU.add), reads=[Mst[hp], WC[hp], pm_], writes=[Mst[hp]])
            k.op("act", lambda e: e.copy(Mbf[hp][P, :], Mst[hp][P, :]), reads=[Mst[hp]], writes=[Mbf[hp]])

        chs = range(nch) if d == 0 else range(nch - 1, -1, -1)
        for ch in chs:
            for hp in range(2):
                for hl in range(2):
                    seq_step(ch, hp, hl)

        def outp(hp):
            for hl in range(2):
                P = slice(hl * 64, hl * 64 + 64)
                pp = PY[hp][hl]
                if hl == 0:
                    k.op("dve", lambda e, P=P, pp=pp: e.tensor_copy(ysb[hp][P, :n], pp[P, :n]), reads=[pp], writes=[ysb[hp]])
                else:
                    k.op("act", lambda e, P=P, pp=pp: e.copy(ysb[hp][P, :n], pp[P, :n]), reads=[pp], writes=[ysb[hp]])
            if d == 0:
                k.op("pool", lambda e: e.dma_start(out=self.rw_yf.ap[hp * 128:(hp + 1) * 128, tok0:tok0 + n], in_=ysb[hp][:, :n]), reads=[ysb[hp]], dma=True)
            else:
                self.rw_finish(l, hp, tok0, n, ysb[hp], yf_in[hp], bon, gat, gn_g, gn_b, nps, sqk, tmp, kx, rn)
        for hp in range(2):
            outp(hp)

    for ti, (tok0, n, stream) in enumerate(order):
        tile_body(ti, tok0, n, stream)
    k.barrier()
    k.emit()
    k.free_to(m)


def rw_finish(self, l, hp, tok0, n, yb, yf, bon, gat, gn_g, gn_b, nps, sqk, tmp, kx, rn):
    k = self.k
    fl = lambda t_: t_[:, :, :].rearrange("p c t -> p (c t)")[:, 0:n]
    k.op("sp", lambda e: e.dma_start(out=yf[:, :n], in_=self.rw_yf.ap[hp * 128:(hp + 1) * 128, tok0:tok0 + n]), writes=[yf], dma=True)
    k.op("sp", lambda e: e.dma_start(out=bon[:, :n], in_=self.rw_bonus.ap[hp * 128:(hp + 1) * 128, tok0:tok0 + n]), writes=[bon], dma=True)
    k.op("sp", lambda e: e.dma_start(out=gat[:, :n], in_=self.rw_gate.ap[hp * 128:(hp + 1) * 128, tok0:tok0 + n]), writes=[gat], dma=True)
    k.op("dve", lambda e: e.tensor_tensor(yb[:, :n], yb[:, :n], yf[:, :n], ALU.add), reads=[yb, yf], writes=[yb])
    k.op("act", lambda e: e.copy(fl(sqk), yb[:, :n]), reads=[yb], writes=[sqk])
    p1 = nps()
    k.op("pe", lambda e: e.matmul(p1[:, :n], lhsT=self.blk[:, :], rhs=fl(sqk), start=True, stop=True), reads=[self.blk, sqk], writes=[p1])
    k.op("dve", lambda e: e.scalar_tensor_tensor(out=fl(kx), in0=p1[:, :n], scalar=-1.0 / 64, in1=yb[:, :n], op0=ALU.mult, op1=ALU.add), reads=[p1, yb], writes=[kx])
    k.op("act", lambda e: e.copy(fl(sqk), fl(kx)), reads=[kx], writes=[sqk])
    p1b = nps()
    k.op("pe", lambda e: e.matmul(p1b[:, :n], lhsT=self.blk[:, :], rhs=fl(sqk), start=True, stop=True), reads=[self.blk, sqk], writes=[p1b])
    k.op("dve", lambda e: e.scalar_tensor_tensor(out=fl(kx), in0=p1b[:, :n], scalar=-1.0 / 64, in1=fl(kx), op0=ALU.mult, op1=ALU.add), reads=[p1b, kx], writes=[kx])
    k.op("act", lambda e: e.activation(out=fl(sqk), in_=fl(kx), func=AF.Square), reads=[kx], writes=[sqk])
    p2 = nps()
    k.op("pe", lambda e: e.matmul(p2[:, :n], lhsT=self.blk[:, :], rhs=fl(sqk), start=True, stop=True), reads=[self.blk, sqk], writes=[p2])
    k.op("act", lambda e: e.activation(out=fl(rn), in_=p2[:, :n], func=AF.Ln, scale=1.0 / 64, bias=64e-5), reads=[p2], writes=[rn])
    k.op("act", lambda e: e.activation(out=fl(rn), in_=fl(rn), func=AF.Exp, scale=-0.5), reads=[rn], writes=[rn])
    k.op("dve", lambda e: e.tensor_tensor(fl(kx), fl(kx), fl(rn), ALU.mult), reads=[kx, rn], writes=[kx])
    k.op("dve", lambda e: e.tensor_scalar(fl(kx), fl(kx), gn_g[:, hp:hp + 1], gn_b[:, hp:hp + 1], ALU.mult, ALU.add), reads=[kx, gn_g, gn_b], writes=[kx])
    k.op("pool", lambda e: e.tensor_tensor(fl(kx), fl(kx), bon[:, :n], ALU.add), reads=[kx, bon], writes=[kx])
    k.op("dve", lambda e: e.tensor_tensor(fl(kx), fl(kx), gat[:, :n], ALU.mult), reads=[kx, gat], writes=[kx])
    k.op("pool", lambda e: e.dma_start(out=self.mixT.ap[hp * 128:(hp + 1) * 128, tok0:tok0 + n], in_=fl(kx)), reads=[kx], dma=True)


MK.rw_consts = _rw_consts
MK.load_consts2 = _load_consts2
MK._pvec = _pvec
MK.phase_rwkv = phase_rwkv_old if os.environ.get('RW_OLD', '0') == '1' else phase_rwkv
MK.rw_finish = rw_finish


GLA_OFF = 2312


def phase_gla(self, l, d):
    k = self.k
    m = k.mark()
    NT = self.NT
    s = 1.0 / 16.0
    qscale = 32 ** -0.5
    if not hasattr(self, "gla_yf"):
        kind = "ExternalOutput" if "gla_yf" in self.dbg else "Internal"
        self.gla_yf = k.dram("gla_yf", [256, NT], F32, kind=kind)
    ga2f = k.sb("ga2f", [16, 2, 128], F32)
    ga2p = k.sb("ga2p", [16, 2, 128], BF16)
    gbp = k.sb("gbp", [128, 2], F32)
    k.op("dve", lambda e: e.memset(ga2f[:, :, :], 0.0), writes=[ga2f])
    k.op("dve", lambda e: e.memset(gbp[:, :], 0.0), writes=[gbp])
    for h in range(4):
        hp, hl = h // 2, h % 2
        k.op("sp", lambda e, h=h, hp=hp, hl=hl: e.dma_start(out=ga2f[:, hp, hl * 64:hl * 64 + 32], in_=self.gla_ga2.ap[l, d, :, h * 32:(h + 1) * 32]), writes=[ga2f], dma=True)
        k.op("sp", lambda e, h=h, hp=hp, hl=hl: e.dma_start(out=gbp[hl * 64:hl * 64 + 32, hp:hp + 1], in_=self.gla_gb.ap[l, d, h * 32:(h + 1) * 32].rearrange("(p o) -> p o", o=1), allow_slow_non_contiguous=True), writes=[gbp], dma=True)
    k.op("dve", lambda e: e.tensor_copy(ga2p[:, :, :], ga2f[:, :, :]), reads=[ga2f], writes=[ga2p])
    ng = self._pvec("gng", self.gla_norm_g.ap[l], 2)

    f32t = lambda n_: k.sb(n_, [128, 8, 64], F32)
    bft = lambda n_: k.sb(n_, [128, 8, 64], BF16)
    q = [[f32t("gq%d%d" % (i, hp)) for hp in range(2)] for i in range(2)]
    kk_ = [[f32t("gk%d%d" % (i, hp)) for hp in range(2)] for i in range(2)]
    vv = [[f32t("gv%d%d" % (i, hp)) for hp in range(2)] for i in range(2)]
    glf = [k.sb("glf%d" % i, [16, 512], F32) for i in range(2)]
    glb = k.sb("glb", [16, 512], BF16)
    for i in range(2):
        for hp in range(2):
            k.op("pool", lambda e, i=i, hp=hp: e.memset(q[i][hp][:, :, :], 0.0), writes=[q[i][hp]])
            k.op("pool", lambda e, i=i, hp=hp: e.memset(kk_[i][hp][:, :, :], 0.0), writes=[kk_[i][hp]])
    lg, cs, dcs, tmp, E, Etrue = [f32t("g_" + z) for z in ("lg", "cs", "dcs", "tmp", "E", "Etrue")]
    qt, kt, Qtrue, Kh, vb, Khtm, Vtm, Ark = [[bft("g_%s%d" % (z, hp)) for hp in range(2)] for z in ("qt", "kt", "Qtrue", "Kh", "vb", "Khtm", "Vtm", "Ark")]
    WC = [k.sb("gWC%d" % hp, [128, 8], F32) for hp in range(2)]
    Mst = [k.sb("gMst%d" % hp, [128, 64], F32) for hp in range(2)]
    Mbf = [k.sb("gMbf%d" % hp, [128, 64], BF16) for hp in range(2)]
    ysb = [k.sb("gysb%d" % hp, [128, 512], F32) for hp in range(2)]
    yfin = [k.sb("gyfin%d" % hp, [128, 512], F32) for hp in range(2)]
    rin = [k.sb("grin%d" % hp, [128, 512], F32) for hp in range(2)]
    sq = k.sb("gsq", [128, 512], BF16)
    rn = k.sb("grn", [128, 512], F32)
    PS = [k.ps("gps%d" % i, [128, 512], F32) for i in range(2)]
    PY = [[k.ps("gpy%d%d" % (i, j), [128, 512], F32) for j in range(2)] for i in range(2)]
    PSB = [k.ps("gpsb%d" % i, [128, 8, 64], BF16) for i in range(2)]
    psi = [0]; psbi = [0]

    def nps():
        p = PS[psi[0] % 2]; psi[0] += 1; return p

    def npsb():
        p = PSB[psbi[0] % 2]; psbi[0] += 1; return p
    for hp in range(2):
        k.op("dve", lambda e, hp=hp: e.memset(Mst[hp][:, :], 0.0), writes=[Mst[hp]])
        k.op("dve", lambda e, hp=hp: e.memset(Mbf[hp][:, :], 0.0), writes=[Mbf[hp]])
    mI = 1 if d == 0 else 3
    tiles = self.tok_tiles()
    lat = tiles[1:]
    order = [tiles[0]] + (lat if d == 0 else lat[::-1])
    O = GLA_OFF

    def tile_body(ti, tok0, n, stream):
        nch = n // 64
        b = ti % 2
        fl = lambda t_: t_[:, :, :].rearrange("p c t -> p (c t)")[:, 0:n]
        for h in range(4):
            hp, hl = h // 2, h % 2
            P32 = slice(hl * 64, hl * 64 + 32)
            k.op("sp", lambda e, h=h, hp=hp, P32=P32: e.dma_start(out=fl(q[b][hp])[P32, :], in_=self.uT.ap[O + h * 32:O + (h + 1) * 32, tok0:tok0 + n]), writes=[q[b][hp]], dma=True)
            k.op("sp", lambda e, h=h, hp=hp, P32=P32: e.dma_start(out=fl(kk_[b][hp])[P32, :], in_=self.uT.ap[O + 128 + h * 32:O + 128 + (h + 1) * 32, tok0:tok0 + n]), writes=[kk_[b][hp]], dma=True)
        for hp in range(2):
            k.op("sp", lambda e, hp=hp: e.dma_start(out=fl(vv[b][hp]), in_=self.uT.ap[O + 256 + hp * 128:O + 256 + (hp + 1) * 128, tok0:tok0 + n]), writes=[vv[b][hp]], dma=True)
        k.op("sp", lambda e: e.dma_start(out=glf[b][:, :n], in_=self.uT.ap[O + 512:O + 528, tok0:tok0 + n]), writes=[glf[b]], dma=True)
        k.op("act", lambda e: e.copy(glb[:, :n], glf[b][:, :n]), reads=[glf[b]], writes=[glb])

        def prep(hp):
            p1 = nps()
            k.op("pe", lambda e: e.matmul(p1[:, :n], lhsT=ga2p[:, hp, :], rhs=glb[:, :n], start=True, stop=True), reads=[ga2p, glb], writes=[p1])
            k.op("act", lambda e: e.activation(out=fl(tmp), in_=p1[:, :n], func=AF.Sigmoid, bias=gbp[:, hp:hp + 1]), reads=[p1, gbp], writes=[tmp])
            k.op("act", lambda e: e.activation(out=fl(lg), in_=fl(tmp), func=AF.Ln), reads=[tmp], writes=[lg])
            k.op("dve", lambda e: e.tensor_tensor_scan(fl(cs), self.reset[:, :n], fl(lg), 0.0, ALU.mult, ALU.add), reads=[self.reset, lg], writes=[cs])
            if d == 1:
                k.op("dve", lambda e: e.tensor_tensor(fl(tmp), fl(lg), fl(cs), ALU.subtract), reads=[lg, cs], writes=[tmp])
                k.op("dve", lambda e: e.tensor_tensor(cs[:, :nch, :], tmp[:, :nch, :], cs[:, :nch, 63:64].to_broadcast([128, nch, 64]), ALU.add), reads=[tmp, cs], writes=[cs])
            endi = 63 if d == 0 else 0
            k.op("pool", lambda e: e.tensor_tensor(dcs[:, :nch, :], cs[:, :nch, :], cs[:, :nch, 32:33].to_broadcast([128, nch, 64]), ALU.subtract), reads=[cs], writes=[dcs])
            k.op("act", lambda e: e.activation(out=fl(E), in_=fl(dcs), func=AF.Exp, scale=s), reads=[dcs], writes=[E])
            k.op("dve", lambda e: e.scalar_tensor_tensor(out=fl(qt[hp]), in0=fl(q[b][hp]), scalar=qscale, in1=fl(E), op0=ALU.mult, op1=ALU.mult), reads=[q[b][hp], E], writes=[qt[hp]])
            k.op("act", lambda e: e.activation(out=fl(E), in_=fl(dcs), func=AF.Exp, scale=-s), reads=[dcs], writes=[E])
            k.op("dve", lambda e: e.tensor_tensor(fl(kt[hp]), fl(kk_[b][hp]), fl(E), ALU.mult), reads=[kk_[b][hp], E], writes=[kt[hp]])
            k.op("act", lambda e: e.activation(out=fl(Etrue), in_=fl(cs), func=AF.Exp, scale=s), reads=[cs], writes=[Etrue])
            k.op("dve", lambda e: e.scalar_tensor_tensor(out=fl(Qtrue[hp]), in0=fl(q[b][hp]), scalar=qscale, in1=fl(Etrue), op0=ALU.mult, op1=ALU.mult), reads=[q[b][hp], Etrue], writes=[Qtrue[hp]])
            k.op("dve", lambda e: e.tensor_copy(WC[hp][:, :nch], Etrue[:, :nch, endi]), reads=[Etrue], writes=[WC[hp]])
            k.op("pool", lambda e: e.tensor_tensor(tmp[:, :nch, :], cs[:, :nch, endi:endi + 1].to_broadcast([128, nch, 64]), cs[:, :nch, :], ALU.subtract), reads=[cs], writes=[tmp])
            k.op("act", lambda e: e.activation(out=fl(E), in_=fl(tmp), func=AF.Exp, scale=s), reads=[tmp], writes=[E])
            k.op("dve", lambda e: e.tensor_tensor(fl(Kh[hp]), fl(kk_[b][hp]), fl(E), ALU.mult), reads=[kk_[b][hp], E], writes=[Kh[hp]])
            k.op("act", lambda e: e.copy(fl(vb[hp]), fl(vv[b][hp])), reads=[vv[b][hp]], writes=[vb[hp]])

        def head_block(hp, hl):
            P = slice(hl * 64, hl * 64 + 64)

            def tm(src, dst):
                pb = npsb()
                for ch in range(nch):
                    k.op("pe", lambda e, ch=ch: e.transpose(pb[P, ch, :], src[hp][P, ch, :], self.ident[P, P]), reads=[src[hp], self.ident], writes=[pb], pe_acc=True)
                k.op("act", lambda e: e.copy(dst[hp][P, :nch, :], pb[P, :nch, :]), reads=[pb], writes=[dst[hp]])
            tm(Kh, Khtm); tm(vb, Vtm)
            pp = nps()
            pv = pp[:, :].rearrange("p (c t) -> p c t", t=64)
            for ch in range(nch):
                k.op("pe", lambda e, ch=ch: e.matmul(pv[P, ch, :], lhsT=kt[hp][P, ch, :], rhs=qt[hp][P, ch, :], start=True, stop=True), reads=[kt[hp], qt[hp]], writes=[pp], pe_acc=True)
            mv = self.masks[:, mI, :].rearrange("p (c t) -> p c t", t=64)
            k.op("dve", lambda e: e.tensor_tensor(Ark[hp][P, :nch, :], pv[P, :nch, :], mv[P, :nch, :], ALU.mult), reads=[pp, self.masks], writes=[Ark[hp]])

        for hp in range(2):
            prep(hp)
            for hl in range(2):
                head_block(hp, hl)

        def seq_step(ch, hp, hl):
            P = slice(hl * 64, hl * 64 + 64)
            pyb = PY[hp][hl]
            pyv = pyb[:, :].rearrange("p (c t) -> p c t", t=64)
            k.op("pe", lambda e: e.matmul(pyv[P, ch, :], lhsT=Mbf[hp][P, :], rhs=Qtrue[hp][P, ch, :], start=True, stop=False), reads=[Mbf[hp], Qtrue[hp]], writes=[pyb], pe_acc=True)
            k.op("pe", lambda e: e.matmul(pyv[P, ch, :], lhsT=Vtm[hp][P, ch, :], rhs=Ark[hp][P, ch, :], start=False, stop=True), reads=[Vtm[hp], Ark[hp]], writes=[pyb], pe_acc=True)
            pm_ = nps()
            k.op("pe", lambda e: e.matmul(pm_[P, 0:64], lhsT=Khtm[hp][P, ch, :], rhs=Vtm[hp][P, ch, :], start=True, stop=True), reads=[Khtm[hp], Vtm[hp]], writes=[pm_])
            k.op("dve", lambda e: e.scalar_tensor_tensor(out=Mst[hp][P, :], in0=Mst[hp][P, :], scalar=WC[hp][P, ch:ch + 1], in1=pm_[P, 0:64], op0=ALU.mult, op1=ALU.add), reads=[Mst[hp], WC[hp], pm_], writes=[Mst[hp]])
            k.op("act", lambda e: e.copy(Mbf[hp][P, :], Mst[hp][P, :]), reads=[Mst[hp]], writes=[Mbf[hp]])

        chs = range(nch) if d == 0 else range(nch - 1, -1, -1)
        for ch in chs:
            for hp in range(2):
                for hl in range(2):
                    seq_step(ch, hp, hl)

        def outp(hp):
            for hl in range(2):
                P = slice(hl * 64, hl * 64 + 64)
                pp = PY[hp][hl]
                if hl == 0:
                    k.op("dve", lambda e, P=P, pp=pp: e.tensor_copy(ysb[hp][P, :n], pp[P, :n]), reads=[pp], writes=[ysb[hp]])
                else:
                    k.op("act", lambda e, P=P, pp=pp: e.copy(ysb[hp][P, :n], pp[P, :n]), reads=[pp], writes=[ysb[hp]])
            if d == 0:
                k.op("pool", lambda e: e.dma_start(out=self.gla_yf.ap[hp * 128:(hp + 1) * 128, tok0:tok0 + n], in_=ysb[hp][:, :n]), reads=[ysb[hp]], dma=True)
            else:
                yb, yf, rr = ysb[hp], yfin[hp], rin[hp]
                k.op("sp", lambda e: e.dma_start(out=yf[:, :n], in_=self.gla_yf.ap[hp * 128:(hp + 1) * 128, tok0:tok0 + n]), writes=[yf], dma=True)
                k.op("sp", lambda e: e.dma_start(out=rr[:, :n], in_=self.uT.ap[O + 528 + hp * 128:O + 528 + (hp + 1) * 128, tok0:tok0 + n]), writes=[rr], dma=True)
                k.op("dve", lambda e: e.tensor_tensor(yb[:, :n], yb[:, :n], yf[:, :n], ALU.add), reads=[yb, yf], writes=[yb])
                k.op("act", lambda e: e.activation(out=sq[:, :n], in_=yb[:, :n], func=AF.Square), reads=[yb], writes=[sq])
                p2 = nps()
                k.op("pe", lambda e: e.matmul(p2[:, :n], lhsT=self.blk[:, :], rhs=sq[:, :n], start=True, stop=True), reads=[self.blk, sq], writes=[p2])
                k.op("act", lambda e: e.activation(out=rn[:, :n], in_=p2[:, :n], func=AF.Sqrt, scale=1.0 / 64, bias=EPS), reads=[p2], writes=[rn])
                k.op("dve", lambda e: e.reciprocal(rn[:, :n], rn[:, :n]), reads=[rn], writes=[rn])
                k.op("dve", lambda e: e.scalar_tensor_tensor(out=yb[:, :n], in0=yb[:, :n], scalar=ng[:, hp:hp + 1], in1=rn[:, :n], op0=ALU.mult, op1=ALU.mult), reads=[yb, ng, rn], writes=[yb])
                k.op("act", lambda e: e.activation(out=rr[:, :n], in_=rr[:, :n], func=AF.Silu), reads=[rr], writes=[rr])
                k.op("dve", lambda e: e.tensor_tensor(yb[:, :n], yb[:, :n], rr[:, :n], ALU.mult), reads=[yb, rr], writes=[yb])
                k.op("pool", lambda e: e.dma_start(out=self.mixT.ap[768 + hp * 128:768 + (hp + 1) * 128, tok0:tok0 + n], in_=yb[:, :n]), reads=[yb], dma=True)
        for hp in range(2):
            outp(hp)

    for ti, (tok0, n, stream) in enumerate(order):
        tile_body(ti, tok0, n, stream)
    k.barrier()
    k.emit()
    k.free_to(m)


MK.phase_gla = phase_gla


SSM_OFF = 1024


def _ssm_decl(self):
    k = self.k
    NT = self.NT
    self.c_m128 = k.dram("c_m128", [5, 128, 128], F32, kind="ExternalInput")
    def S(n, s, dt=F32):
        kind = "ExternalOutput" if n in self.dbg else "Internal"
        return k.dram(n, s, dt, kind=kind)
    self.s_xtm = S("s_xtm", [NT, 768])
    self.s_ztm = S("s_ztm", [NT, 512])
    self.s_dttm = S("s_dttm", [NT, 8])
    self.s_bcT = S("s_bcT", [256, NT])
    self.s_yf = S("s_yf", [NT, 512])


def phase_ssm_conv(self, l):
    k = self.k
    m = k.mark()
    NT, T = self.NT, self.T
    cw = k.sb("cw", [128, 6, 9], F32)
    cbias = self._pvec("cbias", self.ssm_conv_b.ap[l], 6)
    for tap in range(9):
        k.op("sp", lambda e, tap=tap: e.dma_start(out=cw[:, :, tap], in_=self.ssm_conv_w.ap[l, tap // 3, tap % 3].rearrange("(c p) -> p c", p=128), allow_slow_non_contiguous=True), writes=[cw], dma=True)
    xin = [k.sb("cxin%d" % i, [128, 642], F32) for i in range(3)]
    acc = [k.sb("cacc%d" % i, [128, 512], F32) for i in range(2)]
    acc2 = [k.sb("cacc2%d" % i, [128, 512], F32) for i in range(2)]
    res = [k.sb("cres%d" % i, [128, 512], F32) for i in range(2)]
    zin = [k.sb("czin%d" % i, [128, 512], F32) for i in range(2)]
    dtin = [k.sb("cdtin%d" % i, [8, 512], F32) for i in range(2)]
    otm = [k.sb("cotm%d" % i, [128, 4, 128], F32) for i in range(2)]
    odt = [k.sb("codt%d" % i, [128, 4, 8], F32) for i in range(2)]
    PT = [k.ps("cpt%d" % i, [128, 4, 128], F32) for i in range(3)]
    pti = [0]
    cnt = [0]
    O = SSM_OFF

    def transpose_store(src, npart, dst_ap_fn, n, ob):
        pt = PT[pti[0] % 3]; pti[0] += 1
        nb = n // 128
        for tb in range(nb):
            k.op("pe", lambda e, tb=tb: e.transpose(pt[:, tb, :npart], src[:npart, tb * 128:(tb + 1) * 128], self.identf[:npart, :npart]), reads=[src, self.identf], writes=[pt], pe_acc=True)
        eng = "act" if cnt[0] % 2 else "dve"
        cnt[0] += 1
        if eng == "act":
            k.op("act", lambda e: e.copy(ob[:, :nb, :npart], pt[:, :nb, :npart]), reads=[pt], writes=[ob])
        else:
            k.op("dve", lambda e: e.tensor_copy(ob[:, :nb, :npart], pt[:, :nb, :npart]), reads=[pt], writes=[ob])
        k.op("pool", lambda e: e.dma_start(out=dst_ap_fn(nb), in_=ob[:, :nb, :npart]), reads=[ob], dma=True)

    it = [0]
    for (tok0, n, stream) in self.tok_tiles():
        seq0, seq1 = (0, CTX) if stream == 1 else (CTX, NT)
        halo = 65 if stream == 0 else 1
        for c in range(6):
            i = it[0]; it[0] += 1
            xb = xin[i % 3]; ac = acc[i % 2]; ac2 = acc2[i % 2]; rs = res[i % 2]
            lo = max(tok0 - halo, seq0); hi = min(tok0 + n + halo, seq1)
            if lo > tok0 - halo:
                k.op("pool", lambda e, xb=xb, halo=halo: e.memset(xb[:, 0:halo], 0.0), writes=[xb])
            if hi < tok0 + n + halo:
                k.op("pool", lambda e, xb=xb, halo=halo, n=n: e.memset(xb[:, halo + n:halo + n + halo], 0.0), writes=[xb])
            k.op("sp", lambda e, xb=xb, lo=lo, hi=hi, tok0=tok0, halo=halo, c=c: e.dma_start(out=xb[:, lo - (tok0 - halo):hi - (tok0 - halo)], in_=self.uT.ap[O + 512 + c * 128:O + 512 + (c + 1) * 128, lo:hi]), writes=[xb], dma=True)
            first = True
            dys = (-1, 0, 1) if stream == 0 else (0,)
            for dy in dys:
                for dx in (0, -1, 1):
                    tap = (dy + 1) * 3 + (dx + 1)
                    off = halo + dy * 64 + dx
                    if first:
                        k.op("dve", lambda e, ac=ac, xb=xb, off=off, n=n, c=c, tap=tap: e.tensor_scalar(ac[:, :n], xb[:, off:off + n], cw[:, c, tap:tap + 1], None, ALU.mult), reads=[xb, cw], writes=[ac])
                        first = False
                        continue
                    c0, c1 = (0, 64) if (dx == 0 or stream == 1) else ((1, 64) if dx == -1 else (0, 63))
                    def tapop(ac=ac, xb=xb, off=off, n=n, c=c, tap=tap, c0=c0, c1=c1):
                        src = xb[:, off:off + n].rearrange("p (r w) -> p r w", w=64)[:, :, c0:c1]
                        dst = ac[:, :n].rearrange("p (r w) -> p r w", w=64)[:, :, c0:c1]
                        k.op("dve", lambda e: e.scalar_tensor_tensor(out=dst, in0=src, scalar=cw[:, c, tap:tap + 1], in1=dst, op0=ALU.mult, op1=ALU.add), reads=[xb, cw, ac], writes=[ac])
                    tapop()
            k.op("act", lambda e, ac=ac, rs=rs, n=n, c=c: e.activation(out=rs[:, :n], in_=ac[:, :n], func=AF.Silu, bias=cbias[:, c:c + 1]), reads=[ac, cbias], writes=[rs])
            if c >= 4:
                k.op("pool", lambda e, rs=rs, n=n, c=c, tok0=tok0: e.dma_start(out=self.s_bcT.ap[(c - 4) * 128:(c - 3) * 128, tok0:tok0 + n], in_=rs[:, :n]), reads=[rs], dma=True)
            ob = otm[i % 2]
            transpose_store(rs, 128, lambda nb, c=c, tok0=tok0: self.s_xtm.ap[tok0:tok0 + nb * 128, c * 128:(c + 1) * 128].rearrange("(b p) f -> p b f", p=128), n, ob)
        for c in range(4):
            i = it[0]; it[0] += 1
            zb = zin[i % 2]
            k.op("sp", lambda e, zb=zb, c=c, tok0=tok0, n=n: e.dma_start(out=zb[:, :n], in_=self.uT.ap[O + c * 128:O + (c + 1) * 128, tok0:tok0 + n]), writes=[zb], dma=True)
            ob = otm[i % 2]
            transpose_store(zb, 128, lambda nb, c=c, tok0=tok0: self.s_ztm.ap[tok0:tok0 + nb * 128, c * 128:(c + 1) * 128].rearrange("(b p) f -> p b f", p=128), n, ob)
        i = it[0]; it[0] += 1
        db = dtin[i % 2]
        k.op("sp", lambda e, db=db, tok0=tok0, n=n: e.dma_start(out=db[:, :n], in_=self.uT.ap[O + 1280:O + 1288, tok0:tok0 + n]), writes=[db], dma=True)
        ob = odt[i % 2]
        transpose_store(db, 8, lambda nb, tok0=tok0: self.s_dttm.ap[tok0:tok0 + nb * 128, :].rearrange("(b p) f -> p b f", p=128), n, ob)
    k.barrier()
    k.emit()
    k.free_to(m)


def phase_ssm_scan(self, l, d):
    k = self.k
    m = k.mark()
    NT = self.NT
    BIG = 30000.0
    m128 = k.sb("m128", [128, 5, 128], F32)
    for i in range(5):
        k.op("sp", lambda e, i=i: e.dma_start(out=m128[:, i, :], in_=self.c_m128.ap[i]), writes=[m128], dma=True)
    LE, GE, GT, LT, NEGI = 0, 1, 2, 3, 4
    if d == 0:
        mTri, mR, mNeg = LE, GT, GT
    else:
        mTri, mR, mNeg = GE, LT, LT
    onesf = k.sb("onesf", [128, 128], F32)
    k.op("dve", lambda e: e.memset(onesf[:, :], 1.0), writes=[onesf])
    dtb = k.sb("dtb", [128, 8], F32)
    aneg = k.sb("aneg", [128, 8], F32)
    dsk = k.sb("dsk", [128, 8], F32)
    ngb = k.sb("ngb", [128, 512], F32)
    k.op("sp", lambda e: e.dma_start(out=dtb[:, :], in_=self.ssm_dt_bias.ap[l, d].partition_broadcast(128)), writes=[dtb], dma=True)
    k.op("sp", lambda e: e.dma_start(out=aneg[:, :], in_=self.ssm_a_log.ap[l, d].partition_broadcast(128)), writes=[aneg], dma=True)
    k.op("sp", lambda e: e.dma_start(out=dsk[:, :], in_=self.ssm_d.ap[l].partition_broadcast(128)), writes=[dsk], dma=True)
    k.op("sp", lambda e: e.dma_start(out=ngb[:, :], in_=self.ssm_norm_g.ap[l].partition_broadcast(128)), writes=[ngb], dma=True)
    k.op("act", lambda e: e.activation(out=aneg[:, :], in_=aneg[:, :], func=AF.Exp), reads=[aneg], writes=[aneg])
    k.op("dve", lambda e: e.tensor_scalar(aneg[:, :], aneg[:, :], -1.0, None, ALU.mult), reads=[aneg], writes=[aneg])

    xs = [k.sb("sxs%d" % i, [128, 768], F32) for i in range(2)]
    dt = [k.sb("sdt%d" % i, [128, 8], F32) for i in range(2)]
    BT = [[k.sb("sBT%d%d" % (i, g), [64, 128], F32) for g in range(2)] for i in range(2)]
    CT = [[k.sb("sCT%d%d" % (i, g), [64, 128], F32) for g in range(2)] for i in range(2)]
    BTb = [k.sb("sBTb%d" % g, [64, 128], BF16) for g in range(2)]
    CTb = [k.sb("sCTb%d" % g, [64, 128], BF16) for g in range(2)]
    Btm = k.sb("sBtm", [128, 128], BF16)
    zt = [k.sb("szt%d" % i, [128, 512], F32) for i in range(2)]
    yfin = [k.sb("syf%d" % i, [128, 512], F32) for i in range(2)]
    xdt = k.sb("sx", [128, 8], F32)
    dA = k.sb("sdA", [128, 8], F32)
    cs = k.sb("scs", [128, 8], F32)
    dte = k.sb("sdte", [128, 8], F32)
    ecs = k.sb("secs", [128, 8], F32)
    eend = k.sb("seend", [128, 8], F32)
    dtp = k.sb("sdtp", [128, 8], F32)
    Xd = k.sb("sXd", [128, 8, 64], BF16)
    Xdd = k.sb("sXdd", [128, 8, 64], BF16)
    Rm = [k.sb("sRm%d" % i, [128, 128], F32) for i in range(4)]
    Lt = k.sb("sLt", [128, 8, 128], BF16)
    sc = k.sb("ssc", [128, 2, 128], BF16)
    Gt = k.sb("sGt", [128, 8, 128], BF16)
    Yt = k.sb("sYt", [128, 512], F32)
    Y = k.sb("sY", [128, 512], F32)
    junk = k.sb("sjunk", [128, 512], BF16)
    ss = k.sb("sss", [128, 1], F32)
    Ybf = k.sb("sYbf", [128, 512], BF16)
    ofm = k.sb("sofm", [128, 4, 128], F32)
    Sst = k.sb("sSst", [64, 8, 64], F32)
    Stmp = k.sb("sStmp", [64, 8, 64], F32)
    Sbf = k.sb("sSbf", [64, 8, 64], BF16)
    k.op("dve", lambda e: e.memset(Sst[:, :, :], 0.0), writes=[Sst])
    k.op("dve", lambda e: e.memset(Sbf[:, :, :], 0.0), writes=[Sbf])
    pcs = k.ps("spcs", [128, 2, 8], F32)
    pD = [k.ps("spD%d" % i, [128, 4, 128], F32) for i in range(2)]
    psc = k.ps("spsc", [128, 2, 128], F32)
    pYd = k.ps("spYd", [128, 512], F32)
    pYo = k.ps("spYo", [128, 512], F32)
    pSt = k.ps("spSt", [64, 512], F32)
    pT = k.ps("spT", [128, 4, 128], BF16)

    nchunks = NT // 128
    ctxc = [0, 1]
    latc = list(range(2, nchunks))
    order = (ctxc + latc) if d == 0 else (ctxc[::-1] + latc[::-1])
    ri = [0]

    def chunk(ci, c):
        b = ci % 2
        t0 = c * 128
        x_, dt_, z_, yf_ = xs[b], dt[b], zt[b], yfin[b]
        k.op("sp", lambda e: e.dma_start(out=x_[:, :], in_=self.s_xtm.ap[t0:t0 + 128, :]), writes=[x_], dma=True)
        k.op("sp", lambda e: e.dma_start(out=dt_[:, :], in_=self.s_dttm.ap[t0:t0 + 128, :]), writes=[dt_], dma=True)
        for g in range(2):
            k.op("sp", lambda e, g=g: e.dma_start(out=BT[b][g][:, :], in_=self.s_bcT.ap[g * 64:(g + 1) * 64, t0:t0 + 128]), writes=[BT[b][g]], dma=True)
            k.op("sp", lambda e, g=g: e.dma_start(out=CT[b][g][:, :], in_=self.s_bcT.ap[128 + g * 64:128 + (g + 1) * 64, t0:t0 + 128]), writes=[CT[b][g]], dma=True)
        if d == 1:
            k.op("sp", lambda e: e.dma_start(out=z_[:, :], in_=self.s_ztm.ap[t0:t0 + 128, :]), writes=[z_], dma=True)
            k.op("sp", lambda e: e.dma_start(out=yf_[:, :], in_=self.s_yf.ap[t0:t0 + 128, :]), writes=[yf_], dma=True)
        for g in range(2):
            k.op("act", lambda e, g=g: e.copy(BTb[g][:, :], BT[b][g][:, :]), reads=[BT[b][g]], writes=[BTb[g]])
            k.op("act", lambda e, g=g: e.copy(CTb[g][:, :], CT[b][g][:, :]), reads=[CT[b][g]], writes=[CTb[g]])
        k.op("act", lambda e: e.copy(Btm[:, :], x_[:, 512:640]), reads=[x_], writes=[Btm])
        k.op("dve", lambda e: e.tensor_tensor(xdt[:, :], dt_[:, :], dtb[:, :], ALU.add), reads=[dt_, dtb], writes=[xdt])
        k.op("act", lambda e: e.activation(out=xdt[:, :], in_=xdt[:, :], func=AF.Exp), reads=[xdt], writes=[xdt])
        k.op("act", lambda e: e.activation(out=dtp[:, :], in_=xdt[:, :], func=AF.Ln, bias=1.0), reads=[xdt], writes=[dtp])
        k.op("dve", lambda e: e.tensor_tensor(dA[:, :], dtp[:, :], aneg[:, :], ALU.mult), reads=[dtp, aneg], writes=[dA])
        k.op("pe", lambda e: e.matmul(pcs[:, 0, :], lhsT=m128[:, mTri, :], rhs=dA[:, :], start=True, stop=True), reads=[m128, dA], writes=[pcs])
        k.op("pe", lambda e: e.matmul(pcs[:, 1, :], lhsT=onesf[:, :], rhs=dA[:, :], start=True, stop=True), reads=[onesf, dA], writes=[pcs], pe_acc=True)
        k.op("dve", lambda e: e.tensor_copy(cs[:, :], pcs[:, 0, :]), reads=[pcs], writes=[cs])
        k.op("act", lambda e: e.activation(out=ecs[:, :], in_=pcs[:, 0, :], func=AF.Exp), reads=[pcs], writes=[ecs])
        k.op("act", lambda e: e.activation(out=eend[:, :], in_=pcs[:, 1, :], func=AF.Exp), reads=[pcs], writes=[eend])
        k.op("dve", lambda e: e.tensor_tensor(dte[:, :], pcs[:, 1, :], cs[:, :], ALU.subtract), reads=[pcs, cs], writes=[dte])
        k.op("act", lambda e: e.activation(out=dte[:, :], in_=dte[:, :], func=AF.Exp), reads=[dte], writes=[dte])
        xv = x_[:, 0:512].rearrange("p (h e) -> p h e", e=64)
        k.op("dve", lambda e: e.tensor_tensor(Xd[:, :, :], xv, dtp[:, :].unsqueeze(2).to_broadcast([128, 8, 64]), ALU.mult), reads=[x_, dtp], writes=[Xd])
        k.op("pool", lambda e: e.tensor_tensor(Xdd[:, :, :], Xd[:, :, :], dte[:, :].unsqueeze(2).to_broadcast([128, 8, 64]), ALU.mult), reads=[Xd, dte], writes=[Xdd])
        for h in range(8):
            r_ = Rm[ri[0] % 4]; ri[0] += 1
            pd = pD[h // 4]
            k.op("dve" if h % 2 == 0 else "pool", lambda e, h=h, r_=r_: e.tensor_tensor(r_[:, :], m128[:, mR, :], dA[:, h:h + 1].to_broadcast([128, 128]), ALU.mult), reads=[m128, dA], writes=[r_])
            k.op("pe", lambda e, h=h, r_=r_, pd=pd: e.matmul(pd[:, h % 4, :], lhsT=r_[:, :], rhs=m128[:, mTri, :], start=True, stop=False), reads=[r_, m128], writes=[pd], pe_acc=True)
            k.op("pe", lambda e, h=h, pd=pd: e.matmul(pd[:, h % 4, :], lhsT=m128[:, NEGI, :], rhs=m128[:, mNeg, :], start=False, stop=True), reads=[m128], writes=[pd], pe_acc=True)
        for hh in range(2):
            k.op("act", lambda e, hh=hh: e.activation(out=Lt[:, hh * 4:(hh + 1) * 4, :], in_=pD[hh][:, :, :], func=AF.Exp), reads=[pD[hh]], writes=[Lt])
        for g in range(2):
            k.op("pe", lambda e, g=g: e.matmul(psc[:, g, :], lhsT=BTb[g][:, :], rhs=CTb[g][:, :], start=True, stop=True), reads=[BTb[g], CTb[g]], writes=[psc], pe_acc=True)
        k.op("dve", lambda e: e.tensor_copy(sc[:, :, :], psc[:, :, :]), reads=[psc], writes=[sc])
        for g in range(2):
            k.op("dve" if g == 0 else "pool", lambda e, g=g: e.tensor_tensor(Gt[:, g * 4:(g + 1) * 4, :], Lt[:, g * 4:(g + 1) * 4, :], sc[:, g:g + 1, :].to_broadcast([128, 4, 128]), ALU.mult), reads=[Lt, sc], writes=[Gt])
        for h in range(8):
            k.op("pe", lambda e, h=h: e.matmul(pYd[:, h * 64:(h + 1) * 64], lhsT=Gt[:, h, :], rhs=Xd[:, h, :], start=True, stop=True), reads=[Gt, Xd], writes=[pYd], pe_acc=True)
        for g in range(2):
            k.op("pe", lambda e, g=g: e.matmul(pYo[:, g * 256:(g + 1) * 256], lhsT=CTb[g][:, :], rhs=Sbf[:, g * 4:(g + 1) * 4, :].rearrange("p h e -> p (h e)"), start=True, stop=True), reads=[CTb[g], Sbf], writes=[pYo], pe_acc=True)
        k.op("dve", lambda e: e.tensor_tensor(Yt[:, :].rearrange("p (h e) -> p h e", e=64), pYo[:, :].rearrange("p (h e) -> p h e", e=64), ecs[:, :].unsqueeze(2).to_broadcast([128, 8, 64]), ALU.mult), reads=[pYo, ecs], writes=[Yt])
        k.op("dve", lambda e: e.tensor_tensor(Y[:, :], Yt[:, :], pYd[:, :], ALU.add), reads=[Yt, pYd], writes=[Y])
        for g in range(2):
            k.op("pe", lambda e, g=g: e.matmul(pSt[:, g * 256:(g + 1) * 256], lhsT=Btm[:, g * 64:(g + 1) * 64], rhs=Xdd[:, g * 4:(g + 1) * 4, :].rearrange("p h e -> p (h e)"), start=True, stop=True), reads=[Btm, Xdd], writes=[pSt], pe_acc=True)
        k.op("pool", lambda e: e.tensor_tensor(Stmp[:, :, :], Sst[:, :, :], eend[0:64, :].unsqueeze(2).to_broadcast([64, 8, 64]), ALU.mult), reads=[Sst, eend], writes=[Stmp])
        k.op("dve", lambda e: e.tensor_tensor(Sst[:, :, :], Stmp[:, :, :], pSt[:, :].rearrange("p (h e) -> p h e", e=64), ALU.add), reads=[Stmp, pSt], writes=[Sst])
        k.op("act", lambda e: e.copy(Sbf[:, :, :], Sst[:, :, :]), reads=[Sst], writes=[Sbf])
        if d == 0:
            k.op("pool", lambda e: e.dma_start(out=self.s_yf.ap[t0:t0 + 128, :], in_=Y[:, :]), reads=[Y], dma=True)
        else:
            k.op("dve", lambda e: e.tensor_tensor(Y[:, :], Y[:, :], yf_[:, :], ALU.add), reads=[Y, yf_], writes=[Y])
            k.op("pool", lambda e: e.tensor_tensor(Yt[:, :].rearrange("p (h e) -> p h e", e=64), xv, dsk[:, :].unsqueeze(2).to_broadcast([128, 8, 64]), ALU.mult), reads=[x_, dsk], writes=[Yt])
            k.op("dve", lambda e: e.tensor_tensor(Y[:, :], Y[:, :], Yt[:, :], ALU.add), reads=[Y, Yt], writes=[Y])
            k.op("act", lambda e: e.activation(out=z_[:, :], in_=z_[:, :], func=AF.Silu), reads=[z_], writes=[z_])
            k.op("dve", lambda e: e.tensor_tensor(Y[:, :], Y[:, :], z_[:, :], ALU.mult), reads=[Y, z_], writes=[Y])
            k.op("act", lambda e: e.activation(out=junk[:, :], in_=Y[:, :], func=AF.Square, accum_out=ss[:, 0:1]), reads=[Y], writes=[junk, ss])
            k.op("act", lambda e: e.activation(out=ss[:, 0:1], in_=ss[:, 0:1], func=AF.Sqrt, scale=1.0 / 512, bias=EPS), reads=[ss], writes=[ss])
            k.op("dve", lambda e: e.reciprocal(ss[:, 0:1], ss[:, 0:1]), reads=[ss], writes=[ss])
            k.op("dve", lambda e: e.scalar_tensor_tensor(out=Ybf[:, :], in0=Y[:, :], scalar=ss[:, 0:1], in1=ngb[:, :], op0=ALU.mult, op1=ALU.mult), reads=[Y, ss, ngb], writes=[Ybf])
            for j in range(4):
                k.op("pe", lambda e, j=j: e.transpose(pT[:, j, :], Ybf[:, j * 128:(j + 1) * 128], self.ident[:, :]), reads=[Ybf, self.ident], writes=[pT], pe_acc=True)
            k.op("act", lambda e: e.copy(ofm[:, :, :], pT[:, :, :]), reads=[pT], writes=[ofm])
            k.op("pool", lambda e: e.dma_start(out=self.mixT.ap[256:768, t0:t0 + 128].rearrange("(j p) t -> p j t", p=128), in_=ofm[:, :, :]), reads=[ofm], dma=True)

    for ci, c in enumerate(order):
        chunk(ci, c)
    k.barrier()
    k.emit()
    k.free_to(m)


MK.ssm_decl = _ssm_decl
MK.phase_ssm_conv = phase_ssm_conv
MK.phase_ssm_scan = phase_ssm_scan


def _moe_decl(self):
    k = self.k
    NT = self.NT
    def S(n, s, dt=F32):
        kind = "ExternalOutput" if n in self.dbg else "Internal"
        return k.dram(n, s, dt, kind=kind)
    self.h2T = S("h2T", [D, NT], BF16)
    self.combT = S("combT", [16, NT])
    self.c_sel = k.dram("c_sel", [16, 16, 128], F32, kind="ExternalInput")


def phase_outproj(self, l, lat_only):
    k = self.k
    m = k.mark()
    NT = self.NT
    wout = k.sb("wout", [128, 8, D], BF16)
    for c in range(8):
        k.op("pool", lambda e, c=c: e.dma_start(out=wout[:, c, :], in_=self.w_out.ap[l, c * 128:(c + 1) * 128, :]), writes=[wout], dma=True)
    wr = k.sb("wr", [128, 8, 20], F32)
    k.op("sp", lambda e: e.dma_start(out=wr[:, :, 0:4], in_=self.moe_rg_w.ap[l].rearrange("(c p) g -> p c g", p=128)), writes=[wr], dma=True)
    k.op("sp", lambda e: e.dma_start(out=wr[:, :, 4:20], in_=self.moe_re_w.ap[l].rearrange("(c p) g -> p c g", p=128)), writes=[wr], dma=True)
    rb = k.sb("rb", [128, 20], F32)
    k.op("sp", lambda e: e.dma_start(out=rb[:, 0:4], in_=self.moe_rg_b.ap[l].partition_broadcast(128)), writes=[rb], dma=True)
    k.op("sp", lambda e: e.dma_start(out=rb[:, 4:20], in_=self.moe_re_b.ap[l].partition_broadcast(128)), writes=[rb], dma=True)
    mixf = [k.sb("mixf%d" % i, [128, 8, 512], F32) for i in range(2)]
    mixb = k.sb("mixb", [128, 8, 512], BF16)
    xt = [k.sb("oxt%d" % i, [128, 8, 512], F32) for i in range(2)]
    sq = k.sb("osq", [128, 8, 512], BF16)
    rstd = k.sb("orstd", [128, 512], F32)
    hT = k.sb("ohT", [128, 8, 512], BF16)
    pss = k.ps("opss", [128, 512], F32)
    po = [k.ps("opo%d" % i, [128, 512], F32) for i in range(3)]
    plg = k.ps("oplg", [128, 4, 20], F32)
    pct = k.ps("opct", [16, 4, 128], F32)
    R_ = lambda n_, s_: k.sb(n_, [128] + s_, F32)
    lg = R_("rlg", [4, 20]); gmax = R_("rgmax", [4, 1]); ohg = R_("rohg", [4, 4]); eg = R_("reg", [4, 4]); gsum = R_("rgsum", [4, 1])
    pgr = R_("rpgr", [4, 1]); esel3 = R_("resel3", [4, 4, 4]); esel = R_("resel", [4, 4]); m1 = R_("rm1", [4, 1]); oh1 = R_("roh1", [4, 4])
    es2 = R_("res2", [4, 4]); m2 = R_("rm2", [4, 1]); oh2 = R_("roh2", [4, 4]); ex2 = R_("rex2", [4, 1]); den = R_("rden", [4, 1])
    w1_ = R_("rw1", [4, 1]); w2_ = R_("rw2", [4, 1]); cig = R_("rcig", [4, 4]); comb = R_("rcomb", [4, 4, 4]); tmp4 = R_("rtmp4", [4, 4])
    combT_sb = k.sb("combT_sb", [16, 4, 128], F32)
    xTv = self.xT.ap.rearrange("(c p) t -> p c t", p=128)
    mTv = self.mixT.ap.rearrange("(c p) t -> p c t", p=128)
    hTv = self.h2T.ap.rearrange("(c p) t -> p c t", p=128)
    pi = [0]

    def tile_body(ti, tok0, n, stream):
        b = ti % 2
        mf, x_ = mixf[b], xt[b]
        k.op("sp", lambda e: e.dma_start(out=mf[:, :, :n], in_=mTv[:, :, tok0:tok0 + n]), writes=[mf], dma=True)
        k.op("sp", lambda e: e.dma_start(out=x_[:, :, :n], in_=xTv[:, :, tok0:tok0 + n]), writes=[x_], dma=True)
        k.op("act", lambda e: e.copy(mixb[:, 0:4, :n], mf[:, 0:4, :n]), reads=[mf], writes=[mixb])
        k.op("pool", lambda e: e.tensor_copy(mixb[:, 4:8, :n], mf[:, 4:8, :n]), reads=[mf], writes=[mixb])
        for j in range(8):
            p_ = po[pi[0] % 3]; pi[0] += 1
            for c in range(8):
                k.op("pe", lambda e, c=c, j=j, p_=p_: e.matmul(p_[:, :n], lhsT=wout[:, c, j * 128:(j + 1) * 128], rhs=mixb[:, c, :n], start=(c == 0), stop=(c == 7)), reads=[wout, mixb], writes=[p_], pe_acc=True)
            k.op("dve", lambda e, j=j, p_=p_: e.scalar_tensor_tensor(out=x_[:, j, :n], in0=p_[:, :n], scalar=self.modT[:, l, 16 + j, stream:stream + 1], in1=x_[:, j, :n], op0=ALU.mult, op1=ALU.add), reads=[p_, self.modT, x_], writes=[x_])
        k.op("pool", lambda e: e.dma_start(out=xTv[:, :, tok0:tok0 + n], in_=x_[:, :, :n]), reads=[x_], dma=True)
        self.norm_tile(l, 1, tok0, n, stream, x_, sq, pss, rstd, hT, keep_f32=True)
        k.op("pool", lambda e: e.dma_start(out=hTv[:, :, tok0:tok0 + n], in_=hT[:, :, :n]), reads=[hT], dma=True)
        nb = n // 128
        for tb in range(nb):
            for c in range(8):
                k.op("pe", lambda e, tb=tb, c=c: e.matmul(plg[:, tb, :], lhsT=x_[:, c, tb * 128:(tb + 1) * 128], rhs=wr[:, c, :], start=(c == 0), stop=(c == 7)), reads=[x_, wr], writes=[plg], pe_acc=True)
        V = lambda t_, *idx: t_[(slice(None), slice(0, nb)) + idx]
        op = lambda fn, r, w: k.op("dve", fn, reads=r, writes=w)
        op(lambda e: e.tensor_tensor(lg[:, :nb, :], plg[:, :nb, :], rb[:, :].unsqueeze(1).to_broadcast([128, nb, 20]), ALU.add), [plg, rb], [lg])
        op(lambda e: e.tensor_reduce(gmax[:, :nb, :], lg[:, :nb, 0:4], AX.X, ALU.max), [lg], [gmax])
        op(lambda e: e.tensor_tensor(ohg[:, :nb, :], lg[:, :nb, 0:4], gmax[:, :nb, :].to_broadcast([128, nb, 4]), ALU.is_ge), [lg, gmax], [ohg])
        op(lambda e: e.tensor_tensor(eg[:, :nb, :], lg[:, :nb, 0:4], gmax[:, :nb, :].to_broadcast([128, nb, 4]), ALU.subtract), [lg, gmax], [eg])
        k.op("act", lambda e: e.activation(out=eg[:, :nb, :], in_=eg[:, :nb, :], func=AF.Exp), reads=[eg], writes=[eg])
        op(lambda e: e.tensor_reduce(gsum[:, :nb, :], eg[:, :nb, :], AX.X, ALU.add), [eg], [gsum])
        op(lambda e: e.reciprocal(pgr[:, :nb, :], gsum[:, :nb, :]), [gsum], [pgr])
        elv = lg[:, :nb, 4:20].rearrange("p b (g e) -> p b g e", e=4)
        op(lambda e: e.tensor_tensor(esel3[:, :nb, :, :], elv, ohg[:, :nb, :].unsqueeze(3).to_broadcast([128, nb, 4, 4]), ALU.mult), [lg, ohg], [esel3])
        op(lambda e: e.tensor_reduce(esel[:, :nb, :], esel3[:, :nb, :, :].rearrange("p b g e -> p b e g"), AX.X, ALU.add), [esel3], [esel])
        op(lambda e: e.tensor_reduce(m1[:, :nb, :], esel[:, :nb, :], AX.X, ALU.max), [esel], [m1])
        op(lambda e: e.tensor_tensor(oh1[:, :nb, :], esel[:, :nb, :], m1[:, :nb, :].to_broadcast([128, nb, 4]), ALU.is_ge), [esel, m1], [oh1])
        op(lambda e: e.scalar_tensor_tensor(out=es2[:, :nb, :], in0=oh1[:, :nb, :], scalar=-1e30, in1=esel[:, :nb, :], op0=ALU.mult, op1=ALU.add), [oh1, esel], [es2])
        op(lambda e: e.tensor_reduce(m2[:, :nb, :], es2[:, :nb, :], AX.X, ALU.max), [es2], [m2])
        op(lambda e: e.tensor_tensor(oh2[:, :nb, :], es2[:, :nb, :], m2[:, :nb, :].to_broadcast([128, nb, 4]), ALU.is_ge), [es2, m2], [oh2])
        op(lambda e: e.tensor_tensor(ex2[:, :nb, :], m2[:, :nb, :], m1[:, :nb, :], ALU.subtract), [m2, m1], [ex2])
        k.op("act", lambda e: e.activation(out=ex2[:, :nb, :], in_=ex2[:, :nb, :], func=AF.Exp), reads=[ex2], writes=[ex2])
        op(lambda e: e.tensor_scalar(den[:, :nb, :], ex2[:, :nb, :], 1.0, None, ALU.add), [ex2], [den])
        op(lambda e: e.reciprocal(den[:, :nb, :], den[:, :nb, :]), [den], [den])
        op(lambda e: e.tensor_tensor(w1_[:, :nb, :], den[:, :nb, :], pgr[:, :nb, :], ALU.mult), [den, pgr], [w1_])
        op(lambda e: e.tensor_tensor(w2_[:, :nb, :], w1_[:, :nb, :], ex2[:, :nb, :], ALU.mult), [w1_, ex2], [w2_])
        op(lambda e: e.tensor_tensor(cig[:, :nb, :], oh1[:, :nb, :], w1_[:, :nb, :].to_broadcast([128, nb, 4]), ALU.mult), [oh1, w1_], [cig])
        op(lambda e: e.tensor_tensor(tmp4[:, :nb, :], oh2[:, :nb, :], w2_[:, :nb, :].to_broadcast([128, nb, 4]), ALU.mult), [oh2, w2_], [tmp4])
        op(lambda e: e.tensor_tensor(cig[:, :nb, :], cig[:, :nb, :], tmp4[:, :nb, :], ALU.add), [cig, tmp4], [cig])
        for g in range(4):
            op(lambda e, g=g: e.tensor_tensor(comb[:, :nb, g, :], cig[:, :nb, :], ohg[:, :nb, g:g + 1].to_broadcast([128, nb, 4]), ALU.mult), [cig, ohg], [comb])
        for tb in range(nb):
            k.op("pe", lambda e, tb=tb: e.transpose(pct[:, tb, :], comb[:, tb, :, :].rearrange("p g e -> p (g e)"), self.identf[:, :]), reads=[comb, self.identf], writes=[pct], pe_acc=True)
        k.op("act", lambda e: e.copy(combT_sb[:, :nb, :], pct[:, :nb, :]), reads=[pct], writes=[combT_sb])
        k.op("pool", lambda e: e.dma_start(out=self.combT.ap[:, tok0:tok0 + n].rearrange("k (b t) -> k b t", t=128), in_=combT_sb[:, :nb, :]), reads=[combT_sb], dma=True)

    for ti, (tok0, n, stream) in enumerate(self.tok_tiles(lat_only=lat_only)):
        tile_body(ti, tok0, n, stream)
    k.barrier()
    k.emit()
    k.free_to(m)


def phase_moe(self, l, lat_only):
    k = self.k
    m = k.mark()
    NT, T = self.NT, self.T
    TTL = min(2048, T)
    tiles = []
    start = CTX if lat_only else 0
    first = True
    t = start
    while t < NT:
        if first and not lat_only:
            n = CTX + TTL
        else:
            n = TTL
        n = min(n, NT - t)
        tiles.append((t, n))
        t += n
        first = False
    TTmax = max(n for _, n in tiles)
    acc = k.sb("macc", [128, 8, TTmax], F32)
    h2 = k.sb("mh2", [128, 8, TTmax], BF16)
    cTb = k.sb("mcTb", [16, TTmax], BF16)
    w1 = [k.sb("mw1%d" % i, [128, 8, 512], BF16) for i in range(2)]
    w3 = [k.sb("mw3%d" % i, [128, 8, 512], BF16) for i in range(2)]
    w2 = [k.sb("mw2%d" % i, [128, 4, D], BF16) for i in range(2)]
    self_sel = k.sb("msel", [16, 16, 128], BF16)
    k.op("pool", lambda e: e.dma_start(out=self_sel[:, :, :], in_=self.c_sel.ap.rearrange("e k m -> k e m")), writes=[self_sel], dma=True)
    sa = [k.sb("msa%d" % i, [128, 512], BF16) for i in range(2)]
    sa2 = [k.sb("msa2%d" % i, [128, 512], BF16) for i in range(2)]
    hid = [k.sb("mhid%d" % i, [128, 4, 512], BF16) for i in range(2)]
    cb = [k.sb("mcb%d" % i, [128, 512], BF16) for i in range(2)]
    xres = [k.sb("mxres%d" % i, [128, 512], F32) for i in range(2)]
    pa = [k.ps("mpa%d" % i, [128, 512], F32) for i in range(2)]
    pb = [k.ps("mpb%d" % i, [128, 512], F32) for i in range(2)]
    po = [k.ps("mpo%d" % i, [128, 512], F32) for i in range(3)]
    pcb = k.ps("mpcb", [128, 512], F32)
    hTv = self.h2T.ap.rearrange("(c p) t -> p c t", p=128)
    xTv = self.xT.ap.rearrange("(c p) t -> p c t", p=128)
    cnt = {"ab": 0, "o": 0, "h": 0, "w": 0, "s": 0}

    def blocks(t0, n):
        bl = []
        o = 0
        if t0 < CTX:
            bl.append((0, CTX, 1)); o = CTX
        while o < n:
            s_ = min(512, n - o)
            bl.append((o, s_, 0)); o += s_
        return bl

    for (t0, n) in tiles:
        for c in range(8):
            k.op("sp", lambda e, c=c, t0=t0, n=n: e.dma_start(out=h2[:, c, :n], in_=hTv[:, c, t0:t0 + n]), writes=[h2], dma=True)
        k.op("pool", lambda e, t0=t0, n=n: e.dma_start(out=cTb[:, :n], in_=self.combT.ap[:, t0:t0 + n]), writes=[cTb], dma=True)
        bl = blocks(t0, n)
        pending = [None]
        for ex in range(16):
            wb = cnt["w"] % 2; cnt["w"] += 1
            W1, W3, W2 = w1[wb], w3[wb], w2[wb]
            for c in range(8):
                k.op("pool", lambda e, c=c, ex=ex, W1=W1: e.dma_start(out=W1[:, c, :], in_=self.moe_w1.ap[l, ex, c * 128:(c + 1) * 128, :]), writes=[W1], dma=True)
                k.op("pool", lambda e, c=c, ex=ex, W3=W3: e.dma_start(out=W3[:, c, :], in_=self.moe_w3.ap[l, ex, c * 128:(c + 1) * 128, :]), writes=[W3], dma=True)
            for c in range(4):
                k.op("pool", lambda e, c=c, ex=ex, W2=W2: e.dma_start(out=W2[:, c, :], in_=self.moe_w2.ap[l, ex, c * 128:(c + 1) * 128, :]), writes=[W2], dma=True)
            pend = None

            def up(o, s_, stream, W1, W3, ex):
                cbb = cb[cnt["s"] % 2]; cnt["s"] += 1
                k.op("pe", lambda e: e.matmul(pcb[:, :s_], lhsT=self_sel[:, ex, :], rhs=cTb[:, o:o + s_], start=True, stop=True), reads=[self_sel, cTb], writes=[pcb])
                k.op("act", lambda e: e.copy(cbb[:, :s_], pcb[:, :s_]), reads=[pcb], writes=[cbb])
                hd = hid[cnt["h"] % 2]; cnt["h"] += 1
                for fc in range(4):
                    i = cnt["ab"] % 2; cnt["ab"] += 1
                    pa_, pb_, sa_, sa2_ = pa[i], pb[i], sa[i], sa2[i]
                    for c in range(8):
                        k.op("pe", lambda e, c=c, fc=fc, pa_=pa_: e.matmul(pa_[:, :s_], lhsT=W1[:, c, fc * 128:(fc + 1) * 128], rhs=h2[:, c, o:o + s_], start=(c == 0), stop=(c == 7)), reads=[W1, h2], writes=[pa_], pe_acc=True)
                    for c in range(8):
                        k.op("pe", lambda e, c=c, fc=fc, pb_=pb_: e.matmul(pb_[:, :s_], lhsT=W3[:, c, fc * 128:(fc + 1) * 128], rhs=h2[:, c, o:o + s_], start=(c == 0), stop=(c == 7)), reads=[W3, h2], writes=[pb_], pe_acc=True)
                    k.op("act", lambda e, pa_=pa_, sa_=sa_: e.activation(out=sa_[:, :s_], in_=pa_[:, :s_], func=AF.Silu), reads=[pa_], writes=[sa_])
                    k.op("pool", lambda e, sa_=sa_, sa2_=sa2_: e.tensor_tensor(sa2_[:, :s_], sa_[:, :s_], cbb[:, :s_], ALU.mult), reads=[sa_, cbb], writes=[sa2_])
                    k.op("dve", lambda e, sa2_=sa2_, pb_=pb_, fc=fc: e.tensor_tensor(hd[:, fc, :s_], sa2_[:, :s_], pb_[:, :s_], ALU.mult), reads=[sa2_, pb_], writes=[hd])
                return hd

            def down(o, s_, hd, W2_, ex_):
                for j in range(8):
                    po_ = po[cnt["o"] % 3]; cnt["o"] += 1
                    for fc in range(4):
                        k.op("pe", lambda e, fc=fc, j=j, po_=po_: e.matmul(po_[:, :s_], lhsT=W2_[:, fc, j * 128:(j + 1) * 128], rhs=hd[:, fc, :s_], start=(fc == 0), stop=(fc == 3)), reads=[W2_, hd], writes=[po_], pe_acc=True)
                    if ex_ == 0:
                        k.op("dve", lambda e, j=j, po_=po_: e.tensor_copy(acc[:, j, o:o + s_], po_[:, :s_]), reads=[po_], writes=[acc])
                    else:
                        k.op("dve", lambda e, j=j, po_=po_: e.tensor_tensor(acc[:, j, o:o + s_], acc[:, j, o:o + s_], po_[:, :s_], ALU.add), reads=[po_, acc], writes=[acc])

            for (o, s_, stream) in bl:
                hd = up(o, s_, stream, W1, W3, ex)
                if pending[0] is not None:
                    down(*pending[0])
                pending[0] = (o, s_, hd, W2, ex)
        if pending[0] is not None:
            down(*pending[0])
            pending[0] = None
        ri = 0
        for (o, s_, stream) in bl:
            for j in range(8):
                xr = xres[ri % 2]; ri += 1
                k.op("sp", lambda e, xr=xr, o=o, s_=s_, t0=t0, j=j: e.dma_start(out=xr[:, :s_], in_=xTv[:, j, t0 + o:t0 + o + s_]), writes=[xr], dma=True)
                k.op("dve", lambda e, j=j, xr=xr, o=o, s_=s_, stream=stream: e.scalar_tensor_tensor(out=xr[:, :s_], in0=acc[:, j, o:o + s_], scalar=self.modT[:, l, 40 + j, stream:stream + 1], in1=xr[:, :s_], op0=ALU.mult, op1=ALU.add), reads=[acc, self.modT, xr], writes=[xr])
                k.op("pool", lambda e, xr=xr, o=o, s_=s_, t0=t0, j=j: e.dma_start(out=xTv[:, j, t0 + o:t0 + o + s_], in_=xr[:, :s_]), reads=[xr], dma=True)
    k.barrier()
    k.emit()
    k.free_to(m)


def phase_final(self):
    k = self.k
    m = k.mark()
    fg = self._pvec("fg", self.final_g.ap, 8)
    xt = [k.sb("fxt%d" % i, [128, 8, 512], F32) for i in range(2)]
    sq = k.sb("fsq", [128, 8, 512], BF16)
    rstd = k.sb("frstd", [128, 512], F32)
    pss = k.ps("fpss", [128, 512], F32)
    pt = [k.ps("fpt%d" % i, [128, 4, 128], F32) for i in range(2)]
    ot = [k.sb("fot%d" % i, [128, 8, 128], F32) for i in range(2)]
    xTv = self.xT.ap.rearrange("(c p) t -> p c t", p=128)
    cnt = [0]
    for ti, (tok0, n, stream) in enumerate(self.tok_tiles(lat_only=True)):
        x_ = xt[ti % 2]
        k.op("sp", lambda e, x_=x_, tok0=tok0, n=n: e.dma_start(out=x_[:, :, :n], in_=xTv[:, :, tok0:tok0 + n]), writes=[x_], dma=True)
        k.op("act", lambda e, x_=x_, n=n: e.activation(out=sq[:, :, :n], in_=x_[:, :, :n], func=AF.Square), reads=[x_], writes=[sq])
        for c in range(8):
            k.op("pe", lambda e, c=c, n=n: e.matmul(pss[:, :n], lhsT=self.ones[:, :], rhs=sq[:, c, :n], start=(c == 0), stop=(c == 7)), reads=[sq, self.ones], writes=[pss], pe_acc=True)
        k.op("act", lambda e, n=n: e.activation(out=rstd[:, :n], in_=pss[:, :n], func=AF.Sqrt, scale=1.0 / D, bias=EPS), reads=[pss], writes=[rstd])
        k.op("dve", lambda e, n=n: e.reciprocal(rstd[:, :n], rstd[:, :n]), reads=[rstd], writes=[rstd])
        k.op("dve", lambda e, x_=x_, n=n: e.tensor_tensor(x_[:, :, :n], x_[:, :, :n], rstd[:, :n].unsqueeze(1).to_broadcast([128, 8, n]), ALU.mult), reads=[x_, rstd], writes=[x_])
        k.op("pool", lambda e, x_=x_, n=n: e.tensor_tensor(x_[:, :, :n], x_[:, :, :n], fg[:, :].unsqueeze(2).to_broadcast([128, 8, n]), ALU.mult), reads=[x_, fg], writes=[x_])
        for tb in range(n // 128):
            o_ = ot[cnt[0] % 2]; cnt[0] += 1
            for hh in range(2):
                for c in range(4):
                    cc = hh * 4 + c
                    k.op("pe", lambda e, hh=hh, c=c, cc=cc, x_=x_, tb=tb: e.transpose(pt[hh][:, c, :], x_[:, cc, tb * 128:(tb + 1) * 128], self.identf[:, :]), reads=[x_, self.identf], writes=[pt[hh]], pe_acc=True)
                if hh == 0:
                    k.op("dve", lambda e, o_=o_, hh=hh: e.tensor_copy(o_[:, 0:4, :], pt[0][:, :, :]), reads=[pt[0]], writes=[o_])
                else:
                    k.op("act", lambda e, o_=o_, hh=hh: e.copy(o_[:, 4:8, :], pt[1][:, :, :]), reads=[pt[1]], writes=[o_])
            tt = tok0 - CTX + tb * 128
            k.op("pool", lambda e, o_=o_, tt=tt: e.dma_start(out=self.out.ap[tt:tt + 128, :], in_=o_[:, :, :].rearrange("p c f -> p (c f)")), reads=[o_], dma=True)
    k.barrier()
    k.emit()
    k.free_to(m)


MK.moe_decl = _moe_decl
MK.phase_outproj = phase_outproj
MK.phase_moe = phase_moe
MK.phase_final = phase_final


def build_all(T, dbg=None, L=2):
    mk_ = MK(T, L=L, dbg=dbg)
    mk_.consts(); mk_.phase_mod(); mk_.phase_x_in()
    for l in range(L):
        last = (l == L - 1)
        mk_.phase_inproj(l)
        mk_.phase_rwkv(l, 0); mk_.phase_rwkv(l, 1)
        mk_.phase_ssm_conv(l); mk_.phase_ssm_scan(l, 0); mk_.phase_ssm_scan(l, 1)
        mk_.phase_gla(l, 0); mk_.phase_gla(l, 1)
        mk_.phase_outproj(l, lat_only=last)
        mk_.phase_moe(l, lat_only=last)
    mk_.phase_final()
    mk_.finish(None)
    return mk_


def _host_consts():
    c = {}
    c["c_ident"] = np.eye(128, dtype=np.float32)
    p = np.arange(128)[:, None] % 64
    f = np.arange(512)[None, :] % 64
    c["c_masks"] = np.stack([(p < f), (p <= f), (p > f), (p >= f)]).astype(np.float32)
    c["c_id8"] = (p == f).astype(np.float32)
    pp = np.arange(128)
    c["c_blk"] = ((pp[:, None] // 64) == (pp[None, :] // 64)).astype(np.float32)
    j = np.arange(128)[:, None]; f128 = np.arange(128)[None, :]
    c["c_m128"] = np.stack([(j <= f128), (j >= f128), (j > f128), (j < f128), -30000.0 * (j == f128)]).astype(np.float32)
    sel = np.zeros((16, 16, 128), np.float32)
    for e_ in range(16):
        sel[e_, e_, :] = 1.0
    c["c_sel"] = sel
    c["c_reset"] = np.broadcast_to((np.arange(512) % 64 != 0).astype(np.float32)[None, :], (128, 512)).copy()
    return c


_CACHE = {}


def kernel(**inputs):
    from concourse.bass_utils import run_bass_kernel_spmd
    x = np.asarray(inputs["x"])
    B, T, _ = x.shape
    n_cores = 8
    assert B == n_cores
    mk_ = build_all(T)
    consts = _host_consts()
    in_maps = []
    for b in range(n_cores):
        m = {}
        for k_, v in inputs.items():
            v = np.asarray(v, dtype=np.float32)
            if k_ in ("x", "ctx", "c"):
                m[k_] = np.ascontiguousarray(v[b])
            else:
                m[k_] = np.ascontiguousarray(v)
        m.update(consts)
        in_maps.append(m)
    res = run_bass_kernel_spmd(mk_.nc, in_maps, core_ids=list(range(n_cores)))
    out = np.stack([np.asarray(r["out"], dtype=np.float32) for r in res.results], 0)
    return out
```

```python
import os
import numpy as np
import concourse.bass as bass
import concourse.mybir as mybir

F32 = mybir.dt.float32
BF16 = mybir.dt.bfloat16
ALU = mybir.AluOpType
AF = mybir.ActivationFunctionType
AX = mybir.AxisListType

SEM_CHUNK = int(os.environ.get("SEM_CHUNK", 8000))
N_DMA_SEMS = 12


class T:
    __slots__ = ("ap", "w", "r", "name", "psum")

    def __init__(self, ap, name="", psum=False):
        self.psum = psum
        self.ap = ap
        self.w = None
        self.r = []
        self.name = name

    def __getitem__(self, idx):
        return self.ap[idx]


class KB:
    ENGS = ("pe", "act", "dve", "pool", "sp")

    def __init__(self, nc, same_engine_sync=(os.environ.get("SES", "1") == "1")):
        self.nc = nc
        self.ops = {e: [] for e in self.ENGS}
        self.cnt = {e: 0 for e in self.ENGS}
        self.sem_names = []
        self.waited = {e: {} for e in self.ENGS}
        self.dma_rr = {e: 0 for e in self.ENGS}
        self.dma_val = {}
        self.same_engine_sync = same_engine_sync
        self.ctx = []
        self.final_waits = []
        self.sems = {}
        self.sem_guards = []
        self.last_tok = {}

    def sb(self, name, shape, dt):
        self.uid = getattr(self, "uid", 0) + 1
        name = "%s_%d" % (name, self.uid)
        g = self.nc.sbuf_tensor(name, list(shape), dt)
        t = g.__enter__()
        self.ctx.append(g)
        return T(t, name)

    def ps(self, name, shape, dt):
        self.uid = getattr(self, "uid", 0) + 1
        name = "%s_%d" % (name, self.uid)
        esz = 4 if dt == F32 else 2
        full = 2048 // esz
        g = self.nc.psum_tensor(name, [128, full], dt)
        t = g.__enter__()
        self.ctx.append(g)
        shape = list(shape)
        p = shape[0]
        if len(shape) == 2:
            assert shape[1] <= full
            ap = t[0:p, 0:shape[1]]
        else:
            assert len(shape) == 3 and shape[1] * shape[2] <= full
            ap = t[0:p, 0:shape[1] * shape[2]].rearrange("p (a b) -> p a b", b=shape[2])
        return T(ap, name, psum=True)

    def dram(self, name, shape, dt, kind="Internal"):
        t = self.nc.dram_tensor(name, list(shape), dt, kind=kind)
        return T(t.ap(), name)

    def _sem_key(self, key):
        if key not in self.sem_names:
            self.sem_names.append(key)
        return key

    def op(self, eng, fn, reads=(), writes=(), dma=False, pe_acc=False):
        waits = {}
        if any(t.psum for t in reads):
            writes = list(writes) + [t for t in reads if t.psum and t not in writes]
            reads = [t for t in reads if not t.psum]

        def need(dep):
            if dep is None:
                return
            k, v, e = dep
            if e == eng and not dma and not self.same_engine_sync and not k[0] == "dma":
                return
            if e == eng and eng == "pe" and k[0] != "dma":
                return
            if waits.get(k, 0) < v:
                waits[k] = v

        for t in reads:
            need(t.w)
        for t in writes:
            if not (pe_acc and t.w is not None and t.w[2] == "pe" and eng == "pe"):
                need(t.w)
            for d in t.r:
                need(d)
        if dma:
            i = self.dma_rr[eng]
            self.dma_rr[eng] = (i + 1) % N_DMA_SEMS
            key = self._sem_key(("dma", eng, i))
            prev = self.dma_val.get(key, 0)
            if prev:
                if waits.get(key, 0) < prev:
                    waits[key] = prev
            val = prev + 16
            self.dma_val[key] = val
            inc = 16
        else:
            c = self.cnt[eng]
            self.cnt[eng] = c + 1
            key = self._sem_key(("eng", eng, c // SEM_CHUNK))
            val = (c % SEM_CHUNK) + 1
            inc = 1
        wl = []
        wd = self.waited[eng]
        for k, v in waits.items():
            if wd.get(k, 0) >= v:
                continue
            wd[k] = v
            wl.append((k, v))
        self.ops[eng].append((fn, wl, key, inc))
        tok = (key, val, eng)
        self.last_tok[key] = val
        for t in reads:
            t.r.append(tok)
        for t in writes:
            t.w = tok
            t.r = []
        return tok

    def mark(self):
        return len(self.ctx)

    def free_to(self, mark):
        while len(self.ctx) > mark:
            g = self.ctx.pop()
            g.__exit__(None, None, None)

    def barrier(self):
        for eng in self.ENGS:
            wl = []
            wd = self.waited[eng]
            for k, v in self.last_tok.items():
                if k[0] == "eng" and k[1] == eng:
                    continue
                if wd.get(k, 0) >= v:
                    continue
                wd[k] = v
                wl.append((k, v))
            if wl:
                self.ops[eng].append((None, wl, None, 0))

    def finish_wait(self, eng, toks):
        self.final_waits.append((eng, toks))

    def emit(self):
        nc = self.nc
        for key in self.sem_names:
            if key not in self.sems:
                g = nc.semaphore("s%d" % len(self.sems))
                self.sems[key] = g.__enter__()
                self.sem_guards.append(g)
        sems = self.sems
        ops = self.ops
        final_waits = self.final_waits

        def run(engname, h):
            for fn, wl, key, inc in ops[engname]:
                for k, v in wl:
                    h.wait_ge(sems[k], v)
                if fn is not None:
                    ins = fn(h)
                    ins.then_inc(sems[key], inc)
            for e, toks in final_waits:
                if e == engname:
                    for (k, v, _) in toks:
                        h.wait_ge(sems[k], v)

        with nc.Block() as block:
            @block.tensor
            def _(h):
                run("pe", h)

            @block.scalar
            def _(h):
                run("act", h)

            @block.vector
            def _(h):
                run("dve", h)

            @block.gpsimd
            def _(h):
                run("pool", h)

            @block.sync
            def _(h):
                run("sp", h)
        self.ops = {e: [] for e in self.ENGS}
        self.final_waits = []

    def close(self):
        self.free_to(0)
        for g in reversed(self.sem_guards):
            g.__exit__(None, None, None)


D = 1024
CTX = 256
INC = 3096
EPS = 1e-6
NEG_E05 = -0.6065306597126334


class MK:
    def __init__(self, T, L=2, dbg=None):
        self.T = T
        self.L = L
        self.NT = CTX + T
        self.dbg = dbg or set()
        nc = bass.Bass("TRN2", target_bir_lowering=False)
        self.nc = nc
        self.k = KB(nc)
        self.decl()

    def decl(self):
        k = self.k
        T, L, NT = self.T, self.L, self.NT
        I = lambda n, s: k.dram(n, s, F32, kind="ExternalInput")
        self.x = I("x", [T, D]); self.c = I("c", [D]); self.ctx = I("ctx", [CTX, D]); self.c_ctx = I("c_ctx", [D])
        self.ada_w = I("ada_w", [L, D, 6 * D]); self.ada_b = I("ada_b", [L, 6 * D])
        self.norm1_g = I("norm1_g", [L, D]); self.norm2_g = I("norm2_g", [L, D])
        self.w_in = I("w_in", [L, D, INC]); self.w_out = I("w_out", [L, D, D])
        self.rw_mu_prev = I("rw_mu_prev", [L, 1024]); self.rw_mu_next = I("rw_mu_next", [L, 1024])
        self.rw_w0 = I("rw_w0", [L, 2, 256]); self.rw_w2 = I("rw_w2", [L, 2, 64, 256])
        self.rw_a0 = I("rw_a0", [L, 2, 256]); self.rw_a2 = I("rw_a2", [L, 2, 64, 256])
        self.rw_g2 = I("rw_g2", [L, 128, 256])
        self.rw_k_k = I("rw_k_k", [L, 256]); self.rw_k_a = I("rw_k_a", [L, 256]); self.rw_r_k = I("rw_r_k", [L, 256])
        self.rw_gn_g = I("rw_gn_g", [L, 256]); self.rw_gn_b = I("rw_gn_b", [L, 256])
        self.ssm_conv_w = I("ssm_conv_w", [L, 3, 3, 768]); self.ssm_conv_b = I("ssm_conv_b", [L, 768])
        self.ssm_dt_bias = I("ssm_dt_bias", [L, 2, 8]); self.ssm_a_log = I("ssm_a_log", [L, 2, 8])
        self.ssm_d = I("ssm_d", [L, 8]); self.ssm_norm_g = I("ssm_norm_g", [L, 512])
        self.gla_ga2 = I("gla_ga2", [L, 2, 16, 128]); self.gla_gb = I("gla_gb", [L, 2, 128]); self.gla_norm_g = I("gla_norm_g", [L, 256])
        self.moe_rg_w = I("moe_rg_w", [L, D, 4]); self.moe_rg_b = I("moe_rg_b", [L, 4])
        self.moe_re_w = I("moe_re_w", [L, D, 16]); self.moe_re_b = I("moe_re_b", [L, 16])
        self.moe_w1 = I("moe_w1", [L, 16, D, 512]); self.moe_w3 = I("moe_w3", [L, 16, D, 512]); self.moe_w2 = I("moe_w2", [L, 16, 512, D])
        self.final_g = I("final_g", [D])
        self.c_ident = I("c_ident", [128, 128])
        self.out = k.dram("out", [T, D], F32, kind="ExternalOutput")
        def S(n, s, dt=F32):
            kind = "ExternalOutput" if n in self.dbg else "Internal"
            return k.dram(n, s, dt, kind=kind)
        self.xT = S("xT", [D, NT])
        self.uT = S("uT", [3200, NT])
        self.mixT = S("mixT", [D, NT])
        self.rw_consts()
        self.ssm_decl()
        self.moe_decl()

    def consts(self):
        k = self.k
        self.identf = k.sb("identf", [128, 128], F32)
        self.ident = k.sb("ident", [128, 128], BF16)
        self.ones = k.sb("ones", [128, 128], BF16)
        k.op("sp", lambda e: e.dma_start(out=self.identf[:, :], in_=self.c_ident.ap[:, :]), writes=[self.identf], dma=True)
        k.op("dve", lambda e: e.tensor_copy(self.ident[:, :], self.identf[:, :]), reads=[self.identf], writes=[self.ident])
        k.op("dve", lambda e: e.memset(self.ones[:, :], 1.0), writes=[self.ones])
        self.modT = k.sb("modT", [128, self.L, 48, 2], F32)
        self.gm = k.sb("gm", [128, self.L, 2, 8, 2], F32)
        self.load_consts2()

    def phase_mod(self):
        k = self.k
        L = self.L
        m = k.mark()
        cf = k.sb("cf", [128, 8, 2], F32)
        cb = k.sb("cb", [128, 8, 2], BF16)
        adab = k.sb("adab", [128, 48], F32)
        ng = k.sb("ng", [128, 2, 8], F32)
        aw = k.sb("aw", [128, 8, 3072], BF16)
        pm = k.ps("pm", [128, 48, 2], F32)
        k.op("sp", lambda e: e.dma_start(out=cf[:, :, 0], in_=self.c.ap.rearrange("(c p) -> p c", p=128), allow_slow_non_contiguous=True), writes=[cf], dma=True)
        k.op("sp", lambda e: e.dma_start(out=cf[:, :, 1], in_=self.c_ctx.ap.rearrange("(c p) -> p c", p=128), allow_slow_non_contiguous=True), writes=[cf], dma=True)
        k.op("act", lambda e: e.activation(out=cb[:, :, :], in_=cf[:, :, :], func=AF.Silu), reads=[cf], writes=[cb])
        for l in range(L):
            k.op("sp", lambda e, l=l: e.dma_start(out=adab[:, :], in_=self.ada_b.ap[l].rearrange("(c p) -> p c", p=128), allow_slow_non_contiguous=True), writes=[adab], dma=True)
            k.op("sp", lambda e, l=l: e.dma_start(out=ng[:, 0, :], in_=self.norm1_g.ap[l].rearrange("(c p) -> p c", p=128), allow_slow_non_contiguous=True), writes=[ng], dma=True)
            k.op("sp", lambda e, l=l: e.dma_start(out=ng[:, 1, :], in_=self.norm2_g.ap[l].rearrange("(c p) -> p c", p=128), allow_slow_non_contiguous=True), writes=[ng], dma=True)
            for half in range(2):
                for c in range(8):
                    k.op("pool", lambda e, l=l, c=c, half=half: e.dma_start(out=aw[:, c, :], in_=self.ada_w.ap[l, c * 128:(c + 1) * 128, half * 3072:(half + 1) * 3072]), writes=[aw], dma=True)
                for j in range(24):
                    for c in range(8):
                        k.op("pe", lambda e, c=c, j=j, half=half: e.matmul(pm[:, half * 24 + j, :], lhsT=aw[:, c, j * 128:(j + 1) * 128], rhs=cb[:, c, :], start=(c == 0), stop=(c == 7)), reads=[aw, cb], writes=[pm], pe_acc=True)
            k.op("dve", lambda e, l=l: e.tensor_tensor(self.modT[:, l, :, :], pm[:, :, :], adab[:, :].unsqueeze(2).to_broadcast([128, 48, 2]), ALU.add), reads=[pm, adab], writes=[self.modT])
            for w, j in ((0, 1), (1, 4)):
                k.op("dve", lambda e, l=l, w=w, j=j: e.scalar_tensor_tensor(out=self.gm[:, l, w, :, :], in0=self.modT[:, l, j * 8:(j + 1) * 8, :], scalar=1.0, in1=ng[:, w, :].unsqueeze(2).to_broadcast([128, 8, 2]), op0=ALU.add, op1=ALU.mult), reads=[self.modT, ng], writes=[self.gm])
        k.barrier()
        k.emit()
        k.free_to(m)

    def tok_tiles(self, lat_only=False, n=512):
        tiles = []
        if not lat_only:
            tiles.append((0, CTX, 1))
        for i in range(self.T // n):
            tiles.append((CTX + i * n, n, 0))
        return tiles

    def phase_x_in(self):
        k = self.k
        m = k.mark()
        xin = [k.sb("xin%d" % i, [128, D], F32) for i in range(2)]
        pt = [k.ps("ptx%d" % i, [128, 4, 128], F32) for i in range(2)]
        xo = [k.sb("xo%d" % i, [128, 8, 128], F32) for i in range(2)]
        nt = self.NT // 128
        for i in range(nt):
            src = self.ctx.ap[i * 128:(i + 1) * 128, :] if i < 2 else self.x.ap[(i - 2) * 128:(i - 1) * 128, :]
            b = i % 2
            k.op("sp", lambda e, b=b, src=src: e.dma_start(out=xin[b][:, :], in_=src), writes=[xin[b]], dma=True)
            for hh in range(2):
                for c in range(4):
                    cc = hh * 4 + c
                    k.op("pe", lambda e, b=b, hh=hh, c=c, cc=cc: e.transpose(pt[hh][:, c, :], xin[b][:, cc * 128:(cc + 1) * 128], self.identf[:, :]), reads=[xin[b], self.identf], writes=[pt[hh]], pe_acc=True)
                eng = "dve" if hh == 0 else "act"
                if eng == "dve":
                    k.op("dve", lambda e, b=b, hh=hh: e.tensor_copy(xo[b][:, hh * 4:(hh + 1) * 4, :], pt[hh][:, :, :]), reads=[pt[hh]], writes=[xo[b]])
                else:
                    k.op("act", lambda e, b=b, hh=hh: e.copy(xo[b][:, hh * 4:(hh + 1) * 4, :], pt[hh][:, :, :]), reads=[pt[hh]], writes=[xo[b]])
            k.op("pool", lambda e, b=b, i=i: e.dma_start(out=self.xT.ap.rearrange("(c p) t -> p c t", p=128)[:, :, i * 128:(i + 1) * 128], in_=xo[b][:, :, :]), reads=[xo[b]], dma=True)
        k.barrier()
        k.emit()
        k.free_to(m)

    def norm_tile(self, l, which, tok0, n, stream, xt, sq, pss, rstd, hT, keep_f32=False):
        k = self.k
        k.op("act", lambda e: e.activation(out=sq[:, :, :n], in_=xt[:, :, :n], func=AF.Square), reads=[xt], writes=[sq])
        for c in range(8):
            k.op("pe", lambda e, c=c: e.matmul(pss[:, :n], lhsT=self.ones[:, :], rhs=sq[:, c, :n], start=(c == 0), stop=(c == 7)), reads=[sq, self.ones], writes=[pss], pe_acc=True)
        k.op("act", lambda e: e.activation(out=rstd[:, :n], in_=pss[:, :n], func=AF.Sqrt, scale=1.0 / D, bias=EPS), reads=[pss], writes=[rstd])
        k.op("dve", lambda e: e.reciprocal(rstd[:, :n], rstd[:, :n]), reads=[rstd], writes=[rstd])
        k.op("dve", lambda e: e.tensor_tensor(xt[:, :, :n], xt[:, :, :n], rstd[:, :n].unsqueeze(1).to_broadcast([128, 8, n]), ALU.mult), reads=[xt, rstd], writes=[xt])
        sh_j = 0 if which == 0 else 3
        k.op("pool", lambda e: e.tensor_tensor(xt[:, :, :n], xt[:, :, :n], self.gm[:, l, which, :, stream].unsqueeze(2).to_broadcast([128, 8, n]), ALU.mult), reads=[xt, self.gm], writes=[xt])
        if keep_f32:
            k.op("dve", lambda e: e.tensor_tensor(xt[:, :, :n], xt[:, :, :n], self.modT[:, l, sh_j * 8:(sh_j + 1) * 8, stream].unsqueeze(2).to_broadcast([128, 8, n]), ALU.add), reads=[xt, self.modT], writes=[xt])
            k.op("act", lambda e: e.copy(hT[:, :, :n], xt[:, :, :n]), reads=[xt], writes=[hT])
        else:
            k.op("dve", lambda e: e.tensor_tensor(hT[:, :, :n], xt[:, :, :n], self.modT[:, l, sh_j * 8:(sh_j + 1) * 8, stream].unsqueeze(2).to_broadcast([128, 8, n]), ALU.add), reads=[xt, self.modT], writes=[hT])

    def phase_inproj(self, l):
        k = self.k
        m = k.mark()
        win = k.sb("win", [128, 8, INC], BF16)
        for c in range(8):
            k.op("pool", lambda e, c=c: e.dma_start(out=win[:, c, :], in_=self.w_in.ap[l, c * 128:(c + 1) * 128, :]), writes=[win], dma=True)
        xt = [k.sb("xt%d" % i, [128, 8, 512], F32) for i in range(2)]
        sq = k.sb("sq", [128, 8, 512], BF16)
        rstd = k.sb("rstd", [128, 512], F32)
        hT = [k.sb("hT%d" % i, [128, 8, 512], BF16) for i in range(2)]
        pss = k.ps("pss", [128, 512], F32)
        pu = [k.ps("pu%d" % i, [128, 512], F32) for i in range(4)]
        us = [k.sb("us%d" % i, [128, 512], F32) for i in range(4)]
        xTv = self.xT.ap.rearrange("(c p) t -> p c t", p=128)
        nchunk = (INC + 127) // 128
        cnt = 0
        for ti, (tok0, n, stream) in enumerate(self.tok_tiles()):
            b = ti % 2
            k.op("sp", lambda e, b=b, tok0=tok0, n=n: e.dma_start(out=xt[b][:, :, :n], in_=xTv[:, :, tok0:tok0 + n]), writes=[xt[b]], dma=True)
            self.norm_tile(l, 0, tok0, n, stream, xt[b], sq, pss, rstd, hT[b])
            for j in range(nchunk):
                c0 = j * 128
                nc_ = min(128, INC - c0)
                pb = cnt % 4
                cnt += 1
                for c in range(8):
                    k.op("pe", lambda e, c=c, c0=c0, nc_=nc_, pb=pb, b=b, n=n: e.matmul(pu[pb][:nc_, :n], lhsT=win[:, c, c0:c0 + nc_], rhs=hT[b][:, c, :n], start=(c == 0), stop=(c == 7)), reads=[win, hT[b]], writes=[pu[pb]], pe_acc=True)
                if j % 2 == 0:
                    k.op("dve", lambda e, pb=pb, nc_=nc_, n=n: e.tensor_copy(us[pb][:nc_, :n], pu[pb][:nc_, :n]), reads=[pu[pb]], writes=[us[pb]])
                else:
                    k.op("act", lambda e, pb=pb, nc_=nc_, n=n: e.copy(us[pb][:nc_, :n], pu[pb][:nc_, :n]), reads=[pu[pb]], writes=[us[pb]])
                k.op("pool", lambda e, pb=pb, nc_=nc_, n=n, c0=c0, tok0=tok0: e.dma_start(out=self.uT.ap[c0:c0 + nc_, tok0:tok0 + n], in_=us[pb][:nc_, :n]), reads=[us[pb]], dma=True)
        k.barrier()
        k.emit()
        k.free_to(m)

    def finish(self, last_tensor):
        k = self.k
        k.barrier()
        k.emit()
        k.close()


def _rw_consts(self):
    k = self.k
    I = lambda n, s: k.dram(n, s, F32, kind="ExternalInput")
    self.c_masks = I("c_masks", [4, 128, 512])
    self.c_id8 = I("c_id8", [128, 512])
    self.c_blk = I("c_blk", [128, 128])
    self.c_reset = I("c_reset", [128, 512])
    def S(n, s, dt=F32):
        kind = "ExternalOutput" if n in self.dbg else "Internal"
        return k.dram(n, s, dt, kind=kind)
    self.rw_yf = S("rw_yf", [256, self.NT])
    self.rw_bonus = S("rw_bonus", [256, self.NT])
    self.rw_gate = S("rw_gate", [256, self.NT])


def _load_consts2(self):
    k = self.k
    self.masks = k.sb("masks", [128, 4, 512], BF16)
    self.id8 = k.sb("id8", [128, 512], BF16)
    self.blk = k.sb("blk", [128, 128], BF16)
    self.reset = k.sb("reset", [128, 512], F32)
    for i in range(4):
        k.op("pool", lambda e, i=i: e.dma_start(out=self.masks[:, i, :], in_=self.c_masks.ap[i]), writes=[self.masks], dma=True)
    k.op("pool", lambda e: e.dma_start(out=self.id8[:, :], in_=self.c_id8.ap[:, :]), writes=[self.id8], dma=True)
    k.op("pool", lambda e: e.dma_start(out=self.blk[:, :], in_=self.c_blk.ap[:, :]), writes=[self.blk], dma=True)
    k.op("sp", lambda e: e.dma_start(out=self.reset[:, :], in_=self.c_reset.ap[:, :]), writes=[self.reset], dma=True)


def _pvec(self, name, src_ap, ncols, eng="sp", dt=F32):
    k = self.k
    t = k.sb(name, [128, ncols], dt)
    k.op(eng, lambda e: e.dma_start(out=t[:, :], in_=src_ap.rearrange("(c p) -> p c", p=128), allow_slow_non_contiguous=True), writes=[t], dma=True)
    return t


def interleave(gens, weights=None):
    gens = [(g, (weights[i] if weights else 1)) for i, g in enumerate(gens)]
    while gens:
        for item in list(gens):
            g, w = item
            try:
                for _ in range(w):
                    next(g)
            except StopIteration:
                gens.remove(item)


def phase_rwkv(self, l, d):
    k = self.k
    m = k.mark()
    NT = self.NT
    s = NEG_E05
    mp = self._pvec("mp", self.rw_mu_prev.ap[l], 8)
    mn = self._pvec("mn", self.rw_mu_next.ap[l], 8)
    cmix = k.sb("cmix", [128, 8], F32)
    k.op("dve", lambda e: e.tensor_tensor(cmix[:, :], mp[:, :], mn[:, :], ALU.add), reads=[mp, mn], writes=[cmix])
    k.op("dve", lambda e: e.tensor_scalar(cmix[:, :], cmix[:, :], -1.0, 1.0, ALU.mult, ALU.add), reads=[cmix], writes=[cmix])
    w0 = self._pvec("w0", self.rw_w0.ap[l, d], 2)
    a0 = self._pvec("a0", self.rw_a0.ap[l, d], 2)
    k_k = self._pvec("k_k", self.rw_k_k.ap[l], 2)
    k_a = self._pvec("k_a", self.rw_k_a.ap[l], 2)
    r_k = self._pvec("r_k", self.rw_r_k.ap[l], 2)
    gn_g = self._pvec("gn_g", self.rw_gn_g.ap[l], 2)
    gn_b = self._pvec("gn_b", self.rw_gn_b.ap[l], 2)
    w2s = k.sb("w2s", [128, 256], BF16)
    a2s = k.sb("a2s", [128, 256], BF16)
    g2s = k.sb("g2s", [128, 256], BF16)
    k.op("pool", lambda e: e.dma_start(out=w2s[0:64, :], in_=self.rw_w2.ap[l, d]), writes=[w2s], dma=True)
    k.op("pool", lambda e: e.dma_start(out=a2s[64:128, :], in_=self.rw_a2.ap[l, d]), writes=[a2s], dma=True)
    k.op("pool", lambda e: e.dma_start(out=g2s[:, :], in_=self.rw_g2.ap[l]), writes=[g2s], dma=True)

    U = [k.sb("rU%d" % i, [128, 8, 514], F32) for i in range(2)]
    S = k.sb("rS", [128, 8, 512], F32)
    wlt = k.sb("wlt", [128, 512], BF16)
    alb = k.sb("alb", [128, 512], BF16)
    sgl = k.sb("sgl", [128, 512], BF16)
    f32t = lambda n_: k.sb(n_, [128, 8, 64], F32)
    bft = lambda n_: k.sb(n_, [128, 8, 64], BF16)
    sgw, asig, kx, rn, cs, dcs, tmp, E, Etrue = [f32t("r_" + z) for z in ("sgw", "asig", "kx", "rn", "cs", "dcs", "tmp", "E", "Etrue")]
    E2_, E3_ = f32t("r_E2"), f32t("r_E3")
    kkn, bvec, kmod = f32t("kkn"), f32t("bvec"), f32t("kmod")
    sqk = bft("sqk")
    AB = {z: [[bft("r_%s%d%d" % (z, b, hp)) for hp in range(2)] for b in range(2)] for z in ("rt", "kt", "bt", "at", "Rtrue", "Atrue", "Bh", "Kh", "vb")}
    WCs = [[k.sb("WC%d%d" % (b, hp), [128, 8], F32) for hp in range(2)] for b in range(2)]
    Atm, Bhtm, Khtm, Vtm = [[bft("r_%s%d" % (z, hp)) for hp in range(2)] for z in ("Atm", "Bhtm", "Khtm", "Vtm")]
    Zs, Ns, Zs2, Ns2, Aak, Arb, Ark, X, AVs, PaT, Us = [[bft("r_%s%d" % (z, hp)) for hp in range(2)] for z in ("Zs", "Ns", "Zs2", "Ns2", "Aak", "Arb", "Ark", "X", "AVs", "PaT", "Us")]
    Qs = [f32t("Qs%d" % hp) for hp in range(2)]
    Mst = [k.sb("Mst%d" % hp, [128, 64], F32) for hp in range(2)]
    Mbf = [k.sb("Mbf%d" % hp, [128, 64], BF16) for hp in range(2)]
    ysb = [k.sb("ysb%d" % hp, [128, 512], F32) for hp in range(2)]
    bon = k.sb("bon", [128, 512], F32)
    gat = k.sb("gat", [128, 512], F32)
    fbon = k.sb("fbon", [128, 512], F32)
    fgat = k.sb("fgat", [128, 512], F32)
    fkx, frn = f32t("r_fkx"), f32t("r_frn")
    fsq = bft("r_fsq")
    yf_in = [k.sb("yfin%d" % hp, [128, 512], F32) for hp in range(2)]
    PS = [k.ps("rps%d" % i, [128, 512], F32) for i in range(4)]
    PY = [k.ps("rpy%d" % j, [128, 512], F32) for j in range(2)]
    PSB = [k.ps("rpsb%d" % i, [128, 8, 64], BF16) for i in range(2)]
    psi = [0]
    psbi = [0]

    def nps():
        p = PS[psi[0] % 4]
        psi[0] += 1
        return p

    def npsb():
        p = PSB[psbi[0] % 2]
        psbi[0] += 1
        return p

    for hp in range(2):
        k.op("dve", lambda e, hp=hp: e.memset(Mst[hp][:, :], 0.0), writes=[Mst[hp]])
        k.op("dve", lambda e, hp=hp: e.memset(Mbf[hp][:, :], 0.0), writes=[Mbf[hp]])

    M_SL, M_LE, M_SG, M_GE = 0, 1, 2, 3
    if d == 0:
        mZ, mN, mI = M_SL, M_SG, M_LE
    else:
        mZ, mN, mI = M_SG, M_SL, M_GE

    tiles = self.tok_tiles()
    lat = tiles[1:]
    order = [tiles[0]] + (lat if d == 0 else lat[::-1])
    uTv = self.uT.ap[0:1024, :].rearrange("(c p) t -> p c t", p=128)

    def stageA(ti, tok0, n, stream):
        nch = n // 64
        b = ti % 2
        rt, kt, bt, at, Rtrue, Atrue, Bh, Kh, vb = [AB[z][b] for z in ("rt", "kt", "bt", "at", "Rtrue", "Atrue", "Bh", "Kh", "vb")]
        WC = WCs[b]
        seq0, seq1 = (0, CTX) if stream == 1 else (CTX, NT)
        ub = U[ti % 2]
        lo = max(tok0 - 1, seq0)
        hi = min(tok0 + n + 1, seq1)
        if lo > tok0 - 1:
            k.op("pool", lambda e: e.memset(ub[:, :, 0:1], 0.0), writes=[ub])
        if hi < tok0 + n + 1:
            k.op("pool", lambda e: e.memset(ub[:, :, n + 1:n + 2], 0.0), writes=[ub])
        k.op("sp", lambda e: e.dma_start(out=ub[:, :, lo - (tok0 - 1):hi - (tok0 - 1)], in_=uTv[:, :, lo:hi]), writes=[ub], dma=True)
        fl = lambda t_: t_[:, :, :].rearrange("p c t -> p (c t)")[:, 0:n]
        yield

        def shift(c):
            k.op("dve", lambda e: e.tensor_scalar(S[:, c, :n], ub[:, c, 1:n + 1], cmix[:, c:c + 1], None, ALU.mult), reads=[ub, cmix], writes=[S])
            k.op("dve", lambda e: e.scalar_tensor_tensor(out=S[:, c, :n], in0=ub[:, c, 0:n], scalar=mp[:, c:c + 1], in1=S[:, c, :n], op0=ALU.mult, op1=ALU.add), reads=[ub, mp, S], writes=[S])
            k.op("dve", lambda e: e.scalar_tensor_tensor(out=S[:, c, :n], in0=ub[:, c, 2:n + 2], scalar=mn[:, c:c + 1], in1=S[:, c, :n], op0=ALU.mult, op1=ALU.add), reads=[ub, mn, S], writes=[S])
        for c in range(8):
            if d == 1 and c == 7:
                continue
            shift(c)
            yield
        k.op("act", lambda e: e.activation(out=wlt[0:64, :n], in_=S[0:64, 6, :n], func=AF.Tanh), reads=[S], writes=[wlt])
        k.op("act", lambda e: e.copy(alb[64:128, :n], S[64:128, 6, :n]), reads=[S], writes=[alb])
        if d == 0:
            k.op("act", lambda e: e.activation(out=sgl[:, :n], in_=S[:, 7, :n], func=AF.Sigmoid), reads=[S], writes=[sgl])
        yield

        def prep(hp):
            p1 = nps()
            k.op("pe", lambda e: e.matmul(p1[:, :n], lhsT=w2s[0:64, hp * 128:(hp + 1) * 128], rhs=wlt[0:64, :n], start=True, stop=True), reads=[w2s, wlt], writes=[p1])
            k.op("act", lambda e: e.activation(out=fl(sgw), in_=p1[:, :n], func=AF.Sigmoid, bias=w0[:, hp:hp + 1]), reads=[p1, w0], writes=[sgw])
            p2 = nps()
            k.op("pe", lambda e: e.matmul(p2[:, :n], lhsT=a2s[64:128, hp * 128:(hp + 1) * 128], rhs=alb[64:128, :n], start=True, stop=True), reads=[a2s, alb], writes=[p2])
            k.op("act", lambda e: e.activation(out=fl(asig), in_=p2[:, :n], func=AF.Sigmoid, bias=a0[:, hp:hp + 1]), reads=[p2, a0], writes=[asig])
            yield
            k.op("dve", lambda e: e.tensor_scalar(fl(kx), S[:, 2 + hp, :n], k_k[:, hp:hp + 1], None, ALU.mult), reads=[S, k_k], writes=[kx])
            k.op("act", lambda e: e.activation(out=fl(sqk), in_=fl(kx), func=AF.Square), reads=[kx], writes=[sqk])
            p3 = nps()
            k.op("pe", lambda e: e.matmul(p3[:, :n], lhsT=self.blk[:, :], rhs=fl(sqk), start=True, stop=True), reads=[self.blk, sqk], writes=[p3])
            k.op("act", lambda e: e.activation(out=fl(rn), in_=p3[:, :n], func=AF.Ln, bias=1e-12), reads=[p3], writes=[rn])
            k.op("act", lambda e: e.activation(out=fl(rn), in_=fl(rn), func=AF.Exp, scale=-0.5), reads=[rn], writes=[rn])
            yield
            k.op("dve", lambda e: e.tensor_tensor(fl(kkn), fl(kx), fl(rn), ALU.mult), reads=[kx, rn], writes=[kkn])
            k.op("pool", lambda e: e.tensor_tensor(fl(bvec), fl(kkn), fl(asig), ALU.mult), reads=[kkn, asig], writes=[bvec])
            k.op("dve", lambda e: e.tensor_scalar(fl(tmp), fl(asig), -1.0, k_a[:, hp:hp + 1], ALU.add, ALU.mult), reads=[asig, k_a], writes=[tmp])
            k.op("dve", lambda e: e.scalar_tensor_tensor(out=fl(kmod), in0=fl(tmp), scalar=1.0, in1=S[:, 2 + hp, :n], op0=ALU.add, op1=ALU.mult), reads=[tmp, S], writes=[kmod])
            yield
            k.op("dve", lambda e: e.tensor_tensor_scan(fl(cs), self.reset[:, :n], fl(sgw), 0.0, ALU.mult, ALU.add), reads=[self.reset, sgw], writes=[cs])
            if d == 1:
                k.op("dve", lambda e: e.tensor_tensor(fl(tmp), fl(sgw), fl(cs), ALU.subtract), reads=[sgw, cs], writes=[tmp])
                k.op("dve", lambda e: e.tensor_tensor(cs[:, :nch, :], tmp[:, :nch, :], cs[:, :nch, 63:64].to_broadcast([128, nch, 64]), ALU.add), reads=[tmp, cs], writes=[cs])
            endi = 63 if d == 0 else 0
            k.op("pool", lambda e: e.tensor_tensor(dcs[:, :nch, :], cs[:, :nch, :], cs[:, :nch, 32:33].to_broadcast([128, nch, 64]), ALU.subtract), reads=[cs], writes=[dcs])
            yield
            k.op("act", lambda e: e.activation(out=fl(E), in_=fl(dcs), func=AF.Exp, scale=s), reads=[dcs], writes=[E])
            k.op("pool", lambda e: e.tensor_tensor(fl(tmp), fl(dcs), fl(sgw), ALU.subtract), reads=[dcs, sgw], writes=[tmp])
            k.op("act", lambda e: e.activation(out=fl(E2_), in_=fl(tmp), func=AF.Exp, scale=s), reads=[tmp], writes=[E2_])
            k.op("act", lambda e: e.activation(out=fl(E3_), in_=fl(dcs), func=AF.Exp, scale=-s), reads=[dcs], writes=[E3_])
            k.op("act", lambda e: e.activation(out=fl(Etrue), in_=fl(cs), func=AF.Exp, scale=s), reads=[cs], writes=[Etrue])
            yield
            k.op("dve", lambda e: e.tensor_tensor(fl(rt[hp]), S[:, hp, :n], fl(E), ALU.mult), reads=[S, E], writes=[rt[hp]])
            k.op("dve", lambda e: e.scalar_tensor_tensor(out=fl(at[hp]), in0=fl(kkn), scalar=-1.0, in1=fl(E2_), op0=ALU.mult, op1=ALU.mult), reads=[kkn, E2_], writes=[at[hp]])
            k.op("dve", lambda e: e.tensor_tensor(fl(kt[hp]), fl(kmod), fl(E3_), ALU.mult), reads=[kmod, E3_], writes=[kt[hp]])
            k.op("pool", lambda e: e.tensor_tensor(fl(bt[hp]), fl(bvec), fl(E3_), ALU.mult), reads=[bvec, E3_], writes=[bt[hp]])
            yield
            k.op("dve", lambda e: e.tensor_tensor(fl(Rtrue[hp]), S[:, hp, :n], fl(Etrue), ALU.mult), reads=[S, Etrue], writes=[Rtrue[hp]])
            k.op("dve", lambda e: e.tensor_copy(WC[hp][:, :nch], Etrue[:, :nch, endi]), reads=[Etrue], writes=[WC[hp]])
            k.op("pool", lambda e: e.tensor_tensor(fl(tmp), fl(cs), fl(sgw), ALU.subtract), reads=[cs, sgw], writes=[tmp])
            k.op("act", lambda e: e.activation(out=fl(E), in_=fl(tmp), func=AF.Exp, scale=s), reads=[tmp], writes=[E])
            k.op("dve", lambda e: e.scalar_tensor_tensor(out=fl(Atrue[hp]), in0=fl(kkn), scalar=-1.0, in1=fl(E), op0=ALU.mult, op1=ALU.mult), reads=[kkn, E], writes=[Atrue[hp]])
            yield
            k.op("pool", lambda e: e.tensor_tensor(tmp[:, :nch, :], cs[:, :nch, endi:endi + 1].to_broadcast([128, nch, 64]), cs[:, :nch, :], ALU.subtract), reads=[cs], writes=[tmp])
            k.op("act", lambda e: e.activation(out=fl(E2_), in_=fl(tmp), func=AF.Exp, scale=s), reads=[tmp], writes=[E2_])
            k.op("dve", lambda e: e.tensor_tensor(fl(Bh[hp]), fl(bvec), fl(E2_), ALU.mult), reads=[bvec, E2_], writes=[Bh[hp]])
            k.op("pool", lambda e: e.tensor_tensor(fl(Kh[hp]), fl(kmod), fl(E2_), ALU.mult), reads=[kmod, E2_], writes=[Kh[hp]])
            k.op("act", lambda e: e.copy(fl(vb[hp]), S[:, 4 + hp, :n]), reads=[S], writes=[vb[hp]])
            yield
            if d == 0:
                k.op("dve", lambda e: e.scalar_tensor_tensor(out=fl(sqk), in0=S[:, hp, :n], scalar=r_k[:, hp:hp + 1], in1=S[:, 2 + hp, :n], op0=ALU.mult, op1=ALU.mult), reads=[S, r_k], writes=[sqk])
                p4 = nps()
                k.op("pe", lambda e: e.matmul(p4[:, :n], lhsT=self.blk[:, :], rhs=fl(sqk), start=True, stop=True), reads=[self.blk, sqk], writes=[p4])
                k.op("dve", lambda e: e.tensor_tensor(bon[:, :n], p4[:, :n], S[:, 4 + hp, :n], ALU.mult), reads=[p4, S], writes=[bon])
                k.op("pool", lambda e: e.dma_start(out=self.rw_bonus.ap[hp * 128:(hp + 1) * 128, tok0:tok0 + n], in_=bon[:, :n]), reads=[bon], dma=True)
                p5 = nps()
                k.op("pe", lambda e: e.matmul(p5[:, :n], lhsT=g2s[:, hp * 128:(hp + 1) * 128], rhs=sgl[:, :n], start=True, stop=True), reads=[g2s, sgl], writes=[p5])
                k.op("act", lambda e: e.copy(gat[:, :n], p5[:, :n]), reads=[p5], writes=[gat])
                k.op("pool", lambda e: e.dma_start(out=self.rw_gate.ap[hp * 128:(hp + 1) * 128, tok0:tok0 + n], in_=gat[:, :n]), reads=[gat], dma=True)
                yield
        for hp in range(2):
            yield from prep(hp)

    def stageB(ti, tok0, n, stream):
        nch = n // 64
        b = ti % 2
        rt, kt, bt, at, Rtrue, Atrue, Bh, Kh, vb = [AB[z][b] for z in ("rt", "kt", "bt", "at", "Rtrue", "Atrue", "Bh", "Kh", "vb")]
        WC = WCs[b]

        def units(dst_ps, lt, rt_, P, reads):
            pv = dst_ps[:, :].rearrange("p (c t) -> p c t", t=64)
            for ch in range(nch):
                k.op("pe", lambda e, ch=ch: e.matmul(pv[P, ch, :], lhsT=lt[P, ch, :], rhs=rt_[P, ch, :], start=True, stop=True), reads=reads, writes=[dst_ps], pe_acc=True)
            return pv

        def head_block(hp, hl):
            P = slice(hl * 64, hl * 64 + 64)

            def tm(src, dst):
                pb = npsb()
                for ch in range(nch):
                    k.op("pe", lambda e, ch=ch: e.transpose(pb[P, ch, :], src[hp][P, ch, :], self.ident[P, P]), reads=[src[hp], self.ident], writes=[pb], pe_acc=True)
                k.op("act", lambda e: e.copy(dst[hp][P, :nch, :], pb[P, :nch, :]), reads=[pb], writes=[dst[hp]])
            for (a_, b_) in ((Atrue, Atm), (Bh, Bhtm), (Kh, Khtm), (vb, Vtm)):
                tm(a_, b_)
                yield

            def pair(dst, lt, rt_, mask, eng="dve"):
                pp = nps()
                pv = units(pp, lt[hp], rt_[hp], P, [lt[hp], rt_[hp]])
                mv = self.masks[:, mask, :].rearrange("p (c t) -> p c t", t=64)
                k.op(eng, lambda e: e.tensor_tensor(dst[hp][P, :nch, :], pv[P, :nch, :], mv[P, :nch, :], ALU.mult), reads=[pp, self.masks], writes=[dst[hp]])
            for args in ((Zs, bt, at, mZ), (Ns, at, bt, mN), (Aak, kt, at, mZ), (Arb, bt, rt, mI), (Ark, kt, rt, mI)):
                pair(*args)
                yield
            idv = self.id8[:, :].rearrange("p (c t) -> p c t", t=64)
            k.op("pool", lambda e: e.tensor_tensor(X[hp][P, :nch, :], Zs[hp][P, :nch, :], idv[P, :nch, :], ALU.add), reads=[Zs[hp], self.id8], writes=[X[hp]])
            Zc, Nc, Zn, Nn = Zs[hp], Ns[hp], Zs2[hp], Ns2[hp]
            for lev in range(1, 6):
                last = lev == 5

                def level(Zc, Nc, Zn, Nn, last):
                    if not last:
                        pz = nps()
                        pzv = units(pz, Nc, Zc, P, [Nc, Zc])
                    pn = nps()
                    pnv = units(pn, Zc, Nc, P, [Nc, Zc])
                    if not last:
                        k.op("act", lambda e: e.copy(Zn[P, :nch, :], pzv[P, :nch, :]), reads=[pz], writes=[Zn])
                    k.op("dve", lambda e: e.tensor_copy(Nn[P, :nch, :], pnv[P, :nch, :]), reads=[pn], writes=[Nn])
                    yield
                    px = nps()
                    pxv = units(px, Nn, X[hp], P, [Nn, X[hp]])
                    k.op("dve", lambda e: e.tensor_tensor(X[hp][P, :nch, :], pxv[P, :nch, :], X[hp][P, :nch, :], ALU.add), reads=[px, X[hp]], writes=[X[hp]])
                    yield
                yield from level(Zc, Nc, Zn, Nn, last)
                Zc, Nc, Zn, Nn = Zn, Nn, Zc, Nc
            pa = nps()
            pav = units(pa, Aak[hp], Vtm[hp], P, [Aak[hp], Vtm[hp]])
            k.op("act", lambda e: e.copy(AVs[hp][P, :nch, :], pav[P, :nch, :]), reads=[pa], writes=[AVs[hp]])
            yield
            pq = nps()
            pqv = units(pq, X[hp], AVs[hp], P, [X[hp], AVs[hp]])
            k.op("act", lambda e: e.copy(Qs[hp][P, :nch, :], pqv[P, :nch, :]), reads=[pq], writes=[Qs[hp]])
            yield
            pp_ = nps()
            ppv_ = units(pp_, Atm[hp], X[hp], P, [Atm[hp], X[hp]])
            k.op("dve", lambda e: e.tensor_copy(PaT[hp][P, :nch, :], ppv_[P, :nch, :]), reads=[pp_], writes=[PaT[hp]])
            yield

        interleave_here = [head_block(0, 0), head_block(0, 1), head_block(1, 0), head_block(1, 1)]
        alive = list(interleave_here)
        while alive:
            for g in list(alive):
                try:
                    next(g)
                except StopIteration:
                    alive.remove(g)
            yield

        def seq_step(ch, hp, hl):
            P = slice(hl * 64, hl * 64 + 64)
            pyb = PY[hl]
            col = hp * 256 + (ch % 4) * 64
            pu_ = nps()
            k.op("pe", lambda e: e.matmul(pu_[P, 0:64], lhsT=PaT[hp][P, ch, :], rhs=Mbf[hp][P, :], start=True, stop=True), reads=[PaT[hp], Mbf[hp]], writes=[pu_])
            k.op("dve", lambda e: e.tensor_tensor(Us[hp][P, ch, :], pu_[P, 0:64], Qs[hp][P, ch, :], ALU.add), reads=[pu_, Qs[hp]], writes=[Us[hp]])
            yield
            k.op("pe", lambda e: e.matmul(pyb[P, col:col + 64], lhsT=Mbf[hp][P, :], rhs=Rtrue[hp][P, ch, :], start=True, stop=False), reads=[Mbf[hp], Rtrue[hp]], writes=[pyb], pe_acc=True)
            k.op("pe", lambda e: e.matmul(pyb[P, col:col + 64], lhsT=Vtm[hp][P, ch, :], rhs=Ark[hp][P, ch, :], start=False, stop=False), reads=[Vtm[hp], Ark[hp]], writes=[pyb], pe_acc=True)
            k.op("pe", lambda e: e.matmul(pyb[P, col:col + 64], lhsT=Us[hp][P, ch, :], rhs=Arb[hp][P, ch, :], start=False, stop=True), reads=[Us[hp], Arb[hp]], writes=[pyb], pe_acc=True)
            pm_ = nps()
            k.op("pe", lambda e: e.matmul(pm_[P, 0:64], lhsT=Bhtm[hp][P, ch, :], rhs=Us[hp][P, ch, :], start=True, stop=False), reads=[Bhtm[hp], Us[hp]], writes=[pm_], pe_acc=True)
            k.op("pe", lambda e: e.matmul(pm_[P, 0:64], lhsT=Khtm[hp][P, ch, :], rhs=Vtm[hp][P, ch, :], start=False, stop=True), reads=[Khtm[hp], Vtm[hp]], writes=[pm_], pe_acc=True)
            k.op("dve", lambda e: e.scalar_tensor_tensor(out=Mst[hp][P, :], in0=Mst[hp][P, :], scalar=WC[hp][P, ch:ch + 1], in1=pm_[P, 0:64], op0=ALU.mult, op1=ALU.add), reads=[Mst[hp], WC[hp], pm_], writes=[Mst[hp]])
            k.op("act", lambda e: e.copy(Mbf[hp][P, :], Mst[hp][P, :]), reads=[Mst[hp]], writes=[Mbf[hp]])
            yield

        def evac_y(grp):
            for hp in range(2):
                for hl in range(2):
                    P = slice(hl * 64, hl * 64 + 64)
                    pp = PY[hl]
                    nc4 = min(4, nch - grp * 4)
                    src = pp[P, hp * 256:hp * 256 + nc4 * 64]
                    dst = ysb[hp][P, grp * 256:grp * 256 + nc4 * 64]
                    if hl == 0:
                        k.op("dve", lambda e, src=src, dst=dst: e.tensor_copy(dst, src), reads=[pp], writes=[ysb[hp]])
                    else:
                        k.op("act", lambda e, src=src, dst=dst: e.copy(dst, src), reads=[pp], writes=[ysb[hp]])

        chs = list(range(nch)) if d == 0 else list(range(nch - 1, -1, -1))
        done = 0
        for ch in chs:
            gens = [seq_step(ch, hp, hl) for hp in range(2) for hl in range(2)]
            alive = list(gens)
            while alive:
                for g in list(alive):
                    try:
                        next(g)
                    except StopIteration:
                        alive.remove(g)
                yield
            done += 1
            if done % 4 == 0 or done == nch:
                evac_y(ch // 4)
                yield

        def outp(hp):
            if d == 0:
                k.op("pool", lambda e: e.dma_start(out=self.rw_yf.ap[hp * 128:(hp + 1) * 128, tok0:tok0 + n], in_=ysb[hp][:, :n]), reads=[ysb[hp]], dma=True)
            else:
                self.rw_finish(l, hp, tok0, n, ysb[hp], yf_in[hp], fbon, fgat, gn_g, gn_b, nps, fsq, None, fkx, frn)
        for hp in range(2):
            outp(hp)
            yield

    nt = len(order)
    gA = stageA(0, *order[0])
    for _ in gA:
        pass
    for ti in range(nt):
        gens = [stageB(ti, *order[ti])]
        wts = [3]
        if ti + 1 < nt:
            gens.append(stageA(ti + 1, *order[ti + 1]))
            wts.append(1)
        if os.environ.get("RW_IL", "0") == "1":
            interleave(gens, wts)
        else:
            for g_ in gens:
                for _ in g_:
                    pass
    k.barrier()
    k.emit()
    k.free_to(m)


def phase_rwkv_old(self, l, d):
    k = self.k
    m = k.mark()
    NT = self.NT
    s = NEG_E05
    mp = self._pvec("mp", self.rw_mu_prev.ap[l], 8)
    mn = self._pvec("mn", self.rw_mu_next.ap[l], 8)
    cmix = k.sb("cmix", [128, 8], F32)
    k.op("dve", lambda e: e.tensor_tensor(cmix[:, :], mp[:, :], mn[:, :], ALU.add), reads=[mp, mn], writes=[cmix])
    k.op("dve", lambda e: e.tensor_scalar(cmix[:, :], cmix[:, :], -1.0, 1.0, ALU.mult, ALU.add), reads=[cmix], writes=[cmix])
    w0 = self._pvec("w0", self.rw_w0.ap[l, d], 2)
    a0 = self._pvec("a0", self.rw_a0.ap[l, d], 2)
    k_k = self._pvec("k_k", self.rw_k_k.ap[l], 2)
    k_a = self._pvec("k_a", self.rw_k_a.ap[l], 2)
    r_k = self._pvec("r_k", self.rw_r_k.ap[l], 2)
    gn_g = self._pvec("gn_g", self.rw_gn_g.ap[l], 2)
    gn_b = self._pvec("gn_b", self.rw_gn_b.ap[l], 2)
    w2s = k.sb("w2s", [128, 256], BF16)
    a2s = k.sb("a2s", [128, 256], BF16)
    g2s = k.sb("g2s", [128, 256], BF16)
    k.op("pool", lambda e: e.dma_start(out=w2s[0:64, :], in_=self.rw_w2.ap[l, d]), writes=[w2s], dma=True)
    k.op("pool", lambda e: e.dma_start(out=a2s[64:128, :], in_=self.rw_a2.ap[l, d]), writes=[a2s], dma=True)
    k.op("pool", lambda e: e.dma_start(out=g2s[:, :], in_=self.rw_g2.ap[l]), writes=[g2s], dma=True)

    U = [k.sb("rU%d" % i, [128, 8, 514], F32) for i in range(2)]
    S = k.sb("rS", [128, 8, 512], F32)
    wlt = k.sb("wlt", [128, 512], BF16)
    alb = k.sb("alb", [128, 512], BF16)
    sgl = k.sb("sgl", [128, 512], BF16)
    f32t = lambda n_: k.sb(n_, [128, 8, 64], F32)
    bft = lambda n_: k.sb(n_, [128, 8, 64], BF16)
    sgw, asig, kx, rn, cs, dcs, tmp, E, Etrue = [f32t("r_" + z) for z in ("sgw", "asig", "kx", "rn", "cs", "dcs", "tmp", "E", "Etrue")]
    kkn, bvec, kmod = f32t("kkn"), f32t("bvec"), f32t("kmod")
    sqk = bft("sqk")
    rt, kt, bt, at, Rtrue, Atrue, Bh, Kh, vb = [[bft("r_%s%d" % (z, hp)) for hp in range(2)] for z in ("rt", "kt", "bt", "at", "Rtrue", "Atrue", "Bh", "Kh", "vb")]
    WC = [k.sb("WC%d" % hp, [128, 8], F32) for hp in range(2)]
    Atm, Bhtm, Khtm, Vtm = [[bft("r_%s%d" % (z, hp)) for hp in range(2)] for z in ("Atm", "Bhtm", "Khtm", "Vtm")]
    Zs, Ns, Zs2, Ns2, Aak, Arb, Ark, X, AVs, PaT, Us = [[bft("r_%s%d" % (z, hp)) for hp in range(2)] for z in ("Zs", "Ns", "Zs2", "Ns2", "Aak", "Arb", "Ark", "X", "AVs", "PaT", "Us")]
    Qs = [f32t("Qs%d" % hp) for hp in range(2)]
    Mst = [k.sb("Mst%d" % hp, [128, 64], F32) for hp in range(2)]
    Mbf = [k.sb("Mbf%d" % hp, [128, 64], BF16) for hp in range(2)]
    ysb = [k.sb("ysb%d" % hp, [128, 512], F32) for hp in range(2)]
    bon = k.sb("bon", [128, 512], F32)
    gat = k.sb("gat", [128, 512], F32)
    yf_in = [k.sb("yfin%d" % hp, [128, 512], F32) for hp in range(2)]
    PS = [k.ps("rps%d" % i, [128, 512], F32) for i in range(2)]
    PY = [[k.ps("rpy%d%d" % (i, j), [128, 512], F32) for j in range(2)] for i in range(2)]
    PSB = [k.ps("rpsb%d" % i, [128, 8, 64], BF16) for i in range(2)]
    psi = [0]
    psbi = [0]

    def nps():
        p = PS[psi[0] % 2]
        psi[0] += 1
        return p

    def npsb():
        p = PSB[psbi[0] % 2]
        psbi[0] += 1
        return p

    for hp in range(2):
        k.op("dve", lambda e, hp=hp: e.memset(Mst[hp][:, :], 0.0), writes=[Mst[hp]])
        k.op("dve", lambda e, hp=hp: e.memset(Mbf[hp][:, :], 0.0), writes=[Mbf[hp]])

    M_SL, M_LE, M_SG, M_GE = 0, 1, 2, 3
    if d == 0:
        mZ, mN, mI = M_SL, M_SG, M_LE
    else:
        mZ, mN, mI = M_SG, M_SL, M_GE

    tiles = self.tok_tiles()
    lat = tiles[1:]
    order = [tiles[0]] + (lat if d == 0 else lat[::-1])
    uTv = self.uT.ap[0:1024, :].rearrange("(c p) t -> p c t", p=128)

    def v3(t, P, nch):
        return t[P, 0:nch, :]

    def v2(t, P, n):
        return t[P, :, :].rearrange("p c t -> p (c t)")[:, 0:n]

    def tile_body(ti, tok0, n, stream):
        nch = n // 64
        seq0, seq1 = (0, CTX) if stream == 1 else (CTX, NT)
        ub = U[ti % 2]
        lo = max(tok0 - 1, seq0)
        hi = min(tok0 + n + 1, seq1)
        if lo > tok0 - 1:
            k.op("pool", lambda e: e.memset(ub[:, :, 0:1], 0.0), writes=[ub])
        if hi < tok0 + n + 1:
            k.op("pool", lambda e: e.memset(ub[:, :, n + 1:n + 2], 0.0), writes=[ub])
        k.op("sp", lambda e: e.dma_start(out=ub[:, :, lo - (tok0 - 1):hi - (tok0 - 1)], in_=uTv[:, :, lo:hi]), writes=[ub], dma=True)
        fl = lambda t_: t_[:, :, :].rearrange("p c t -> p (c t)")[:, 0:n]

        def shift(c):
            k.op("dve", lambda e: e.tensor_scalar(S[:, c, :n], ub[:, c, 1:n + 1], cmix[:, c:c + 1], None, ALU.mult), reads=[ub, cmix], writes=[S])
            k.op("dve", lambda e: e.scalar_tensor_tensor(out=S[:, c, :n], in0=ub[:, c, 0:n], scalar=mp[:, c:c + 1], in1=S[:, c, :n], op0=ALU.mult, op1=ALU.add), reads=[ub, mp, S], writes=[S])
            k.op("dve", lambda e: e.scalar_tensor_tensor(out=S[:, c, :n], in0=ub[:, c, 2:n + 2], scalar=mn[:, c:c + 1], in1=S[:, c, :n], op0=ALU.mult, op1=ALU.add), reads=[ub, mn, S], writes=[S])
        for c in range(8):
            if d == 1 and c == 7:
                continue
            shift(c)
        k.op("act", lambda e: e.activation(out=wlt[0:64, :n], in_=S[0:64, 6, :n], func=AF.Tanh), reads=[S], writes=[wlt])
        k.op("act", lambda e: e.copy(alb[64:128, :n], S[64:128, 6, :n]), reads=[S], writes=[alb])
        if d == 0:
            k.op("act", lambda e: e.activation(out=sgl[:, :n], in_=S[:, 7, :n], func=AF.Sigmoid), reads=[S], writes=[sgl])

        def prep(hp):
            p1 = nps()
            k.op("pe", lambda e: e.matmul(p1[:, :n], lhsT=w2s[0:64, hp * 128:(hp + 1) * 128], rhs=wlt[0:64, :n], start=True, stop=True), reads=[w2s, wlt], writes=[p1])
            k.op("act", lambda e: e.activation(out=fl(sgw), in_=p1[:, :n], func=AF.Sigmoid, bias=w0[:, hp:hp + 1]), reads=[p1, w0], writes=[sgw])
            p2 = nps()
            k.op("pe", lambda e: e.matmul(p2[:, :n], lhsT=a2s[64:128, hp * 128:(hp + 1) * 128], rhs=alb[64:128, :n], start=True, stop=True), reads=[a2s, alb], writes=[p2])
            k.op("act", lambda e: e.activation(out=fl(asig), in_=p2[:, :n], func=AF.Sigmoid, bias=a0[:, hp:hp + 1]), reads=[p2, a0], writes=[asig])
            k.op("dve", lambda e: e.tensor_scalar(fl(kx), S[:, 2 + hp, :n], k_k[:, hp:hp + 1], None, ALU.mult), reads=[S, k_k], writes=[kx])
            k.op("act", lambda e: e.activation(out=fl(sqk), in_=fl(kx), func=AF.Square), reads=[kx], writes=[sqk])
            p3 = nps()
            k.op("pe", lambda e: e.matmul(p3[:, :n], lhsT=self.blk[:, :], rhs=fl(sqk), start=True, stop=True), reads=[self.blk, sqk], writes=[p3])
            k.op("act", lambda e: e.activation(out=fl(rn), in_=p3[:, :n], func=AF.Sqrt, bias=1e-12), reads=[p3], writes=[rn])
            k.op("dve", lambda e: e.reciprocal(fl(rn), fl(rn)), reads=[rn], writes=[rn])
            k.op("dve", lambda e: e.tensor_tensor(fl(kkn), fl(kx), fl(rn), ALU.mult), reads=[kx, rn], writes=[kkn])
            k.op("pool", lambda e: e.tensor_tensor(fl(bvec), fl(kkn), fl(asig), ALU.mult), reads=[kkn, asig], writes=[bvec])
            k.op("dve", lambda e: e.tensor_scalar(fl(tmp), fl(asig), -1.0, k_a[:, hp:hp + 1], ALU.add, ALU.mult), reads=[asig, k_a], writes=[tmp])
            k.op("dve", lambda e: e.scalar_tensor_tensor(out=fl(kmod), in0=fl(tmp), scalar=1.0, in1=S[:, 2 + hp, :n], op0=ALU.add, op1=ALU.mult), reads=[tmp, S], writes=[kmod])
            k.op("dve", lambda e: e.tensor_tensor_scan(fl(cs), self.reset[:, :n], fl(sgw), 0.0, ALU.mult, ALU.add), reads=[self.reset, sgw], writes=[cs])
            if d == 1:
                k.op("dve", lambda e: e.tensor_tensor(fl(tmp), fl(sgw), fl(cs), ALU.subtract), reads=[sgw, cs], writes=[tmp])
                k.op("dve", lambda e: e.tensor_tensor(cs[:, :nch, :], tmp[:, :nch, :], cs[:, :nch, 63:64].to_broadcast([128, nch, 64]), ALU.add), reads=[tmp, cs], writes=[cs])
            endi = 63 if d == 0 else 0
            k.op("pool", lambda e: e.tensor_tensor(dcs[:, :nch, :], cs[:, :nch, :], cs[:, :nch, 32:33].to_broadcast([128, nch, 64]), ALU.subtract), reads=[cs], writes=[dcs])
            k.op("act", lambda e: e.activation(out=fl(E), in_=fl(dcs), func=AF.Exp, scale=s), reads=[dcs], writes=[E])
            k.op("dve", lambda e: e.tensor_tensor(fl(rt[hp]), S[:, hp, :n], fl(E), ALU.mult), reads=[S, E], writes=[rt[hp]])
            k.op("pool", lambda e: e.tensor_tensor(fl(tmp), fl(dcs), fl(sgw), ALU.subtract), reads=[dcs, sgw], writes=[tmp])
            k.op("act", lambda e: e.activation(out=fl(E), in_=fl(tmp), func=AF.Exp, scale=s), reads=[tmp], writes=[E])
            k.op("dve", lambda e: e.scalar_tensor_tensor(out=fl(at[hp]), in0=fl(kkn), scalar=-1.0, in1=fl(E), op0=ALU.mult, op1=ALU.mult), reads=[kkn, E], writes=[at[hp]])
            k.op("act", lambda e: e.activation(out=fl(E), in_=fl(dcs), func=AF.Exp, scale=-s), reads=[dcs], writes=[E])
            k.op("dve", lambda e: e.tensor_tensor(fl(kt[hp]), fl(kmod), fl(E), ALU.mult), reads=[kmod, E], writes=[kt[hp]])
            k.op("pool", lambda e: e.tensor_tensor(fl(bt[hp]), fl(bvec), fl(E), ALU.mult), reads=[bvec, E], writes=[bt[hp]])
            k.op("act", lambda e: e.activation(out=fl(Etrue), in_=fl(cs), func=AF.Exp, scale=s), reads=[cs], writes=[Etrue])
            k.op("dve", lambda e: e.tensor_tensor(fl(Rtrue[hp]), S[:, hp, :n], fl(Etrue), ALU.mult), reads=[S, Etrue], writes=[Rtrue[hp]])
            k.op("dve", lambda e: e.tensor_copy(WC[hp][:, :nch], Etrue[:, :nch, endi]), reads=[Etrue], writes=[WC[hp]])
            k.op("pool", lambda e: e.tensor_tensor(fl(tmp), fl(cs), fl(sgw), ALU.subtract), reads=[cs, sgw], writes=[tmp])
            k.op("act", lambda e: e.activation(out=fl(E), in_=fl(tmp), func=AF.Exp, scale=s), reads=[tmp], writes=[E])
            k.op("dve", lambda e: e.scalar_tensor_tensor(out=fl(Atrue[hp]), in0=fl(kkn), scalar=-1.0, in1=fl(E), op0=ALU.mult, op1=ALU.mult), reads=[kkn, E], writes=[Atrue[hp]])
            k.op("pool", lambda e: e.tensor_tensor(tmp[:, :nch, :], cs[:, :nch, endi:endi + 1].to_broadcast([128, nch, 64]), cs[:, :nch, :], ALU.subtract), reads=[cs], writes=[tmp])
            k.op("act", lambda e: e.activation(out=fl(E), in_=fl(tmp), func=AF.Exp, scale=s), reads=[tmp], writes=[E])
            k.op("dve", lambda e: e.tensor_tensor(fl(Bh[hp]), fl(bvec), fl(E), ALU.mult), reads=[bvec, E], writes=[Bh[hp]])
            k.op("pool", lambda e: e.tensor_tensor(fl(Kh[hp]), fl(kmod), fl(E), ALU.mult), reads=[kmod, E], writes=[Kh[hp]])
            k.op("act", lambda e: e.copy(fl(vb[hp]), S[:, 4 + hp, :n]), reads=[S], writes=[vb[hp]])
            if d == 0:
                k.op("dve", lambda e: e.scalar_tensor_tensor(out=fl(sqk), in0=S[:, hp, :n], scalar=r_k[:, hp:hp + 1], in1=S[:, 2 + hp, :n], op0=ALU.mult, op1=ALU.mult), reads=[S, r_k], writes=[sqk])
                p4 = nps()
                k.op("pe", lambda e: e.matmul(p4[:, :n], lhsT=self.blk[:, :], rhs=fl(sqk), start=True, stop=True), reads=[self.blk, sqk], writes=[p4])
                k.op("dve", lambda e: e.tensor_tensor(bon[:, :n], p4[:, :n], S[:, 4 + hp, :n], ALU.mult), reads=[p4, S], writes=[bon])
                k.op("pool", lambda e: e.dma_start(out=self.rw_bonus.ap[hp * 128:(hp + 1) * 128, tok0:tok0 + n], in_=bon[:, :n]), reads=[bon], dma=True)
                p5 = nps()
                k.op("pe", lambda e: e.matmul(p5[:, :n], lhsT=g2s[:, hp * 128:(hp + 1) * 128], rhs=sgl[:, :n], start=True, stop=True), reads=[g2s, sgl], writes=[p5])
                k.op("act", lambda e: e.copy(gat[:, :n], p5[:, :n]), reads=[p5], writes=[gat])
                k.op("pool", lambda e: e.dma_start(out=self.rw_gate.ap[hp * 128:(hp + 1) * 128, tok0:tok0 + n], in_=gat[:, :n]), reads=[gat], dma=True)

        def units(dst_ps, lt, rt_, P, reads):
            pv = dst_ps[:, :].rearrange("p (c t) -> p c t", t=64)
            for ch in range(nch):
                k.op("pe", lambda e, ch=ch: e.matmul(pv[P, ch, :], lhsT=lt[P, ch, :], rhs=rt_[P, ch, :], start=True, stop=True), reads=reads, writes=[dst_ps], pe_acc=True)
            return pv

        def head_block(hp, hl):
            P = slice(hl * 64, hl * 64 + 64)

            def tm(src, dst):
                pb = npsb()
                for ch in range(nch):
                    k.op("pe", lambda e, ch=ch: e.transpose(pb[P, ch, :], src[hp][P, ch, :], self.ident[P, P]), reads=[src[hp], self.ident], writes=[pb], pe_acc=True)
                k.op("act", lambda e: e.copy(dst[hp][P, :nch, :], pb[P, :nch, :]), reads=[pb], writes=[dst[hp]])
            tm(Atrue, Atm); tm(Bh, Bhtm); tm(Kh, Khtm); tm(vb, Vtm)

            def pair(dst, lt, rt_, mask, eng="dve"):
                pp = nps()
                pv = units(pp, lt[hp], rt_[hp], P, [lt[hp], rt_[hp]])
                mv = self.masks[:, mask, :].rearrange("p (c t) -> p c t", t=64)
                k.op(eng, lambda e: e.tensor_tensor(dst[hp][P, :nch, :], pv[P, :nch, :], mv[P, :nch, :], ALU.mult), reads=[pp, self.masks], writes=[dst[hp]])
            pair(Zs, bt, at, mZ)
            pair(Ns, at, bt, mN)
            pair(Aak, kt, at, mZ)
            pair(Arb, bt, rt, mI)
            pair(Ark, kt, rt, mI)
            idv = self.id8[:, :].rearrange("p (c t) -> p c t", t=64)
            k.op("pool", lambda e: e.tensor_tensor(X[hp][P, :nch, :], Zs[hp][P, :nch, :], idv[P, :nch, :], ALU.add), reads=[Zs[hp], self.id8], writes=[X[hp]])
            Zc, Nc, Zn, Nn = Zs[hp], Ns[hp], Zs2[hp], Ns2[hp]
            for lev in range(1, 6):
                last = lev == 5

                def level(Zc, Nc, Zn, Nn, last):
                    if not last:
                        pz = nps()
                        pzv = units(pz, Nc, Zc, P, [Nc, Zc])
                    pn = nps()
                    pnv = units(pn, Zc, Nc, P, [Nc, Zc])
                    if not last:
                        k.op("act", lambda e: e.copy(Zn[P, :nch, :], pzv[P, :nch, :]), reads=[pz], writes=[Zn])
                    k.op("dve", lambda e: e.tensor_copy(Nn[P, :nch, :], pnv[P, :nch, :]), reads=[pn], writes=[Nn])
                    px = nps()
                    pxv = units(px, Nn, X[hp], P, [Nn, X[hp]])
                    k.op("dve", lambda e: e.tensor_tensor(X[hp][P, :nch, :], pxv[P, :nch, :], X[hp][P, :nch, :], ALU.add), reads=[px, X[hp]], writes=[X[hp]])
                level(Zc, Nc, Zn, Nn, last)
                Zc, Nc, Zn, Nn = Zn, Nn, Zc, Nc
            pa = nps()
            pav = units(pa, Aak[hp], Vtm[hp], P, [Aak[hp], Vtm[hp]])
            k.op("act", lambda e: e.copy(AVs[hp][P, :nch, :], pav[P, :nch, :]), reads=[pa], writes=[AVs[hp]])
            pq = nps()
            pqv = units(pq, X[hp], AVs[hp], P, [X[hp], AVs[hp]])
            k.op("act", lambda e: e.copy(Qs[hp][P, :nch, :], pqv[P, :nch, :]), reads=[pq], writes=[Qs[hp]])
            pp_ = nps()
            ppv_ = units(pp_, Atm[hp], X[hp], P, [Atm[hp], X[hp]])
            k.op("dve", lambda e: e.tensor_copy(PaT[hp][P, :nch, :], ppv_[P, :nch, :]), reads=[pp_], writes=[PaT[hp]])

        for hp in range(2):
            prep(hp)
            for hl in range(2):
                head_block(hp, hl)

        def seq_step(ch, hp, hl):
            P = slice(hl * 64, hl * 64 + 64)
            pyb = PY[hp][hl]
            pu_ = nps()
            k.op("pe", lambda e: e.matmul(pu_[P, 0:64], lhsT=PaT[hp][P, ch, :], rhs=Mbf[hp][P, :], start=True, stop=True), reads=[PaT[hp], Mbf[hp]], writes=[pu_])
            k.op("dve", lambda e: e.tensor_tensor(Us[hp][P, ch, :], pu_[P, 0:64], Qs[hp][P, ch, :], ALU.add), reads=[pu_, Qs[hp]], writes=[Us[hp]])
            pyv = pyb[:, :].rearrange("p (c t) -> p c t", t=64)
            k.op("pe", lambda e: e.matmul(pyv[P, ch, :], lhsT=Mbf[hp][P, :], rhs=Rtrue[hp][P, ch, :], start=True, stop=False), reads=[Mbf[hp], Rtrue[hp]], writes=[pyb], pe_acc=True)
            k.op("pe", lambda e: e.matmul(pyv[P, ch, :], lhsT=Vtm[hp][P, ch, :], rhs=Ark[hp][P, ch, :], start=False, stop=False), reads=[Vtm[hp], Ark[hp]], writes=[pyb], pe_acc=True)
            k.op("pe", lambda e: e.matmul(pyv[P, ch, :], lhsT=Us[hp][P, ch, :], rhs=Arb[hp][P, ch, :], start=False, stop=True), reads=[Us[hp], Arb[hp]], writes=[pyb], pe_acc=True)
            pm_ = nps()
            k.op("pe", lambda e: e.matmul(pm_[P, 0:64], lhsT=Bhtm[hp][P, ch, :], rhs=Us[hp][P, ch, :], start=True, stop=False), reads=[Bhtm[hp], Us[hp]], writes=[pm_], pe_acc=True)
            k.op("pe", lambda e: e.matmul(pm_[P, 0:64], lhsT=Khtm[hp][P, ch, :], rhs=Vtm[hp][P, ch, :], start=False, stop=True), reads=[Khtm[hp], Vtm[hp]], writes=[pm_], pe_acc=True)
            k.op("dve", lambda e: e.scalar_tensor_tensor(out=Mst[hp][P, :], in0=Mst[hp][P, :], scalar=WC[hp][P, ch:ch + 1], in1=pm_[P, 0:64], op0=ALU.mult, op1=ALU.add), reads=[Mst[hp], WC[hp], pm_], writes=[Mst[hp]])
            k.op("act", lambda e: e.copy(Mbf[hp][P, :], Mst[hp][P, :]), reads=[Mst[hp]], writes=[Mbf[hp]])

        chs = range(nch) if d == 0 else range(nch - 1, -1, -1)
        for ch in chs:
            for hp in range(2):
                for hl in range(2):
                    seq_step(ch, hp, hl)

        def outp(hp):
            for hl in range(2):
                P = slice(hl * 64, hl * 64 + 64)
                pp = PY[hp][hl]
                if hl == 0:
                    k.op("dve", lambda e, P=P, pp=pp: e.tensor_copy(ysb[hp][P, :n], pp[P, :n]), reads=[pp], writes=[ysb[hp]])
                else:
                    k.op("act", lambda e, P=P, pp=pp: e.copy(ysb[hp][P, :n], pp[P, :n]), reads=[pp], writes=[ysb[hp]])
            if d == 0:
                k.op("pool", lambda e: e.dma_start(out=self.rw_yf.ap[hp * 128:(hp + 1) * 128, tok0:tok0 + n], in_=ysb[hp][:, :n]), reads=[ysb[hp]], dma=True)
            else:
                self.rw_finish(l, hp, tok0, n, ysb[hp], yf_in[hp], bon, gat, gn_g, gn_b, nps, sqk, tmp, kx, rn)
        for hp in range(2):
            outp(hp)

    for ti, (tok0, n, stream) in enumerate(order):
        tile_body(ti, tok0, n, stream)
    k.barrier()
    k.emit()
    k.free_to(m)


def rw_finish(self, l, hp, tok0, n, yb, yf, bon, gat, gn_g, gn_b, nps, sqk, tmp, kx, rn):
    k = self.k
    fl = lambda t_: t_[:, :, :].rearrange("p c t -> p (c t)")[:, 0:n]
    k.op("sp", lambda e: e.dma_start(out=yf[:, :n], in_=self.rw_yf.ap[hp * 128:(hp + 1) * 128, tok0:tok0 + n]), writes=[yf], dma=True)
    k.op("sp", lambda e: e.dma_start(out=bon[:, :n], in_=self.rw_bonus.ap[hp * 128:(hp + 1) * 128, tok0:tok0 + n]), writes=[bon], dma=True)
    k.op("sp", lambda e: e.dma_start(out=gat[:, :n], in_=self.rw_gate.ap[hp * 128:(hp + 1) * 128, tok0:tok0 + n]), writes=[gat], dma=True)
    k.op("dve", lambda e: e.tensor_tensor(yb[:, :n], yb[:, :n], yf[:, :n], ALU.add), reads=[yb, yf], writes=[yb])
    k.op("act", lambda e: e.copy(fl(sqk), yb[:, :n]), reads=[yb], writes=[sqk])
    p1 = nps()
    k.op("pe", lambda e: e.matmul(p1[:, :n], lhsT=self.blk[:, :], rhs=fl(sqk), start=True, stop=True), reads=[self.blk, sqk], writes=[p1])
    k.op("dve", lambda e: e.scalar_tensor_tensor(out=fl(kx), in0=p1[:, :n], scalar=-1.0 / 64, in1=yb[:, :n], op0=ALU.mult, op1=ALU.add), reads=[p1, yb], writes=[kx])
    k.op("act", lambda e: e.copy(fl(sqk), fl(kx)), reads=[kx], writes=[sqk])
    p1b = nps()
    k.op("pe", lambda e: e.matmul(p1b[:, :n], lhsT=self.blk[:, :], rhs=fl(sqk), start=True, stop=True), reads=[self.blk, sqk], writes=[p1b])
    k.op("dve", lambda e: e.scalar_tensor_tensor(out=fl(kx), in0=p1b[:, :n], scalar=-1.0 / 64, in1=fl(kx), op0=ALU.mult, op1=ALU.add), reads=[p1b, kx], writes=[kx])
    k.op("act", lambda e: e.activation(out=fl(sqk), in_=fl(kx), func=AF.Square), reads=[kx], writes=[sqk])
    p2 = nps()
    k.op("pe", lambda e: e.matmul(p2[:, :n], lhsT=self.blk[:, :], rhs=fl(sqk), start=True, stop=True), reads=[self.blk, sqk], writes=[p2])
    k.op("act", lambda e: e.activation(out=fl(rn), in_=p2[:, :n], func=AF.Ln, scale=1.0 / 64, bias=64e-5), reads=[p2], writes=[rn])
    k.op("act", lambda e: e.activation(out=fl(rn), in_=fl(rn), func=AF.Exp, scale=-0.5), reads=[rn], writes=[rn])
    k.op("dve", lambda e: e.tensor_tensor(fl(kx), fl(kx), fl(rn), ALU.mult), reads=[kx, rn], writes=[kx])
    k.op("dve", lambda e: e.tensor_scalar(fl(kx), fl(kx), gn_g[:, hp:hp + 1], gn_b[:, hp:hp + 1], ALU.mult, ALU.add), reads=[kx, gn_g, gn_b], writes=[kx])
    k.op("pool", lambda e: e.tensor_tensor(fl(kx), fl(kx), bon[:, :n], ALU.add), reads=[kx, bon], writes=[kx])
    k.op("dve", lambda e: e.tensor_tensor(fl(kx), fl(kx), gat[:, :n], ALU.mult), reads=[kx, gat], writes=[kx])
    k.op("pool", lambda e: e.dma_start(out=self.mixT.ap[hp * 128:(hp + 1) * 128, tok0:tok0 + n], in_=fl(kx)), reads=[kx], dma=True)


MK.rw_consts = _rw_consts
MK.load_consts2 = _load_consts2
MK._pvec = _pvec
MK.phase_rwkv = phase_rwkv_old if os.environ.get('RW_OLD', '0') == '1' else phase_rwkv
MK.rw_finish = rw_finish


GLA_OFF = 2312


def phase_gla(self, l, d):
    k = self.k
    m = k.mark()
    NT = self.NT
    s = 1.0 / 16.0
    qscale = 32 ** -0.5
    if not hasattr(self, "gla_yf"):
        kind = "ExternalOutput" if "gla_yf" in self.dbg else "Internal"
        self.gla_yf = k.dram("gla_yf", [256, NT], F32, kind=kind)
    ga2f = k.sb("ga2f", [16, 2, 128], F32)
    ga2p = k.sb("ga2p", [16, 2, 128], BF16)
    gbp = k.sb("gbp", [128, 2], F32)
    k.op("dve", lambda e: e.memset(ga2f[:, :, :], 0.0), writes=[ga2f])
    k.op("dve", lambda e: e.memset(gbp[:, :], 0.0), writes=[gbp])
    for h in range(4):
        hp, hl = h // 2, h % 2
        k.op("sp", lambda e, h=h, hp=hp, hl=hl: e.dma_start(out=ga2f[:, hp, hl * 64:hl * 64 + 32], in_=self.gla_ga2.ap[l, d, :, h * 32:(h + 1) * 32]), writes=[ga2f], dma=True)
        k.op("sp", lambda e, h=h, hp=hp, hl=hl: e.dma_start(out=gbp[hl * 64:hl * 64 + 32, hp:hp + 1], in_=self.gla_gb.ap[l, d, h * 32:(h + 1) * 32].rearrange("(p o) -> p o", o=1), allow_slow_non_contiguous=True), writes=[gbp], dma=True)
    k.op("dve", lambda e: e.tensor_copy(ga2p[:, :, :], ga2f[:, :, :]), reads=[ga2f], writes=[ga2p])
    ng = self._pvec("gng", self.gla_norm_g.ap[l], 2)

    f32t = lambda n_: k.sb(n_, [128, 8, 64], F32)
    bft = lambda n_: k.sb(n_, [128, 8, 64], BF16)
    q = [[f32t("gq%d%d" % (i, hp)) for hp in range(2)] for i in range(2)]
    kk_ = [[f32t("gk%d%d" % (i, hp)) for hp in range(2)] for i in range(2)]
    vv = [[f32t("gv%d%d" % (i, hp)) for hp in range(2)] for i in range(2)]
    glf = [k.sb("glf%d" % i, [16, 512], F32) for i in range(2)]
    glb = k.sb("glb", [16, 512], BF16)
    for i in range(2):
        for hp in range(2):
            k.op("pool", lambda e, i=i, hp=hp: e.memset(q[i][hp][:, :, :], 0.0), writes=[q[i][hp]])
            k.op("pool", lambda e, i=i, hp=hp: e.memset(kk_[i][hp][:, :, :], 0.0), writes=[kk_[i][hp]])
    lg, cs, dcs, tmp, E, Etrue = [f32t("g_" + z) for z in ("lg", "cs", "dcs", "tmp", "E", "Etrue")]
    qt, kt, Qtrue, Kh, vb, Khtm, Vtm, Ark = [[bft("g_%s%d" % (z, hp)) for hp in range(2)] for z in ("qt", "kt", "Qtrue", "Kh", "vb", "Khtm", "Vtm", "Ark")]
    WC = [k.sb("gWC%d" % hp, [128, 8], F32) for hp in range(2)]
    Mst = [k.sb("gMst%d" % hp, [128, 64], F32) for hp in range(2)]
    Mbf = [k.sb("gMbf%d" % hp, [128, 64], BF16) for hp in range(2)]
    ysb = [k.sb("gysb%d" % hp, [128, 512], F32) for hp in range(2)]
    yfin = [k.sb("gyfin%d" % hp, [128, 512], F32) for hp in range(2)]
    rin = [k.sb("grin%d" % hp, [128, 512], F32) for hp in range(2)]
    sq = k.sb("gsq", [128, 512], BF16)
    rn = k.sb("grn", [128, 512], F32)
    PS = [k.ps("gps%d" % i, [128, 512], F32) for i in range(2)]
    PY = [[k.ps("gpy%d%d" % (i, j), [128, 512], F32) for j in range(2)] for i in range(2)]
    PSB = [k.ps("gpsb%d" % i, [128, 8, 64], BF16) for i in range(2)]
    psi = [0]; psbi = [0]

    def nps():
        p = PS[psi[0] % 2]; psi[0] += 1; return p

    def npsb():
        p = PSB[psbi[0] % 2]; psbi[0] += 1; return p
    for hp in range(2):
        k.op("dve", lambda e, hp=hp: e.memset(Mst[hp][:, :], 0.0), writes=[Mst[hp]])
        k.op("dve", lambda e, hp=hp: e.memset(Mbf[hp][:, :], 0.0), writes=[Mbf[hp]])
    mI = 1 if d == 0 else 3
    tiles = self.tok_tiles()
    lat = tiles[1:]
    order = [tiles[0]] + (lat if d == 0 else lat[::-1])
    O = GLA_OFF

    def tile_body(ti, tok0, n, stream):
        nch = n // 64
        b = ti % 2
        fl = lambda t_: t_[:, :, :].rearrange("p c t -> p (c t)")[:, 0:n]
        for h in range(4):
            hp, hl = h // 2, h % 2
            P32 = slice(hl * 64, hl * 64 + 32)
            k.op("sp", lambda e, h=h, hp=hp, P32=P32: e.dma_start(out=fl(q[b][hp])[P32, :], in_=self.uT.ap[O + h * 32:O + (h + 1) * 32, tok0:tok0 + n]), writes=[q[b][hp]], dma=True)
            k.op("sp", lambda e, h=h, hp=hp, P32=P32: e.dma_start(out=fl(kk_[b][hp])[P32, :], in_=self.uT.ap[O + 128 + h * 32:O + 128 + (h + 1) * 32, tok0:tok0 + n]), writes=[kk_[b][hp]], dma=True)
        for hp in range(2):
            k.op("sp", lambda e, hp=hp: e.dma_start(out=fl(vv[b][hp]), in_=self.uT.ap[O + 256 + hp * 128:O + 256 + (hp + 1) * 128, tok0:tok0 + n]), writes=[vv[b][hp]], dma=True)
        k.op("sp", lambda e: e.dma_start(out=glf[b][:, :n], in_=self.uT.ap[O + 512:O + 528, tok0:tok0 + n]), writes=[glf[b]], dma=True)
        k.op("act", lambda e: e.copy(glb[:, :n], glf[b][:, :n]), reads=[glf[b]], writes=[glb])

        def prep(hp):
            p1 = nps()
            k.op("pe", lambda e: e.matmul(p1[:, :n], lhsT=ga2p[:, hp, :], rhs=glb[:, :n], start=True, stop=True), reads=[ga2p, glb], writes=[p1])
            k.op("act", lambda e: e.activation(out=fl(tmp), in_=p1[:, :n], func=AF.Sigmoid, bias=gbp[:, hp:hp + 1]), reads=[p1, gbp], writes=[tmp])
            k.op("act", lambda e: e.activation(out=fl(lg), in_=fl(tmp), func=AF.Ln), reads=[tmp], writes=[lg])
            k.op("dve", lambda e: e.tensor_tensor_scan(fl(cs), self.reset[:, :n], fl(lg), 0.0, ALU.mult, ALU.add), reads=[self.reset, lg], writes=[cs])
            if d == 1:
                k.op("dve", lambda e: e.tensor_tensor(fl(tmp), fl(lg), fl(cs), ALU.subtract), reads=[lg, cs], writes=[tmp])
                k.op("dve", lambda e: e.tensor_tensor(cs[:, :nch, :], tmp[:, :nch, :], cs[:, :nch, 63:64].to_broadcast([128, nch, 64]), ALU.add), reads=[tmp, cs], writes=[cs])
            endi = 63 if d == 0 else 0
            k.op("pool", lambda e: e.tensor_tensor(dcs[:, :nch, :], cs[:, :nch, :], cs[:, :nch, 32:33].to_broadcast([128, nch, 64]), ALU.subtract), reads=[cs], writes=[dcs])
            k.op("act", lambda e: e.activation(out=fl(E), in_=fl(dcs), func=AF.Exp, scale=s), reads=[dcs], writes=[E])
            k.op("dve", lambda e: e.scalar_tensor_tensor(out=fl(qt[hp]), in0=fl(q[b][hp]), scalar=qscale, in1=fl(E), op0=ALU.mult, op1=ALU.mult), reads=[q[b][hp], E], writes=[qt[hp]])
            k.op("act", lambda e: e.activation(out=fl(E), in_=fl(dcs), func=AF.Exp, scale=-s), reads=[dcs], writes=[E])
            k.op("dve", lambda e: e.tensor_tensor(fl(kt[hp]), fl(kk_[b][hp]), fl(E), ALU.mult), reads=[kk_[b][hp], E], writes=[kt[hp]])
            k.op("act", lambda e: e.activation(out=fl(Etrue), in_=fl(cs), func=AF.Exp, scale=s), reads=[cs], writes=[Etrue])
            k.op("dve", lambda e: e.scalar_tensor_tensor(out=fl(Qtrue[hp]), in0=fl(q[b][hp]), scalar=qscale, in1=fl(Etrue), op0=ALU.mult, op1=ALU.mult), reads=[q[b][hp], Etrue], writes=[Qtrue[hp]])
            k.op("dve", lambda e: e.tensor_copy(WC[hp][:, :nch], Etrue[:, :nch, endi]), reads=[Etrue], writes=[WC[hp]])
            k.op("pool", lambda e: e.tensor_tensor(tmp[:, :nch, :], cs[:, :nch, endi:endi + 1].to_broadcast([128, nch, 64]), cs[:, :nch, :], ALU.subtract), reads=[cs], writes=[tmp])
            k.op("act", lambda e: e.activation(out=fl(E), in_=fl(tmp), func=AF.Exp, scale=s), reads=[tmp], writes=[E])
            k.op("dve", lambda e: e.tensor_tensor(fl(Kh[hp]), fl(kk_[b][hp]), fl(E), ALU.mult), reads=[kk_[b][hp], E], writes=[Kh[hp]])
            k.op("act", lambda e: e.copy(fl(vb[hp]), fl(vv[b][hp])), reads=[vv[b][hp]], writes=[vb[hp]])

        def head_block(hp, hl):
            P = slice(hl * 64, hl * 64 + 64)

            def tm(src, dst):
                pb = npsb()
                for ch in range(nch):
                    k.op("pe", lambda e, ch=ch: e.transpose(pb[P, ch, :], src[hp][P, ch, :], self.ident[P, P]), reads=[src[hp], self.ident], writes=[pb], pe_acc=True)
                k.op("act", lambda e: e.copy(dst[hp][P, :nch, :], pb[P, :nch, :]), reads=[pb], writes=[dst[hp]])
            tm(Kh, Khtm); tm(vb, Vtm)
            pp = nps()
            pv = pp[:, :].rearrange("p (c t) -> p c t", t=64)
            for ch in range(nch):
                k.op("pe", lambda e, ch=ch: e.matmul(pv[P, ch, :], lhsT=kt[hp][P, ch, :], rhs=qt[hp][P, ch, :], start=True, stop=True), reads=[kt[hp], qt[hp]], writes=[pp], pe_acc=True)
            mv = self.masks[:, mI, :].rearrange("p (c t) -> p c t", t=64)
            k.op("dve", lambda e: e.tensor_tensor(Ark[hp][P, :nch, :], pv[P, :nch, :], mv[P, :nch, :], ALU.mult), reads=[pp, self.masks], writes=[Ark[hp]])

        for hp in range(2):
            prep(hp)
            for hl in range(2):
                head_block(hp, hl)

        def seq_step(ch, hp, hl):
            P = slice(hl * 64, hl * 64 + 64)
            pyb = PY[hp][hl]
            pyv = pyb[:, :].rearrange("p (c t) -> p c t", t=64)
            k.op("pe", lambda e: e.matmul(pyv[P, ch, :], lhsT=Mbf[hp][P, :], rhs=Qtrue[hp][P, ch, :], start=True, stop=False), reads=[Mbf[hp], Qtrue[hp]], writes=[pyb], pe_acc=True)
            k.op("pe", lambda e: e.matmul(pyv[P, ch, :], lhsT=Vtm[hp][P, ch, :], rhs=Ark[hp][P, ch, :], start=False, stop=True), reads=[Vtm[hp], Ark[hp]], writes=[pyb], pe_acc=True)
            pm_ = nps()
            k.op("pe", lambda e: e.matmul(pm_[P, 0:64], lhsT=Khtm[hp][P, ch, :], rhs=Vtm[hp][P, ch, :], start=True, stop=True), reads=[Khtm[hp], Vtm[hp]], writes=[pm_])
            k.op("dve", lambda e: e.scalar_tensor_tensor(out=Mst[hp][P, :], in0=Mst[hp][P, :], scalar=WC[hp][P, ch:ch + 1], in1=pm_[P, 0:64], op0=ALU.mult, op1=ALU.add), reads=[Mst[hp], WC[hp], pm_], writes=[Mst[hp]])
            k.op("act", lambda e: e.copy(Mbf[hp][P, :], Mst[hp][P, :]), reads=[Mst[hp]], writes=[Mbf[hp]])

        chs = range(nch) if d == 0 else range(nch - 1, -1, -1)
        for ch in chs:
            for hp in range(2):
                for hl in range(2):
                    seq_step(ch, hp, hl)

        def outp(hp):
            for hl in range(2):
                P = slice(hl * 64, hl * 64 + 64)
                pp = PY[hp][hl]
                if hl == 0:
                    k.op("dve", lambda e, P=P, pp=pp: e.tensor_copy(ysb[hp][P, :n], pp[P, :n]), reads=[pp], writes=[ysb[hp]])
                else:
                    k.op("act", lambda e, P=P, pp=pp: e.copy(ysb[hp][P, :n], pp[P, :n]), reads=[pp], writes=[ysb[hp]])
            if d == 0:
                k.op("pool", lambda e: e.dma_start(out=self.gla_yf.ap[hp * 128:(hp + 1) * 128, tok0:tok0 + n], in_=ysb[hp][:, :n]), reads=[ysb[hp]], dma=True)
            else:
                yb, yf, rr = ysb[hp], yfin[hp], rin[hp]
                k.op("sp", lambda e: e.dma_start(out=yf[:, :n], in_=self.gla_yf.ap[hp * 128:(hp + 1) * 128, tok0:tok0 + n]), writes=[yf], dma=True)
                k.op("sp", lambda e: e.dma_start(out=rr[:, :n], in_=self.uT.ap[O + 528 + hp * 128:O + 528 + (hp + 1) * 128, tok0:tok0 + n]), writes=[rr], dma=True)
                k.op("dve", lambda e: e.tensor_tensor(yb[:, :n], yb[:, :n], yf[:, :n], ALU.add), reads=[yb, yf], writes=[yb])
                k.op("act", lambda e: e.activation(out=sq[:, :n], in_=yb[:, :n], func=AF.Square), reads=[yb], writes=[sq])
                p2 = nps()
                k.op("pe", lambda e: e.matmul(p2[:, :n], lhsT=self.blk[:, :], rhs=sq[:, :n], start=True, stop=True), reads=[self.blk, sq], writes=[p2])
                k.op("act", lambda e: e.activation(out=rn[:, :n], in_=p2[:, :n], func=AF.Sqrt, scale=1.0 / 64, bias=EPS), reads=[p2], writes=[rn])
                k.op("dve", lambda e: e.reciprocal(rn[:, :n], rn[:, :n]), reads=[rn], writes=[rn])
                k.op("dve", lambda e: e.scalar_tensor_tensor(out=yb[:, :n], in0=yb[:, :n], scalar=ng[:, hp:hp + 1], in1=rn[:, :n], op0=ALU.mult, op1=ALU.mult), reads=[yb, ng, rn], writes=[yb])
                k.op("act", lambda e: e.activation(out=rr[:, :n], in_=rr[:, :n], func=AF.Silu), reads=[rr], writes=[rr])
                k.op("dve", lambda e: e.tensor_tensor(yb[:, :n], yb[:, :n], rr[:, :n], ALU.mult), reads=[yb, rr], writes=[yb])
                k.op("pool", lambda e: e.dma_start(out=self.mixT.ap[768 + hp * 128:768 + (hp + 1) * 128, tok0:tok0 + n], in_=yb[:, :n]), reads=[yb], dma=True)
        for hp in range(2):
            outp(hp)

    for ti, (tok0, n, stream) in enumerate(order):
        tile_body(ti, tok0, n, stream)
    k.barrier()
    k.emit()
    k.free_to(m)


MK.phase_gla = phase_gla


SSM_OFF = 1024


def _ssm_decl(self):
    k = self.k
    NT = self.NT
    self.c_m128 = k.dram("c_m128", [5, 128, 128], F32, kind="ExternalInput")
    def S(n, s, dt=F32):
        kind = "ExternalOutput" if n in self.dbg else "Internal"
        return k.dram(n, s, dt, kind=kind)
    self.s_xtm = S("s_xtm", [NT, 768])
    self.s_ztm = S("s_ztm", [NT, 512])
    self.s_dttm = S("s_dttm", [NT, 8])
    self.s_bcT = S("s_bcT", [256, NT])
    self.s_yf = S("s_yf", [NT, 512])


def phase_ssm_conv(self, l):
    k = self.k
    m = k.mark()
    NT, T = self.NT, self.T
    cw = k.sb("cw", [128, 6, 9], F32)
    cbias = self._pvec("cbias", self.ssm_conv_b.ap[l], 6)
    for tap in range(9):
        k.op("sp", lambda e, tap=tap: e.dma_start(out=cw[:, :, tap], in_=self.ssm_conv_w.ap[l, tap // 3, tap % 3].rearrange("(c p) -> p c", p=128), allow_slow_non_contiguous=True), writes=[cw], dma=True)
    xin = [k.sb("cxin%d" % i, [128, 642], F32) for i in range(3)]
    acc = [k.sb("cacc%d" % i, [128, 512], F32) for i in range(2)]
    acc2 = [k.sb("cacc2%d" % i, [128, 512], F32) for i in range(2)]
    res = [k.sb("cres%d" % i, [128, 512], F32) for i in range(2)]
    zin = [k.sb("czin%d" % i, [128, 512], F32) for i in range(2)]
    dtin = [k.sb("cdtin%d" % i, [8, 512], F32) for i in range(2)]
    otm = [k.sb("cotm%d" % i, [128, 4, 128], F32) for i in range(2)]
    odt = [k.sb("codt%d" % i, [128, 4, 8], F32) for i in range(2)]
    PT = [k.ps("cpt%d" % i, [128, 4, 128], F32) for i in range(3)]
    pti = [0]
    cnt = [0]
    O = SSM_OFF

    def transpose_store(src, npart, dst_ap_fn, n, ob):
        pt = PT[pti[0] % 3]; pti[0] += 1
        nb = n // 128
        for tb in range(nb):
            k.op("pe", lambda e, tb=tb: e.transpose(pt[:, tb, :npart], src[:npart, tb * 128:(tb + 1) * 128], self.identf[:npart, :npart]), reads=[src, self.identf], writes=[pt], pe_acc=True)
        eng = "act" if cnt[0] % 2 else "dve"
        cnt[0] += 1
        if eng == "act":
            k.op("act", lambda e: e.copy(ob[:, :nb, :npart], pt[:, :nb, :npart]), reads=[pt], writes=[ob])
        else:
            k.op("dve", lambda e: e.tensor_copy(ob[:, :nb, :npart], pt[:, :nb, :npart]), reads=[pt], writes=[ob])
        k.op("pool", lambda e: e.dma_start(out=dst_ap_fn(nb), in_=ob[:, :nb, :npart]), reads=[ob], dma=True)

    it = [0]
    for (tok0, n, stream) in self.tok_tiles():
        seq0, seq1 = (0, CTX) if stream == 1 else (CTX, NT)
        halo = 65 if stream == 0 else 1
        for c in range(6):
            i = it[0]; it[0] += 1
            xb = xin[i % 3]; ac = acc[i % 2]; ac2 = acc2[i % 2]; rs = res[i % 2]
            lo = max(tok0 - halo, seq0); hi = min(tok0 + n + halo, seq1)
            if lo > tok0 - halo:
                k.op("pool", lambda e, xb=xb, halo=halo: e.memset(xb[:, 0:halo], 0.0), writes=[xb])
            if hi < tok0 + n + halo:
                k.op("pool", lambda e, xb=xb, halo=halo, n=n: e.memset(xb[:, halo + n:halo + n + halo], 0.0), writes=[xb])
            k.op("sp", lambda e, xb=xb, lo=lo, hi=hi, tok0=tok0, halo=halo, c=c: e.dma_start(out=xb[:, lo - (tok0 - halo):hi - (tok0 - halo)], in_=self.uT.ap[O + 512 + c * 128:O + 512 + (c + 1) * 128, lo:hi]), writes=[xb], dma=True)
            first = True
            dys = (-1, 0, 1) if stream == 0 else (0,)
            for dy in dys:
                for dx in (0, -1, 1):
                    tap = (dy + 1) * 3 + (dx + 1)
                    off = halo + dy * 64 + dx
                    if first:
                        k.op("dve", lambda e, ac=ac, xb=xb, off=off, n=n, c=c, tap=tap: e.tensor_scalar(ac[:, :n], xb[:, off:off + n], cw[:, c, tap:tap + 1], None, ALU.mult), reads=[xb, cw], writes=[ac])
                        first = False
                        continue
                    c0, c1 = (0, 64) if (dx == 0 or stream == 1) else ((1, 64) if dx == -1 else (0, 63))
                    def tapop(ac=ac, xb=xb, off=off, n=n, c=c, tap=tap, c0=c0, c1=c1):
                        src = xb[:, off:off + n].rearrange("p (r w) -> p r w", w=64)[:, :, c0:c1]
                        dst = ac[:, :n].rearrange("p (r w) -> p r w", w=64)[:, :, c0:c1]
                        k.op("dve", lambda e: e.scalar_tensor_tensor(out=dst, in0=src, scalar=cw[:, c, tap:tap + 1], in1=dst, op0=ALU.mult, op1=ALU.add), reads=[xb, cw, ac], writes=[ac])
                    tapop()
            k.op("act", lambda e, ac=ac, rs=rs, n=n, c=c: e.activation(out=rs[:, :n], in_=ac[:, :n], func=AF.Silu, bias=cbias[:, c:c + 1]), reads=[ac, cbias], writes=[rs])
            if c >= 4:
                k.op("pool", lambda e, rs=rs, n=n, c=c, tok0=tok0: e.dma_start(out=self.s_bcT.ap[(c - 4) * 128:(c - 3) * 128, tok0:tok0 + n], in_=rs[:, :n]), reads=[rs], dma=True)
            ob = otm[i % 2]
            transpose_store(rs, 128, lambda nb, c=c, tok0=tok0: self.s_xtm.ap[tok0:tok0 + nb * 128, c * 128:(c + 1) * 128].rearrange("(b p) f -> p b f", p=128), n, ob)
        for c in range(4):
            i = it[0]; it[0] += 1
            zb = zin[i % 2]
            k.op("sp", lambda e, zb=zb, c=c, tok0=tok0, n=n: e.dma_start(out=zb[:, :n], in_=self.uT.ap[O + c * 128:O + (c + 1) * 128, tok0:tok0 + n]), writes=[zb], dma=True)
            ob = otm[i % 2]
            transpose_store(zb, 128, lambda nb, c=c, tok0=tok0: self.s_ztm.ap[tok0:tok0 + nb * 128, c * 128:(c + 1) * 128].rearrange("(b p) f -> p b f", p=128), n, ob)
        i = it[0]; it[0] += 1
        db = dtin[i % 2]
        k.op("sp", lambda e, db=db, tok0=tok0, n=n: e.dma_start(out=db[:, :n], in_=self.uT.ap[O + 1280:O + 1288, tok0:tok0 + n]), writes=[db], dma=True)
        ob = odt[i % 2]
        transpose_store(db, 8, lambda nb, tok0=tok0: self.s_dttm.ap[tok0:tok0 + nb * 128, :].rearrange("(b p) f -> p b f", p=128), n, ob)
    k.barrier()
    k.emit()
    k.free_to(m)


def phase_ssm_scan(self, l, d):
    k = self.k
    m = k.mark()
    NT = self.NT
    BIG = 30000.0
    m128 = k.sb("m128", [128, 5, 128], F32)
    for i in range(5):
        k.op("sp", lambda e, i=i: e.dma_start(out=m128[:, i, :], in_=self.c_m128.ap[i]), writes=[m128], dma=True)
    LE, GE, GT, LT, NEGI = 0, 1, 2, 3, 4
    if d == 0:
        mTri, mR, mNeg = LE, GT, GT
    else:
        mTri, mR, mNeg = GE, LT, LT
    onesf = k.sb("onesf", [128, 128], F32)
    k.op("dve", lambda e: e.memset(onesf[:, :], 1.0), writes=[onesf])
    dtb = k.sb("dtb", [128, 8], F32)
    aneg = k.sb("aneg", [128, 8], F32)
    dsk = k.sb("dsk", [128, 8], F32)
    ngb = k.sb("ngb", [128, 512], F32)
    k.op("sp", lambda e: e.dma_start(out=dtb[:, :], in_=self.ssm_dt_bias.ap[l, d].partition_broadcast(128)), writes=[dtb], dma=True)
    k.op("sp", lambda e: e.dma_start(out=aneg[:, :], in_=self.ssm_a_log.ap[l, d].partition_broadcast(128)), writes=[aneg], dma=True)
    k.op("sp", lambda e: e.dma_start(out=dsk[:, :], in_=self.ssm_d.ap[l].partition_broadcast(128)), writes=[dsk], dma=True)
    k.op("sp", lambda e: e.dma_start(out=ngb[:, :], in_=self.ssm_norm_g.ap[l].partition_broadcast(128)), writes=[ngb], dma=True)
    k.op("act", lambda e: e.activation(out=aneg[:, :], in_=aneg[:, :], func=AF.Exp), reads=[aneg], writes=[aneg])
    k.op("dve", lambda e: e.tensor_scalar(aneg[:, :], aneg[:, :], -1.0, None, ALU.mult), reads=[aneg], writes=[aneg])

    xs = [k.sb("sxs%d" % i, [128, 768], F32) for i in range(2)]
    dt = [k.sb("sdt%d" % i, [128, 8], F32) for i in range(2)]
    BT = [[k.sb("sBT%d%d" % (i, g), [64, 128], F32) for g in range(2)] for i in range(2)]
    CT = [[k.sb("sCT%d%d" % (i, g), [64, 128], F32) for g in range(2)] for i in range(2)]
    BTb = [k.sb("sBTb%d" % g, [64, 128], BF16) for g in range(2)]
    CTb = [k.sb("sCTb%d" % g, [64, 128], BF16) for g in range(2)]
    Btm = k.sb("sBtm", [128, 128], BF16)
    zt = [k.sb("szt%d" % i, [128, 512], F32) for i in range(2)]
    yfin = [k.sb("syf%d" % i, [128, 512], F32) for i in range(2)]
    xdt = k.sb("sx", [128, 8], F32)
    dA = k.sb("sdA", [128, 8], F32)
    cs = k.sb("scs", [128, 8], F32)
    dte = k.sb("sdte", [128, 8], F32)
    ecs = k.sb("secs", [128, 8], F32)
    eend = k.sb("seend", [128, 8], F32)
    dtp = k.sb("sdtp", [128, 8], F32)
    Xd = k.sb("sXd", [128, 8, 64], BF16)
    Xdd = k.sb("sXdd", [128, 8, 64], BF16)
    Rm = [k.sb("sRm%d" % i, [128, 128], F32) for i in range(4)]
    Lt = k.sb("sLt", [128, 8, 128], BF16)
    sc = k.sb("ssc", [128, 2, 128], BF16)
    Gt = k.sb("sGt", [128, 8, 128], BF16)
    Yt = k.sb("sYt", [128, 512], F32)
    Y = k.sb("sY", [128, 512], F32)
    junk = k.sb("sjunk", [128, 512], BF16)
    ss = k.sb("sss", [128, 1], F32)
    Ybf = k.sb("sYbf", [128, 512], BF16)
    ofm = k.sb("sofm", [128, 4, 128], F32)
    Sst = k.sb("sSst", [64, 8, 64], F32)
    Stmp = k.sb("sStmp", [64, 8, 64], F32)
    Sbf = k.sb("sSbf", [64, 8, 64], BF16)
    k.op("dve", lambda e: e.memset(Sst[:, :, :], 0.0), writes=[Sst])
    k.op("dve", lambda e: e.memset(Sbf[:, :, :], 0.0), writes=[Sbf])
    pcs = k.ps("spcs", [128, 2, 8], F32)
    pD = [k.ps("spD%d" % i, [128, 4, 128], F32) for i in range(2)]
    psc = k.ps("spsc", [128, 2, 128], F32)
    pYd = k.ps("spYd", [128, 512], F32)
    pYo = k.ps("spYo", [128, 512], F32)
    pSt = k.ps("spSt", [64, 512], F32)
    pT = k.ps("spT", [128, 4, 128], BF16)

    nchunks = NT // 128
    ctxc = [0, 1]
    latc = list(range(2, nchunks))
    order = (ctxc + latc) if d == 0 else (ctxc[::-1] + latc[::-1])
    ri = [0]

    def chunk(ci, c):
        b = ci % 2
        t0 = c * 128
        x_, dt_, z_, yf_ = xs[b], dt[b], zt[b], yfin[b]
        k.op("sp", lambda e: e.dma_start(out=x_[:, :], in_=self.s_xtm.ap[t0:t0 + 128, :]), writes=[x_], dma=True)
        k.op("sp", lambda e: e.dma_start(out=dt_[:, :], in_=self.s_dttm.ap[t0:t0 + 128, :]), writes=[dt_], dma=True)
        for g in range(2):
            k.op("sp", lambda e, g=g: e.dma_start(out=BT[b][g][:, :], in_=self.s_bcT.ap[g * 64:(g + 1) * 64, t0:t0 + 128]), writes=[BT[b][g]], dma=True)
            k.op("sp", lambda e, g=g: e.dma_start(out=CT[b][g][:, :], in_=self.s_bcT.ap[128 + g * 64:128 + (g + 1) * 64, t0:t0 + 128]), writes=[CT[b][g]], dma=True)
        if d == 1:
            k.op("sp", lambda e: e.dma_start(out=z_[:, :], in_=self.s_ztm.ap[t0:t0 + 128, :]), writes=[z_], dma=True)
            k.op("sp", lambda e: e.dma_start(out=yf_[:, :], in_=self.s_yf.ap[t0:t0 + 128, :]), writes=[yf_], dma=True)
        for g in range(2):
            k.op("act", lambda e, g=g: e.copy(BTb[g][:, :], BT[b][g][:, :]), reads=[BT[b][g]], writes=[BTb[g]])
            k.op("act", lambda e, g=g: e.copy(CTb[g][:, :], CT[b][g][:, :]), reads=[CT[b][g]], writes=[CTb[g]])
        k.op("act", lambda e: e.copy(Btm[:, :], x_[:, 512:640]), reads=[x_], writes=[Btm])
        k.op("dve", lambda e: e.tensor_tensor(xdt[:, :], dt_[:, :], dtb[:, :], ALU.add), reads=[dt_, dtb], writes=[xdt])
        k.op("act", lambda e: e.activation(out=xdt[:, :], in_=xdt[:, :], func=AF.Exp), reads=[xdt], writes=[xdt])
        k.op("act", lambda e: e.activation(out=dtp[:, :], in_=xdt[:, :], func=AF.Ln, bias=1.0), reads=[xdt], writes=[dtp])
        k.op("dve", lambda e: e.tensor_tensor(dA[:, :], dtp[:, :], aneg[:, :], ALU.mult), reads=[dtp, aneg], writes=[dA])
        k.op("pe", lambda e: e.matmul(pcs[:, 0, :], lhsT=m128[:, mTri, :], rhs=dA[:, :], start=True, stop=True), reads=[m128, dA], writes=[pcs])
        k.op("pe", lambda e: e.matmul(pcs[:, 1, :], lhsT=onesf[:, :], rhs=dA[:, :], start=True, stop=True), reads=[onesf, dA], writes=[pcs], pe_acc=True)
        k.op("dve", lambda e: e.tensor_copy(cs[:, :], pcs[:, 0, :]), reads=[pcs], writes=[cs])
        k.op("act", lambda e: e.activation(out=ecs[:, :], in_=pcs[:, 0, :], func=AF.Exp), reads=[pcs], writes=[ecs])
        k.op("act", lambda e: e.activation(out=eend[:, :], in_=pcs[:, 1, :], func=AF.Exp), reads=[pcs], writes=[eend])
        k.op("dve", lambda e: e.tensor_tensor(dte[:, :], pcs[:, 1, :], cs[:, :], ALU.subtract), reads=[pcs, cs], writes=[dte])
        k.op("act", lambda e: e.activation(out=dte[:, :], in_=dte[:, :], func=AF.Exp), reads=[dte], writes=[dte])
        xv = x_[:, 0:512].rearrange("p (h e) -> p h e", e=64)
        k.op("dve", lambda e: e.tensor_tensor(Xd[:, :, :], xv, dtp[:, :].unsqueeze(2).to_broadcast([128, 8, 64]), ALU.mult), reads=[x_, dtp], writes=[Xd])
        k.op("pool", lambda e: e.tensor_tensor(Xdd[:, :, :], Xd[:, :, :], dte[:, :].unsqueeze(2).to_broadcast([128, 8, 64]), ALU.mult), reads=[Xd, dte], writes=[Xdd])
        for h in range(8):
            r_ = Rm[ri[0] % 4]; ri[0] += 1
            pd = pD[h // 4]
            k.op("dve" if h % 2 == 0 else "pool", lambda e, h=h, r_=r_: e.tensor_tensor(r_[:, :], m128[:, mR, :], dA[:, h:h + 1].to_broadcast([128, 128]), ALU.mult), reads=[m128, dA], writes=[r_])
            k.op("pe", lambda e, h=h, r_=r_, pd=pd: e.matmul(pd[:, h % 4, :], lhsT=r_[:, :], rhs=m128[:, mTri, :], start=True, stop=False), reads=[r_, m128], writes=[pd], pe_acc=True)
            k.op("pe", lambda e, h=h, pd=pd: e.matmul(pd[:, h % 4, :], lhsT=m128[:, NEGI, :], rhs=m128[:, mNeg, :], start=False, stop=True), reads=[m128], writes=[pd], pe_acc=True)
        for hh in range(2):
            k.op("act", lambda e, hh=hh: e.activation(out=Lt[:, hh * 4:(hh + 1) * 4, :], in_=pD[hh][:, :, :], func=AF.Exp), reads=[pD[hh]], writes=[Lt])
        for g in range(2):
            k.op("pe", lambda e, g=g: e.matmul(psc[:, g, :], lhsT=BTb[g][:, :], rhs=CTb[g][:, :], start=True, stop=True), reads=[BTb[g], CTb[g]], writes=[psc], pe_acc=True)
        k.op("dve", lambda e: e.tensor_copy(sc[:, :, :], psc[:, :, :]), reads=[psc], writes=[sc])
        for g in range(2):
            k.op("dve" if g == 0 else "pool", lambda e, g=g: e.tensor_tensor(Gt[:, g * 4:(g + 1) * 4, :], Lt[:, g * 4:(g + 1) * 4, :], sc[:, g:g + 1, :].to_broadcast([128, 4, 128]), ALU.mult), reads=[Lt, sc], writes=[Gt])
        for h in range(8):
            k.op("pe", lambda e, h=h: e.matmul(pYd[:, h * 64:(h + 1) * 64], lhsT=Gt[:, h, :], rhs=Xd[:, h, :], start=True, stop=True), reads=[Gt, Xd], writes=[pYd], pe_acc=True)
        for g in range(2):
            k.op("pe", lambda e, g=g: e.matmul(pYo[:, g * 256:(g + 1) * 256], lhsT=CTb[g][:, :], rhs=Sbf[:, g * 4:(g + 1) * 4, :].rearrange("p h e -> p (h e)"), start=True, stop=True), reads=[CTb[g], Sbf], writes=[pYo], pe_acc=True)
        k.op("dve", lambda e: e.tensor_tensor(Yt[:, :].rearrange("p (h e) -> p h e", e=64), pYo[:, :].rearrange("p (h e) -> p h e", e=64), ecs[:, :].unsqueeze(2).to_broadcast([128, 8, 64]), ALU.mult), reads=[pYo, ecs], writes=[Yt])
        k.op("dve", lambda e: e.tensor_tensor(Y[:, :], Yt[:, :], pYd[:, :], ALU.add), reads=[Yt, pYd], writes=[Y])
        for g in range(2):
            k.op("pe", lambda e, g=g: e.matmul(pSt[:, g * 256:(g + 1) * 256], lhsT=Btm[:, g * 64:(g + 1) * 64], rhs=Xdd[:, g * 4:(g + 1) * 4, :].rearrange("p h e -> p (h e)"), start=True, stop=True), reads=[Btm, Xdd], writes=[pSt], pe_acc=True)
        k.op("pool", lambda e: e.tensor_tensor(Stmp[:, :, :], Sst[:, :, :], eend[0:64, :].unsqueeze(2).to_broadcast([64, 8, 64]), ALU.mult), reads=[Sst, eend], writes=[Stmp])
        k.op("dve", lambda e: e.tensor_tensor(Sst[:, :, :], Stmp[:, :, :], pSt[:, :].rearrange("p (h e) -> p h e", e=64), ALU.add), reads=[Stmp, pSt], writes=[Sst])
        k.op("act", lambda e: e.copy(Sbf[:, :, :], Sst[:, :, :]), reads=[Sst], writes=[Sbf])
        if d == 0:
            k.op("pool", lambda e: e.dma_start(out=self.s_yf.ap[t0:t0 + 128, :], in_=Y[:, :]), reads=[Y], dma=True)
        else:
            k.op("dve", lambda e: e.tensor_tensor(Y[:, :], Y[:, :], yf_[:, :], ALU.add), reads=[Y, yf_], writes=[Y])
            k.op("pool", lambda e: e.tensor_tensor(Yt[:, :].rearrange("p (h e) -> p h e", e=64), xv, dsk[:, :].unsqueeze(2).to_broadcast([128, 8, 64]), ALU.mult), reads=[x_, dsk], writes=[Yt])
            k.op("dve", lambda e: e.tensor_tensor(Y[:, :], Y[:, :], Yt[:, :], ALU.add), reads=[Y, Yt], writes=[Y])
            k.op("act", lambda e: e.activation(out=z_[:, :], in_=z_[:, :], func=AF.Silu), reads=[z_], writes=[z_])
            k.op("dve", lambda e: e.tensor_tensor(Y[:, :], Y[:, :], z_[:, :], ALU.mult), reads=[Y, z_], writes=[Y])
            k.op("act", lambda e: e.activation(out=junk[:, :], in_=Y[:, :], func=AF.Square, accum_out=ss[:, 0:1]), reads=[Y], writes=[junk, ss])
            k.op("act", lambda e: e.activation(out=ss[:, 0:1], in_=ss[:, 0:1], func=AF.Sqrt, scale=1.0 / 512, bias=EPS), reads=[ss], writes=[ss])
            k.op("dve", lambda e: e.reciprocal(ss[:, 0:1], ss[:, 0:1]), reads=[ss], writes=[ss])
            k.op("dve", lambda e: e.scalar_tensor_tensor(out=Ybf[:, :], in0=Y[:, :], scalar=ss[:, 0:1], in1=ngb[:, :], op0=ALU.mult, op1=ALU.mult), reads=[Y, ss, ngb], writes=[Ybf])
            for j in range(4):
                k.op("pe", lambda e, j=j: e.transpose(pT[:, j, :], Ybf[:, j * 128:(j + 1) * 128], self.ident[:, :]), reads=[Ybf, self.ident], writes=[pT], pe_acc=True)
            k.op("act", lambda e: e.copy(ofm[:, :, :], pT[:, :, :]), reads=[pT], writes=[ofm])
            k.op("pool", lambda e: e.dma_start(out=self.mixT.ap[256:768, t0:t0 + 128].rearrange("(j p) t -> p j t", p=128), in_=ofm[:, :, :]), reads=[ofm], dma=True)

    for ci, c in enumerate(order):
        chunk(ci, c)
    k.barrier()
    k.emit()
    k.free_to(m)


MK.ssm_decl = _ssm_decl
MK.phase_ssm_conv = phase_ssm_conv
MK.phase_ssm_scan = phase_ssm_scan


def _moe_decl(self):
    k = self.k
    NT = self.NT
    def S(n, s, dt=F32):
        kind = "ExternalOutput" if n in self.dbg else "Internal"
        return k.dram(n, s, dt, kind=kind)
    self.h2T = S("h2T", [D, NT], BF16)
    self.combT = S("combT", [16, NT])
    self.c_sel = k.dram("c_sel", [16, 16, 128], F32, kind="ExternalInput")


def phase_outproj(self, l, lat_only):
    k = self.k
    m = k.mark()
    NT = self.NT
    wout = k.sb("wout", [128, 8, D], BF16)
    for c in range(8):
        k.op("pool", lambda e, c=c: e.dma_start(out=wout[:, c, :], in_=self.w_out.ap[l, c * 128:(c + 1) * 128, :]), writes=[wout], dma=True)
    wr = k.sb("wr", [128, 8, 20], F32)
    k.op("sp", lambda e: e.dma_start(out=wr[:, :, 0:4], in_=self.moe_rg_w.ap[l].rearrange("(c p) g -> p c g", p=128)), writes=[wr], dma=True)
    k.op("sp", lambda e: e.dma_start(out=wr[:, :, 4:20], in_=self.moe_re_w.ap[l].rearrange("(c p) g -> p c g", p=128)), writes=[wr], dma=True)
    rb = k.sb("rb", [128, 20], F32)
    k.op("sp", lambda e: e.dma_start(out=rb[:, 0:4], in_=self.moe_rg_b.ap[l].partition_broadcast(128)), writes=[rb], dma=True)
    k.op("sp", lambda e: e.dma_start(out=rb[:, 4:20], in_=self.moe_re_b.ap[l].partition_broadcast(128)), writes=[rb], dma=True)
    mixf = [k.sb("mixf%d" % i, [128, 8, 512], F32) for i in range(2)]
    mixb = k.sb("mixb", [128, 8, 512], BF16)
    xt = [k.sb("oxt%d" % i, [128, 8, 512], F32) for i in range(2)]
    sq = k.sb("osq", [128, 8, 512], BF16)
    rstd = k.sb("orstd", [128, 512], F32)
    hT = k.sb("ohT", [128, 8, 512], BF16)
    pss = k.ps("opss", [128, 512], F32)
    po = [k.ps("opo%d" % i, [128, 512], F32) for i in range(3)]
    plg = k.ps("oplg", [128, 4, 20], F32)
    pct = k.ps("opct", [16, 4, 128], F32)
    R_ = lambda n_, s_: k.sb(n_, [128] + s_, F32)
    lg = R_("rlg", [4, 20]); gmax = R_("rgmax", [4, 1]); ohg = R_("rohg", [4, 4]); eg = R_("reg", [4, 4]); gsum = R_("rgsum", [4, 1])
    pgr = R_("rpgr", [4, 1]); esel3 = R_("resel3", [4, 4, 4]); esel = R_("resel", [4, 4]); m1 = R_("rm1", [4, 1]); oh1 = R_("roh1", [4, 4])
    es2 = R_("res2", [4, 4]); m2 = R_("rm2", [4, 1]); oh2 = R_("roh2", [4, 4]); ex2 = R_("rex2", [4, 1]); den = R_("rden", [4, 1])
    w1_ = R_("rw1", [4, 1]); w2_ = R_("rw2", [4, 1]); cig = R_("rcig", [4, 4]); comb = R_("rcomb", [4, 4, 4]); tmp4 = R_("rtmp4", [4, 4])
    combT_sb = k.sb("combT_sb", [16, 4, 128], F32)
    xTv = self.xT.ap.rearrange("(c p) t -> p c t", p=128)
    mTv = self.mixT.ap.rearrange("(c p) t -> p c t", p=128)
    hTv = self.h2T.ap.rearrange("(c p) t -> p c t", p=128)
    pi = [0]

    def tile_body(ti, tok0, n, stream):
        b = ti % 2
        mf, x_ = mixf[b], xt[b]
        k.op("sp", lambda e: e.dma_start(out=mf[:, :, :n], in_=mTv[:, :, tok0:tok0 + n]), writes=[mf], dma=True)
        k.op("sp", lambda e: e.dma_start(out=x_[:, :, :n], in_=xTv[:, :, tok0:tok0 + n]), writes=[x_], dma=True)
        k.op("act", lambda e: e.copy(mixb[:, 0:4, :n], mf[:, 0:4, :n]), reads=[mf], writes=[mixb])
        k.op("pool", lambda e: e.tensor_copy(mixb[:, 4:8, :n], mf[:, 4:8, :n]), reads=[mf], writes=[mixb])
        for j in range(8):
            p_ = po[pi[0] % 3]; pi[0] += 1
            for c in range(8):
                k.op("pe", lambda e, c=c, j=j, p_=p_: e.matmul(p_[:, :n], lhsT=wout[:, c, j * 128:(j + 1) * 128], rhs=mixb[:, c, :n], start=(c == 0), stop=(c == 7)), reads=[wout, mixb], writes=[p_], pe_acc=True)
            k.op("dve", lambda e, j=j, p_=p_: e.scalar_tensor_tensor(out=x_[:, j, :n], in0=p_[:, :n], scalar=self.modT[:, l, 16 + j, stream:stream + 1], in1=x_[:, j, :n], op0=ALU.mult, op1=ALU.add), reads=[p_, self.modT, x_], writes=[x_])
        k.op("pool", lambda e: e.dma_start(out=xTv[:, :, tok0:tok0 + n], in_=x_[:, :, :n]), reads=[x_], dma=True)
        self.norm_tile(l, 1, tok0, n, stream, x_, sq, pss, rstd, hT, keep_f32=True)
        k.op("pool", lambda e: e.dma_start(out=hTv[:, :, tok0:tok0 + n], in_=hT[:, :, :n]), reads=[hT], dma=True)
        nb = n // 128
        for tb in range(nb):
            for c in range(8):
                k.op("pe", lambda e, tb=tb, c=c: e.matmul(plg[:, tb, :], lhsT=x_[:, c, tb * 128:(tb + 1) * 128], rhs=wr[:, c, :], start=(c == 0), stop=(c == 7)), reads=[x_, wr], writes=[plg], pe_acc=True)
        V = lambda t_, *idx: t_[(slice(None), slice(0, nb)) + idx]
        op = lambda fn, r, w: k.op("dve", fn, reads=r, writes=w)
        op(lambda e: e.tensor_tensor(lg[:, :nb, :], plg[:, :nb, :], rb[:, :].unsqueeze(1).to_broadcast([128, nb, 20]), ALU.add), [plg, rb], [lg])
        op(lambda e: e.tensor_reduce(gmax[:, :nb, :], lg[:, :nb, 0:4], AX.X, ALU.max), [lg], [gmax])
        op(lambda e: e.tensor_tensor(ohg[:, :nb, :], lg[:, :nb, 0:4], gmax[:, :nb, :].to_broadcast([128, nb, 4]), ALU.is_ge), [lg, gmax], [ohg])
        op(lambda e: e.tensor_tensor(eg[:, :nb, :], lg[:, :nb, 0:4], gmax[:, :nb, :].to_broadcast([128, nb, 4]), ALU.subtract), [lg, gmax], [eg])
        k.op("act", lambda e: e.activation(out=eg[:, :nb, :], in_=eg[:, :nb, :], func=AF.Exp), reads=[eg], writes=[eg])
        op(lambda e: e.tensor_reduce(gsum[:, :nb, :], eg[:, :nb, :], AX.X, ALU.add), [eg], [gsum])
        op(lambda e: e.reciprocal(pgr[:, :nb, :], gsum[:, :nb, :]), [gsum], [pgr])
        elv = lg[:, :nb, 4:20].rearrange("p b (g e) -> p b g e", e=4)
        op(lambda e: e.tensor_tensor(esel3[:, :nb, :, :], elv, ohg[:, :nb, :].unsqueeze(3).to_broadcast([128, nb, 4, 4]), ALU.mult), [lg, ohg], [esel3])
        op(lambda e: e.tensor_reduce(esel[:, :nb, :], esel3[:, :nb, :, :].rearrange("p b g e -> p b e g"), AX.X, ALU.add), [esel3], [esel])
        op(lambda e: e.tensor_reduce(m1[:, :nb, :], esel[:, :nb, :], AX.X, ALU.max), [esel], [m1])
        op(lambda e: e.tensor_tensor(oh1[:, :nb, :], esel[:, :nb, :], m1[:, :nb, :].to_broadcast([128, nb, 4]), ALU.is_ge), [esel, m1], [oh1])
        op(lambda e: e.scalar_tensor_tensor(out=es2[:, :nb, :], in0=oh1[:, :nb, :], scalar=-1e30, in1=esel[:, :nb, :], op0=ALU.mult, op1=ALU.add), [oh1, esel], [es2])
        op(lambda e: e.tensor_reduce(m2[:, :nb, :], es2[:, :nb, :], AX.X, ALU.max), [es2], [m2])
        op(lambda e: e.tensor_tensor(oh2[:, :nb, :], es2[:, :nb, :], m2[:, :nb, :].to_broadcast([128, nb, 4]), ALU.is_ge), [es2, m2], [oh2])
        op(lambda e: e.tensor_tensor(ex2[:, :nb, :], m2[:, :nb, :], m1[:, :nb, :], ALU.subtract), [m2, m1], [ex2])
        k.op("act", lambda e: e.activation(out=ex2[:, :nb, :], in_=ex2[:, :nb, :], func=AF.Exp), reads=[ex2], writes=[ex2])
        op(lambda e: e.tensor_scalar(den[:, :nb, :], ex2[:, :nb, :], 1.0, None, ALU.add), [ex2], [den])
        op(lambda e: e.reciprocal(den[:, :nb, :], den[:, :nb, :]), [den], [den])
        op(lambda e: e.tensor_tensor(w1_[:, :nb, :], den[:, :nb, :], pgr[:, :nb, :], ALU.mult), [den, pgr], [w1_])
        op(lambda e: e.tensor_tensor(w2_[:, :nb, :], w1_[:, :nb, :], ex2[:, :nb, :], ALU.mult), [w1_, ex2], [w2_])
        op(lambda e: e.tensor_tensor(cig[:, :nb, :], oh1[:, :nb, :], w1_[:, :nb, :].to_broadcast([128, nb, 4]), ALU.mult), [oh1, w1_], [cig])
        op(lambda e: e.tensor_tensor(tmp4[:, :nb, :], oh2[:, :nb, :], w2_[:, :nb, :].to_broadcast([128, nb, 4]), ALU.mult), [oh2, w2_], [tmp4])
        op(lambda e: e.tensor_tensor(cig[:, :nb, :], cig[:, :nb, :], tmp4[:, :nb, :], ALU.add), [cig, tmp4], [cig])
        for g in range(4):
            op(lambda e, g=g: e.tensor_tensor(comb[:, :nb, g, :], cig[:, :nb, :], ohg[:, :nb, g:g + 1].to_broadcast([128, nb, 4]), ALU.mult), [cig, ohg], [comb])
        for tb in range(nb):
            k.op("pe", lambda e, tb=tb: e.transpose(pct[:, tb, :], comb[:, tb, :, :].rearrange("p g e -> p (g e)"), self.identf[:, :]), reads=[comb, self.identf], writes=[pct], pe_acc=True)
        k.op("act", lambda e: e.copy(combT_sb[:, :nb, :], pct[:, :nb, :]), reads=[pct], writes=[combT_sb])
        k.op("pool", lambda e: e.dma_start(out=self.combT.ap[:, tok0:tok0 + n].rearrange("k (b t) -> k b t", t=128), in_=combT_sb[:, :nb, :]), reads=[combT_sb], dma=True)

    for ti, (tok0, n, stream) in enumerate(self.tok_tiles(lat_only=lat_only)):
        tile_body(ti, tok0, n, stream)
    k.barrier()
    k.emit()
    k.free_to(m)


def phase_moe(self, l, lat_only):
    k = self.k
    m = k.mark()
    NT, T = self.NT, self.T
    TTL = min(2048, T)
    tiles = []
    start = CTX if lat_only else 0
    first = True
    t = start
    while t < NT:
        if first and not lat_only:
            n = CTX + TTL
        else:
            n = TTL
        n = min(n, NT - t)
        tiles.append((t, n))
        t += n
        first = False
    TTmax = max(n for _, n in tiles)
    acc = k.sb("macc", [128, 8, TTmax], F32)
    h2 = k.sb("mh2", [128, 8, TTmax], BF16)
    cTb = k.sb("mcTb", [16, TTmax], BF16)
    w1 = [k.sb("mw1%d" % i, [128, 8, 512], BF16) for i in range(2)]
    w3 = [k.sb("mw3%d" % i, [128, 8, 512], BF16) for i in range(2)]
    w2 = [k.sb("mw2%d" % i, [128, 4, D], BF16) for i in range(2)]
    self_sel = k.sb("msel", [16, 16, 128], BF16)
    k.op("pool", lambda e: e.dma_start(out=self_sel[:, :, :], in_=self.c_sel.ap.rearrange("e k m -> k e m")), writes=[self_sel], dma=True)
    sa = [k.sb("msa%d" % i, [128, 512], BF16) for i in range(2)]
    sa2 = [k.sb("msa2%d" % i, [128, 512], BF16) for i in range(2)]
    hid = [k.sb("mhid%d" % i, [128, 4, 512], BF16) for i in range(2)]
    cb = [k.sb("mcb%d" % i, [128, 512], BF16) for i in range(2)]
    xres = [k.sb("mxres%d" % i, [128, 512], F32) for i in range(2)]
    pa = [k.ps("mpa%d" % i, [128, 512], F32) for i in range(2)]
    pb = [k.ps("mpb%d" % i, [128, 512], F32) for i in range(2)]
    po = [k.ps("mpo%d" % i, [128, 512], F32) for i in range(3)]
    pcb = k.ps("mpcb", [128, 512], F32)
    hTv = self.h2T.ap.rearrange("(c p) t -> p c t", p=128)
    xTv = self.xT.ap.rearrange("(c p) t -> p c t", p=128)
    cnt = {"ab": 0, "o": 0, "h": 0, "w": 0, "s": 0}

    def blocks(t0, n):
        bl = []
        o = 0
        if t0 < CTX:
            bl.append((0, CTX, 1)); o = CTX
        while o < n:
            s_ = min(512, n - o)
            bl.append((o, s_, 0)); o += s_
        return bl

    nsteps = 16 * len(tiles)

    def load_w(si, which):
        if si >= nsteps:
            return
        ex = si % 16
        wb = si % 2
        W1, W3, W2 = w1[wb], w3[wb], w2[wb]
        if which == "w13":
            for c in range(8):
                k.op("pool", lambda e, c=c: e.dma_start(out=W1[:, c, :], in_=self.moe_w1.ap[l, ex, c * 128:(c + 1) * 128, :]), writes=[W1], dma=True)
                k.op("pool", lambda e, c=c: e.dma_start(out=W3[:, c, :], in_=self.moe_w3.ap[l, ex, c * 128:(c + 1) * 128, :]), writes=[W3], dma=True)
        else:
            for c in range(4):
                k.op("pool", lambda e, c=c: e.dma_start(out=W2[:, c, :], in_=self.moe_w2.ap[l, ex, c * 128:(c + 1) * 128, :]), writes=[W2], dma=True)

    for (t0, n) in tiles:
        for c in range(8):
            k.op("sp", lambda e, c=c, t0=t0, n=n: e.dma_start(out=h2[:, c, :n], in_=hTv[:, c, t0:t0 + n]), writes=[h2], dma=True)
        k.op("pool", lambda e, t0=t0, n=n: e.dma_start(out=cTb[:, :n], in_=self.combT.ap[:, t0:t0 + n]), writes=[cTb], dma=True)
        bl = blocks(t0, n)
        pending = [None]
        for ex in range(16):
            si = cnt["w"]; cnt["w"] += 1
            wb = si % 2
            W1, W3, W2 = w1[wb], w3[wb], w2[wb]
            if si == 0:
                load_w(0, "w13"); load_w(0, "w2")
            load_w(si + 1, "w13")
            first_blk = [True]
            pend = None

            def up(o, s_, stream, W1, W3, ex):
                cbb = cb[cnt["s"] % 2]; cnt["s"] += 1
                k.op("pe", lambda e: e.matmul(pcb[:, :s_], lhsT=self_sel[:, ex, :], rhs=cTb[:, o:o + s_], start=True, stop=True), reads=[self_sel, cTb], writes=[pcb])
                k.op("act", lambda e: e.copy(cbb[:, :s_], pcb[:, :s_]), reads=[pcb], writes=[cbb])
                hd = hid[cnt["h"] % 2]; cnt["h"] += 1
                for fc in range(4):
                    i = cnt["ab"] % 2; cnt["ab"] += 1
                    pa_, pb_, sa_, sa2_ = pa[i], pb[i], sa[i], sa2[i]
                    for c in range(8):
                        k.op("pe", lambda e, c=c, fc=fc, pa_=pa_: e.matmul(pa_[:, :s_], lhsT=W1[:, c, fc * 128:(fc + 1) * 128], rhs=h2[:, c, o:o + s_], start=(c == 0), stop=(c == 7)), reads=[W1, h2], writes=[pa_], pe_acc=True)
                    for c in range(8):
                        k.op("pe", lambda e, c=c, fc=fc, pb_=pb_: e.matmul(pb_[:, :s_], lhsT=W3[:, c, fc * 128:(fc + 1) * 128], rhs=h2[:, c, o:o + s_], start=(c == 0), stop=(c == 7)), reads=[W3, h2], writes=[pb_], pe_acc=True)
                    k.op("act", lambda e, pa_=pa_, sa_=sa_: e.activation(out=sa_[:, :s_], in_=pa_[:, :s_], func=AF.Silu), reads=[pa_], writes=[sa_])
                    k.op("pool", lambda e, sa_=sa_, sa2_=sa2_: e.tensor_tensor(sa2_[:, :s_], sa_[:, :s_], cbb[:, :s_], ALU.mult), reads=[sa_, cbb], writes=[sa2_])
                    k.op("dve", lambda e, sa2_=sa2_, pb_=pb_, fc=fc: e.tensor_tensor(hd[:, fc, :s_], sa2_[:, :s_], pb_[:, :s_], ALU.mult), reads=[sa2_, pb_], writes=[hd])
                return hd

            def down(o, s_, hd, W2_, ex_):
                for j in range(8):
                    po_ = po[cnt["o"] % 3]; cnt["o"] += 1
                    for fc in range(4):
                        k.op("pe", lambda e, fc=fc, j=j, po_=po_: e.matmul(po_[:, :s_], lhsT=W2_[:, fc, j * 128:(j + 1) * 128], rhs=hd[:, fc, :s_], start=(fc == 0), stop=(fc == 3)), reads=[W2_, hd], writes=[po_], pe_acc=True)
                    if ex_ == 0:
                        k.op("dve", lambda e, j=j, po_=po_: e.tensor_copy(acc[:, j, o:o + s_], po_[:, :s_]), reads=[po_], writes=[acc])
                    else:
                        k.op("dve", lambda e, j=j, po_=po_: e.tensor_tensor(acc[:, j, o:o + s_], acc[:, j, o:o + s_], po_[:, :s_], ALU.add), reads=[po_, acc], writes=[acc])

            for (o, s_, stream) in bl:
                hd = up(o, s_, stream, W1, W3, ex)
                if pending[0] is not None:
                    down(*pending[0])
                pending[0] = (o, s_, hd, W2, ex)
                if first_blk[0]:
                    first_blk[0] = False
                    load_w(si + 1, "w2")
        if pending[0] is not None:
            down(*pending[0])
            pending[0] = None
        ri = 0
        for (o, s_, stream) in bl:
            for j in range(8):
                xr = xres[ri % 2]; ri += 1
                k.op("sp", lambda e, xr=xr, o=o, s_=s_, t0=t0, j=j: e.dma_start(out=xr[:, :s_], in_=xTv[:, j, t0 + o:t0 + o + s_]), writes=[xr], dma=True)
                k.op("dve", lambda e, j=j, xr=xr, o=o, s_=s_, stream=stream: e.scalar_tensor_tensor(out=xr[:, :s_], in0=acc[:, j, o:o + s_], scalar=self.modT[:, l, 40 + j, stream:stream + 1], in1=xr[:, :s_], op0=ALU.mult, op1=ALU.add), reads=[acc, self.modT, xr], writes=[xr])
                k.op("pool", lambda e, xr=xr, o=o, s_=s_, t0=t0, j=j: e.dma_start(out=xTv[:, j, t0 + o:t0 + o + s_], in_=xr[:, :s_]), reads=[xr], dma=True)
    k.barrier()
    k.emit()
    k.free_to(m)


def phase_final(self):
    k = self.k
    m = k.mark()
    fg = self._pvec("fg", self.final_g.ap, 8)
    xt = [k.sb("fxt%d" % i, [128, 8, 512], F32) for i in range(2)]
    sq = k.sb("fsq", [128, 8, 512], BF16)
    rstd = k.sb("frstd", [128, 512], F32)
    pss = k.ps("fpss", [128, 512], F32)
    pt = [k.ps("fpt%d" % i, [128, 4, 128], F32) for i in range(2)]
    ot = [k.sb("fot%d" % i, [128, 8, 128], F32) for i in range(2)]
    xTv = self.xT.ap.rearrange("(c p) t -> p c t", p=128)
    cnt = [0]
    for ti, (tok0, n, stream) in enumerate(self.tok_tiles(lat_only=True)):
        x_ = xt[ti % 2]
        k.op("sp", lambda e, x_=x_, tok0=tok0, n=n: e.dma_start(out=x_[:, :, :n], in_=xTv[:, :, tok0:tok0 + n]), writes=[x_], dma=True)
        k.op("act", lambda e, x_=x_, n=n: e.activation(out=sq[:, :, :n], in_=x_[:, :, :n], func=AF.Square), reads=[x_], writes=[sq])
        for c in range(8):
            k.op("pe", lambda e, c=c, n=n: e.matmul(pss[:, :n], lhsT=self.ones[:, :], rhs=sq[:, c, :n], start=(c == 0), stop=(c == 7)), reads=[sq, self.ones], writes=[pss], pe_acc=True)
        k.op("act", lambda e, n=n: e.activation(out=rstd[:, :n], in_=pss[:, :n], func=AF.Sqrt, scale=1.0 / D, bias=EPS), reads=[pss], writes=[rstd])
        k.op("dve", lambda e, n=n: e.reciprocal(rstd[:, :n], rstd[:, :n]), reads=[rstd], writes=[rstd])
        k.op("dve", lambda e, x_=x_, n=n: e.tensor_tensor(x_[:, :, :n], x_[:, :, :n], rstd[:, :n].unsqueeze(1).to_broadcast([128, 8, n]), ALU.mult), reads=[x_, rstd], writes=[x_])
        k.op("pool", lambda e, x_=x_, n=n: e.tensor_tensor(x_[:, :, :n], x_[:, :, :n], fg[:, :].unsqueeze(2).to_broadcast([128, 8, n]), ALU.mult), reads=[x_, fg], writes=[x_])
        for tb in range(n // 128):
            o_ = ot[cnt[0] % 2]; cnt[0] += 1
            for hh in range(2):
                for c in range(4):
                    cc = hh * 4 + c
                    k.op("pe", lambda e, hh=hh, c=c, cc=cc, x_=x_, tb=tb: e.transpose(pt[hh][:, c, :], x_[:, cc, tb * 128:(tb + 1) * 128], self.identf[:, :]), reads=[x_, self.identf], writes=[pt[hh]], pe_acc=True)
                if hh == 0:
                    k.op("dve", lambda e, o_=o_, hh=hh: e.tensor_copy(o_[:, 0:4, :], pt[0][:, :, :]), reads=[pt[0]], writes=[o_])
                else:
                    k.op("act", lambda e, o_=o_, hh=hh: e.copy(o_[:, 4:8, :], pt[1][:, :, :]), reads=[pt[1]], writes=[o_])
            tt = tok0 - CTX + tb * 128
            k.op("pool", lambda e, o_=o_, tt=tt: e.dma_start(out=self.out.ap[tt:tt + 128, :], in_=o_[:, :, :].rearrange("p c f -> p (c f)")), reads=[o_], dma=True)
    k.barrier()
    k.emit()
    k.free_to(m)


MK.moe_decl = _moe_decl
MK.phase_outproj = phase_outproj
MK.phase_moe = phase_moe
MK.phase_final = phase_final


def build_all(T, dbg=None, L=2):
    mk_ = MK(T, L=L, dbg=dbg)
    mk_.consts(); mk_.phase_mod(); mk_.phase_x_in()
    for l in range(L):
        last = (l == L - 1)
        mk_.phase_inproj(l)
        mk_.phase_rwkv(l, 0); mk_.phase_rwkv(l, 1)
        mk_.phase_ssm_conv(l); mk_.phase_ssm_scan(l, 0); mk_.phase_ssm_scan(l, 1)
        mk_.phase_gla(l, 0); mk_.phase_gla(l, 1)
        mk_.phase_outproj(l, lat_only=last)
        mk_.phase_moe(l, lat_only=last)
    mk_.phase_final()
    mk_.finish(None)
    return mk_


def _host_consts():
    c = {}
    c["c_ident"] = np.eye(128, dtype=np.float32)
    p = np.arange(128)[:, None] % 64
    f = np.arange(512)[None, :] % 64
    c["c_masks"] = np.stack([(p < f), (p <= f), (p > f), (p >= f)]).astype(np.float32)
    c["c_id8"] = (p == f).astype(np.float32)
    pp = np.arange(128)
    c["c_blk"] = ((pp[:, None] // 64) == (pp[None, :] // 64)).astype(np.float32)
    j = np.arange(128)[:, None]; f128 = np.arange(128)[None, :]
    c["c_m128"] = np.stack([(j <= f128), (j >= f128), (j > f128), (j < f128), -30000.0 * (j == f128)]).astype(np.float32)
    sel = np.zeros((16, 16, 128), np.float32)
    for e_ in range(16):
        sel[e_, e_, :] = 1.0
    c["c_sel"] = sel
    c["c_reset"] = np.broadcast_to((np.arange(512) % 64 != 0).astype(np.float32)[None, :], (128, 512)).copy()
    return c


_CACHE = {}


def kernel(**inputs):
    from concourse.bass_utils import run_bass_kernel_spmd
    x = np.asarray(inputs["x"])
    B, T, _ = x.shape
    n_cores = 8
    assert B == n_cores
    mk_ = build_all(T)
    consts = _host_consts()
    in_maps = []
    for b in range(n_cores):
        m = {}
        for k_, v in inputs.items():
            v = np.asarray(v, dtype=np.float32)
            if k_ in ("x", "ctx", "c"):
                m[k_] = np.ascontiguousarray(v[b])
            else:
                m[k_] = np.ascontiguousarray(v)
        m.update(consts)
        in_maps.append(m)
    res = run_bass_kernel_spmd(mk_.nc, in_maps, core_ids=list(range(n_cores)))
    out = np.stack([np.asarray(r["out"], dtype=np.float32) for r in res.results], 0)
    return out
```

```python
import os
import numpy as np
import concourse.bass as bass
import concourse.mybir as mybir

F32 = mybir.dt.float32
BF16 = mybir.dt.bfloat16
ALU = mybir.AluOpType
AF = mybir.ActivationFunctionType
AX = mybir.AxisListType

SEM_CHUNK = int(os.environ.get("SEM_CHUNK", 8000))
N_DMA_SEMS = 12


class T:
    __slots__ = ("ap", "w", "r", "name", "psum")

    def __init__(self, ap, name="", psum=False):
        self.psum = psum
        self.ap = ap
        self.w = None
        self.r = []
        self.name = name

    def __getitem__(self, idx):
        return self.ap[idx]


class KB:
    ENGS = ("pe", "act", "dve", "pool", "sp")

    def __init__(self, nc, same_engine_sync=(os.environ.get("SES", "1") == "1")):
        self.nc = nc
        self.ops = {e: [] for e in self.ENGS}
        self.cnt = {e: 0 for e in self.ENGS}
        self.sem_names = []
        self.waited = {e: {} for e in self.ENGS}
        self.dma_rr = {e: 0 for e in self.ENGS}
        self.dma_val = {}
        self.same_engine_sync = same_engine_sync
        self.ctx = []
        self.final_waits = []
        self.sems = {}
        self.sem_guards = []
        self.last_tok = {}

    def sb(self, name, shape, dt):
        self.uid = getattr(self, "uid", 0) + 1
        name = "%s_%d" % (name, self.uid)
        g = self.nc.sbuf_tensor(name, list(shape), dt)
        t = g.__enter__()
        self.ctx.append(g)
        return T(t, name)

    def ps(self, name, shape, dt):
        self.uid = getattr(self, "uid", 0) + 1
        name = "%s_%d" % (name, self.uid)
        esz = 4 if dt == F32 else 2
        full = 2048 // esz
        g = self.nc.psum_tensor(name, [128, full], dt)
        t = g.__enter__()
        self.ctx.append(g)
        shape = list(shape)
        p = shape[0]
        if len(shape) == 2:
            assert shape[1] <= full
            ap = t[0:p, 0:shape[1]]
        else:
            assert len(shape) == 3 and shape[1] * shape[2] <= full
            ap = t[0:p, 0:shape[1] * shape[2]].rearrange("p (a b) -> p a b", b=shape[2])
        return T(ap, name, psum=True)

    def dram(self, name, shape, dt, kind="Internal"):
        t = self.nc.dram_tensor(name, list(shape), dt, kind=kind)
        return T(t.ap(), name)

    def _sem_key(self, key):
        if key not in self.sem_names:
            self.sem_names.append(key)
        return key

    def op(self, eng, fn, reads=(), writes=(), dma=False, pe_acc=False):
        waits = {}
        if any(t.psum for t in reads):
            writes = list(writes) + [t for t in reads if t.psum and t not in writes]
            reads = [t for t in reads if not t.psum]

        def need(dep):
            if dep is None:
                return
            k, v, e = dep
            if e == eng and not dma and not self.same_engine_sync and not k[0] == "dma":
                return
            if e == eng and eng == "pe" and k[0] != "dma":
                return
            if waits.get(k, 0) < v:
                waits[k] = v

        for t in reads:
            need(t.w)
        for t in writes:
            if not (pe_acc and t.w is not None and t.w[2] == "pe" and eng == "pe"):
                need(t.w)
            for d in t.r:
                need(d)
        if dma:
            i = self.dma_rr[eng]
            self.dma_rr[eng] = (i + 1) % N_DMA_SEMS
            key = self._sem_key(("dma", eng, i))
            prev = self.dma_val.get(key, 0)
            if prev:
                if waits.get(key, 0) < prev:
                    waits[key] = prev
            val = prev + 16
            self.dma_val[key] = val
            inc = 16
        else:
            c = self.cnt[eng]
            self.cnt[eng] = c + 1
            key = self._sem_key(("eng", eng, c // SEM_CHUNK))
            val = (c % SEM_CHUNK) + 1
            inc = 1
        wl = []
        wd = self.waited[eng]
        for k, v in waits.items():
            if wd.get(k, 0) >= v:
                continue
            wd[k] = v
            wl.append((k, v))
        self.ops[eng].append((fn, wl, key, inc))
        tok = (key, val, eng)
        self.last_tok[key] = val
        for t in reads:
            t.r.append(tok)
        for t in writes:
            t.w = tok
            t.r = []
        return tok

    def mark(self):
        return len(self.ctx)

    def free_to(self, mark):
        while len(self.ctx) > mark:
            g = self.ctx.pop()
            g.__exit__(None, None, None)

    def barrier(self):
        for eng in self.ENGS:
            wl = []
            wd = self.waited[eng]
            for k, v in self.last_tok.items():
                if k[0] == "eng" and k[1] == eng:
                    continue
                if wd.get(k, 0) >= v:
                    continue
                wd[k] = v
                wl.append((k, v))
            if wl:
                self.ops[eng].append((None, wl, None, 0))

    def finish_wait(self, eng, toks):
        self.final_waits.append((eng, toks))

    def emit(self):
        nc = self.nc
        for key in self.sem_names:
            if key not in self.sems:
                g = nc.semaphore("s%d" % len(self.sems))
                self.sems[key] = g.__enter__()
                self.sem_guards.append(g)
        sems = self.sems
        ops = self.ops
        final_waits = self.final_waits

        def run(engname, h):
            for fn, wl, key, inc in ops[engname]:
                for k, v in wl:
                    h.wait_ge(sems[k], v)
                if fn is not None:
                    ins = fn(h)
                    ins.then_inc(sems[key], inc)
            for e, toks in final_waits:
                if e == engname:
                    for (k, v, _) in toks:
                        h.wait_ge(sems[k], v)

        with nc.Block() as block:
            @block.tensor
            def _(h):
                run("pe", h)

            @block.scalar
            def _(h):
                run("act", h)

            @block.vector
            def _(h):
                run("dve", h)

            @block.gpsimd
            def _(h):
                run("pool", h)

            @block.sync
            def _(h):
                run("sp", h)
        self.ops = {e: [] for e in self.ENGS}
        self.final_waits = []

    def close(self):
        self.free_to(0)
        for g in reversed(self.sem_guards):
            g.__exit__(None, None, None)


D = 1024
CTX = 256
INC = 3096
EPS = 1e-6
NEG_E05 = -0.6065306597126334


class MK:
    def __init__(self, T, L=2, dbg=None):
        self.T = T
        self.L = L
        self.NT = CTX + T
        self.dbg = dbg or set()
        nc = bass.Bass("TRN2", target_bir_lowering=False)
        self.nc = nc
        self.k = KB(nc)
        self.decl()

    def decl(self):
        k = self.k
        T, L, NT = self.T, self.L, self.NT
        I = lambda n, s: k.dram(n, s, F32, kind="ExternalInput")
        self.x = I("x", [T, D]); self.c = I("c", [D]); self.ctx = I("ctx", [CTX, D]); self.c_ctx = I("c_ctx", [D])
        self.ada_w = I("ada_w", [L, D, 6 * D]); self.ada_b = I("ada_b", [L, 6 * D])
        self.norm1_g = I("norm1_g", [L, D]); self.norm2_g = I("norm2_g", [L, D])
        self.w_in = I("w_in", [L, D, INC]); self.w_out = I("w_out", [L, D, D])
        self.rw_mu_prev = I("rw_mu_prev", [L, 1024]); self.rw_mu_next = I("rw_mu_next", [L, 1024])
        self.rw_w0 = I("rw_w0", [L, 2, 256]); self.rw_w2 = I("rw_w2", [L, 2, 64, 256])
        self.rw_a0 = I("rw_a0", [L, 2, 256]); self.rw_a2 = I("rw_a2", [L, 2, 64, 256])
        self.rw_g2 = I("rw_g2", [L, 128, 256])
        self.rw_k_k = I("rw_k_k", [L, 256]); self.rw_k_a = I("rw_k_a", [L, 256]); self.rw_r_k = I("rw_r_k", [L, 256])
        self.rw_gn_g = I("rw_gn_g", [L, 256]); self.rw_gn_b = I("rw_gn_b", [L, 256])
        self.ssm_conv_w = I("ssm_conv_w", [L, 3, 3, 768]); self.ssm_conv_b = I("ssm_conv_b", [L, 768])
        self.ssm_dt_bias = I("ssm_dt_bias", [L, 2, 8]); self.ssm_a_log = I("ssm_a_log", [L, 2, 8])
        self.ssm_d = I("ssm_d", [L, 8]); self.ssm_norm_g = I("ssm_norm_g", [L, 512])
        self.gla_ga2 = I("gla_ga2", [L, 2, 16, 128]); self.gla_gb = I("gla_gb", [L, 2, 128]); self.gla_norm_g = I("gla_norm_g", [L, 256])
        self.moe_rg_w = I("moe_rg_w", [L, D, 4]); self.moe_rg_b = I("moe_rg_b", [L, 4])
        self.moe_re_w = I("moe_re_w", [L, D, 16]); self.moe_re_b = I("moe_re_b", [L, 16])
        self.moe_w1 = I("moe_w1", [L, 16, D, 512]); self.moe_w3 = I("moe_w3", [L, 16, D, 512]); self.moe_w2 = I("moe_w2", [L, 16, 512, D])
        self.final_g = I("final_g", [D])
        self.c_ident = I("c_ident", [128, 128])
        self.out = k.dram("out", [T, D], F32, kind="ExternalOutput")
        def S(n, s, dt=F32):
            kind = "ExternalOutput" if n in self.dbg else "Internal"
            return k.dram(n, s, dt, kind=kind)
        self.xT = S("xT", [D, NT])
        self.uT = S("uT", [3200, NT])
        self.mixT = S("mixT", [D, NT])
        self.rw_consts()
        self.ssm_decl()
        self.moe_decl()

    def consts(self):
        k = self.k
        self.identf = k.sb("identf", [128, 128], F32)
        self.ident = k.sb("ident", [128, 128], BF16)
        self.ones = k.sb("ones", [128, 128], BF16)
        k.op("sp", lambda e: e.dma_start(out=self.identf[:, :], in_=self.c_ident.ap[:, :]), writes=[self.identf], dma=True)
        k.op("dve", lambda e: e.tensor_copy(self.ident[:, :], self.identf[:, :]), reads=[self.identf], writes=[self.ident])
        k.op("dve", lambda e: e.memset(self.ones[:, :], 1.0), writes=[self.ones])
        self.modT = k.sb("modT", [128, self.L, 48, 2], F32)
        self.gm = k.sb("gm", [128, self.L, 2, 8, 2], F32)
        self.load_consts2()

    def phase_mod(self):
        k = self.k
        L = self.L
        m = k.mark()
        cf = k.sb("cf", [128, 8, 2], F32)
        cb = k.sb("cb", [128, 8, 2], BF16)
        adab = k.sb("adab", [128, 48], F32)
        ng = k.sb("ng", [128, 2, 8], F32)
        aw = k.sb("aw", [128, 8, 3072], BF16)
        pm = k.ps("pm", [128, 48, 2], F32)
        k.op("sp", lambda e: e.dma_start(out=cf[:, :, 0], in_=self.c.ap.rearrange("(c p) -> p c", p=128), allow_slow_non_contiguous=True), writes=[cf], dma=True)
        k.op("sp", lambda e: e.dma_start(out=cf[:, :, 1], in_=self.c_ctx.ap.rearrange("(c p) -> p c", p=128), allow_slow_non_contiguous=True), writes=[cf], dma=True)
        k.op("act", lambda e: e.activation(out=cb[:, :, :], in_=cf[:, :, :], func=AF.Silu), reads=[cf], writes=[cb])
        for l in range(L):
            k.op("sp", lambda e, l=l: e.dma_start(out=adab[:, :], in_=self.ada_b.ap[l].rearrange("(c p) -> p c", p=128), allow_slow_non_contiguous=True), writes=[adab], dma=True)
            k.op("sp", lambda e, l=l: e.dma_start(out=ng[:, 0, :], in_=self.norm1_g.ap[l].rearrange("(c p) -> p c", p=128), allow_slow_non_contiguous=True), writes=[ng], dma=True)
            k.op("sp", lambda e, l=l: e.dma_start(out=ng[:, 1, :], in_=self.norm2_g.ap[l].rearrange("(c p) -> p c", p=128), allow_slow_non_contiguous=True), writes=[ng], dma=True)
            for half in range(2):
                for c in range(8):
                    k.op("pool", lambda e, l=l, c=c, half=half: e.dma_start(out=aw[:, c, :], in_=self.ada_w.ap[l, c * 128:(c + 1) * 128, half * 3072:(half + 1) * 3072]), writes=[aw], dma=True)
                for j in range(24):
                    for c in range(8):
                        k.op("pe", lambda e, c=c, j=j, half=half: e.matmul(pm[:, half * 24 + j, :], lhsT=aw[:, c, j * 128:(j + 1) * 128], rhs=cb[:, c, :], start=(c == 0), stop=(c == 7)), reads=[aw, cb], writes=[pm], pe_acc=True)
            k.op("dve", lambda e, l=l: e.tensor_tensor(self.modT[:, l, :, :], pm[:, :, :], adab[:, :].unsqueeze(2).to_broadcast([128, 48, 2]), ALU.add), reads=[pm, adab], writes=[self.modT])
            for w, j in ((0, 1), (1, 4)):
                k.op("dve", lambda e, l=l, w=w, j=j: e.scalar_tensor_tensor(out=self.gm[:, l, w, :, :], in0=self.modT[:, l, j * 8:(j + 1) * 8, :], scalar=1.0, in1=ng[:, w, :].unsqueeze(2).to_broadcast([128, 8, 2]), op0=ALU.add, op1=ALU.mult), reads=[self.modT, ng], writes=[self.gm])
        k.barrier()
        k.emit()
        k.free_to(m)

    def tok_tiles(self, lat_only=False, n=512):
        tiles = []
        if not lat_only:
            tiles.append((0, CTX, 1))
        for i in range(self.T // n):
            tiles.append((CTX + i * n, n, 0))
        return tiles

    def phase_x_in(self):
        k = self.k
        m = k.mark()
        xin = [k.sb("xin%d" % i, [128, D], F32) for i in range(2)]
        pt = [k.ps("ptx%d" % i, [128, 4, 128], F32) for i in range(2)]
        xo = [k.sb("xo%d" % i, [128, 8, 128], F32) for i in range(2)]
        nt = self.NT // 128
        for i in range(nt):
            src = self.ctx.ap[i * 128:(i + 1) * 128, :] if i < 2 else self.x.ap[(i - 2) * 128:(i - 1) * 128, :]
            b = i % 2
            k.op("sp", lambda e, b=b, src=src: e.dma_start(out=xin[b][:, :], in_=src), writes=[xin[b]], dma=True)
            for hh in range(2):
                for c in range(4):
                    cc = hh * 4 + c
                    k.op("pe", lambda e, b=b, hh=hh, c=c, cc=cc: e.transpose(pt[hh][:, c, :], xin[b][:, cc * 128:(cc + 1) * 128], self.identf[:, :]), reads=[xin[b], self.identf], writes=[pt[hh]], pe_acc=True)
                eng = "dve" if hh == 0 else "act"
                if eng == "dve":
                    k.op("dve", lambda e, b=b, hh=hh: e.tensor_copy(xo[b][:, hh * 4:(hh + 1) * 4, :], pt[hh][:, :, :]), reads=[pt[hh]], writes=[xo[b]])
                else:
                    k.op("act", lambda e, b=b, hh=hh: e.copy(xo[b][:, hh * 4:(hh + 1) * 4, :], pt[hh][:, :, :]), reads=[pt[hh]], writes=[xo[b]])
            k.op("pool", lambda e, b=b, i=i: e.dma_start(out=self.xT.ap.rearrange("(c p) t -> p c t", p=128)[:, :, i * 128:(i + 1) * 128], in_=xo[b][:, :, :]), reads=[xo[b]], dma=True)
        k.barrier()
        k.emit()
        k.free_to(m)

    def norm_tile(self, l, which, tok0, n, stream, xt, sq, pss, rstd, hT, keep_f32=False):
        k = self.k
        k.op("act", lambda e: e.activation(out=sq[:, :, :n], in_=xt[:, :, :n], func=AF.Square), reads=[xt], writes=[sq])
        for c in range(8):
            k.op("pe", lambda e, c=c: e.matmul(pss[:, :n], lhsT=self.ones[:, :], rhs=sq[:, c, :n], start=(c == 0), stop=(c == 7)), reads=[sq, self.ones], writes=[pss], pe_acc=True)
        k.op("act", lambda e: e.activation(out=rstd[:, :n], in_=pss[:, :n], func=AF.Sqrt, scale=1.0 / D, bias=EPS), reads=[pss], writes=[rstd])
        k.op("dve", lambda e: e.reciprocal(rstd[:, :n], rstd[:, :n]), reads=[rstd], writes=[rstd])
        k.op("dve", lambda e: e.tensor_tensor(xt[:, :, :n], xt[:, :, :n], rstd[:, :n].unsqueeze(1).to_broadcast([128, 8, n]), ALU.mult), reads=[xt, rstd], writes=[xt])
        sh_j = 0 if which == 0 else 3
        k.op("pool", lambda e: e.tensor_tensor(xt[:, :, :n], xt[:, :, :n], self.gm[:, l, which, :, stream].unsqueeze(2).to_broadcast([128, 8, n]), ALU.mult), reads=[xt, self.gm], writes=[xt])
        if keep_f32:
            k.op("dve", lambda e: e.tensor_tensor(xt[:, :, :n], xt[:, :, :n], self.modT[:, l, sh_j * 8:(sh_j + 1) * 8, stream].unsqueeze(2).to_broadcast([128, 8, n]), ALU.add), reads=[xt, self.modT], writes=[xt])
            k.op("act", lambda e: e.copy(hT[:, :, :n], xt[:, :, :n]), reads=[xt], writes=[hT])
        else:
            k.op("dve", lambda e: e.tensor_tensor(hT[:, :, :n], xt[:, :, :n], self.modT[:, l, sh_j * 8:(sh_j + 1) * 8, stream].unsqueeze(2).to_broadcast([128, 8, n]), ALU.add), reads=[xt, self.modT], writes=[hT])

    def phase_inproj(self, l):
        k = self.k
        m = k.mark()
        win = k.sb("win", [128, 8, INC], BF16)
        for c in range(8):
            k.op("pool", lambda e, c=c: e.dma_start(out=win[:, c, :], in_=self.w_in.ap[l, c * 128:(c + 1) * 128, :]), writes=[win], dma=True)
        xt = [k.sb("xt%d" % i, [128, 8, 512], F32) for i in range(2)]
        sq = k.sb("sq", [128, 8, 512], BF16)
        rstd = k.sb("rstd", [128, 512], F32)
        hT = [k.sb("hT%d" % i, [128, 8, 512], BF16) for i in range(2)]
        pss = k.ps("pss", [128, 512], F32)
        pu = [k.ps("pu%d" % i, [128, 512], F32) for i in range(4)]
        us = [k.sb("us%d" % i, [128, 512], F32) for i in range(4)]
        xTv = self.xT.ap.rearrange("(c p) t -> p c t", p=128)
        nchunk = (INC + 127) // 128
        cnt = 0
        for ti, (tok0, n, stream) in enumerate(self.tok_tiles()):
            b = ti % 2
            k.op("sp", lambda e, b=b, tok0=tok0, n=n: e.dma_start(out=xt[b][:, :, :n], in_=xTv[:, :, tok0:tok0 + n]), writes=[xt[b]], dma=True)
            self.norm_tile(l, 0, tok0, n, stream, xt[b], sq, pss, rstd, hT[b])
            for j in range(nchunk):
                c0 = j * 128
                nc_ = min(128, INC - c0)
                pb = cnt % 4
                cnt += 1
                for c in range(8):
                    k.op("pe", lambda e, c=c, c0=c0, nc_=nc_, pb=pb, b=b, n=n: e.matmul(pu[pb][:nc_, :n], lhsT=win[:, c, c0:c0 + nc_], rhs=hT[b][:, c, :n], start=(c == 0), stop=(c == 7)), reads=[win, hT[b]], writes=[pu[pb]], pe_acc=True)
                if j % 2 == 0:
                    k.op("dve", lambda e, pb=pb, nc_=nc_, n=n: e.tensor_copy(us[pb][:nc_, :n], pu[pb][:nc_, :n]), reads=[pu[pb]], writes=[us[pb]])
                else:
                    k.op("act", lambda e, pb=pb, nc_=nc_, n=n: e.copy(us[pb][:nc_, :n], pu[pb][:nc_, :n]), reads=[pu[pb]], writes=[us[pb]])
                k.op("pool", lambda e, pb=pb, nc_=nc_, n=n, c0=c0, tok0=tok0: e.dma_start(out=self.uT.ap[c0:c0 + nc_, tok0:tok0 + n], in_=us[pb][:nc_, :n]), reads=[us[pb]], dma=True)
        k.barrier()
        k.emit()
        k.free_to(m)

    def finish(self, last_tensor):
        k = self.k
        k.barrier()
        k.emit()
        k.close()


def _rw_consts(self):
    k = self.k
    I = lambda n, s: k.dram(n, s, F32, kind="ExternalInput")
    self.c_masks = I("c_masks", [4, 128, 512])
    self.c_id8 = I("c_id8", [128, 512])
    self.c_blk = I("c_blk", [128, 128])
    self.c_reset = I("c_reset", [128, 512])
    def S(n, s, dt=F32):
        kind = "ExternalOutput" if n in self.dbg else "Internal"
        return k.dram(n, s, dt, kind=kind)
    self.rw_yf = S("rw_yf", [256, self.NT])
    self.rw_bonus = S("rw_bonus", [256, self.NT])
    self.rw_gate = S("rw_gate", [256, self.NT])


def _load_consts2(self):
    k = self.k
    self.masks = k.sb("masks", [128, 4, 512], BF16)
    self.id8 = k.sb("id8", [128, 512], BF16)
    self.blk = k.sb("blk", [128, 128], BF16)
    self.reset = k.sb("reset", [128, 512], F32)
    for i in range(4):
        k.op("pool", lambda e, i=i: e.dma_start(out=self.masks[:, i, :], in_=self.c_masks.ap[i]), writes=[self.masks], dma=True)
    k.op("pool", lambda e: e.dma_start(out=self.id8[:, :], in_=self.c_id8.ap[:, :]), writes=[self.id8], dma=True)
    k.op("pool", lambda e: e.dma_start(out=self.blk[:, :], in_=self.c_blk.ap[:, :]), writes=[self.blk], dma=True)
    k.op("sp", lambda e: e.dma_start(out=self.reset[:, :], in_=self.c_reset.ap[:, :]), writes=[self.reset], dma=True)


def _pvec(self, name, src_ap, ncols, eng="sp", dt=F32):
    k = self.k
    t = k.sb(name, [128, ncols], dt)
    k.op(eng, lambda e: e.dma_start(out=t[:, :], in_=src_ap.rearrange("(c p) -> p c", p=128), allow_slow_non_contiguous=True), writes=[t], dma=True)
    return t


def interleave(gens, weights=None):
    gens = [(g, (weights[i] if weights else 1)) for i, g in enumerate(gens)]
    while gens:
        for item in list(gens):
            g, w = item
            try:
                for _ in range(w):
                    next(g)
            except StopIteration:
                gens.remove(item)


def phase_rwkv(self, l, d):
    k = self.k
    m = k.mark()
    NT = self.NT
    s = NEG_E05
    mp = self._pvec("mp", self.rw_mu_prev.ap[l], 8)
    mn = self._pvec("mn", self.rw_mu_next.ap[l], 8)
    cmix = k.sb("cmix", [128, 8], F32)
    k.op("dve", lambda e: e.tensor_tensor(cmix[:, :], mp[:, :], mn[:, :], ALU.add), reads=[mp, mn], writes=[cmix])
    k.op("dve", lambda e: e.tensor_scalar(cmix[:, :], cmix[:, :], -1.0, 1.0, ALU.mult, ALU.add), reads=[cmix], writes=[cmix])
    w0 = self._pvec("w0", self.rw_w0.ap[l, d], 2)
    a0 = self._pvec("a0", self.rw_a0.ap[l, d], 2)
    k_k = self._pvec("k_k", self.rw_k_k.ap[l], 2)
    k_a = self._pvec("k_a", self.rw_k_a.ap[l], 2)
    r_k = self._pvec("r_k", self.rw_r_k.ap[l], 2)
    gn_g = self._pvec("gn_g", self.rw_gn_g.ap[l], 2)
    gn_b = self._pvec("gn_b", self.rw_gn_b.ap[l], 2)
    w2s = k.sb("w2s", [128, 256], BF16)
    a2s = k.sb("a2s", [128, 256], BF16)
    g2s = k.sb("g2s", [128, 256], BF16)
    k.op("pool", lambda e: e.dma_start(out=w2s[0:64, :], in_=self.rw_w2.ap[l, d]), writes=[w2s], dma=True)
    k.op("pool", lambda e: e.dma_start(out=a2s[64:128, :], in_=self.rw_a2.ap[l, d]), writes=[a2s], dma=True)
    k.op("pool", lambda e: e.dma_start(out=g2s[:, :], in_=self.rw_g2.ap[l]), writes=[g2s], dma=True)

    U = [k.sb("rU%d" % i, [128, 8, 514], F32) for i in range(2)]
    S = k.sb("rS", [128, 8, 512], F32)
    wlt = k.sb("wlt", [128, 512], BF16)
    alb = k.sb("alb", [128, 512], BF16)
    sgl = k.sb("sgl", [128, 512], BF16)
    f32t = lambda n_: k.sb(n_, [128, 8, 64], F32)
    bft = lambda n_: k.sb(n_, [128, 8, 64], BF16)
    sgw, asig, kx, rn, cs, dcs, tmp, E, Etrue = [f32t("r_" + z) for z in ("sgw", "asig", "kx", "rn", "cs", "dcs", "tmp", "E", "Etrue")]
    E2_, E3_ = f32t("r_E2"), f32t("r_E3")
    kkn, bvec, kmod = f32t("kkn"), f32t("bvec"), f32t("kmod")
    sqk = bft("sqk")
    AB = {z: [[bft("r_%s%d%d" % (z, b, hp)) for hp in range(2)] for b in range(2)] for z in ("rt", "kt", "bt", "at", "Rtrue", "Atrue", "Bh", "Kh", "vb")}
    WCs = [[k.sb("WC%d%d" % (b, hp), [128, 8], F32) for hp in range(2)] for b in range(2)]
    Atm, Bhtm, Khtm, Vtm = [[bft("r_%s%d" % (z, hp)) for hp in range(2)] for z in ("Atm", "Bhtm", "Khtm", "Vtm")]
    Zs, Ns, Zs2, Ns2, Aak, Arb, Ark, X, AVs, PaT, Us = [[bft("r_%s%d" % (z, hp)) for hp in range(2)] for z in ("Zs", "Ns", "Zs2", "Ns2", "Aak", "Arb", "Ark", "X", "AVs", "PaT", "Us")]
    Qs = [f32t("Qs%d" % hp) for hp in range(2)]
    Mst = [k.sb("Mst%d" % hp, [128, 64], F32) for hp in range(2)]
    Mbf = [k.sb("Mbf%d" % hp, [128, 64], BF16) for hp in range(2)]
    ysb = [k.sb("ysb%d" % hp, [128, 512], F32) for hp in range(2)]
    bon = k.sb("bon", [128, 512], F32)
    gat = k.sb("gat", [128, 512], F32)
    fbon = k.sb("fbon", [128, 512], F32)
    fgat = k.sb("fgat", [128, 512], F32)
    fkx, frn = f32t("r_fkx"), f32t("r_frn")
    fsq = bft("r_fsq")
    yf_in = [k.sb("yfin%d" % hp, [128, 512], F32) for hp in range(2)]
    PS = [k.ps("rps%d" % i, [128, 512], F32) for i in range(4)]
    PY = [k.ps("rpy%d" % j, [128, 512], F32) for j in range(2)]
    PSB = [k.ps("rpsb%d" % i, [128, 8, 64], BF16) for i in range(2)]
    psi = [0]
    psbi = [0]

    def nps():
        p = PS[psi[0] % 4]
        psi[0] += 1
        return p

    def npsb():
        p = PSB[psbi[0] % 2]
        psbi[0] += 1
        return p

    for hp in range(2):
        k.op("dve", lambda e, hp=hp: e.memset(Mst[hp][:, :], 0.0), writes=[Mst[hp]])
        k.op("dve", lambda e, hp=hp: e.memset(Mbf[hp][:, :], 0.0), writes=[Mbf[hp]])

    M_SL, M_LE, M_SG, M_GE = 0, 1, 2, 3
    if d == 0:
        mZ, mN, mI = M_SL, M_SG, M_LE
    else:
        mZ, mN, mI = M_SG, M_SL, M_GE

    tiles = self.tok_tiles()
    lat = tiles[1:]
    order = [tiles[0]] + (lat if d == 0 else lat[::-1])
    uTv = self.uT.ap[0:1024, :].rearrange("(c p) t -> p c t", p=128)

    def stageA(ti, tok0, n, stream):
        nch = n // 64
        b = ti % 2
        rt, kt, bt, at, Rtrue, Atrue, Bh, Kh, vb = [AB[z][b] for z in ("rt", "kt", "bt", "at", "Rtrue", "Atrue", "Bh", "Kh", "vb")]
        WC = WCs[b]
        seq0, seq1 = (0, CTX) if stream == 1 else (CTX, NT)
        ub = U[ti % 2]
        lo = max(tok0 - 1, seq0)
        hi = min(tok0 + n + 1, seq1)
        if lo > tok0 - 1:
            k.op("pool", lambda e: e.memset(ub[:, :, 0:1], 0.0), writes=[ub])
        if hi < tok0 + n + 1:
            k.op("pool", lambda e: e.memset(ub[:, :, n + 1:n + 2], 0.0), writes=[ub])
        k.op("sp", lambda e: e.dma_start(out=ub[:, :, lo - (tok0 - 1):hi - (tok0 - 1)], in_=uTv[:, :, lo:hi]), writes=[ub], dma=True)
        fl = lambda t_: t_[:, :, :].rearrange("p c t -> p (c t)")[:, 0:n]
        yield

        def shift(c):
            k.op("dve", lambda e: e.tensor_scalar(S[:, c, :n], ub[:, c, 1:n + 1], cmix[:, c:c + 1], None, ALU.mult), reads=[ub, cmix], writes=[S])
            k.op("dve", lambda e: e.scalar_tensor_tensor(out=S[:, c, :n], in0=ub[:, c, 0:n], scalar=mp[:, c:c + 1], in1=S[:, c, :n], op0=ALU.mult, op1=ALU.add), reads=[ub, mp, S], writes=[S])
            k.op("dve", lambda e: e.scalar_tensor_tensor(out=S[:, c, :n], in0=ub[:, c, 2:n + 2], scalar=mn[:, c:c + 1], in1=S[:, c, :n], op0=ALU.mult, op1=ALU.add), reads=[ub, mn, S], writes=[S])
        for c in range(8):
            if d == 1 and c == 7:
                continue
            shift(c)
            yield
        k.op("act", lambda e: e.activation(out=wlt[0:64, :n], in_=S[0:64, 6, :n], func=AF.Tanh), reads=[S], writes=[wlt])
        k.op("act", lambda e: e.copy(alb[64:128, :n], S[64:128, 6, :n]), reads=[S], writes=[alb])
        if d == 0:
            k.op("act", lambda e: e.activation(out=sgl[:, :n], in_=S[:, 7, :n], func=AF.Sigmoid), reads=[S], writes=[sgl])
        yield

        def prep(hp):
            p1 = nps()
            k.op("pe", lambda e: e.matmul(p1[:, :n], lhsT=w2s[0:64, hp * 128:(hp + 1) * 128], rhs=wlt[0:64, :n], start=True, stop=True), reads=[w2s, wlt], writes=[p1])
            k.op("act", lambda e: e.activation(out=fl(sgw), in_=p1[:, :n], func=AF.Sigmoid, bias=w0[:, hp:hp + 1]), reads=[p1, w0], writes=[sgw])
            p2 = nps()
            k.op("pe", lambda e: e.matmul(p2[:, :n], lhsT=a2s[64:128, hp * 128:(hp + 1) * 128], rhs=alb[64:128, :n], start=True, stop=True), reads=[a2s, alb], writes=[p2])
            k.op("act", lambda e: e.activation(out=fl(asig), in_=p2[:, :n], func=AF.Sigmoid, bias=a0[:, hp:hp + 1]), reads=[p2, a0], writes=[asig])
            yield
            k.op("dve", lambda e: e.tensor_scalar(fl(kx), S[:, 2 + hp, :n], k_k[:, hp:hp + 1], None, ALU.mult), reads=[S, k_k], writes=[kx])
            k.op("act", lambda e: e.activation(out=fl(sqk), in_=fl(kx), func=AF.Square), reads=[kx], writes=[sqk])
            p3 = nps()
            k.op("pe", lambda e: e.matmul(p3[:, :n], lhsT=self.blk[:, :], rhs=fl(sqk), start=True, stop=True), reads=[self.blk, sqk], writes=[p3])
            k.op("act", lambda e: e.activation(out=fl(rn), in_=p3[:, :n], func=AF.Ln, bias=1e-12), reads=[p3], writes=[rn])
            k.op("act", lambda e: e.activation(out=fl(rn), in_=fl(rn), func=AF.Exp, scale=-0.5), reads=[rn], writes=[rn])
            yield
            k.op("dve", lambda e: e.tensor_tensor(fl(kkn), fl(kx), fl(rn), ALU.mult), reads=[kx, rn], writes=[kkn])
            k.op("pool", lambda e: e.tensor_tensor(fl(bvec), fl(kkn), fl(asig), ALU.mult), reads=[kkn, asig], writes=[bvec])
            k.op("dve", lambda e: e.tensor_scalar(fl(tmp), fl(asig), -1.0, k_a[:, hp:hp + 1], ALU.add, ALU.mult), reads=[asig, k_a], writes=[tmp])
            k.op("dve", lambda e: e.scalar_tensor_tensor(out=fl(kmod), in0=fl(tmp), scalar=1.0, in1=S[:, 2 + hp, :n], op0=ALU.add, op1=ALU.mult), reads=[tmp, S], writes=[kmod])
            yield
            k.op("dve", lambda e: e.tensor_tensor_scan(fl(cs), self.reset[:, :n], fl(sgw), 0.0, ALU.mult, ALU.add), reads=[self.reset, sgw], writes=[cs])
            if d == 1:
                k.op("dve", lambda e: e.tensor_tensor(fl(tmp), fl(sgw), fl(cs), ALU.subtract), reads=[sgw, cs], writes=[tmp])
                k.op("dve", lambda e: e.tensor_tensor(cs[:, :nch, :], tmp[:, :nch, :], cs[:, :nch, 63:64].to_broadcast([128, nch, 64]), ALU.add), reads=[tmp, cs], writes=[cs])
            endi = 63 if d == 0 else 0
            k.op("pool", lambda e: e.tensor_tensor(dcs[:, :nch, :], cs[:, :nch, :], cs[:, :nch, 32:33].to_broadcast([128, nch, 64]), ALU.subtract), reads=[cs], writes=[dcs])
            yield
            k.op("act", lambda e: e.activation(out=fl(E), in_=fl(dcs), func=AF.Exp, scale=s), reads=[dcs], writes=[E])
            k.op("pool", lambda e: e.tensor_tensor(fl(tmp), fl(dcs), fl(sgw), ALU.subtract), reads=[dcs, sgw], writes=[tmp])
            k.op("act", lambda e: e.activation(out=fl(E2_), in_=fl(tmp), func=AF.Exp, scale=s), reads=[tmp], writes=[E2_])
            k.op("act", lambda e: e.activation(out=fl(E3_), in_=fl(dcs), func=AF.Exp, scale=-s), reads=[dcs], writes=[E3_])
            k.op("act", lambda e: e.activation(out=fl(Etrue), in_=fl(cs), func=AF.Exp, scale=s), reads=[cs], writes=[Etrue])
            yield
            k.op("dve", lambda e: e.tensor_tensor(fl(rt[hp]), S[:, hp, :n], fl(E), ALU.mult), reads=[S, E], writes=[rt[hp]])
            k.op("dve", lambda e: e.scalar_tensor_tensor(out=fl(at[hp]), in0=fl(kkn), scalar=-1.0, in1=fl(E2_), op0=ALU.mult, op1=ALU.mult), reads=[kkn, E2_], writes=[at[hp]])
            k.op("dve", lambda e: e.tensor_tensor(fl(kt[hp]), fl(kmod), fl(E3_), ALU.mult), reads=[kmod, E3_], writes=[kt[hp]])
            k.op("pool", lambda e: e.tensor_tensor(fl(bt[hp]), fl(bvec), fl(E3_), ALU.mult), reads=[bvec, E3_], writes=[bt[hp]])
            yield
            k.op("dve", lambda e: e.tensor_tensor(fl(Rtrue[hp]), S[:, hp, :n], fl(Etrue), ALU.mult), reads=[S, Etrue], writes=[Rtrue[hp]])
            k.op("dve", lambda e: e.tensor_copy(WC[hp][:, :nch], Etrue[:, :nch, endi]), reads=[Etrue], writes=[WC[hp]])
            k.op("pool", lambda e: e.tensor_tensor(fl(tmp), fl(cs), fl(sgw), ALU.subtract), reads=[cs, sgw], writes=[tmp])
            k.op("act", lambda e: e.activation(out=fl(E), in_=fl(tmp), func=AF.Exp, scale=s), reads=[tmp], writes=[E])
            k.op("dve", lambda e: e.scalar_tensor_tensor(out=fl(Atrue[hp]), in0=fl(kkn), scalar=-1.0, in1=fl(E), op0=ALU.mult, op1=ALU.mult), reads=[kkn, E], writes=[Atrue[hp]])
            yield
            k.op("pool", lambda e: e.tensor_tensor(tmp[:, :nch, :], cs[:, :nch, endi:endi + 1].to_broadcast([128, nch, 64]), cs[:, :nch, :], ALU.subtract), reads=[cs], writes=[tmp])
            k.op("act", lambda e: e.activation(out=fl(E2_), in_=fl(tmp), func=AF.Exp, scale=s), reads=[tmp], writes=[E2_])
            k.op("dve", lambda e: e.tensor_tensor(fl(Bh[hp]), fl(bvec), fl(E2_), ALU.mult), reads=[bvec, E2_], writes=[Bh[hp]])
            k.op("pool", lambda e: e.tensor_tensor(fl(Kh[hp]), fl(kmod), fl(E2_), ALU.mult), reads=[kmod, E2_], writes=[Kh[hp]])
            k.op("act", lambda e: e.copy(fl(vb[hp]), S[:, 4 + hp, :n]), reads=[S], writes=[vb[hp]])
            yield
            if d == 0:
                k.op("dve", lambda e: e.scalar_tensor_tensor(out=fl(sqk), in0=S[:, hp, :n], scalar=r_k[:, hp:hp + 1], in1=S[:, 2 + hp, :n], op0=ALU.mult, op1=ALU.mult), reads=[S, r_k], writes=[sqk])
                p4 = nps()
                k.op("pe", lambda e: e.matmul(p4[:, :n], lhsT=self.blk[:, :], rhs=fl(sqk), start=True, stop=True), reads=[self.blk, sqk], writes=[p4])
                k.op("dve", lambda e: e.tensor_tensor(bon[:, :n], p4[:, :n], S[:, 4 + hp, :n], ALU.mult), reads=[p4, S], writes=[bon])
                k.op("pool", lambda e: e.dma_start(out=self.rw_bonus.ap[hp * 128:(hp + 1) * 128, tok0:tok0 + n], in_=bon[:, :n]), reads=[bon], dma=True)
                p5 = nps()
                k.op("pe", lambda e: e.matmul(p5[:, :n], lhsT=g2s[:, hp * 128:(hp + 1) * 128], rhs=sgl[:, :n], start=True, stop=True), reads=[g2s, sgl], writes=[p5])
                k.op("act", lambda e: e.copy(gat[:, :n], p5[:, :n]), reads=[p5], writes=[gat])
                k.op("pool", lambda e: e.dma_start(out=self.rw_gate.ap[hp * 128:(hp + 1) * 128, tok0:tok0 + n], in_=gat[:, :n]), reads=[gat], dma=True)
                yield
        for hp in range(2):
            yield from prep(hp)

    def stageB(ti, tok0, n, stream):
        nch = n // 64
        b = ti % 2
        rt, kt, bt, at, Rtrue, Atrue, Bh, Kh, vb = [AB[z][b] for z in ("rt", "kt", "bt", "at", "Rtrue", "Atrue", "Bh", "Kh", "vb")]
        WC = WCs[b]

        def units(dst_ps, lt, rt_, P, reads):
            pv = dst_ps[:, :].rearrange("p (c t) -> p c t", t=64)
            for ch in range(nch):
                k.op("pe", lambda e, ch=ch: e.matmul(pv[P, ch, :], lhsT=lt[P, ch, :], rhs=rt_[P, ch, :], start=True, stop=True), reads=reads, writes=[dst_ps], pe_acc=True)
            return pv

        def head_block(hp, hl):
            P = slice(hl * 64, hl * 64 + 64)

            def tm(src, dst):
                pb = npsb()
                for ch in range(nch):
                    k.op("pe", lambda e, ch=ch: e.transpose(pb[P, ch, :], src[hp][P, ch, :], self.ident[P, P]), reads=[src[hp], self.ident], writes=[pb], pe_acc=True)
                k.op("act", lambda e: e.copy(dst[hp][P, :nch, :], pb[P, :nch, :]), reads=[pb], writes=[dst[hp]])
            for (a_, b_) in ((Atrue, Atm), (Bh, Bhtm), (Kh, Khtm), (vb, Vtm)):
                tm(a_, b_)
                yield

            def pair(dst, lt, rt_, mask, eng="dve"):
                pp = nps()
                pv = units(pp, lt[hp], rt_[hp], P, [lt[hp], rt_[hp]])
                mv = self.masks[:, mask, :].rearrange("p (c t) -> p c t", t=64)
                k.op(eng, lambda e: e.tensor_tensor(dst[hp][P, :nch, :], pv[P, :nch, :], mv[P, :nch, :], ALU.mult), reads=[pp, self.masks], writes=[dst[hp]])
            for args in ((Zs, bt, at, mZ), (Ns, at, bt, mN), (Aak, kt, at, mZ), (Arb, bt, rt, mI), (Ark, kt, rt, mI)):
                pair(*args)
                yield
            idv = self.id8[:, :].rearrange("p (c t) -> p c t", t=64)
            k.op("pool", lambda e: e.tensor_tensor(X[hp][P, :nch, :], Zs[hp][P, :nch, :], idv[P, :nch, :], ALU.add), reads=[Zs[hp], self.id8], writes=[X[hp]])
            Zc, Nc, Zn, Nn = Zs[hp], Ns[hp], Zs2[hp], Ns2[hp]
            for lev in range(1, 6):
                last = lev == 5

                def level(Zc, Nc, Zn, Nn, last):
                    if not last:
                        pz = nps()
                        pzv = units(pz, Nc, Zc, P, [Nc, Zc])
                    pn = nps()
                    pnv = units(pn, Zc, Nc, P, [Nc, Zc])
                    if not last:
                        k.op("act", lambda e: e.copy(Zn[P, :nch, :], pzv[P, :nch, :]), reads=[pz], writes=[Zn])
                    k.op("dve", lambda e: e.tensor_copy(Nn[P, :nch, :], pnv[P, :nch, :]), reads=[pn], writes=[Nn])
                    yield
                    px = nps()
                    pxv = units(px, Nn, X[hp], P, [Nn, X[hp]])
                    k.op("dve", lambda e: e.tensor_tensor(X[hp][P, :nch, :], pxv[P, :nch, :], X[hp][P, :nch, :], ALU.add), reads=[px, X[hp]], writes=[X[hp]])
                    yield
                yield from level(Zc, Nc, Zn, Nn, last)
                Zc, Nc, Zn, Nn = Zn, Nn, Zc, Nc
            pa = nps()
            pav = units(pa, Aak[hp], Vtm[hp], P, [Aak[hp], Vtm[hp]])
            k.op("act", lambda e: e.copy(AVs[hp][P, :nch, :], pav[P, :nch, :]), reads=[pa], writes=[AVs[hp]])
            yield
            pq = nps()
            pqv = units(pq, X[hp], AVs[hp], P, [X[hp], AVs[hp]])
            k.op("act", lambda e: e.copy(Qs[hp][P, :nch, :], pqv[P, :nch, :]), reads=[pq], writes=[Qs[hp]])
            yield
            pp_ = nps()
            ppv_ = units(pp_, Atm[hp], X[hp], P, [Atm[hp], X[hp]])
            k.op("dve", lambda e: e.tensor_copy(PaT[hp][P, :nch, :], ppv_[P, :nch, :]), reads=[pp_], writes=[PaT[hp]])
            yield

        interleave_here = [head_block(0, 0), head_block(0, 1), head_block(1, 0), head_block(1, 1)]
        alive = list(interleave_here)
        while alive:
            for g in list(alive):
                try:
                    next(g)
                except StopIteration:
                    alive.remove(g)
            yield

        def seq_step(ch, hp, hl):
            P = slice(hl * 64, hl * 64 + 64)
            pyb = PY[hl]
            col = hp * 256 + (ch % 4) * 64
            pu_ = nps()
            k.op("pe", lambda e: e.matmul(pu_[P, 0:64], lhsT=PaT[hp][P, ch, :], rhs=Mbf[hp][P, :], start=True, stop=True), reads=[PaT[hp], Mbf[hp]], writes=[pu_])
            k.op("dve", lambda e: e.tensor_tensor(Us[hp][P, ch, :], pu_[P, 0:64], Qs[hp][P, ch, :], ALU.add), reads=[pu_, Qs[hp]], writes=[Us[hp]])
            yield
            k.op("pe", lambda e: e.matmul(pyb[P, col:col + 64], lhsT=Mbf[hp][P, :], rhs=Rtrue[hp][P, ch, :], start=True, stop=False), reads=[Mbf[hp], Rtrue[hp]], writes=[pyb], pe_acc=True)
            k.op("pe", lambda e: e.matmul(pyb[P, col:col + 64], lhsT=Vtm[hp][P, ch, :], rhs=Ark[hp][P, ch, :], start=False, stop=False), reads=[Vtm[hp], Ark[hp]], writes=[pyb], pe_acc=True)
            k.op("pe", lambda e: e.matmul(pyb[P, col:col + 64], lhsT=Us[hp][P, ch, :], rhs=Arb[hp][P, ch, :], start=False, stop=True), reads=[Us[hp], Arb[hp]], writes=[pyb], pe_acc=True)
            pm_ = nps()
            k.op("pe", lambda e: e.matmul(pm_[P, 0:64], lhsT=Bhtm[hp][P, ch, :], rhs=Us[hp][P, ch, :], start=True, stop=False), reads=[Bhtm[hp], Us[hp]], writes=[pm_], pe_acc=True)
            k.op("pe", lambda e: e.matmul(pm_[P, 0:64], lhsT=Khtm[hp][P, ch, :], rhs=Vtm[hp][P, ch, :], start=False, stop=True), reads=[Khtm[hp], Vtm[hp]], writes=[pm_], pe_acc=True)
            k.op("dve", lambda e: e.scalar_tensor_tensor(out=Mst[hp][P, :], in0=Mst[hp][P, :], scalar=WC[hp][P, ch:ch + 1], in1=pm_[P, 0:64], op0=ALU.mult, op1=ALU.add), reads=[Mst[hp], WC[hp], pm_], writes=[Mst[hp]])
            k.op("act", lambda e: e.copy(Mbf[hp][P, :], Mst[hp][P, :]), reads=[Mst[hp]], writes=[Mbf[hp]])
            yield

        def evac_y(grp):
            for hp in range(2):
                for hl in range(2):
                    P = slice(hl * 64, hl * 64 + 64)
                    pp = PY[hl]
                    nc4 = min(4, nch - grp * 4)
                    src = pp[P, hp * 256:hp * 256 + nc4 * 64]
                    dst = ysb[hp][P, grp * 256:grp * 256 + nc4 * 64]
                    if hl == 0:
                        k.op("dve", lambda e, src=src, dst=dst: e.tensor_copy(dst, src), reads=[pp], writes=[ysb[hp]])
                    else:
                        k.op("act", lambda e, src=src, dst=dst: e.copy(dst, src), reads=[pp], writes=[ysb[hp]])

        chs = list(range(nch)) if d == 0 else list(range(nch - 1, -1, -1))
        done = 0
        for ch in chs:
            gens = [seq_step(ch, hp, hl) for hp in range(2) for hl in range(2)]
            alive = list(gens)
            while alive:
                for g in list(alive):
                    try:
                        next(g)
                    except StopIteration:
                        alive.remove(g)
                yield
            done += 1
            if done % 4 == 0 or done == nch:
                evac_y(ch // 4)
                yield

        def outp(hp):
            if d == 0:
                k.op("pool", lambda e: e.dma_start(out=self.rw_yf.ap[hp * 128:(hp + 1) * 128, tok0:tok0 + n], in_=ysb[hp][:, :n]), reads=[ysb[hp]], dma=True)
            else:
                self.rw_finish(l, hp, tok0, n, ysb[hp], yf_in[hp], fbon, fgat, gn_g, gn_b, nps, fsq, None, fkx, frn)
        for hp in range(2):
            outp(hp)
            yield

    nt = len(order)
    gA = stageA(0, *order[0])
    for _ in gA:
        pass
    for ti in range(nt):
        gens = [stageB(ti, *order[ti])]
        wts = [3]
        if ti + 1 < nt:
            gens.append(stageA(ti + 1, *order[ti + 1]))
            wts.append(1)
        if os.environ.get("RW_IL", "0") == "1":
            interleave(gens, wts)
        else:
            for g_ in gens:
                for _ in g_:
                    pass
    k.barrier()
    k.emit()
    k.free_to(m)


def phase_rwkv_old(self, l, d):
    k = self.k
    m = k.mark()
    NT = self.NT
    s = NEG_E05
    mp = self._pvec("mp", self.rw_mu_prev.ap[l], 8)
    mn = self._pvec("mn", self.rw_mu_next.ap[l], 8)
    cmix = k.sb("cmix", [128, 8], F32)
    k.op("dve", lambda e: e.tensor_tensor(cmix[:, :], mp[:, :], mn[:, :], ALU.add), reads=[mp, mn], writes=[cmix])
    k.op("dve", lambda e: e.tensor_scalar(cmix[:, :], cmix[:, :], -1.0, 1.0, ALU.mult, ALU.add), reads=[cmix], writes=[cmix])
    w0 = self._pvec("w0", self.rw_w0.ap[l, d], 2)
    a0 = self._pvec("a0", self.rw_a0.ap[l, d], 2)
    k_k = self._pvec("k_k", self.rw_k_k.ap[l], 2)
    k_a = self._pvec("k_a", self.rw_k_a.ap[l], 2)
    r_k = self._pvec("r_k", self.rw_r_k.ap[l], 2)
    gn_g = self._pvec("gn_g", self.rw_gn_g.ap[l], 2)
    gn_b = self._pvec("gn_b", self.rw_gn_b.ap[l], 2)
    w2s = k.sb("w2s", [128, 256], BF16)
    a2s = k.sb("a2s", [128, 256], BF16)
    g2s = k.sb("g2s", [128, 256], BF16)
    k.op("pool", lambda e: e.dma_start(out=w2s[0:64, :], in_=self.rw_w2.ap[l, d]), writes=[w2s], dma=True)
    k.op("pool", lambda e: e.dma_start(out=a2s[64:128, :], in_=self.rw_a2.ap[l, d]), writes=[a2s], dma=True)
    k.op("pool", lambda e: e.dma_start(out=g2s[:, :], in_=self.rw_g2.ap[l]), writes=[g2s], dma=True)

    U = [k.sb("rU%d" % i, [128, 8, 514], F32) for i in range(2)]
    S = k.sb("rS", [128, 8, 512], F32)
    wlt = k.sb("wlt", [128, 512], BF16)
    alb = k.sb("alb", [128, 512], BF16)
    sgl = k.sb("sgl", [128, 512], BF16)
    f32t = lambda n_: k.sb(n_, [128, 8, 64], F32)
    bft = lambda n_: k.sb(n_, [128, 8, 64], BF16)
    sgw, asig, kx, rn, cs, dcs, tmp, E, Etrue = [f32t("r_" + z) for z in ("sgw", "asig", "kx", "rn", "cs", "dcs", "tmp", "E", "Etrue")]
    kkn, bvec, kmod = f32t("kkn"), f32t("bvec"), f32t("kmod")
    sqk = bft("sqk")
    rt, kt, bt, at, Rtrue, Atrue, Bh, Kh, vb = [[bft("r_%s%d" % (z, hp)) for hp in range(2)] for z in ("rt", "kt", "bt", "at", "Rtrue", "Atrue", "Bh", "Kh", "vb")]
    WC = [k.sb("WC%d" % hp, [128, 8], F32) for hp in range(2)]
    Atm, Bhtm, Khtm, Vtm = [[bft("r_%s%d" % (z, hp)) for hp in range(2)] for z in ("Atm", "Bhtm", "Khtm", "Vtm")]
    Zs, Ns, Zs2, Ns2, Aak, Arb, Ark, X, AVs, PaT, Us = [[bft("r_%s%d" % (z, hp)) for hp in range(2)] for z in ("Zs", "Ns", "Zs2", "Ns2", "Aak", "Arb", "Ark", "X", "AVs", "PaT", "Us")]
    Qs = [f32t("Qs%d" % hp) for hp in range(2)]
    Mst = [k.sb("Mst%d" % hp, [128, 64], F32) for hp in range(2)]
    Mbf = [k.sb("Mbf%d" % hp, [128, 64], BF16) for hp in range(2)]
    ysb = [k.sb("ysb%d" % hp, [128, 512], F32) for hp in range(2)]
    bon = k.sb("bon", [128, 512], F32)
    gat = k.sb("gat", [128, 512], F32)
    yf_in = [k.sb("yfin%d" % hp, [128, 512], F32) for hp in range(2)]
    PS = [k.ps("rps%d" % i, [128, 512], F32) for i in range(2)]
    PY = [[k.ps("rpy%d%d" % (i, j), [128, 512], F32) for j in range(2)] for i in range(2)]
    PSB = [k.ps("rpsb%d" % i, [128, 8, 64], BF16) for i in range(2)]
    psi = [0]
    psbi = [0]

    def nps():
        p = PS[psi[0] % 2]
        psi[0] += 1
        return p

    def npsb():
        p = PSB[psbi[0] % 2]
        psbi[0] += 1
        return p

    for hp in range(2):
        k.op("dve", lambda e, hp=hp: e.memset(Mst[hp][:, :], 0.0), writes=[Mst[hp]])
        k.op("dve", lambda e, hp=hp: e.memset(Mbf[hp][:, :], 0.0), writes=[Mbf[hp]])

    M_SL, M_LE, M_SG, M_GE = 0, 1, 2, 3
    if d == 0:
        mZ, mN, mI = M_SL, M_SG, M_LE
    else:
        mZ, mN, mI = M_SG, M_SL, M_GE

    tiles = self.tok_tiles()
    lat = tiles[1:]
    order = [tiles[0]] + (lat if d == 0 else lat[::-1])
    uTv = self.uT.ap[0:1024, :].rearrange("(c p) t -> p c t", p=128)

    def v3(t, P, nch):
        return t[P, 0:nch, :]

    def v2(t, P, n):
        return t[P, :, :].rearrange("p c t -> p (c t)")[:, 0:n]

    def tile_body(ti, tok0, n, stream):
        nch = n // 64
        seq0, seq1 = (0, CTX) if stream == 1 else (CTX, NT)
        ub = U[ti % 2]
        lo = max(tok0 - 1, seq0)
        hi = min(tok0 + n + 1, seq1)
        if lo > tok0 - 1:
            k.op("pool", lambda e: e.memset(ub[:, :, 0:1], 0.0), writes=[ub])
        if hi < tok0 + n + 1:
            k.op("pool", lambda e: e.memset(ub[:, :, n + 1:n + 2], 0.0), writes=[ub])
        k.op("sp", lambda e: e.dma_start(out=ub[:, :, lo - (tok0 - 1):hi - (tok0 - 1)], in_=uTv[:, :, lo:hi]), writes=[ub], dma=True)
        fl = lambda t_: t_[:, :, :].rearrange("p c t -> p (c t)")[:, 0:n]

        def shift(c):
            k.op("dve", lambda e: e.tensor_scalar(S[:, c, :n], ub[:, c, 1:n + 1], cmix[:, c:c + 1], None, ALU.mult), reads=[ub, cmix], writes=[S])
            k.op("dve", lambda e: e.scalar_tensor_tensor(out=S[:, c, :n], in0=ub[:, c, 0:n], scalar=mp[:, c:c + 1], in1=S[:, c, :n], op0=ALU.mult, op1=ALU.add), reads=[ub, mp, S], writes=[S])
            k.op("dve", lambda e: e.scalar_tensor_tensor(out=S[:, c, :n], in0=ub[:, c, 2:n + 2], scalar=mn[:, c:c + 1], in1=S[:, c, :n], op0=ALU.mult, op1=ALU.add), reads=[ub, mn, S], writes=[S])
        for c in range(8):
            if d == 1 and c == 7:
                continue
            shift(c)
        k.op("act", lambda e: e.activation(out=wlt[0:64, :n], in_=S[0:64, 6, :n], func=AF.Tanh), reads=[S], writes=[wlt])
        k.op("act", lambda e: e.copy(alb[64:128, :n], S[64:128, 6, :n]), reads=[S], writes=[alb])
        if d == 0:
            k.op("act", lambda e: e.activation(out=sgl[:, :n], in_=S[:, 7, :n], func=AF.Sigmoid), reads=[S], writes=[sgl])

        def prep(hp):
            p1 = nps()
            k.op("pe", lambda e: e.matmul(p1[:, :n], lhsT=w2s[0:64, hp * 128:(hp + 1) * 128], rhs=wlt[0:64, :n], start=True, stop=True), reads=[w2s, wlt], writes=[p1])
            k.op("act", lambda e: e.activation(out=fl(sgw), in_=p1[:, :n], func=AF.Sigmoid, bias=w0[:, hp:hp + 1]), reads=[p1, w0], writes=[sgw])
            p2 = nps()
            k.op("pe", lambda e: e.matmul(p2[:, :n], lhsT=a2s[64:128, hp * 128:(hp + 1) * 128], rhs=alb[64:128, :n], start=True, stop=True), reads=[a2s, alb], writes=[p2])
            k.op("act", lambda e: e.activation(out=fl(asig), in_=p2[:, :n], func=AF.Sigmoid, bias=a0[:, hp:hp + 1]), reads=[p2, a0], writes=[asig])
            k.op("dve", lambda e: e.tensor_scalar(fl(kx), S[:, 2 + hp, :n], k_k[:, hp:hp + 1], None, ALU.mult), reads=[S, k_k], writes=[kx])
            k.op("act", lambda e: e.activation(out=fl(sqk), in_=fl(kx), func=AF.Square), reads=[kx], writes=[sqk])
            p3 = nps()
            k.op("pe", lambda e: e.matmul(p3[:, :n], lhsT=self.blk[:, :], rhs=fl(sqk), start=True, stop=True), reads=[self.blk, sqk], writes=[p3])
            k.op("act", lambda e: e.activation(out=fl(rn), in_=p3[:, :n], func=AF.Sqrt, bias=1e-12), reads=[p3], writes=[rn])
            k.op("dve", lambda e: e.reciprocal(fl(rn), fl(rn)), reads=[rn], writes=[rn])
            k.op("dve", lambda e: e.tensor_tensor(fl(kkn), fl(kx), fl(rn), ALU.mult), reads=[kx, rn], writes=[kkn])
            k.op("pool", lambda e: e.tensor_tensor(fl(bvec), fl(kkn), fl(asig), ALU.mult), reads=[kkn, asig], writes=[bvec])
            k.op("dve", lambda e: e.tensor_scalar(fl(tmp), fl(asig), -1.0, k_a[:, hp:hp + 1], ALU.add, ALU.mult), reads=[asig, k_a], writes=[tmp])
            k.op("dve", lambda e: e.scalar_tensor_tensor(out=fl(kmod), in0=fl(tmp), scalar=1.0, in1=S[:, 2 + hp, :n], op0=ALU.add, op1=ALU.mult), reads=[tmp, S], writes=[kmod])
            k.op("dve", lambda e: e.tensor_tensor_scan(fl(cs), self.reset[:, :n], fl(sgw), 0.0, ALU.mult, ALU.add), reads=[self.reset, sgw], writes=[cs])
            if d == 1:
                k.op("dve", lambda e: e.tensor_tensor(fl(tmp), fl(sgw), fl(cs), ALU.subtract), reads=[sgw, cs], writes=[tmp])
                k.op("dve", lambda e: e.tensor_tensor(cs[:, :nch, :], tmp[:, :nch, :], cs[:, :nch, 63:64].to_broadcast([128, nch, 64]), ALU.add), reads=[tmp, cs], writes=[cs])
            endi = 63 if d == 0 else 0
            k.op("pool", lambda e: e.tensor_tensor(dcs[:, :nch, :], cs[:, :nch, :], cs[:, :nch, 32:33].to_broadcast([128, nch, 64]), ALU.subtract), reads=[cs], writes=[dcs])
            k.op("act", lambda e: e.activation(out=fl(E), in_=fl(dcs), func=AF.Exp, scale=s), reads=[dcs], writes=[E])
            k.op("dve", lambda e: e.tensor_tensor(fl(rt[hp]), S[:, hp, :n], fl(E), ALU.mult), reads=[S, E], writes=[rt[hp]])
            k.op("pool", lambda e: e.tensor_tensor(fl(tmp), fl(dcs), fl(sgw), ALU.subtract), reads=[dcs, sgw], writes=[tmp])
            k.op("act", lambda e: e.activation(out=fl(E), in_=fl(tmp), func=AF.Exp, scale=s), reads=[tmp], writes=[E])
            k.op("dve", lambda e: e.scalar_tensor_tensor(out=fl(at[hp]), in0=fl(kkn), scalar=-1.0, in1=fl(E), op0=ALU.mult, op1=ALU.mult), reads=[kkn, E], writes=[at[hp]])
            k.op("act", lambda e: e.activation(out=fl(E), in_=fl(dcs), func=AF.Exp, scale=-s), reads=[dcs], writes=[E])
            k.op("dve", lambda e: e.tensor_tensor(fl(kt[hp]), fl(kmod), fl(E), ALU.mult), reads=[kmod, E], writes=[kt[hp]])
            k.op("pool", lambda e: e.tensor_tensor(fl(bt[hp]), fl(bvec), fl(E), ALU.mult), reads=[bvec, E], writes=[bt[hp]])
            k.op("act", lambda e: e.activation(out=fl(Etrue), in_=fl(cs), func=AF.Exp, scale=s), reads=[cs], writes=[Etrue])
            k.op("dve", lambda e: e.tensor_tensor(fl(Rtrue[hp]), S[:, hp, :n], fl(Etrue), ALU.mult), reads=[S, Etrue], writes=[Rtrue[hp]])
            k.op("dve", lambda e: e.tensor_copy(WC[hp][:, :nch], Etrue[:, :nch, endi]), reads=[Etrue], writes=[WC[hp]])
            k.op("pool", lambda e: e.tensor_tensor(fl(tmp), fl(cs), fl(sgw), ALU.subtract), reads=[cs, sgw], writes=[tmp])
            k.op("act", lambda e: e.activation(out=fl(E), in_=fl(tmp), func=AF.Exp, scale=s), reads=[tmp], writes=[E])
            k.op("dve", lambda e: e.scalar_tensor_tensor(out=fl(Atrue[hp]), in0=fl(kkn), scalar=-1.0, in1=fl(E), op0=ALU.mult, op1=ALU.mult), reads=[kkn, E], writes=[Atrue[hp]])
            k.op("pool", lambda e: e.tensor_tensor(tmp[:, :nch, :], cs[:, :nch, endi:endi + 1].to_broadcast([128, nch, 64]), cs[:, :nch, :], ALU.subtract), reads=[cs], writes=[tmp])
            k.op("act", lambda e: e.activation(out=fl(E), in_=fl(tmp), func=AF.Exp, scale=s), reads=[tmp], writes=[E])
            k.op("dve", lambda e: e.tensor_tensor(fl(Bh[hp]), fl(bvec), fl(E), ALU.mult), reads=[bvec, E], writes=[Bh[hp]])
            k.op("pool", lambda e: e.tensor_tensor(fl(Kh[hp]), fl(kmod), fl(E), ALU.mult), reads=[kmod, E], writes=[Kh[hp]])
            k.op("act", lambda e: e.copy(fl(vb[hp]), S[:, 4 + hp, :n]), reads=[S], writes=[vb[hp]])
            if d == 0:
                k.op("dve", lambda e: e.scalar_tensor_tensor(out=fl(sqk), in0=S[:, hp, :n], scalar=r_k[:, hp:hp + 1], in1=S[:, 2 + hp, :n], op0=ALU.mult, op1=ALU.mult), reads=[S, r_k], writes=[sqk])
                p4 = nps()
                k.op("pe", lambda e: e.matmul(p4[:, :n], lhsT=self.blk[:, :], rhs=fl(sqk), start=True, stop=True), reads=[self.blk, sqk], writes=[p4])
                k.op("dve", lambda e: e.tensor_tensor(bon[:, :n], p4[:, :n], S[:, 4 + hp, :n], ALU.mult), reads=[p4, S], writes=[bon])
                k.op("pool", lambda e: e.dma_start(out=self.rw_bonus.ap[hp * 128:(hp + 1) * 128, tok0:tok0 + n], in_=bon[:, :n]), reads=[bon], dma=True)
                p5 = nps()
                k.op("pe", lambda e: e.matmul(p5[:, :n], lhsT=g2s[:, hp * 128:(hp + 1) * 128], rhs=sgl[:, :n], start=True, stop=True), reads=[g2s, sgl], writes=[p5])
                k.op("act", lambda e: e.copy(gat[:, :n], p5[:, :n]), reads=[p5], writes=[gat])
                k.op("pool", lambda e: e.dma_start(out=self.rw_gate.ap[hp * 128:(hp + 1) * 128, tok0:tok0 + n], in_=gat[:, :n]), reads=[gat], dma=True)

        def units(dst_ps, lt, rt_, P, reads):
            pv = dst_ps[:, :].rearrange("p (c t) -> p c t", t=64)
            for ch in range(nch):
                k.op("pe", lambda e, ch=ch: e.matmul(pv[P, ch, :], lhsT=lt[P, ch, :], rhs=rt_[P, ch, :], start=True, stop=True), reads=reads, writes=[dst_ps], pe_acc=True)
            return pv

        def head_block(hp, hl):
            P = slice(hl * 64, hl * 64 + 64)

            def tm(src, dst):
                pb = npsb()
                for ch in range(nch):
                    k.op("pe", lambda e, ch=ch: e.transpose(pb[P, ch, :], src[hp][P, ch, :], self.ident[P, P]), reads=[src[hp], self.ident], writes=[pb], pe_acc=True)
                k.op("act", lambda e: e.copy(dst[hp][P, :nch, :], pb[P, :nch, :]), reads=[pb], writes=[dst[hp]])
            tm(Atrue, Atm); tm(Bh, Bhtm); tm(Kh, Khtm); tm(vb, Vtm)

            def pair(dst, lt, rt_, mask, eng="dve"):
                pp = nps()
                pv = units(pp, lt[hp], rt_[hp], P, [lt[hp], rt_[hp]])
                mv = self.masks[:, mask, :].rearrange("p (c t) -> p c t", t=64)
                k.op(eng, lambda e: e.tensor_tensor(dst[hp][P, :nch, :], pv[P, :nch, :], mv[P, :nch, :], ALU.mult), reads=[pp, self.masks], writes=[dst[hp]])
            pair(Zs, bt, at, mZ)
            pair(Ns, at, bt, mN)
            pair(Aak, kt, at, mZ)
            pair(Arb, bt, rt, mI)
            pair(Ark, kt, rt, mI)
            idv = self.id8[:, :].rearrange("p (c t) -> p c t", t=64)
            k.op("pool", lambda e: e.tensor_tensor(X[hp][P, :nch, :], Zs[hp][P, :nch, :], idv[P, :nch, :], ALU.add), reads=[Zs[hp], self.id8], writes=[X[hp]])
            Zc, Nc, Zn, Nn = Zs[hp], Ns[hp], Zs2[hp], Ns2[hp]
            for lev in range(1, 6):
                last = lev == 5

                def level(Zc, Nc, Zn, Nn, last):
                    if not last:
                        pz = nps()
                        pzv = units(pz, Nc, Zc, P, [Nc, Zc])
                    pn = nps()
                    pnv = units(pn, Zc, Nc, P, [Nc, Zc])
                    if not last:
                        k.op("act", lambda e: e.copy(Zn[P, :nch, :], pzv[P, :nch, :]), reads=[pz], writes=[Zn])
                    k.op("dve", lambda e: e.tensor_copy(Nn[P, :nch, :], pnv[P, :nch, :]), reads=[pn], writes=[Nn])
                    px = nps()
                    pxv = units(px, Nn, X[hp], P, [Nn, X[hp]])
                    k.op("dve", lambda e: e.tensor_tensor(X[hp][P, :nch, :], pxv[P, :nch, :], X[hp][P, :nch, :], ALU.add), reads=[px, X[hp]], writes=[X[hp]])
                level(Zc, Nc, Zn, Nn, last)
                Zc, Nc, Zn, Nn = Zn, Nn, Zc, Nc
            pa = nps()
            pav = units(pa, Aak[hp], Vtm[hp], P, [Aak[hp], Vtm[hp]])
            k.op("act", lambda e: e.copy(AVs[hp][P, :nch, :], pav[P, :nch, :]), reads=[pa], writes=[AVs[hp]])
            pq = nps()
            pqv = units(pq, X[hp], AVs[hp], P, [X[hp], AVs[hp]])
            k.op("act", lambda e: e.copy(Qs[hp][P, :nch, :], pqv[P, :nch, :]), reads=[pq], writes=[Qs[hp]])
            pp_ = nps()
            ppv_ = units(pp_, Atm[hp], X[hp], P, [Atm[hp], X[hp]])
            k.op("dve", lambda e: e.tensor_copy(PaT[hp][P, :nch, :], ppv_[P, :nch, :]), reads=[pp_], writes=[PaT[hp]])

        for hp in range(2):
            prep(hp)
            for hl in range(2):
                head_block(hp, hl)

        def seq_step(ch, hp, hl):
            P = slice(hl * 64, hl * 64 + 64)
            pyb = PY[hp][hl]
            pu_ = nps()
            k.op("pe", lambda e: e.matmul(pu_[P, 0:64], lhsT=PaT[hp][P, ch, :], rhs=Mbf[hp][P, :], start=True, stop=True), reads=[PaT[hp], Mbf[hp]], writes=[pu_])
            k.op("dve", lambda e: e.tensor_tensor(Us[hp][P, ch, :], pu_[P, 0:64], Qs[hp][P, ch, :], ALU.add), reads=[pu_, Qs[hp]], writes=[Us[hp]])
            pyv = pyb[:, :].rearrange("p (c t) -> p c t", t=64)
            k.op("pe", lambda e: e.matmul(pyv[P, ch, :], lhsT=Mbf[hp][P, :], rhs=Rtrue[hp][P, ch, :], start=True, stop=False), reads=[Mbf[hp], Rtrue[hp]], writes=[pyb], pe_acc=True)
            k.op("pe", lambda e: e.matmul(pyv[P, ch, :], lhsT=Vtm[hp][P, ch, :], rhs=Ark[hp][P, ch, :], start=False, stop=False), reads=[Vtm[hp], Ark[hp]], writes=[pyb], pe_acc=True)
            k.op("pe", lambda e: e.matmul(pyv[P, ch, :], lhsT=Us[hp][P, ch, :], rhs=Arb[hp][P, ch, :], start=False, stop=True), reads=[Us[hp], Arb[hp]], writes=[pyb], pe_acc=True)
            pm_ = nps()
            k.op("pe", lambda e: e.matmul(pm_[P, 0:64], lhsT=Bhtm[hp][P, ch, :], rhs=Us[hp][P, ch, :], start=True, stop=False), reads=[Bhtm[hp], Us[hp]], writes=[pm_], pe_acc=True)
            k.op("pe", lambda e: e.matmul(pm_[P, 0:64], lhsT=Khtm[hp][P, ch, :], rhs=Vtm[hp][P, ch, :], start=False, stop=True), reads=[Khtm[hp], Vtm[hp]], writes=[pm_], pe_acc=True)
            k.op("dve", lambda e: e.scalar_tensor_tensor(out=Mst[hp][P, :], in0=Mst[hp][P, :], scalar=WC[hp][P, ch:ch + 1], in1=pm_[P, 0:64], op0=ALU.mult, op1=ALU.add), reads=[Mst[hp], WC[hp], pm_], writes=[Mst[hp]])
            k.op("act", lambda e: e.copy(Mbf[hp][P, :], Mst[hp][P, :]), reads=[Mst[hp]], writes=[Mbf[hp]])

        chs = range(nch) if d == 0 else range(nch - 1, -1, -1)
        for ch in chs:
            for hp in range(2):
                for hl in range(2):
                    seq_step(ch, hp, hl)

        def outp(hp):
            for hl in range(2):
                P = slice(hl * 64, hl * 64 + 64)
                pp = PY[hp][hl]
                if hl == 0:
                    k.op("dve", lambda e, P=P, pp=pp: e.tensor_copy(ysb[hp][P, :n], pp[P, :n]), reads=[pp], writes=[ysb[hp]])
                else:
                    k.op("act", lambda e, P=P, pp=pp: e.copy(ysb[hp][P, :n], pp[P, :n]), reads=[pp], writes=[ysb[hp]])
            if d == 0:
                k.op("pool", lambda e: e.dma_start(out=self.rw_yf.ap[hp * 128:(hp + 1) * 128, tok0:tok0 + n], in_=ysb[hp][:, :n]), reads=[ysb[hp]], dma=True)
            else:
                self.rw_finish(l, hp, tok0, n, ysb[hp], yf_in[hp], bon, gat, gn_g, gn_b, nps, sqk, tmp, kx, rn)
        for hp in range(2):
            outp(hp)

    for ti, (tok0, n, stream) in enumerate(order):
        tile_body(ti, tok0, n, stream)
    k.barrier()
    k.emit()
    k.free_to(m)


def rw_finish(self, l, hp, tok0, n, yb, yf, bon, gat, gn_g, gn_b, nps, sqk, tmp, kx, rn):
    k = self.k
    fl = lambda t_: t_[:, :, :].rearrange("p c t -> p (c t)")[:, 0:n]
    k.op("sp", lambda e: e.dma_start(out=yf[:, :n], in_=self.rw_yf.ap[hp * 128:(hp + 1) * 128, tok0:tok0 + n]), writes=[yf], dma=True)
    k.op("sp", lambda e: e.dma_start(out=bon[:, :n], in_=self.rw_bonus.ap[hp * 128:(hp + 1) * 128, tok0:tok0 + n]), writes=[bon], dma=True)
    k.op("sp", lambda e: e.dma_start(out=gat[:, :n], in_=self.rw_gate.ap[hp * 128:(hp + 1) * 128, tok0:tok0 + n]), writes=[gat], dma=True)
    k.op("dve", lambda e: e.tensor_tensor(yb[:, :n], yb[:, :n], yf[:, :n], ALU.add), reads=[yb, yf], writes=[yb])
    k.op("act", lambda e: e.copy(fl(sqk), yb[:, :n]), reads=[yb], writes=[sqk])
    p1 = nps()
    k.op("pe", lambda e: e.matmul(p1[:, :n], lhsT=self.blk[:, :], rhs=fl(sqk), start=True, stop=True), reads=[self.blk, sqk], writes=[p1])
    k.op("dve", lambda e: e.scalar_tensor_tensor(out=fl(kx), in0=p1[:, :n], scalar=-1.0 / 64, in1=yb[:, :n], op0=ALU.mult, op1=ALU.add), reads=[p1, yb], writes=[kx])
    k.op("act", lambda e: e.copy(fl(sqk), fl(kx)), reads=[kx], writes=[sqk])
    p1b = nps()
    k.op("pe", lambda e: e.matmul(p1b[:, :n], lhsT=self.blk[:, :], rhs=fl(sqk), start=True, stop=True), reads=[self.blk, sqk], writes=[p1b])
    k.op("dve", lambda e: e.scalar_tensor_tensor(out=fl(kx), in0=p1b[:, :n], scalar=-1.0 / 64, in1=fl(kx), op0=ALU.mult, op1=ALU.add), reads=[p1b, kx], writes=[kx])
    k.op("act", lambda e: e.activation(out=fl(sqk), in_=fl(kx), func=AF.Square), reads=[kx], writes=[sqk])
    p2 = nps()
    k.op("pe", lambda e: e.matmul(p2[:, :n], lhsT=self.blk[:, :], rhs=fl(sqk), start=True, stop=True), reads=[self.blk, sqk], writes=[p2])
    k.op("act", lambda e: e.activation(out=fl(rn), in_=p2[:, :n], func=AF.Ln, scale=1.0 / 64, bias=64e-5), reads=[p2], writes=[rn])
    k.op("act", lambda e: e.activation(out=fl(rn), in_=fl(rn), func=AF.Exp, scale=-0.5), reads=[rn], writes=[rn])
    k.op("dve", lambda e: e.tensor_tensor(fl(kx), fl(kx), fl(rn), ALU.mult), reads=[kx, rn], writes=[kx])
    k.op("dve", lambda e: e.tensor_scalar(fl(kx), fl(kx), gn_g[:, hp:hp + 1], gn_b[:, hp:hp + 1], ALU.mult, ALU.add), reads=[kx, gn_g, gn_b], writes=[kx])
    k.op("pool", lambda e: e.tensor_tensor(fl(kx), fl(kx), bon[:, :n], ALU.add), reads=[kx, bon], writes=[kx])
    k.op("dve", lambda e: e.tensor_tensor(fl(kx), fl(kx), gat[:, :n], ALU.mult), reads=[kx, gat], writes=[kx])
    k.op("pool", lambda e: e.dma_start(out=self.mixT.ap[hp * 128:(hp + 1) * 128, tok0:tok0 + n], in_=fl(kx)), reads=[kx], dma=True)


MK.rw_consts = _rw_consts
MK.load_consts2 = _load_consts2
MK._pvec = _pvec
MK.phase_rwkv = phase_rwkv_old if os.environ.get('RW_OLD', '0') == '1' else phase_rwkv
MK.rw_finish = rw_finish


GLA_OFF = 2312


def phase_gla(self, l, d):
    k = self.k
    m = k.mark()
    NT = self.NT
    s = 1.0 / 16.0
    qscale = 32 ** -0.5
    if not hasattr(self, "gla_yf"):
        kind = "ExternalOutput" if "gla_yf" in self.dbg else "Internal"
        self.gla_yf = k.dram("gla_yf", [256, NT], F32, kind=kind)
    ga2f = k.sb("ga2f", [16, 2, 128], F32)
    ga2p = k.sb("ga2p", [16, 2, 128], BF16)
    gbp = k.sb("gbp", [128, 2], F32)
    k.op("dve", lambda e: e.memset(ga2f[:, :, :], 0.0), writes=[ga2f])
    k.op("dve", lambda e: e.memset(gbp[:, :], 0.0), writes=[gbp])
    for h in range(4):
        hp, hl = h // 2, h % 2
        k.op("sp", lambda e, h=h, hp=hp, hl=hl: e.dma_start(out=ga2f[:, hp, hl * 64:hl * 64 + 32], in_=self.gla_ga2.ap[l, d, :, h * 32:(h + 1) * 32]), writes=[ga2f], dma=True)
        k.op("sp", lambda e, h=h, hp=hp, hl=hl: e.dma_start(out=gbp[hl * 64:hl * 64 + 32, hp:hp + 1], in_=self.gla_gb.ap[l, d, h * 32:(h + 1) * 32].rearrange("(p o) -> p o", o=1), allow_slow_non_contiguous=True), writes=[gbp], dma=True)
    k.op("dve", lambda e: e.tensor_copy(ga2p[:, :, :], ga2f[:, :, :]), reads=[ga2f], writes=[ga2p])
    ng = self._pvec("gng", self.gla_norm_g.ap[l], 2)

    f32t = lambda n_: k.sb(n_, [128, 8, 64], F32)
    bft = lambda n_: k.sb(n_, [128, 8, 64], BF16)
    q = [[f32t("gq%d%d" % (i, hp)) for hp in range(2)] for i in range(2)]
    kk_ = [[f32t("gk%d%d" % (i, hp)) for hp in range(2)] for i in range(2)]
    vv = [[f32t("gv%d%d" % (i, hp)) for hp in range(2)] for i in range(2)]
    glf = [k.sb("glf%d" % i, [16, 512], F32) for i in range(2)]
    glb = k.sb("glb", [16, 512], BF16)
    for i in range(2):
        for hp in range(2):
            k.op("pool", lambda e, i=i, hp=hp: e.memset(q[i][hp][:, :, :], 0.0), writes=[q[i][hp]])
            k.op("pool", lambda e, i=i, hp=hp: e.memset(kk_[i][hp][:, :, :], 0.0), writes=[kk_[i][hp]])
    lg, cs, dcs, tmp, E, Etrue = [f32t("g_" + z) for z in ("lg", "cs", "dcs", "tmp", "E", "Etrue")]
    qt, kt, Qtrue, Kh, vb, Khtm, Vtm, Ark = [[bft("g_%s%d" % (z, hp)) for hp in range(2)] for z in ("qt", "kt", "Qtrue", "Kh", "vb", "Khtm", "Vtm", "Ark")]
    WC = [k.sb("gWC%d" % hp, [128, 8], F32) for hp in range(2)]
    Mst = [k.sb("gMst%d" % hp, [128, 64], F32) for hp in range(2)]
    Mbf = [k.sb("gMbf%d" % hp, [128, 64], BF16) for hp in range(2)]
    ysb = [k.sb("gysb%d" % hp, [128, 512], F32) for hp in range(2)]
    yfin = [k.sb("gyfin%d" % hp, [128, 512], F32) for hp in range(2)]
    rin = [k.sb("grin%d" % hp, [128, 512], F32) for hp in range(2)]
    sq = k.sb("gsq", [128, 512], BF16)
    rn = k.sb("grn", [128, 512], F32)
    PS = [k.ps("gps%d" % i, [128, 512], F32) for i in range(2)]
    PY = [[k.ps("gpy%d%d" % (i, j), [128, 512], F32) for j in range(2)] for i in range(2)]
    PSB = [k.ps("gpsb%d" % i, [128, 8, 64], BF16) for i in range(2)]
    psi = [0]; psbi = [0]

    def nps():
        p = PS[psi[0] % 2]; psi[0] += 1; return p

    def npsb():
        p = PSB[psbi[0] % 2]; psbi[0] += 1; return p
    for hp in range(2):
        k.op("dve", lambda e, hp=hp: e.memset(Mst[hp][:, :], 0.0), writes=[Mst[hp]])
        k.op("dve", lambda e, hp=hp: e.memset(Mbf[hp][:, :], 0.0), writes=[Mbf[hp]])
    mI = 1 if d == 0 else 3
    tiles = self.tok_tiles()
    lat = tiles[1:]
    order = [tiles[0]] + (lat if d == 0 else lat[::-1])
    O = GLA_OFF

    def tile_body(ti, tok0, n, stream):
        nch = n // 64
        b = ti % 2
        fl = lambda t_: t_[:, :, :].rearrange("p c t -> p (c t)")[:, 0:n]
        for h in range(4):
            hp, hl = h // 2, h % 2
            P32 = slice(hl * 64, hl * 64 + 32)
            k.op("sp", lambda e, h=h, hp=hp, P32=P32: e.dma_start(out=fl(q[b][hp])[P32, :], in_=self.uT.ap[O + h * 32:O + (h + 1) * 32, tok0:tok0 + n]), writes=[q[b][hp]], dma=True)
            k.op("sp", lambda e, h=h, hp=hp, P32=P32: e.dma_start(out=fl(kk_[b][hp])[P32, :], in_=self.uT.ap[O + 128 + h * 32:O + 128 + (h + 1) * 32, tok0:tok0 + n]), writes=[kk_[b][hp]], dma=True)
        for hp in range(2):
            k.op("sp", lambda e, hp=hp: e.dma_start(out=fl(vv[b][hp]), in_=self.uT.ap[O + 256 + hp * 128:O + 256 + (hp + 1) * 128, tok0:tok0 + n]), writes=[vv[b][hp]], dma=True)
        k.op("sp", lambda e: e.dma_start(out=glf[b][:, :n], in_=self.uT.ap[O + 512:O + 528, tok0:tok0 + n]), writes=[glf[b]], dma=True)
        k.op("act", lambda e: e.copy(glb[:, :n], glf[b][:, :n]), reads=[glf[b]], writes=[glb])

        def prep(hp):
            p1 = nps()
            k.op("pe", lambda e: e.matmul(p1[:, :n], lhsT=ga2p[:, hp, :], rhs=glb[:, :n], start=True, stop=True), reads=[ga2p, glb], writes=[p1])
            k.op("act", lambda e: e.activation(out=fl(tmp), in_=p1[:, :n], func=AF.Sigmoid, bias=gbp[:, hp:hp + 1]), reads=[p1, gbp], writes=[tmp])
            k.op("act", lambda e: e.activation(out=fl(lg), in_=fl(tmp), func=AF.Ln), reads=[tmp], writes=[lg])
            k.op("dve", lambda e: e.tensor_tensor_scan(fl(cs), self.reset[:, :n], fl(lg), 0.0, ALU.mult, ALU.add), reads=[self.reset, lg], writes=[cs])
            if d == 1:
                k.op("dve", lambda e: e.tensor_tensor(fl(tmp), fl(lg), fl(cs), ALU.subtract), reads=[lg, cs], writes=[tmp])
                k.op("dve", lambda e: e.tensor_tensor(cs[:, :nch, :], tmp[:, :nch, :], cs[:, :nch, 63:64].to_broadcast([128, nch, 64]), ALU.add), reads=[tmp, cs], writes=[cs])
            endi = 63 if d == 0 else 0
            k.op("pool", lambda e: e.tensor_tensor(dcs[:, :nch, :], cs[:, :nch, :], cs[:, :nch, 32:33].to_broadcast([128, nch, 64]), ALU.subtract), reads=[cs], writes=[dcs])
            k.op("act", lambda e: e.activation(out=fl(E), in_=fl(dcs), func=AF.Exp, scale=s), reads=[dcs], writes=[E])
            k.op("dve", lambda e: e.scalar_tensor_tensor(out=fl(qt[hp]), in0=fl(q[b][hp]), scalar=qscale, in1=fl(E), op0=ALU.mult, op1=ALU.mult), reads=[q[b][hp], E], writes=[qt[hp]])
            k.op("act", lambda e: e.activation(out=fl(E), in_=fl(dcs), func=AF.Exp, scale=-s), reads=[dcs], writes=[E])
            k.op("dve", lambda e: e.tensor_tensor(fl(kt[hp]), fl(kk_[b][hp]), fl(E), ALU.mult), reads=[kk_[b][hp], E], writes=[kt[hp]])
            k.op("act", lambda e: e.activation(out=fl(Etrue), in_=fl(cs), func=AF.Exp, scale=s), reads=[cs], writes=[Etrue])
            k.op("dve", lambda e: e.scalar_tensor_tensor(out=fl(Qtrue[hp]), in0=fl(q[b][hp]), scalar=qscale, in1=fl(Etrue), op0=ALU.mult, op1=ALU.mult), reads=[q[b][hp], Etrue], writes=[Qtrue[hp]])
            k.op("dve", lambda e: e.tensor_copy(WC[hp][:, :nch], Etrue[:, :nch, endi]), reads=[Etrue], writes=[WC[hp]])
            k.op("pool", lambda e: e.tensor_tensor(tmp[:, :nch, :], cs[:, :nch, endi:endi + 1].to_broadcast([128, nch, 64]), cs[:, :nch, :], ALU.subtract), reads=[cs], writes=[tmp])
            k.op("act", lambda e: e.activation(out=fl(E), in_=fl(tmp), func=AF.Exp, scale=s), reads=[tmp], writes=[E])
            k.op("dve", lambda e: e.tensor_tensor(fl(Kh[hp]), fl(kk_[b][hp]), fl(E), ALU.mult), reads=[kk_[b][hp], E], writes=[Kh[hp]])
            k.op("act", lambda e: e.copy(fl(vb[hp]), fl(vv[b][hp])), reads=[vv[b][hp]], writes=[vb[hp]])

        def head_block(hp, hl):
            P = slice(hl * 64, hl * 64 + 64)

            def tm(src, dst):
                pb = npsb()
                for ch in range(nch):
                    k.op("pe", lambda e, ch=ch: e.transpose(pb[P, ch, :], src[hp][P, ch, :], self.ident[P, P]), reads=[src[hp], self.ident], writes=[pb], pe_acc=True)
                k.op("act", lambda e: e.copy(dst[hp][P, :nch, :], pb[P, :nch, :]), reads=[pb], writes=[dst[hp]])
            tm(Kh, Khtm); tm(vb, Vtm)
            pp = nps()
            pv = pp[:, :].rearrange("p (c t) -> p c t", t=64)
            for ch in range(nch):
                k.op("pe", lambda e, ch=ch: e.matmul(pv[P, ch, :], lhsT=kt[hp][P, ch, :], rhs=qt[hp][P, ch, :], start=True, stop=True), reads=[kt[hp], qt[hp]], writes=[pp], pe_acc=True)
            mv = self.masks[:, mI, :].rearrange("p (c t) -> p c t", t=64)
            k.op("dve", lambda e: e.tensor_tensor(Ark[hp][P, :nch, :], pv[P, :nch, :], mv[P, :nch, :], ALU.mult), reads=[pp, self.masks], writes=[Ark[hp]])

        for hp in range(2):
            prep(hp)
            for hl in range(2):
                head_block(hp, hl)

        def seq_step(ch, hp, hl):
            P = slice(hl * 64, hl * 64 + 64)
            pyb = PY[hp][hl]
            pyv = pyb[:, :].rearrange("p (c t) -> p c t", t=64)
            k.op("pe", lambda e: e.matmul(pyv[P, ch, :], lhsT=Mbf[hp][P, :], rhs=Qtrue[hp][P, ch, :], start=True, stop=False), reads=[Mbf[hp], Qtrue[hp]], writes=[pyb], pe_acc=True)
            k.op("pe", lambda e: e.matmul(pyv[P, ch, :], lhsT=Vtm[hp][P, ch, :], rhs=Ark[hp][P, ch, :], start=False, stop=True), reads=[Vtm[hp], Ark[hp]], writes=[pyb], pe_acc=True)
            pm_ = nps()
            k.op("pe", lambda e: e.matmul(pm_[P, 0:64], lhsT=Khtm[hp][P, ch, :], rhs=Vtm[hp][P, ch, :], start=True, stop=True), reads=[Khtm[hp], Vtm[hp]], writes=[pm_])
            k.op("dve", lambda e: e.scalar_tensor_tensor(out=Mst[hp][P, :], in0=Mst[hp][P, :], scalar=WC[hp][P, ch:ch + 1], in1=pm_[P, 0:64], op0=ALU.mult, op1=ALU.add), reads=[Mst[hp], WC[hp], pm_], writes=[Mst[hp]])
            k.op("act", lambda e: e.copy(Mbf[hp][P, :], Mst[hp][P, :]), reads=[Mst[hp]], writes=[Mbf[hp]])

        chs = range(nch) if d == 0 else range(nch - 1, -1, -1)
        for ch in chs:
            for hp in range(2):
                for hl in range(2):
                    seq_step(ch, hp, hl)

        def outp(hp):
            for hl in range(2):
                P = slice(hl * 64, hl * 64 + 64)
                pp = PY[hp][hl]
                if hl == 0:
                    k.op("dve", lambda e, P=P, pp=pp: e.tensor_copy(ysb[hp][P, :n], pp[P, :n]), reads=[pp], writes=[ysb[hp]])
                else:
                    k.op("act", lambda e, P=P, pp=pp: e.copy(ysb[hp][P, :n], pp[P, :n]), reads=[pp], writes=[ysb[hp]])
            if d == 0:
                k.op("pool", lambda e: e.dma_start(out=self.gla_yf.ap[hp * 128:(hp + 1) * 128, tok0:tok0 + n], in_=ysb[hp][:, :n]), reads=[ysb[hp]], dma=True)
            else:
                yb, yf, rr = ysb[hp], yfin[hp], rin[hp]
                k.op("sp", lambda e: e.dma_start(out=yf[:, :n], in_=self.gla_yf.ap[hp * 128:(hp + 1) * 128, tok0:tok0 + n]), writes=[yf], dma=True)
                k.op("sp", lambda e: e.dma_start(out=rr[:, :n], in_=self.uT.ap[O + 528 + hp * 128:O + 528 + (hp + 1) * 128, tok0:tok0 + n]), writes=[rr], dma=True)
                k.op("dve", lambda e: e.tensor_tensor(yb[:, :n], yb[:, :n], yf[:, :n], ALU.add), reads=[yb, yf], writes=[yb])
                k.op("act", lambda e: e.activation(out=sq[:, :n], in_=yb[:, :n], func=AF.Square), reads=[yb], writes=[sq])
                p2 = nps()
                k.op("pe", lambda e: e.matmul(p2[:, :n], lhsT=self.blk[:, :], rhs=sq[:, :n], start=True, stop=True), reads=[self.blk, sq], writes=[p2])
                k.op("act", lambda e: e.activation(out=rn[:, :n], in_=p2[:, :n], func=AF.Sqrt, scale=1.0 / 64, bias=EPS), reads=[p2], writes=[rn])
                k.op("dve", lambda e: e.reciprocal(rn[:, :n], rn[:, :n]), reads=[rn], writes=[rn])
                k.op("dve", lambda e: e.scalar_tensor_tensor(out=yb[:, :n], in0=yb[:, :n], scalar=ng[:, hp:hp + 1], in1=rn[:, :n], op0=ALU.mult, op1=ALU.mult), reads=[yb, ng, rn], writes=[yb])
                k.op("act", lambda e: e.activation(out=rr[:, :n], in_=rr[:, :n], func=AF.Silu), reads=[rr], writes=[rr])
                k.op("dve", lambda e: e.tensor_tensor(yb[:, :n], yb[:, :n], rr[:, :n], ALU.mult), reads=[yb, rr], writes=[yb])
                k.op("pool", lambda e: e.dma_start(out=self.mixT.ap[768 + hp * 128:768 + (hp + 1) * 128, tok0:tok0 + n], in_=yb[:, :n]), reads=[yb], dma=True)
        for hp in range(2):
            outp(hp)

    for ti, (tok0, n, stream) in enumerate(order):
        tile_body(ti, tok0, n, stream)
    k.barrier()
    k.emit()
    k.free_to(m)


MK.phase_gla = phase_gla


SSM_OFF = 1024


def _ssm_decl(self):
    k = self.k
    NT = self.NT
    self.c_m128 = k.dram("c_m128", [5, 128, 128], F32, kind="ExternalInput")
    def S(n, s, dt=F32):
        kind = "ExternalOutput" if n in self.dbg else "Internal"
        return k.dram(n, s, dt, kind=kind)
    self.s_xtm = S("s_xtm", [NT, 768])
    self.s_ztm = S("s_ztm", [NT, 512])
    self.s_dttm = S("s_dttm", [NT, 8])
    self.s_bcT = S("s_bcT", [256, NT])
    self.s_yf = S("s_yf", [NT, 512])


def phase_ssm_conv(self, l):
    k = self.k
    m = k.mark()
    NT, T = self.NT, self.T
    cw = k.sb("cw", [128, 6, 9], F32)
    cbias = self._pvec("cbias", self.ssm_conv_b.ap[l], 6)
    for tap in range(9):
        k.op("sp", lambda e, tap=tap: e.dma_start(out=cw[:, :, tap], in_=self.ssm_conv_w.ap[l, tap // 3, tap % 3].rearrange("(c p) -> p c", p=128), allow_slow_non_contiguous=True), writes=[cw], dma=True)
    xin = [k.sb("cxin%d" % i, [128, 642], F32) for i in range(3)]
    acc = [k.sb("cacc%d" % i, [128, 512], F32) for i in range(2)]
    acc2 = [k.sb("cacc2%d" % i, [128, 512], F32) for i in range(2)]
    res = [k.sb("cres%d" % i, [128, 512], F32) for i in range(2)]
    zin = [k.sb("czin%d" % i, [128, 512], F32) for i in range(2)]
    dtin = [k.sb("cdtin%d" % i, [8, 512], F32) for i in range(2)]
    otm = [k.sb("cotm%d" % i, [128, 4, 128], F32) for i in range(2)]
    odt = [k.sb("codt%d" % i, [128, 4, 8], F32) for i in range(2)]
    PT = [k.ps("cpt%d" % i, [128, 4, 128], F32) for i in range(3)]
    pti = [0]
    cnt = [0]
    O = SSM_OFF

    def transpose_store(src, npart, dst_ap_fn, n, ob):
        pt = PT[pti[0] % 3]; pti[0] += 1
        nb = n // 128
        for tb in range(nb):
            k.op("pe", lambda e, tb=tb: e.transpose(pt[:, tb, :npart], src[:npart, tb * 128:(tb + 1) * 128], self.identf[:npart, :npart]), reads=[src, self.identf], writes=[pt], pe_acc=True)
        eng = "act" if cnt[0] % 2 else "dve"
        cnt[0] += 1
        if eng == "act":
            k.op("act", lambda e: e.copy(ob[:, :nb, :npart], pt[:, :nb, :npart]), reads=[pt], writes=[ob])
        else:
            k.op("dve", lambda e: e.tensor_copy(ob[:, :nb, :npart], pt[:, :nb, :npart]), reads=[pt], writes=[ob])
        k.op("pool", lambda e: e.dma_start(out=dst_ap_fn(nb), in_=ob[:, :nb, :npart]), reads=[ob], dma=True)

    it = [0]
    for (tok0, n, stream) in self.tok_tiles():
        seq0, seq1 = (0, CTX) if stream == 1 else (CTX, NT)
        halo = 65 if stream == 0 else 1
        for c in range(6):
            i = it[0]; it[0] += 1
            xb = xin[i % 3]; ac = acc[i % 2]; ac2 = acc2[i % 2]; rs = res[i % 2]
            lo = max(tok0 - halo, seq0); hi = min(tok0 + n + halo, seq1)
            if lo > tok0 - halo:
                k.op("pool", lambda e, xb=xb, halo=halo: e.memset(xb[:, 0:halo], 0.0), writes=[xb])
            if hi < tok0 + n + halo:
                k.op("pool", lambda e, xb=xb, halo=halo, n=n: e.memset(xb[:, halo + n:halo + n + halo], 0.0), writes=[xb])
            k.op("sp", lambda e, xb=xb, lo=lo, hi=hi, tok0=tok0, halo=halo, c=c: e.dma_start(out=xb[:, lo - (tok0 - halo):hi - (tok0 - halo)], in_=self.uT.ap[O + 512 + c * 128:O + 512 + (c + 1) * 128, lo:hi]), writes=[xb], dma=True)
            first = True
            dys = (-1, 0, 1) if stream == 0 else (0,)
            for dy in dys:
                for dx in (0, -1, 1):
                    tap = (dy + 1) * 3 + (dx + 1)
                    off = halo + dy * 64 + dx
                    if first:
                        k.op("dve", lambda e, ac=ac, xb=xb, off=off, n=n, c=c, tap=tap: e.tensor_scalar(ac[:, :n], xb[:, off:off + n], cw[:, c, tap:tap + 1], None, ALU.mult), reads=[xb, cw], writes=[ac])
                        first = False
                        continue
                    c0, c1 = (0, 64) if (dx == 0 or stream == 1) else ((1, 64) if dx == -1 else (0, 63))
                    def tapop(ac=ac, xb=xb, off=off, n=n, c=c, tap=tap, c0=c0, c1=c1):
                        src = xb[:, off:off + n].rearrange("p (r w) -> p r w", w=64)[:, :, c0:c1]
                        dst = ac[:, :n].rearrange("p (r w) -> p r w", w=64)[:, :, c0:c1]
                        k.op("dve", lambda e: e.scalar_tensor_tensor(out=dst, in0=src, scalar=cw[:, c, tap:tap + 1], in1=dst, op0=ALU.mult, op1=ALU.add), reads=[xb, cw, ac], writes=[ac])
                    tapop()
            k.op("act", lambda e, ac=ac, rs=rs, n=n, c=c: e.activation(out=rs[:, :n], in_=ac[:, :n], func=AF.Silu, bias=cbias[:, c:c + 1]), reads=[ac, cbias], writes=[rs])
            if c >= 4:
                k.op("pool", lambda e, rs=rs, n=n, c=c, tok0=tok0: e.dma_start(out=self.s_bcT.ap[(c - 4) * 128:(c - 3) * 128, tok0:tok0 + n], in_=rs[:, :n]), reads=[rs], dma=True)
            ob = otm[i % 2]
            transpose_store(rs, 128, lambda nb, c=c, tok0=tok0: self.s_xtm.ap[tok0:tok0 + nb * 128, c * 128:(c + 1) * 128].rearrange("(b p) f -> p b f", p=128), n, ob)
        for c in range(4):
            i = it[0]; it[0] += 1
            zb = zin[i % 2]
            k.op("sp", lambda e, zb=zb, c=c, tok0=tok0, n=n: e.dma_start(out=zb[:, :n], in_=self.uT.ap[O + c * 128:O + (c + 1) * 128, tok0:tok0 + n]), writes=[zb], dma=True)
            ob = otm[i % 2]
            transpose_store(zb, 128, lambda nb, c=c, tok0=tok0: self.s_ztm.ap[tok0:tok0 + nb * 128, c * 128:(c + 1) * 128].rearrange("(b p) f -> p b f", p=128), n, ob)
        i = it[0]; it[0] += 1
        db = dtin[i % 2]
        k.op("sp", lambda e, db=db, tok0=tok0, n=n: e.dma_start(out=db[:, :n], in_=self.uT.ap[O + 1280:O + 1288, tok0:tok0 + n]), writes=[db], dma=True)
        ob = odt[i % 2]
        transpose_store(db, 8, lambda nb, tok0=tok0: self.s_dttm.ap[tok0:tok0 + nb * 128, :].rearrange("(b p) f -> p b f", p=128), n, ob)
    k.barrier()
    k.emit()
    k.free_to(m)


def phase_ssm_scan(self, l, d):
    k = self.k
    m = k.mark()
    NT = self.NT
    BIG = 30000.0
    m128 = k.sb("m128", [128, 5, 128], F32)
    for i in range(5):
        k.op("sp", lambda e, i=i: e.dma_start(out=m128[:, i, :], in_=self.c_m128.ap[i]), writes=[m128], dma=True)
    LE, GE, GT, LT, NEGI = 0, 1, 2, 3, 4
    if d == 0:
        mTri, mR, mNeg = LE, GT, GT
    else:
        mTri, mR, mNeg = GE, LT, LT
    onesf = k.sb("onesf", [128, 128], F32)
    k.op("dve", lambda e: e.memset(onesf[:, :], 1.0), writes=[onesf])
    dtb = k.sb("dtb", [128, 8], F32)
    aneg = k.sb("aneg", [128, 8], F32)
    dsk = k.sb("dsk", [128, 8], F32)
    ngb = k.sb("ngb", [128, 512], F32)
    k.op("sp", lambda e: e.dma_start(out=dtb[:, :], in_=self.ssm_dt_bias.ap[l, d].partition_broadcast(128)), writes=[dtb], dma=True)
    k.op("sp", lambda e: e.dma_start(out=aneg[:, :], in_=self.ssm_a_log.ap[l, d].partition_broadcast(128)), writes=[aneg], dma=True)
    k.op("sp", lambda e: e.dma_start(out=dsk[:, :], in_=self.ssm_d.ap[l].partition_broadcast(128)), writes=[dsk], dma=True)
    k.op("sp", lambda e: e.dma_start(out=ngb[:, :], in_=self.ssm_norm_g.ap[l].partition_broadcast(128)), writes=[ngb], dma=True)
    k.op("act", lambda e: e.activation(out=aneg[:, :], in_=aneg[:, :], func=AF.Exp), reads=[aneg], writes=[aneg])
    k.op("dve", lambda e: e.tensor_scalar(aneg[:, :], aneg[:, :], -1.0, None, ALU.mult), reads=[aneg], writes=[aneg])

    xs = [k.sb("sxs%d" % i, [128, 768], F32) for i in range(2)]
    dt = [k.sb("sdt%d" % i, [128, 8], F32) for i in range(2)]
    BT = [[k.sb("sBT%d%d" % (i, g), [64, 128], F32) for g in range(2)] for i in range(2)]
    CT = [[k.sb("sCT%d%d" % (i, g), [64, 128], F32) for g in range(2)] for i in range(2)]
    BTb = [k.sb("sBTb%d" % g, [64, 128], BF16) for g in range(2)]
    CTb = [k.sb("sCTb%d" % g, [64, 128], BF16) for g in range(2)]
    Btm = k.sb("sBtm", [128, 128], BF16)
    zt = [k.sb("szt%d" % i, [128, 512], F32) for i in range(2)]
    yfin = [k.sb("syf%d" % i, [128, 512], F32) for i in range(2)]
    xdt = k.sb("sx", [128, 8], F32)
    dA = k.sb("sdA", [128, 8], F32)
    cs = k.sb("scs", [128, 8], F32)
    dte = k.sb("sdte", [128, 8], F32)
    ecs = k.sb("secs", [128, 8], F32)
    eend = k.sb("seend", [128, 8], F32)
    dtp = k.sb("sdtp", [128, 8], F32)
    Xd = k.sb("sXd", [128, 8, 64], BF16)
    Xdd = k.sb("sXdd", [128, 8, 64], BF16)
    Rm = [k.sb("sRm%d" % i, [128, 128], F32) for i in range(4)]
    Lt = k.sb("sLt", [128, 8, 128], BF16)
    sc = k.sb("ssc", [128, 2, 128], BF16)
    Gt = k.sb("sGt", [128, 8, 128], BF16)
    Yt = k.sb("sYt", [128, 512], F32)
    Y = k.sb("sY", [128, 512], F32)
    junk = k.sb("sjunk", [128, 512], BF16)
    ss = k.sb("sss", [128, 1], F32)
    Ybf = k.sb("sYbf", [128, 512], BF16)
    ofm = k.sb("sofm", [128, 4, 128], F32)
    Sst = k.sb("sSst", [64, 8, 64], F32)
    Stmp = k.sb("sStmp", [64, 8, 64], F32)
    Sbf = k.sb("sSbf", [64, 8, 64], BF16)
    k.op("dve", lambda e: e.memset(Sst[:, :, :], 0.0), writes=[Sst])
    k.op("dve", lambda e: e.memset(Sbf[:, :, :], 0.0), writes=[Sbf])
    pcs = k.ps("spcs", [128, 2, 8], F32)
    pD = [k.ps("spD%d" % i, [128, 4, 128], F32) for i in range(2)]
    psc = k.ps("spsc", [128, 2, 128], F32)
    pYd = k.ps("spYd", [128, 512], F32)
    pYo = k.ps("spYo", [128, 512], F32)
    pSt = k.ps("spSt", [64, 512], F32)
    pT = k.ps("spT", [128, 4, 128], BF16)

    nchunks = NT // 128
    ctxc = [0, 1]
    latc = list(range(2, nchunks))
    order = (ctxc + latc) if d == 0 else (ctxc[::-1] + latc[::-1])
    ri = [0]

    def chunk(ci, c):
        b = ci % 2
        t0 = c * 128
        x_, dt_, z_, yf_ = xs[b], dt[b], zt[b], yfin[b]
        k.op("sp", lambda e: e.dma_start(out=x_[:, :], in_=self.s_xtm.ap[t0:t0 + 128, :]), writes=[x_], dma=True)
        k.op("sp", lambda e: e.dma_start(out=dt_[:, :], in_=self.s_dttm.ap[t0:t0 + 128, :]), writes=[dt_], dma=True)
        for g in range(2):
            k.op("sp", lambda e, g=g: e.dma_start(out=BT[b][g][:, :], in_=self.s_bcT.ap[g * 64:(g + 1) * 64, t0:t0 + 128]), writes=[BT[b][g]], dma=True)
            k.op("sp", lambda e, g=g: e.dma_start(out=CT[b][g][:, :], in_=self.s_bcT.ap[128 + g * 64:128 + (g + 1) * 64, t0:t0 + 128]), writes=[CT[b][g]], dma=True)
        if d == 1:
            k.op("sp", lambda e: e.dma_start(out=z_[:, :], in_=self.s_ztm.ap[t0:t0 + 128, :]), writes=[z_], dma=True)
            k.op("sp", lambda e: e.dma_start(out=yf_[:, :], in_=self.s_yf.ap[t0:t0 + 128, :]), writes=[yf_], dma=True)
        for g in range(2):
            k.op("act", lambda e, g=g: e.copy(BTb[g][:, :], BT[b][g][:, :]), reads=[BT[b][g]], writes=[BTb[g]])
            k.op("act", lambda e, g=g: e.copy(CTb[g][:, :], CT[b][g][:, :]), reads=[CT[b][g]], writes=[CTb[g]])
        k.op("act", lambda e: e.copy(Btm[:, :], x_[:, 512:640]), reads=[x_], writes=[Btm])
        k.op("dve", lambda e: e.tensor_tensor(xdt[:, :], dt_[:, :], dtb[:, :], ALU.add), reads=[dt_, dtb], writes=[xdt])
        k.op("act", lambda e: e.activation(out=xdt[:, :], in_=xdt[:, :], func=AF.Exp), reads=[xdt], writes=[xdt])
        k.op("act", lambda e: e.activation(out=dtp[:, :], in_=xdt[:, :], func=AF.Ln, bias=1.0), reads=[xdt], writes=[dtp])
        k.op("dve", lambda e: e.tensor_tensor(dA[:, :], dtp[:, :], aneg[:, :], ALU.mult), reads=[dtp, aneg], writes=[dA])
        k.op("pe", lambda e: e.matmul(pcs[:, 0, :], lhsT=m128[:, mTri, :], rhs=dA[:, :], start=True, stop=True), reads=[m128, dA], writes=[pcs])
        k.op("pe", lambda e: e.matmul(pcs[:, 1, :], lhsT=onesf[:, :], rhs=dA[:, :], start=True, stop=True), reads=[onesf, dA], writes=[pcs], pe_acc=True)
        k.op("dve", lambda e: e.tensor_copy(cs[:, :], pcs[:, 0, :]), reads=[pcs], writes=[cs])
        k.op("act", lambda e: e.activation(out=ecs[:, :], in_=pcs[:, 0, :], func=AF.Exp), reads=[pcs], writes=[ecs])
        k.op("act", lambda e: e.activation(out=eend[:, :], in_=pcs[:, 1, :], func=AF.Exp), reads=[pcs], writes=[eend])
        k.op("dve", lambda e: e.tensor_tensor(dte[:, :], pcs[:, 1, :], cs[:, :], ALU.subtract), reads=[pcs, cs], writes=[dte])
        k.op("act", lambda e: e.activation(out=dte[:, :], in_=dte[:, :], func=AF.Exp), reads=[dte], writes=[dte])
        xv = x_[:, 0:512].rearrange("p (h e) -> p h e", e=64)
        k.op("dve", lambda e: e.tensor_tensor(Xd[:, :, :], xv, dtp[:, :].unsqueeze(2).to_broadcast([128, 8, 64]), ALU.mult), reads=[x_, dtp], writes=[Xd])
        k.op("pool", lambda e: e.tensor_tensor(Xdd[:, :, :], Xd[:, :, :], dte[:, :].unsqueeze(2).to_broadcast([128, 8, 64]), ALU.mult), reads=[Xd, dte], writes=[Xdd])
        for h in range(8):
            r_ = Rm[ri[0] % 4]; ri[0] += 1
            pd = pD[h // 4]
            k.op("dve" if h % 2 == 0 else "pool", lambda e, h=h, r_=r_: e.tensor_tensor(r_[:, :], m128[:, mR, :], dA[:, h:h + 1].to_broadcast([128, 128]), ALU.mult), reads=[m128, dA], writes=[r_])
            k.op("pe", lambda e, h=h, r_=r_, pd=pd: e.matmul(pd[:, h % 4, :], lhsT=r_[:, :], rhs=m128[:, mTri, :], start=True, stop=False), reads=[r_, m128], writes=[pd], pe_acc=True)
            k.op("pe", lambda e, h=h, pd=pd: e.matmul(pd[:, h % 4, :], lhsT=m128[:, NEGI, :], rhs=m128[:, mNeg, :], start=False, stop=True), reads=[m128], writes=[pd], pe_acc=True)
        for hh in range(2):
            k.op("act", lambda e, hh=hh: e.activation(out=Lt[:, hh * 4:(hh + 1) * 4, :], in_=pD[hh][:, :, :], func=AF.Exp), reads=[pD[hh]], writes=[Lt])
        for g in range(2):
            k.op("pe", lambda e, g=g: e.matmul(psc[:, g, :], lhsT=BTb[g][:, :], rhs=CTb[g][:, :], start=True, stop=True), reads=[BTb[g], CTb[g]], writes=[psc], pe_acc=True)
        k.op("dve", lambda e: e.tensor_copy(sc[:, :, :], psc[:, :, :]), reads=[psc], writes=[sc])
        for g in range(2):
            k.op("dve" if g == 0 else "pool", lambda e, g=g: e.tensor_tensor(Gt[:, g * 4:(g + 1) * 4, :], Lt[:, g * 4:(g + 1) * 4, :], sc[:, g:g + 1, :].to_broadcast([128, 4, 128]), ALU.mult), reads=[Lt, sc], writes=[Gt])
        for h in range(8):
            k.op("pe", lambda e, h=h: e.matmul(pYd[:, h * 64:(h + 1) * 64], lhsT=Gt[:, h, :], rhs=Xd[:, h, :], start=True, stop=True), reads=[Gt, Xd], writes=[pYd], pe_acc=True)
        for g in range(2):
            k.op("pe", lambda e, g=g: e.matmul(pYo[:, g * 256:(g + 1) * 256], lhsT=CTb[g][:, :], rhs=Sbf[:, g * 4:(g + 1) * 4, :].rearrange("p h e -> p (h e)"), start=True, stop=True), reads=[CTb[g], Sbf], writes=[pYo], pe_acc=True)
        k.op("dve", lambda e: e.tensor_tensor(Yt[:, :].rearrange("p (h e) -> p h e", e=64), pYo[:, :].rearrange("p (h e) -> p h e", e=64), ecs[:, :].unsqueeze(2).to_broadcast([128, 8, 64]), ALU.mult), reads=[pYo, ecs], writes=[Yt])
        k.op("dve", lambda e: e.tensor_tensor(Y[:, :], Yt[:, :], pYd[:, :], ALU.add), reads=[Yt, pYd], writes=[Y])
        for g in range(2):
            k.op("pe", lambda e, g=g: e.matmul(pSt[:, g * 256:(g + 1) * 256], lhsT=Btm[:, g * 64:(g + 1) * 64], rhs=Xdd[:, g * 4:(g + 1) * 4, :].rearrange("p h e -> p (h e)"), start=True, stop=True), reads=[Btm, Xdd], writes=[pSt], pe_acc=True)
        k.op("pool", lambda e: e.tensor_tensor(Stmp[:, :, :], Sst[:, :, :], eend[0:64, :].unsqueeze(2).to_broadcast([64, 8, 64]), ALU.mult), reads=[Sst, eend], writes=[Stmp])
        k.op("dve", lambda e: e.tensor_tensor(Sst[:, :, :], Stmp[:, :, :], pSt[:, :].rearrange("p (h e) -> p h e", e=64), ALU.add), reads=[Stmp, pSt], writes=[Sst])
        k.op("act", lambda e: e.copy(Sbf[:, :, :], Sst[:, :, :]), reads=[Sst], writes=[Sbf])
        if d == 0:
            k.op("pool", lambda e: e.dma_start(out=self.s_yf.ap[t0:t0 + 128, :], in_=Y[:, :]), reads=[Y], dma=True)
        else:
            k.op("dve", lambda e: e.tensor_tensor(Y[:, :], Y[:, :], yf_[:, :], ALU.add), reads=[Y, yf_], writes=[Y])
            k.op("pool", lambda e: e.tensor_tensor(Yt[:, :].rearrange("p (h e) -> p h e", e=64), xv, dsk[:, :].unsqueeze(2).to_broadcast([128, 8, 64]), ALU.mult), reads=[x_, dsk], writes=[Yt])
            k.op("dve", lambda e: e.tensor_tensor(Y[:, :], Y[:, :], Yt[:, :], ALU.add), reads=[Y, Yt], writes=[Y])
            k.op("act", lambda e: e.activation(out=z_[:, :], in_=z_[:, :], func=AF.Silu), reads=[z_], writes=[z_])
            k.op("dve", lambda e: e.tensor_tensor(Y[:, :], Y[:, :], z_[:, :], ALU.mult), reads=[Y, z_], writes=[Y])
            k.op("act", lambda e: e.activation(out=junk[:, :], in_=Y[:, :], func=AF.Square, accum_out=ss[:, 0:1]), reads=[Y], writes=[junk, ss])
            k.op("act", lambda e: e.activation(out=ss[:, 0:1], in_=ss[:, 0:1], func=AF.Sqrt, scale=1.0 / 512, bias=EPS), reads=[ss], writes=[ss])
            k.op("dve", lambda e: e.reciprocal(ss[:, 0:1], ss[:, 0:1]), reads=[ss], writes=[ss])
            k.op("dve", lambda e: e.scalar_tensor_tensor(out=Ybf[:, :], in0=Y[:, :], scalar=ss[:, 0:1], in1=ngb[:, :], op0=ALU.mult, op1=ALU.mult), reads=[Y, ss, ngb], writes=[Ybf])
            for j in range(4):
                k.op("pe", lambda e, j=j: e.transpose(pT[:, j, :], Ybf[:, j * 128:(j + 1) * 128], self.ident[:, :]), reads=[Ybf, self.ident], writes=[pT], pe_acc=True)
            k.op("act", lambda e: e.copy(ofm[:, :, :], pT[:, :, :]), reads=[pT], writes=[ofm])
            k.op("pool", lambda e: e.dma_start(out=self.mixT.ap[256:768, t0:t0 + 128].rearrange("(j p) t -> p j t", p=128), in_=ofm[:, :, :]), reads=[ofm], dma=True)

    for ci, c in enumerate(order):
        chunk(ci, c)
    k.barrier()
    k.emit()
    k.free_to(m)


MK.ssm_decl = _ssm_decl
MK.phase_ssm_conv = phase_ssm_conv
MK.phase_ssm_scan = phase_ssm_scan


def _moe_decl(self):
    k = self.k
    NT = self.NT
    def S(n, s, dt=F32):
        kind = "ExternalOutput" if n in self.dbg else "Internal"
        return k.dram(n, s, dt, kind=kind)
    self.h2T = S("h2T", [D, NT], BF16)
    self.combT = S("combT", [16, NT])
    self.c_sel = k.dram("c_sel", [16, 16, 128], F32, kind="ExternalInput")


def phase_outproj(self, l, lat_only):
    k = self.k
    m = k.mark()
    NT = self.NT
    wout = k.sb("wout", [128, 8, D], BF16)
    for c in range(8):
        k.op("pool", lambda e, c=c: e.dma_start(out=wout[:, c, :], in_=self.w_out.ap[l, c * 128:(c + 1) * 128, :]), writes=[wout], dma=True)
    wr = k.sb("wr", [128, 8, 20], F32)
    k.op("sp", lambda e: e.dma_start(out=wr[:, :, 0:4], in_=self.moe_rg_w.ap[l].rearrange("(c p) g -> p c g", p=128)), writes=[wr], dma=True)
    k.op("sp", lambda e: e.dma_start(out=wr[:, :, 4:20], in_=self.moe_re_w.ap[l].rearrange("(c p) g -> p c g", p=128)), writes=[wr], dma=True)
    rb = k.sb("rb", [128, 20], F32)
    k.op("sp", lambda e: e.dma_start(out=rb[:, 0:4], in_=self.moe_rg_b.ap[l].partition_broadcast(128)), writes=[rb], dma=True)
    k.op("sp", lambda e: e.dma_start(out=rb[:, 4:20], in_=self.moe_re_b.ap[l].partition_broadcast(128)), writes=[rb], dma=True)
    mixf = [k.sb("mixf%d" % i, [128, 8, 512], F32) for i in range(2)]
    mixb = k.sb("mixb", [128, 8, 512], BF16)
    xt = [k.sb("oxt%d" % i, [128, 8, 512], F32) for i in range(2)]
    sq = k.sb("osq", [128, 8, 512], BF16)
    rstd = k.sb("orstd", [128, 512], F32)
    hT = k.sb("ohT", [128, 8, 512], BF16)
    pss = k.ps("opss", [128, 512], F32)
    po = [k.ps("opo%d" % i, [128, 512], F32) for i in range(3)]
    plg = k.ps("oplg", [128, 4, 20], F32)
    pct = k.ps("opct", [16, 4, 128], F32)
    R_ = lambda n_, s_: k.sb(n_, [128] + s_, F32)
    lg = R_("rlg", [4, 20]); gmax = R_("rgmax", [4, 1]); ohg = R_("rohg", [4, 4]); eg = R_("reg", [4, 4]); gsum = R_("rgsum", [4, 1])
    pgr = R_("rpgr", [4, 1]); esel3 = R_("resel3", [4, 4, 4]); esel = R_("resel", [4, 4]); m1 = R_("rm1", [4, 1]); oh1 = R_("roh1", [4, 4])
    es2 = R_("res2", [4, 4]); m2 = R_("rm2", [4, 1]); oh2 = R_("roh2", [4, 4]); ex2 = R_("rex2", [4, 1]); den = R_("rden", [4, 1])
    w1_ = R_("rw1", [4, 1]); w2_ = R_("rw2", [4, 1]); cig = R_("rcig", [4, 4]); comb = R_("rcomb", [4, 4, 4]); tmp4 = R_("rtmp4", [4, 4])
    combT_sb = k.sb("combT_sb", [16, 4, 128], F32)
    xTv = self.xT.ap.rearrange("(c p) t -> p c t", p=128)
    mTv = self.mixT.ap.rearrange("(c p) t -> p c t", p=128)
    hTv = self.h2T.ap.rearrange("(c p) t -> p c t", p=128)
    pi = [0]

    def tile_body(ti, tok0, n, stream):
        b = ti % 2
        mf, x_ = mixf[b], xt[b]
        k.op("sp", lambda e: e.dma_start(out=mf[:, :, :n], in_=mTv[:, :, tok0:tok0 + n]), writes=[mf], dma=True)
        k.op("sp", lambda e: e.dma_start(out=x_[:, :, :n], in_=xTv[:, :, tok0:tok0 + n]), writes=[x_], dma=True)
        k.op("act", lambda e: e.copy(mixb[:, 0:4, :n], mf[:, 0:4, :n]), reads=[mf], writes=[mixb])
        k.op("pool", lambda e: e.tensor_copy(mixb[:, 4:8, :n], mf[:, 4:8, :n]), reads=[mf], writes=[mixb])
        for j in range(8):
            p_ = po[pi[0] % 3]; pi[0] += 1
            for c in range(8):
                k.op("pe", lambda e, c=c, j=j, p_=p_: e.matmul(p_[:, :n], lhsT=wout[:, c, j * 128:(j + 1) * 128], rhs=mixb[:, c, :n], start=(c == 0), stop=(c == 7)), reads=[wout, mixb], writes=[p_], pe_acc=True)
            k.op("dve", lambda e, j=j, p_=p_: e.scalar_tensor_tensor(out=x_[:, j, :n], in0=p_[:, :n], scalar=self.modT[:, l, 16 + j, stream:stream + 1], in1=x_[:, j, :n], op0=ALU.mult, op1=ALU.add), reads=[p_, self.modT, x_], writes=[x_])
        k.op("pool", lambda e: e.dma_start(out=xTv[:, :, tok0:tok0 + n], in_=x_[:, :, :n]), reads=[x_], dma=True)
        self.norm_tile(l, 1, tok0, n, stream, x_, sq, pss, rstd, hT, keep_f32=True)
        k.op("pool", lambda e: e.dma_start(out=hTv[:, :, tok0:tok0 + n], in_=hT[:, :, :n]), reads=[hT], dma=True)
        nb = n // 128
        for tb in range(nb):
            for c in range(8):
                k.op("pe", lambda e, tb=tb, c=c: e.matmul(plg[:, tb, :], lhsT=x_[:, c, tb * 128:(tb + 1) * 128], rhs=wr[:, c, :], start=(c == 0), stop=(c == 7)), reads=[x_, wr], writes=[plg], pe_acc=True)
        V = lambda t_, *idx: t_[(slice(None), slice(0, nb)) + idx]
        op = lambda fn, r, w: k.op("dve", fn, reads=r, writes=w)
        op(lambda e: e.tensor_tensor(lg[:, :nb, :], plg[:, :nb, :], rb[:, :].unsqueeze(1).to_broadcast([128, nb, 20]), ALU.add), [plg, rb], [lg])
        op(lambda e: e.tensor_reduce(gmax[:, :nb, :], lg[:, :nb, 0:4], AX.X, ALU.max), [lg], [gmax])
        op(lambda e: e.tensor_tensor(ohg[:, :nb, :], lg[:, :nb, 0:4], gmax[:, :nb, :].to_broadcast([128, nb, 4]), ALU.is_ge), [lg, gmax], [ohg])
        op(lambda e: e.tensor_tensor(eg[:, :nb, :], lg[:, :nb, 0:4], gmax[:, :nb, :].to_broadcast([128, nb, 4]), ALU.subtract), [lg, gmax], [eg])
        k.op("act", lambda e: e.activation(out=eg[:, :nb, :], in_=eg[:, :nb, :], func=AF.Exp), reads=[eg], writes=[eg])
        op(lambda e: e.tensor_reduce(gsum[:, :nb, :], eg[:, :nb, :], AX.X, ALU.add), [eg], [gsum])
        op(lambda e: e.reciprocal(pgr[:, :nb, :], gsum[:, :nb, :]), [gsum], [pgr])
        elv = lg[:, :nb, 4:20].rearrange("p b (g e) -> p b g e", e=4)
        op(lambda e: e.tensor_tensor(esel3[:, :nb, :, :], elv, ohg[:, :nb, :].unsqueeze(3).to_broadcast([128, nb, 4, 4]), ALU.mult), [lg, ohg], [esel3])
        op(lambda e: e.tensor_reduce(esel[:, :nb, :], esel3[:, :nb, :, :].rearrange("p b g e -> p b e g"), AX.X, ALU.add), [esel3], [esel])
        op(lambda e: e.tensor_reduce(m1[:, :nb, :], esel[:, :nb, :], AX.X, ALU.max), [esel], [m1])
        op(lambda e: e.tensor_tensor(oh1[:, :nb, :], esel[:, :nb, :], m1[:, :nb, :].to_broadcast([128, nb, 4]), ALU.is_ge), [esel, m1], [oh1])
        op(lambda e: e.scalar_tensor_tensor(out=es2[:, :nb, :], in0=oh1[:, :nb, :], scalar=-1e30, in1=esel[:, :nb, :], op0=ALU.mult, op1=ALU.add), [oh1, esel], [es2])
        op(lambda e: e.tensor_reduce(m2[:, :nb, :], es2[:, :nb, :], AX.X, ALU.max), [es2], [m2])
        op(lambda e: e.tensor_tensor(oh2[:, :nb, :], es2[:, :nb, :], m2[:, :nb, :].to_broadcast([128, nb, 4]), ALU.is_ge), [es2, m2], [oh2])
        op(lambda e: e.tensor_tensor(ex2[:, :nb, :], m2[:, :nb, :], m1[:, :nb, :], ALU.subtract), [m2, m1], [ex2])
        k.op("act", lambda e: e.activation(out=ex2[:, :nb, :], in_=ex2[:, :nb, :], func=AF.Exp), reads=[ex2], writes=[ex2])
        op(lambda e: e.tensor_scalar(den[:, :nb, :], ex2[:, :nb, :], 1.0, None, ALU.add), [ex2], [den])
        op(lambda e: e.reciprocal(den[:, :nb, :], den[:, :nb, :]), [den], [den])
        op(lambda e: e.tensor_tensor(w1_[:, :nb, :], den[:, :nb, :], pgr[:, :nb, :], ALU.mult), [den, pgr], [w1_])
        op(lambda e: e.tensor_tensor(w2_[:, :nb, :], w1_[:, :nb, :], ex2[:, :nb, :], ALU.mult), [w1_, ex2], [w2_])
        op(lambda e: e.tensor_tensor(cig[:, :nb, :], oh1[:, :nb, :], w1_[:, :nb, :].to_broadcast([128, nb, 4]), ALU.mult), [oh1, w1_], [cig])
        op(lambda e: e.tensor_tensor(tmp4[:, :nb, :], oh2[:, :nb, :], w2_[:, :nb, :].to_broadcast([128, nb, 4]), ALU.mult), [oh2, w2_], [tmp4])
        op(lambda e: e.tensor_tensor(cig[:, :nb, :], cig[:, :nb, :], tmp4[:, :nb, :], ALU.add), [cig, tmp4], [cig])
        for g in range(4):
            op(lambda e, g=g: e.tensor_tensor(comb[:, :nb, g, :], cig[:, :nb, :], ohg[:, :nb, g:g + 1].to_broadcast([128, nb, 4]), ALU.mult), [cig, ohg], [comb])
        for tb in range(nb):
            k.op("pe", lambda e, tb=tb: e.transpose(pct[:, tb, :], comb[:, tb, :, :].rearrange("p g e -> p (g e)"), self.identf[:, :]), reads=[comb, self.identf], writes=[pct], pe_acc=True)
        k.op("act", lambda e: e.copy(combT_sb[:, :nb, :], pct[:, :nb, :]), reads=[pct], writes=[combT_sb])
        k.op("pool", lambda e: e.dma_start(out=self.combT.ap[:, tok0:tok0 + n].rearrange("k (b t) -> k b t", t=128), in_=combT_sb[:, :nb, :]), reads=[combT_sb], dma=True)

    for ti, (tok0, n, stream) in enumerate(self.tok_tiles(lat_only=lat_only)):
        tile_body(ti, tok0, n, stream)
    k.barrier()
    k.emit()
    k.free_to(m)


def phase_moe(self, l, lat_only):
    k = self.k
    m = k.mark()
    NT, T = self.NT, self.T
    TTL = min(2048, T)
    tiles = []
    start = CTX if lat_only else 0
    first = True
    t = start
    while t < NT:
        if first and not lat_only:
            n = CTX + TTL
        else:
            n = TTL
        n = min(n, NT - t)
        tiles.append((t, n))
        t += n
        first = False
    TTmax = max(n for _, n in tiles)
    acc = k.sb("macc", [128, 8, TTmax], F32)
    h2 = k.sb("mh2", [128, 8, TTmax], BF16)
    cTb = k.sb("mcTb", [16, TTmax], BF16)
    w1 = [k.sb("mw1%d" % i, [128, 8, 512], BF16) for i in range(2)]
    w3 = [k.sb("mw3%d" % i, [128, 8, 512], BF16) for i in range(2)]
    w2 = [k.sb("mw2%d" % i, [128, 4, D], BF16) for i in range(2)]
    self_sel = k.sb("msel", [16, 16, 128], BF16)
    k.op("pool", lambda e: e.dma_start(out=self_sel[:, :, :], in_=self.c_sel.ap.rearrange("e k m -> k e m")), writes=[self_sel], dma=True)
    sa = [k.sb("msa%d" % i, [128, 512], BF16) for i in range(2)]
    sa2 = [k.sb("msa2%d" % i, [128, 512], BF16) for i in range(2)]
    hid = [k.sb("mhid%d" % i, [128, 4, 512], BF16) for i in range(2)]
    cb = [k.sb("mcb%d" % i, [128, 512], BF16) for i in range(2)]
    xres = [k.sb("mxres%d" % i, [128, 512], F32) for i in range(2)]
    pa = [k.ps("mpa%d" % i, [128, 512], F32) for i in range(2)]
    pb = [k.ps("mpb%d" % i, [128, 512], F32) for i in range(2)]
    po = [k.ps("mpo%d" % i, [128, 512], F32) for i in range(3)]
    pcb = k.ps("mpcb", [128, 512], F32)
    hTv = self.h2T.ap.rearrange("(c p) t -> p c t", p=128)
    xTv = self.xT.ap.rearrange("(c p) t -> p c t", p=128)
    cnt = {"ab": 0, "o": 0, "h": 0, "w": 0, "s": 0}

    def blocks(t0, n):
        bl = []
        o = 0
        if t0 < CTX:
            bl.append((0, CTX, 1)); o = CTX
        while o < n:
            s_ = min(512, n - o)
            bl.append((o, s_, 0)); o += s_
        return bl

    nsteps = 16 * len(tiles)

    def load_w(si, which):
        th = []
        if si >= nsteps:
            return th
        ex = si % 16
        wb = si % 2
        W1, W3, W2 = w1[wb], w3[wb], w2[wb]
        if which == "w13":
            for c in range(8):
                th.append(lambda c=c: k.op("pool", lambda e: e.dma_start(out=W1[:, c, :], in_=self.moe_w1.ap[l, ex, c * 128:(c + 1) * 128, :]), writes=[W1], dma=True))
                th.append(lambda c=c: k.op("pool", lambda e: e.dma_start(out=W3[:, c, :], in_=self.moe_w3.ap[l, ex, c * 128:(c + 1) * 128, :]), writes=[W3], dma=True))
        else:
            for c in range(4):
                th.append(lambda c=c: k.op("pool", lambda e: e.dma_start(out=W2[:, c, :], in_=self.moe_w2.ap[l, ex, c * 128:(c + 1) * 128, :]), writes=[W2], dma=True))
        return th

    for (t0, n) in tiles:
        for c in range(8):
            k.op("sp", lambda e, c=c, t0=t0, n=n: e.dma_start(out=h2[:, c, :n], in_=hTv[:, c, t0:t0 + n]), writes=[h2], dma=True)
        k.op("pool", lambda e, t0=t0, n=n: e.dma_start(out=cTb[:, :n], in_=self.combT.ap[:, t0:t0 + n]), writes=[cTb], dma=True)
        bl = blocks(t0, n)
        pending = [None]
        for ex in range(16):
            si = cnt["w"]; cnt["w"] += 1
            wb = si % 2
            W1, W3, W2 = w1[wb], w3[wb], w2[wb]
            if si == 0:
                for f_ in load_w(0, "w13") + load_w(0, "w2"):
                    f_()
            nxt = load_w(si + 1, "w13") + load_w(si + 1, "w2")
            nblk = max(1, len(bl) - 1)
            per = (len(nxt) + nblk - 1) // nblk
            pend = None

            def up(o, s_, stream, W1, W3, ex):
                cbb = cb[cnt["s"] % 2]; cnt["s"] += 1
                k.op("pe", lambda e: e.matmul(pcb[:, :s_], lhsT=self_sel[:, ex, :], rhs=cTb[:, o:o + s_], start=True, stop=True), reads=[self_sel, cTb], writes=[pcb])
                k.op("act", lambda e: e.copy(cbb[:, :s_], pcb[:, :s_]), reads=[pcb], writes=[cbb])
                hd = hid[cnt["h"] % 2]; cnt["h"] += 1
                for fc in range(4):
                    i = cnt["ab"] % 2; cnt["ab"] += 1
                    pa_, pb_, sa_, sa2_ = pa[i], pb[i], sa[i], sa2[i]
                    for c in range(8):
                        k.op("pe", lambda e, c=c, fc=fc, pa_=pa_: e.matmul(pa_[:, :s_], lhsT=W1[:, c, fc * 128:(fc + 1) * 128], rhs=h2[:, c, o:o + s_], start=(c == 0), stop=(c == 7)), reads=[W1, h2], writes=[pa_], pe_acc=True)
                    for c in range(8):
                        k.op("pe", lambda e, c=c, fc=fc, pb_=pb_: e.matmul(pb_[:, :s_], lhsT=W3[:, c, fc * 128:(fc + 1) * 128], rhs=h2[:, c, o:o + s_], start=(c == 0), stop=(c == 7)), reads=[W3, h2], writes=[pb_], pe_acc=True)
                    k.op("act", lambda e, pa_=pa_, sa_=sa_: e.activation(out=sa_[:, :s_], in_=pa_[:, :s_], func=AF.Silu), reads=[pa_], writes=[sa_])
                    k.op("pool", lambda e, sa_=sa_, sa2_=sa2_: e.tensor_tensor(sa2_[:, :s_], sa_[:, :s_], cbb[:, :s_], ALU.mult), reads=[sa_, cbb], writes=[sa2_])
                    k.op("dve", lambda e, sa2_=sa2_, pb_=pb_, fc=fc: e.tensor_tensor(hd[:, fc, :s_], sa2_[:, :s_], pb_[:, :s_], ALU.mult), reads=[sa2_, pb_], writes=[hd])
                return hd

            def down(o, s_, hd, W2_, ex_):
                for j in range(8):
                    po_ = po[cnt["o"] % 3]; cnt["o"] += 1
                    for fc in range(4):
                        k.op("pe", lambda e, fc=fc, j=j, po_=po_: e.matmul(po_[:, :s_], lhsT=W2_[:, fc, j * 128:(j + 1) * 128], rhs=hd[:, fc, :s_], start=(fc == 0), stop=(fc == 3)), reads=[W2_, hd], writes=[po_], pe_acc=True)
                    if ex_ == 0:
                        k.op("dve", lambda e, j=j, po_=po_: e.tensor_copy(acc[:, j, o:o + s_], po_[:, :s_]), reads=[po_], writes=[acc])
                    else:
                        k.op("dve", lambda e, j=j, po_=po_: e.tensor_tensor(acc[:, j, o:o + s_], acc[:, j, o:o + s_], po_[:, :s_], ALU.add), reads=[po_, acc], writes=[acc])

            for (o, s_, stream) in bl:
                hd = up(o, s_, stream, W1, W3, ex)
                if pending[0] is not None:
                    down(*pending[0])
                pending[0] = (o, s_, hd, W2, ex)
                for f_ in nxt[:per]:
                    f_()
                nxt = nxt[per:]
        if pending[0] is not None:
            down(*pending[0])
            pending[0] = None
        ri = 0
        for (o, s_, stream) in bl:
            for j in range(8):
                xr = xres[ri % 2]; ri += 1
                k.op("sp", lambda e, xr=xr, o=o, s_=s_, t0=t0, j=j: e.dma_start(out=xr[:, :s_], in_=xTv[:, j, t0 + o:t0 + o + s_]), writes=[xr], dma=True)
                k.op("dve", lambda e, j=j, xr=xr, o=o, s_=s_, stream=stream: e.scalar_tensor_tensor(out=xr[:, :s_], in0=acc[:, j, o:o + s_], scalar=self.modT[:, l, 40 + j, stream:stream + 1], in1=xr[:, :s_], op0=ALU.mult, op1=ALU.add), reads=[acc, self.modT, xr], writes=[xr])
                k.op("pool", lambda e, xr=xr, o=o, s_=s_, t0=t0, j=j: e.dma_start(out=xTv[:, j, t0 + o:t0 + o + s_], in_=xr[:, :s_]), reads=[xr], dma=True)
    k.barrier()
    k.emit()
    k.free_to(m)


def phase_final(self):
    k = self.k
    m = k.mark()
    fg = self._pvec("fg", self.final_g.ap, 8)
    xt = [k.sb("fxt%d" % i, [128, 8, 512], F32) for i in range(2)]
    sq = k.sb("fsq", [128, 8, 512], BF16)
    rstd = k.sb("frstd", [128, 512], F32)
    pss = k.ps("fpss", [128, 512], F32)
    pt = [k.ps("fpt%d" % i, [128, 4, 128], F32) for i in range(2)]
    ot = [k.sb("fot%d" % i, [128, 8, 128], F32) for i in range(2)]
    xTv = self.xT.ap.rearrange("(c p) t -> p c t", p=128)
    cnt = [0]
    for ti, (tok0, n, stream) in enumerate(self.tok_tiles(lat_only=True)):
        x_ = xt[ti % 2]
        k.op("sp", lambda e, x_=x_, tok0=tok0, n=n: e.dma_start(out=x_[:, :, :n], in_=xTv[:, :, tok0:tok0 + n]), writes=[x_], dma=True)
        k.op("act", lambda e, x_=x_, n=n: e.activation(out=sq[:, :, :n], in_=x_[:, :, :n], func=AF.Square), reads=[x_], writes=[sq])
        for c in range(8):
            k.op("pe", lambda e, c=c, n=n: e.matmul(pss[:, :n], lhsT=self.ones[:, :], rhs=sq[:, c, :n], start=(c == 0), stop=(c == 7)), reads=[sq, self.ones], writes=[pss], pe_acc=True)
        k.op("act", lambda e, n=n: e.activation(out=rstd[:, :n], in_=pss[:, :n], func=AF.Sqrt, scale=1.0 / D, bias=EPS), reads=[pss], writes=[rstd])
        k.op("dve", lambda e, n=n: e.reciprocal(rstd[:, :n], rstd[:, :n]), reads=[rstd], writes=[rstd])
        k.op("dve", lambda e, x_=x_, n=n: e.tensor_tensor(x_[:, :, :n], x_[:, :, :n], rstd[:, :n].unsqueeze(1).to_broadcast([128, 8, n]), ALU.mult), reads=[x_, rstd], writes=[x_])
        k.op("pool", lambda e, x_=x_, n=n: e.tensor_tensor(x_[:, :, :n], x_[:, :, :n], fg[:, :].unsqueeze(2).to_broadcast([128, 8, n]), ALU.mult), reads=[x_, fg], writes=[x_])
        for tb in range(n // 128):
            o_ = ot[cnt[0] % 2]; cnt[0] += 1
            for hh in range(2):
                for c in range(4):
                    cc = hh * 4 + c
                    k.op("pe", lambda e, hh=hh, c=c, cc=cc, x_=x_, tb=tb: e.transpose(pt[hh][:, c, :], x_[:, cc, tb * 128:(tb + 1) * 128], self.identf[:, :]), reads=[x_, self.identf], writes=[pt[hh]], pe_acc=True)
                if hh == 0:
                    k.op("dve", lambda e, o_=o_, hh=hh: e.tensor_copy(o_[:, 0:4, :], pt[0][:, :, :]), reads=[pt[0]], writes=[o_])
                else:
                    k.op("act", lambda e, o_=o_, hh=hh: e.copy(o_[:, 4:8, :], pt[1][:, :, :]), reads=[pt[1]], writes=[o_])
            tt = tok0 - CTX + tb * 128
            k.op("pool", lambda e, o_=o_, tt=tt: e.dma_start(out=self.out.ap[tt:tt + 128, :], in_=o_[:, :, :].rearrange("p c f -> p (c f)")), reads=[o_], dma=True)
    k.barrier()
    k.emit()
    k.free_to(m)


MK.moe_decl = _moe_decl
MK.phase_outproj = phase_outproj
MK.phase_moe = phase_moe
MK.phase_final = phase_final


def build_all(T, dbg=None, L=2):
    mk_ = MK(T, L=L, dbg=dbg)
    mk_.consts(); mk_.phase_mod(); mk_.phase_x_in()
    for l in range(L):
        last = (l == L - 1)
        mk_.phase_inproj(l)
        mk_.phase_rwkv(l, 0); mk_.phase_rwkv(l, 1)
        mk_.phase_ssm_conv(l); mk_.phase_ssm_scan(l, 0); mk_.phase_ssm_scan(l, 1)
        mk_.phase_gla(l, 0); mk_.phase_gla(l, 1)
        mk_.phase_outproj(l, lat_only=last)
        mk_.phase_moe(l, lat_only=last)
    mk_.phase_final()
    mk_.finish(None)
    return mk_


def _host_consts():
    c = {}
    c["c_ident"] = np.eye(128, dtype=np.float32)
    p = np.arange(128)[:, None] % 64
    f = np.arange(512)[None, :] % 64
    c["c_masks"] = np.stack([(p < f), (p <= f), (p > f), (p >= f)]).astype(np.float32)
    c["c_id8"] = (p == f).astype(np.float32)
    pp = np.arange(128)
    c["c_blk"] = ((pp[:, None] // 64) == (pp[None, :] // 64)).astype(np.float32)
    j = np.arange(128)[:, None]; f128 = np.arange(128)[None, :]
    c["c_m128"] = np.stack([(j <= f128), (j >= f128), (j > f128), (j < f128), -30000.0 * (j == f128)]).astype(np.float32)
    sel = np.zeros((16, 16, 128), np.float32)
    for e_ in range(16):
        sel[e_, e_, :] = 1.0
    c["c_sel"] = sel
    c["c_reset"] = np.broadcast_to((np.arange(512) % 64 != 0).astype(np.float32)[None, :], (128, 512)).copy()
    return c


_CACHE = {}


def kernel(**inputs):
    from concourse.bass_utils import run_bass_kernel_spmd
    x = np.asarray(inputs["x"])
    B, T, _ = x.shape
    n_cores = 8
    assert B == n_cores
    mk_ = build_all(T)
    consts = _host_consts()
    in_maps = []
    for b in range(n_cores):
        m = {}
        for k_, v in inputs.items():
            v = np.asarray(v, dtype=np.float32)
            if k_ in ("x", "ctx", "c"):
                m[k_] = np.ascontiguousarray(v[b])
            else:
                m[k_] = np.ascontiguousarray(v)
        m.update(consts)
        in_maps.append(m)
    res = run_bass_kernel_spmd(mk_.nc, in_maps, core_ids=list(range(n_cores)))
    out = np.stack([np.asarray(r["out"], dtype=np.float32) for r in res.results], 0)
    return out
```
